# Optimizing a Trainium2 kernel written in Bass

```python
import math
import jax, jax.numpy as jnp
from jax import lax
import numpy as np

D_MODEL = 1024
BATCH = 16
SEQ = 2048
DEPTH = 4

CHUNK = 64
DENSE_Q_BLOCK = 128
SPARSE_Q_BLOCK = 64

HA = 4
DA = 64
HB = 4
DB = 64
BAND_CHUNKS = 8
REL_CLIP = 128
HC = 4
DC = 64
HI = 8
DI = 32
TOPK_MAX = 256

N_ALIBI = HA + HC

COL_SIZES = (HA * 2 * DA, HA * 2 * DA, HA * 2 * DA,
             HB * DB, HB * DB, HB * DB,
             HC * DC, HC * DC, HC * DC,
             HI * DI, DI, HI)
SPLIT_POINTS = tuple(sum(COL_SIZES[:i + 1]) for i in range(len(COL_SIZES) - 1))
D_IN = sum(COL_SIZES)
D_MIX = HA * 2 * DA + HB * DB + HC * DC

N_EXPERTS = 32
TOP_K = 4
D_EXPERT = D_MODEL
SWIGLU_LIMIT = 7.0
SWIGLU_ALPHA = 1.702
EXPERT_BLOCK = 128

DEEPNORM_ALPHA = (2 * DEPTH) ** 0.25
DEEPNORM_BETA = (8 * DEPTH) ** -0.25
EPS = 1e-5
NEG_INF = -1e30

kernel_name = 'hybrid_chunk_causal_diffattn_band_dsa_moe_deepnorm'


def layer_norm(x, g, b):
    xf = x.astype(jnp.float32)
    mu = jnp.mean(xf, axis=-1, keepdims=True)
    var = jnp.mean(jnp.square(xf - mu), axis=-1, keepdims=True)
    return ((xf - mu) * lax.rsqrt(var + EPS) * g.astype(jnp.float32) + b.astype(jnp.float32)).astype(x.dtype)


def rms_norm(x, g):
    xf = x.astype(jnp.float32)
    ms = jnp.mean(jnp.square(xf), axis=-1, keepdims=True)
    return (xf * lax.rsqrt(ms + EPS) * g.astype(jnp.float32)).astype(x.dtype)


def alibi_slopes(n):
    return jnp.exp2(-8.0 * jnp.arange(1, n + 1, dtype=jnp.float32) / n)


def to_blocks(a, blk):
    b, s = a.shape[:2]
    return jnp.moveaxis(a.reshape((b, s // blk, blk) + a.shape[2:]), 1, 0)


def from_blocks(a):
    a = jnp.moveaxis(a, 0, 1)
    return a.reshape((a.shape[0], a.shape[1] * a.shape[2]) + a.shape[3:])


def diff_attention(q, k, v, lam, lam_init, sub_g, slopes):
    bsz, seq = q.shape[:2]
    pos = jnp.arange(seq)
    key_chunk = pos // CHUNK
    scale = DA ** -0.5
    k1, k2 = k[..., 0, :], k[..., 1, :]

    def block(args):
        qb, qpos = args
        s1 = jnp.einsum('bqhd,bshd->bhqs', qb[..., 0, :], k1).astype(jnp.float32) * scale
        s2 = jnp.einsum('bqhd,bshd->bhqs', qb[..., 1, :], k2).astype(jnp.float32) * scale
        dist = jnp.abs(qpos[:, None] - pos[None, :]).astype(jnp.float32)
        allowed = key_chunk[None, :] <= (qpos // CHUNK)[:, None]
        bias = jnp.where(allowed[None], -slopes[:, None, None] * dist[None], NEG_INF)
        p1 = jax.nn.softmax(s1 + bias, axis=-1)
        p2 = jax.nn.softmax(s2 + bias, axis=-1)
        a = (p1 - lam * p2).astype(v.dtype)
        return jnp.einsum('bhqs,bshe->bqhe', a, v)

    qpos_blocks = pos.reshape(-1, DENSE_Q_BLOCK)
    out = from_blocks(lax.map(block, (to_blocks(q, DENSE_Q_BLOCK), qpos_blocks)))
    out = rms_norm(out, sub_g) * (1.0 - lam_init)
    return out.reshape(bsz, seq, HA * 2 * DA)


def band_attention(q, k, v, rel_bias):
    bsz, seq = q.shape[:2]
    nc = seq // CHUNK
    pad = BAND_CHUNKS * CHUNK
    band = (BAND_CHUNKS + 1) * CHUNK
    scale = DB ** -0.5

    def gather_band(a):
        ap = jnp.pad(a, ((0, 0), (pad, 0), (0, 0), (0, 0)))
        ac = ap.reshape(bsz, nc + BAND_CHUNKS, CHUNK, HB, DB)
        return jnp.concatenate([ac[:, j:j + nc] for j in range(BAND_CHUNKS + 1)], axis=2)

    kb, vb = gather_band(k), gather_band(v)
    qc = q.reshape(bsz, nc, CHUNK, HB, DB)
    s = jnp.einsum('bcqhd,bckhd->bhcqk', qc, kb).astype(jnp.float32) * scale
    i = jnp.arange(CHUNK)
    j = jnp.arange(band)
    rel = pad + i[:, None] - j[None, :]
    bias = rel_bias.astype(jnp.float32)[:, jnp.clip(rel, -REL_CLIP, REL_CLIP) + REL_CLIP]
    key_pos = (jnp.arange(nc)[:, None] - BAND_CHUNKS) * CHUNK + j[None, :]
    valid = key_pos >= 0
    s = jnp.where(valid[None, None, :, None, :], s + bias[None, :, None], NEG_INF)
    p = jax.nn.softmax(s, axis=-1).astype(v.dtype)
    out = jnp.einsum('bhcqk,bckhd->bcqhd', p, vb)
    return out.reshape(bsz, seq, HB * DB)


def dsa_attention(q, k, v, q_idx, k_idx, w_idx, slopes):
    bsz, seq = q.shape[:2]
    n_sel = min(TOPK_MAX, seq // 4)
    pos = jnp.arange(seq)
    key_chunk = pos // CHUNK
    scale = DC ** -0.5
    w_scale = (HI ** -0.5) * (DI ** -0.5)
    gather_rows = jax.vmap(lambda a, idx: a[idx])

    def block(args):
        qb, qib, wb, qpos = args
        q_chunk = qpos // CHUNK
        rel = jax.nn.relu(jnp.einsum('bqhd,bsd->bqhs', qib, k_idx).astype(jnp.float32))
        score = jnp.einsum('bqh,bqhs->bqs', wb.astype(jnp.float32) * w_scale, rel)
        allowed = key_chunk[None, :] <= q_chunk[:, None]
        score = jnp.where(allowed[None], score, NEG_INF)
        _, idx = lax.top_k(score, n_sel)
        ks = gather_rows(k, idx)
        vs = gather_rows(v, idx)
        s = jnp.einsum('bqhd,bqkhd->bhqk', qb, ks).astype(jnp.float32) * scale
        dist = jnp.abs(qpos[None, :, None] - idx).astype(jnp.float32)
        valid = (idx // CHUNK) <= q_chunk[None, :, None]
        s = jnp.where(valid[:, None], s - slopes[None, :, None, None] * dist[:, None], NEG_INF)
        p = jax.nn.softmax(s, axis=-1).astype(v.dtype)
        return jnp.einsum('bhqk,bqkhd->bqhd', p, vs)

    qpos_blocks = pos.reshape(-1, SPARSE_Q_BLOCK)
    out = lax.map(block, (to_blocks(q, SPARSE_Q_BLOCK), to_blocks(q_idx, SPARSE_Q_BLOCK),
                          to_blocks(w_idx, SPARSE_Q_BLOCK), qpos_blocks))
    return from_blocks(out).reshape(bsz, seq, HC * DC)


def moe_ffn(x, w_router, b_router, w_gu, b_gu, w_down, b_down):
    bsz, seq, d = x.shape
    n_tok = bsz * seq
    xf = x.reshape(n_tok, d)
    logits = (xf @ w_router).astype(jnp.float32) + b_router.astype(jnp.float32)
    top_val, top_e = lax.top_k(logits, TOP_K)
    gates = jax.nn.softmax(top_val, axis=-1)
    e_flat = top_e.reshape(-1)
    n_asg = n_tok * TOP_K
    order = jnp.argsort(e_flat)
    e_sorted = e_flat[order]
    counts = jnp.bincount(e_flat, length=N_EXPERTS)
    padded = (counts + EXPERT_BLOCK - 1) // EXPERT_BLOCK * EXPERT_BLOCK
    start = jnp.cumsum(counts) - counts
    pend = jnp.cumsum(padded)
    pstart = pend - padded
    rank = jnp.arange(n_asg, dtype=jnp.int32) - start[e_sorted]
    dest = jnp.zeros((n_asg,), jnp.int32).at[order].set(pstart[e_sorted] + rank)
    n_rows = n_asg + N_EXPERTS * EXPERT_BLOCK
    n_blocks = n_rows // EXPERT_BLOCK
    row_token = jnp.full((n_rows,), n_tok, jnp.int32).at[dest].set(
        jnp.arange(n_asg, dtype=jnp.int32) // TOP_K)
    x_pad = jnp.concatenate([xf, jnp.zeros((1, d), xf.dtype)], axis=0)
    x_rows = x_pad[row_token].reshape(n_blocks, EXPERT_BLOCK, d)
    block_expert = jnp.minimum(
        jnp.searchsorted(pend, jnp.arange(n_blocks) * EXPERT_BLOCK, side='right'), N_EXPERTS - 1)

    def expert_block(args):
        xb, e = args
        h = xb @ w_gu[e] + b_gu[e]
        gate = jnp.minimum(h[:, :D_EXPERT], SWIGLU_LIMIT)
        up = jnp.clip(h[:, D_EXPERT:], -SWIGLU_LIMIT, SWIGLU_LIMIT)
        glu = gate * jax.nn.sigmoid(SWIGLU_ALPHA * gate)
        return ((up + 1.0) * glu) @ w_down[e] + b_down[e]

    y_rows = lax.map(expert_block, (x_rows, block_expert)).reshape(n_rows, d)
    y = y_rows[dest].reshape(n_tok, TOP_K, d)
    out = jnp.einsum('tk,tkd->td', gates.astype(y.dtype), y)
    return out.reshape(bsz, seq, d)


def setup_inputs(seed: int = 0) -> dict:
    key = jax.random.key(seed)
    ks = jax.random.split(key, 19)
    f32 = jnp.float32

    def nrm(k, shape, scale):
        return jax.random.normal(k, shape, f32) * scale

    col_scale = np.ones((D_IN,), np.float32)
    for part in (2, 5, 8):
        col_scale[SPLIT_POINTS[part - 1]:SPLIT_POINTS[part]] = DEEPNORM_BETA

    x = nrm(ks[0], (BATCH, SEQ, D_MODEL), 1.0)
    w_in = nrm(ks[1], (DEPTH, D_MODEL, D_IN), D_MODEL ** -0.5) * jnp.asarray(col_scale)
    lam_q1 = nrm(ks[2], (DEPTH, DA), 0.1)
    lam_k1 = nrm(ks[3], (DEPTH, DA), 0.1)
    lam_q2 = nrm(ks[4], (DEPTH, DA), 0.1)
    lam_k2 = nrm(ks[5], (DEPTH, DA), 0.1)
    subln_g = 1.0 + nrm(ks[6], (DEPTH, 2 * DA), 0.02)
    rel_bias = nrm(ks[7], (DEPTH, HB, 2 * REL_CLIP + 1), 0.2)
    w_out = nrm(ks[8], (DEPTH, D_MIX, D_MODEL), D_MIX ** -0.5 * DEEPNORM_BETA)
    ln1_g = 1.0 + nrm(ks[9], (DEPTH, D_MODEL), 0.02)
    ln1_b = nrm(ks[10], (DEPTH, D_MODEL), 0.02)
    w_router = nrm(ks[11], (DEPTH, D_MODEL, N_EXPERTS), D_MODEL ** -0.5)
    b_router = nrm(ks[12], (DEPTH, N_EXPERTS), 0.01)
    w_gu = nrm(ks[13], (DEPTH, N_EXPERTS, D_MODEL, 2 * D_EXPERT), D_MODEL ** -0.5)
    b_gu = nrm(ks[14], (DEPTH, N_EXPERTS, 2 * D_EXPERT), 0.02)
    w_down = nrm(ks[15], (DEPTH, N_EXPERTS, D_EXPERT, D_MODEL), D_EXPERT ** -0.5 * DEEPNORM_BETA)
    b_down = nrm(ks[16], (DEPTH, N_EXPERTS, D_MODEL), 0.02)
    ln2_g = 1.0 + nrm(ks[17], (DEPTH, D_MODEL), 0.02)
    ln2_b = nrm(ks[18], (DEPTH, D_MODEL), 0.02)
    return {'x': x, 'w_in': w_in, 'lam_q1': lam_q1, 'lam_k1': lam_k1, 'lam_q2': lam_q2,
            'lam_k2': lam_k2, 'subln_g': subln_g, 'rel_bias': rel_bias, 'w_out': w_out,
            'ln1_g': ln1_g, 'ln1_b': ln1_b, 'w_router': w_router, 'b_router': b_router,
            'w_gu': w_gu, 'b_gu': b_gu, 'w_down': w_down, 'b_down': b_down,
            'ln2_g': ln2_g, 'ln2_b': ln2_b}


def reference(x, w_in, lam_q1, lam_k1, lam_q2, lam_k2, subln_g, rel_bias, w_out,
              ln1_g, ln1_b, w_router, b_router, w_gu, b_gu, w_down, b_down, ln2_g, ln2_b):
    bsz, seq, _ = x.shape
    slopes = alibi_slopes(N_ALIBI)
    slopes_a, slopes_c = slopes[0::2], slopes[1::2]
    for l in range(DEPTH):
        h = x @ w_in[l]
        qa, ka, va, qb, kb, vb, qc, kc, vc, qi, ki, wi = jnp.split(h, SPLIT_POINTS, axis=-1)
        lam_init = 0.8 - 0.6 * math.exp(-0.3 * l)
        lam = (jnp.exp(jnp.sum(lam_q1[l].astype(jnp.float32) * lam_k1[l].astype(jnp.float32)))
               - jnp.exp(jnp.sum(lam_q2[l].astype(jnp.float32) * lam_k2[l].astype(jnp.float32)))
               + lam_init)
        out_a = diff_attention(qa.reshape(bsz, seq, HA, 2, DA), ka.reshape(bsz, seq, HA, 2, DA),
                               va.reshape(bsz, seq, HA, 2 * DA), lam, lam_init, subln_g[l], slopes_a)
        out_b = band_attention(qb.reshape(bsz, seq, HB, DB), kb.reshape(bsz, seq, HB, DB),
                               vb.reshape(bsz, seq, HB, DB), rel_bias[l])
        out_c = dsa_attention(qc.reshape(bsz, seq, HC, DC), kc.reshape(bsz, seq, HC, DC),
                              vc.reshape(bsz, seq, HC, DC), qi.reshape(bsz, seq, HI, DI), ki, wi, slopes_c)
        mix = jnp.concatenate([out_a, out_b, out_c], axis=-1)
        x = layer_norm(DEEPNORM_ALPHA * x + mix @ w_out[l], ln1_g[l], ln1_b[l])
        ffn = moe_ffn(x, w_router[l], b_router[l], w_gu[l], b_gu[l], w_down[l], b_down[l])
        x = layer_norm(DEEPNORM_ALPHA * x + ffn, ln2_g[l], ln2_b[l])
    return x
```

```python
import math
from contextlib import ExitStack
import numpy as np
import ml_dtypes
import concourse.bass as bass
import concourse.mybir as mybir
from concourse.bass_utils import run_bass_kernel_spmd

F32 = mybir.dt.float32
BF16 = mybir.dt.bfloat16
I32 = mybir.dt.int32
AF = mybir.ActivationFunctionType
ALU = mybir.AluOpType
AX = mybir.AxisListType

NCORES = 8
DEPTH = 4
S = 2048
D = 1024
NBL = 2
T = NBL * S
DIN = 3368
NE = 32
CAP = 768
NSLOT = NE * CAP
ALPHA = (2 * DEPTH) ** 0.25
EPS = 1e-5
NEG = -30000.0
SIG_A = [2.0 ** -1, 2.0 ** -3, 2.0 ** -5, 2.0 ** -7]
SIG_C = [2.0 ** -2, 2.0 ** -4, 2.0 ** -6, 2.0 ** -8]
SIG8 = SIG_A + SIG_C
W_SCALE = (8 ** -0.5) * (32 ** -0.5)
NIT = 20
C_QA, C_KA, C_VA, C_QB, C_KB, C_VB, C_QC, C_KC, C_VC, C_QI, C_KI, C_WI = (
    0, 512, 1024, 1536, 1792, 2048, 2304, 2560, 2816, 3072, 3328, 3360)


class Res:
    __slots__ = ("name", "w", "r", "multi")

    def __init__(self, name, multi=False):
        self.name = name
        self.w = {}
        self.r = {}
        self.multi = multi


class Prog:
    def __init__(self, nc, ndma=(("sp", 40), ("pool", 40), ("act", 8))):
        self.nc = nc
        self.E = {"pe": nc.tensor, "act": nc.scalar, "dve": nc.vector, "pool": nc.gpsimd, "sp": nc.sync}
        self.esem = {k: nc.alloc_semaphore("e_" + k) for k in self.E}
        self.ecnt = {k: 0 for k in self.E}
        self.waited = {k: {} for k in self.E}
        self.dsem = {}
        for k, n in ndma:
            self.dsem[k] = [[nc.alloc_semaphore("d_%s%d" % (k, i)), 0, "d_%s%d" % (k, i)] for i in range(n)]
        self.dnext = {k: 0 for k in self.dsem}
        self.nins = 0

    def _wait(self, eng, ev):
        key, sem, val = ev
        if self.waited[eng].get(key, 0) >= val:
            return
        self.E[eng].wait_ge(sem, val)
        self.waited[eng][key] = val
        self.nins += 1

    def _deps(self, eng, R, W, skip_self):
        for r in R:
            for ev in r.w.values():
                if not (skip_self and ev[0] == eng):
                    self._wait(eng, ev)
        for w in W:
            if not w.multi:
                for ev in w.w.values():
                    if not (skip_self and ev[0] == eng):
                        self._wait(eng, ev)
            for ev in w.r.values():
                if not (skip_self and ev[0] == eng):
                    self._wait(eng, ev)

    def _record(self, ev, R, W):
        for r in R:
            r.r[ev[0]] = ev
        for w in W:
            if w.multi:
                w.w[ev[0]] = ev
            else:
                w.w = {ev[0]: ev}
            w.r = {}

    def op(self, eng, fn, R=(), W=(), skip_self=False):
        self._deps(eng, R, W, skip_self)
        ins = fn(self.E[eng])
        self.ecnt[eng] += 1
        ins.then_inc(self.esem[eng], 1)
        ev = (eng, self.esem[eng], self.ecnt[eng])
        self._record(ev, R, W)
        self.nins += 1
        return ev

    def pe(self, fn, R=(), W=()):
        return self.op("pe", fn, R, W, skip_self=True)

    def dma(self, eng, fn, R=(), W=()):
        self._deps(eng, R, W, False)
        pool = self.dsem[eng]
        slot = pool[self.dnext[eng] % len(pool)]
        self.dnext[eng] += 1
        if slot[1] > 0:
            self._wait(eng, (slot[2], slot[0], slot[1]))
        ins = fn(self.E[eng])
        slot[1] += 16
        ins.then_inc(slot[0], 16)
        ev = (slot[2], slot[0], slot[1])
        self._record(ev, R, W)
        self.nins += 1
        return ev

    def barrier(self):
        evs = [(k, self.esem[k], self.ecnt[k]) for k in self.E if self.ecnt[k] > 0]
        for k in self.dsem:
            for s in self.dsem[k]:
                if s[1] > 0:
                    evs.append((s[2], s[0], s[1]))
        for e in self.E:
            for ev in evs:
                self._wait(e, ev)

    def finish(self):
        self.barrier()


def build(NL, lam_inits, dbg=None):
    nc = bass.Bass("TRN2", target_bir_lowering=False)
    P = Prog(nc)

    def din(name, shape, dt=F32):
        return nc.dram_tensor(name, list(shape), dt, kind="ExternalInput").ap()

    x_in = din("x", [T, D])
    w_in = din("w_in", [NL, D, DIN])
    lamv = din("lamv", [NL, 4, 64])
    subg = din("subln_g", [NL, 128])
    relb = din("relb", [NL, 128, 16, 128])
    w_out = din("w_out", [NL, D, D])
    ln1g = din("ln1_g", [NL, D]); ln1b = din("ln1_b", [NL, D])
    w_rt = din("w_router", [NL, D, NE]); b_rt = din("b_router", [NL, NE])
    if dbg is None or dbg == "moe":
        w_gu = din("w_gu", [NL, NE, D, 2 * D]); w_dn = din("w_down", [NL, NE, D, D])
    b_gu = din("b_gu", [NL, NE * 16, 128]); b_dn = din("b_down", [NL, NE, D])
    ln2g = din("ln2_g", [NL, D]); ln2b = din("ln2_b", [NL, D])
    c_ident = din("c_ident", [128, 128], BF16)
    c_identf = din("c_identf", [128, 128], F32)
    c_ltri = din("c_ltri", [128, 128], BF16)
    c_dmask = din("c_dmask", [128, 8, 128], BF16)
    c_qaug = din("c_qaug", [3, S], BF16)
    c_kaug = din("c_kaug", [8, 3, S], BF16)
    c_eoff = din("c_eoff", [128, NE], F32)
    y_out = nc.dram_tensor("y", [T, D], F32, kind="ExternalOutput").ap()
    dbg_out = None
    if dbg == "mix":
        dbg_out = nc.dram_tensor("dbg", [NBL, 128, 8, S], BF16, kind="ExternalOutput").ap()
    if dbg == "x1":
        dbg_out = nc.dram_tensor("dbg", [T, D], F32, kind="ExternalOutput").ap()
    xcur = nc.dram_tensor("xcur", [T, D], F32).ap()
    x1s = nc.dram_tensor("x1s", [T, D], F32).ap()
    xg = nc.dram_tensor("xg", [NSLOT, D], BF16).ap()
    yg = nc.dram_tensor("yg", [NSLOT, D], F32).ap()
    R_xin = Res("xin", True); R_xcur = Res("xcur", True); R_x1s = Res("x1s", True)
    R_xg = Res("xg", True); R_yg = Res("yg", True); R_y = Res("y", True); R_dbg = Res("dbg", True)
    R_const = Res("const", True)

    def sb(name, shape, dt):
        return nc.alloc_sbuf_tensor(name, list(shape), dt)

    ident_b = sb("ident_b", [128, 128], BF16)
    ident_f = sb("ident_f", [128, 128], F32)
    ltri_b = sb("ltri_b", [128, 128], BF16)
    ones_b = sb("ones_b", [128, 128], BF16)
    zeros_b = sb("zeros_b", [128, 512], BF16)
    dmask_b = sb("dmask_b", [128, 8, 128], BF16)
    eoff = sb("eoff", [128, NE], F32)
    base_cnt = sb("base_cnt", [128, NE], F32)
    slotf = sb("slotf", [128, 32 * 4], F32)
    sloti = sb("sloti", [128, 32 * 4], I32)
    gates = sb("gates", [128, 32 * 4], F32)
    R_slot = Res("slot"); R_gates = Res("gates"); R_base = Res("base")
    for t_, src in ((ident_b, c_ident), (ident_f, c_identf), (ltri_b, c_ltri), (dmask_b, c_dmask), (eoff, c_eoff)):
        P.dma("sp", lambda e, t_=t_, src=src: e.dma_start(out=t_[:], in_=src), W=[R_const])
    P.op("pool", lambda e: e.memset(ones_b[:], 1.0), W=[R_const])
    P.op("pool", lambda e: e.memset(zeros_b[:], 0.0), W=[R_const])

    ps = [nc.alloc_psum_tensor("ps%d" % i, [128, 512], F32) for i in range(7)]
    R_ps = [Res("ps%d" % i) for i in range(7)]
    psT = nc.alloc_psum_tensor("psT", [128, 8, 128], BF16)
    R_psT = Res("psT")

    rot = {}

    def nxt(key, n):
        rot[key] = (rot.get(key, -1) + 1) % n
        return rot[key]

    def layer_norm(v, R_v, g_t, b_t, R_gb, out_t, R_out, st, R_st, junk, R_junk):
        P.op("dve", lambda e: e.reduce_sum(out=st[:, 0:1], in_=v[:], axis=AX.X), R=[R_v], W=[R_st])
        P.op("dve", lambda e: e.tensor_scalar(out=st[:, 1:2], in0=st[:, 0:1], scalar1=-1.0 / D, scalar2=None, op0=ALU.mult), R=[R_st], W=[R_st])
        P.op("act", lambda e: e.activation(out=junk[:], in_=v[:], func=AF.Square, bias=st[:, 1:2], scale=1.0, accum_out=st[:, 2:3]), R=[R_v, R_st], W=[R_junk, R_st])
        P.op("dve", lambda e: e.tensor_scalar(out=st[:, 3:4], in0=st[:, 2:3], scalar1=1.0 / D, scalar2=EPS, op0=ALU.mult, op1=ALU.add), R=[R_st], W=[R_st])
        P.op("act", lambda e: e.activation(out=st[:, 4:5], in_=st[:, 3:4], func=AF.Ln), R=[R_st], W=[R_st])
        P.op("act", lambda e: e.activation(out=st[:, 5:6], in_=st[:, 4:5], func=AF.Exp, scale=-0.5), R=[R_st], W=[R_st])
        P.op("dve", lambda e: e.tensor_scalar(out=v[:], in0=v[:], scalar1=st[:, 1:2], scalar2=st[:, 5:6], op0=ALU.add, op1=ALU.mult), R=[R_v, R_st], W=[R_v])
        P.op("pool", lambda e: e.tensor_tensor(out=v[:], in0=v[:], in1=g_t[:], op=ALU.mult), R=[R_v, R_gb], W=[R_v])
        P.op("dve", lambda e: e.tensor_tensor(out=out_t[:], in0=v[:], in1=b_t[:], op=ALU.add), R=[R_v, R_gb], W=[R_out])

    for l in range(NL):
        lam_init = lam_inits[l]
        xsrc, R_xsrc = (x_in, R_xin) if l == 0 else (xcur, R_xcur)
        last = (l == NL - 1)
        xdst, R_xdst = (y_out, R_y) if last else (xcur, R_xcur)
        P.op("pool", lambda e: e.memset(base_cnt[:], 0.0), W=[R_base])

        for b in range(NBL):
            tok0 = b * S
            with ExitStack() as es_b:
                def sbc(es, name, shape, dt):
                    return es.enter_context(nc.sbuf_tensor(name + "_%d_%d" % (l, b), list(shape), dt))
                xT = sbc(es_b, "xT", [128, 8, S], BF16); R_xT = [Res("xT%d" % i) for i in range(4)]
                mixT = sbc(es_b, "mixT", [128, 8, S], BF16)
                R_mix = [[Res("mix%d_%d" % (c, j)) for j in range(4)] for c in range(8)]

                with ExitStack() as es:
                    xst = [sbc(es, "xst%d" % i, [128, D], F32) for i in range(2)]; R_xst = [Res("xst") for _ in range(2)]
                    xbs = [sbc(es, "xbs%d" % i, [128, D], BF16) for i in range(2)]; R_xbs = [Res("xbs") for _ in range(2)]
                    for tt in range(16):
                        i2 = tt % 2
                        P.dma("sp", lambda e: e.dma_start(out=xst[i2][:], in_=xsrc[tok0 + tt * 128: tok0 + (tt + 1) * 128, :]), R=[R_xsrc], W=[R_xst[i2]])
                        P.op("act", lambda e: e.activation(out=xbs[i2][:], in_=xst[i2][:], func=AF.Copy), R=[R_xst[i2]], W=[R_xbs[i2]])
                        for k in range(8):
                            P.pe(lambda e: e.transpose(out=psT[:, k, :], in_=xbs[i2][:, k * 128:(k + 1) * 128], identity=ident_b[:]), R=[R_xbs[i2], R_const], W=[R_psT])
                        P.op("dve", lambda e: e.tensor_copy(out=xT[:, :, tt * 128:(tt + 1) * 128], in_=psT[:, :, :]), R=[R_psT], W=[R_xT[tt // 4]])
                    P.barrier()

                with ExitStack() as es:
                    QK = [sbc(es, "qk%d" % i, [128, S], BF16) for i in range(12)]
                    R_QK = [Res("qk%d" % i) for i in range(12)]
                    VV = sbc(es, "vv", [128, 16, 512], BF16); R_VV = Res("vv")
                    wst = [sbc(es, "wst%d" % i, [128, 8, 128], F32) for i in range(2)]; R_wst = [Res("wst") for _ in range(2)]
                    wbf = [sbc(es, "wbf%d" % i, [128, 8, 128], BF16) for i in range(4)]; R_wbf = [Res("wbf") for _ in range(4)]
                    PT = [sbc(es, "pt%d" % i, [128, 512], BF16) for i in range(4)]; R_PT = [Res("pt") for _ in range(4)]
                    relb_b = sbc(es, "relb_b", [128, 16, 128], BF16); R_relb = Res("relb")
                    score = sbc(es, "score", [128, S], F32); R_score = Res("score")
                    relu_sb = [sbc(es, "relu%d" % i, [128, 512], BF16) for i in range(3)]; R_relu = [Res("relu") for _ in range(3)]
                    negmask = sbc(es, "negmask", [128, S], BF16); R_negm = Res("negm")
                    nmT = sbc(es, "nmT", [128, 16, 512], BF16); R_nmT = Res("nmT")
                    diag = sbc(es, "diag", [128, 8, 128], BF16); R_diag = Res("diag")
                    wtok = sbc(es, "wtok", [128, 16, 8], F32); R_wtok = Res("wtok")
                    ft = [sbc(es, "ft%d" % i, [128, 512], F32) for i in range(4)]; R_ft = [Res("ft%d" % i) for i in range(4)]
                    sqb = sbc(es, "sqb", [128, 512], BF16); R_sqb = Res("sqb")
                    sm = sbc(es, "sm", [128, 16], F32); R_sm = Res("sm")
                    lamt = sbc(es, "lamt", [128, 4, 64], F32); R_lamt = Res("lamt")
                    gsc = sbc(es, "gsc", [128, 2], F32)

                    P.dma("sp", lambda e: e.dma_start(out=lamt[:], in_=lamv[l:l + 1, :, :].to_broadcast([128, 4, 64])), W=[R_lamt])
                    P.dma("sp", lambda e: e.dma_start(out=gsc[:, 0:1], in_=subg[l, :].rearrange("(p o) -> p o", o=1)), W=[R_sm])
                    P.op("dve", lambda e: e.tensor_tensor(out=lamt[:, 0, :], in0=lamt[:, 0, :], in1=lamt[:, 1, :], op=ALU.mult), R=[R_lamt], W=[R_lamt])
                    P.op("dve", lambda e: e.tensor_tensor(out=lamt[:, 2, :], in0=lamt[:, 2, :], in1=lamt[:, 3, :], op=ALU.mult), R=[R_lamt], W=[R_lamt])
                    P.op("dve", lambda e: e.reduce_sum(out=sm[:, 0:1], in_=lamt[:, 0, :], axis=AX.X), R=[R_lamt], W=[R_sm])
                    P.op("dve", lambda e: e.reduce_sum(out=sm[:, 1:2], in_=lamt[:, 2, :], axis=AX.X), R=[R_lamt], W=[R_sm])
                    P.op("act", lambda e: e.activation(out=sm[:, 2:4], in_=sm[:, 0:2], func=AF.Exp), R=[R_sm], W=[R_sm])
                    P.op("dve", lambda e: e.tensor_tensor(out=sm[:, 4:5], in0=sm[:, 3:4], in1=sm[:, 2:3], op=ALU.subtract), R=[R_sm], W=[R_sm])
                    P.op("dve", lambda e: e.tensor_scalar(out=sm[:, 5:6], in0=sm[:, 4:5], scalar1=-lam_init, scalar2=None, op0=ALU.add), R=[R_sm], W=[R_sm])
                    P.op("dve", lambda e: e.tensor_scalar(out=gsc[:, 1:2], in0=gsc[:, 0:1], scalar1=1.0 - lam_init, scalar2=None, op0=ALU.mult), R=[R_sm], W=[R_sm])
                    neglam = sm[:, 5:6]
                    gscale = gsc[:, 1:2]

                    P.dma("sp", lambda e: e.dma_start(out=score[:, :].rearrange("p (a b) -> p a b", a=16), in_=relb[l]), W=[R_score])
                    P.op("pool", lambda e: e.tensor_copy(out=relb_b[:], in_=score[:, :].rearrange("p (a b) -> p a b", a=16)), R=[R_score], W=[R_relb])

                    def load_w(c0, ncol, rep=1):
                        si = nxt("wst", 2); bi = nxt("wbf", 4)
                        P.dma("sp", lambda e: e.dma_start(out=wst[si][:, :, 0:ncol], in_=w_in[l, :, c0:c0 + ncol].rearrange("(k p) c -> p k c", p=128)), W=[R_wst[si]])
                        for r in range(rep):
                            P.op("pool", lambda e: e.tensor_copy(out=wbf[bi][:, :, r * ncol:(r + 1) * ncol], in_=wst[si][:, :, 0:ncol]), R=[R_wst[si]], W=[R_wbf[bi]])
                        return bi

                    def proj_feat(bi, c_lo, m, dst, R_dst, p_lo, scale):
                        for tb in range(4):
                            pi = 4 + nxt("pj", 2)
                            for k in range(8):
                                P.pe(lambda e: e.matmul(ps[pi][0:m, :], lhsT=wbf[bi][:, k, c_lo:c_lo + m], rhs=xT[:, k, tb * 512:(tb + 1) * 512], start=(k == 0), stop=(k == 7)),
                                     R=[R_wbf[bi], R_xT[tb]], W=[R_ps[pi]])
                            P.op("act", lambda e: e.activation(out=dst[p_lo:p_lo + m, tb * 512:(tb + 1) * 512], in_=ps[pi][p_lo:p_lo + m, :], func=AF.Copy, scale=scale),
                                 R=[R_ps[pi]], W=[R_dst])

                    def proj_feat_split(bi, dst0, R0, dst1, R1, scale):
                        for tb in range(4):
                            pi = 4 + nxt("pj", 2)
                            for k in range(8):
                                P.pe(lambda e: e.matmul(ps[pi][:, :], lhsT=wbf[bi][:, k, :], rhs=xT[:, k, tb * 512:(tb + 1) * 512], start=(k == 0), stop=(k == 7)),
                                     R=[R_wbf[bi], R_xT[tb]], W=[R_ps[pi]])
                            P.op("act", lambda e: e.activation(out=dst0[0:64, tb * 512:(tb + 1) * 512], in_=ps[pi][0:64, :], func=AF.Copy, scale=scale), R=[R_ps[pi]], W=[R0])
                            P.op("act", lambda e: e.activation(out=dst1[64:128, tb * 512:(tb + 1) * 512], in_=ps[pi][64:128, :], func=AF.Copy, scale=scale), R=[R_ps[pi]], W=[R1])

                    def proj_tok(bi, ncol, dst_fn, R_dst, act_eng="act"):
                        for st_ in range(16):
                            pi = 4 + nxt("pj", 2)
                            for k in range(8):
                                P.pe(lambda e: e.matmul(ps[pi][:, 0:ncol], lhsT=xT[:, k, st_ * 128:(st_ + 1) * 128], rhs=wbf[bi][:, k, 0:ncol], start=(k == 0), stop=(k == 7)),
                                     R=[R_wbf[bi], R_xT[st_ // 4]], W=[R_ps[pi]])
                            P.op("act", lambda e: e.activation(out=dst_fn(st_), in_=ps[pi][:, 0:ncol], func=AF.Copy), R=[R_ps[pi]], W=[R_dst])

                    def recip_safe(dst, R_dst, src_ps, R_src, p0, p1):
                        P.op("dve", lambda e: e.tensor_scalar(out=dst[p0:p1, :], in0=src_ps[p0:p1, :], scalar1=1e-30, scalar2=None, op0=ALU.max), R=[R_src], W=[R_dst])
                        P.op("dve", lambda e: e.reciprocal(out=dst[p0:p1, :], in_=dst[p0:p1, :]), R=[R_dst], W=[R_dst])

                    def attn_block(J, i_list, colrange_fn, score_mms, cfn, Vl_fn, R_V, accs, R_extra):
                        for (oi, si_, m) in accs:
                            for bi_ in (oi, si_):
                                P.pe(lambda e: e.matmul(ps[bi_][:, :], lhsT=zeros_b[:, 0:128], rhs=zeros_b[:, :], start=True, stop=False), R=[R_const], W=[R_ps[bi_]])
                        n_i = len(i_list)
                        for ii, i in enumerate(i_list):
                            c_lo, c_hi = colrange_fn(i)
                            for (oi, si_, m) in accs:
                                sci = 4 + nxt("scb", 3)
                                score_mms(sci, i, m, c_lo, c_hi)
                                pti = nxt("pt", 4)
                                P.op("act", lambda e: e.activation(out=PT[pti][:, c_lo:c_hi], in_=ps[sci][:, c_lo:c_hi], func=AF.Exp, bias=float(cfn(i)), scale=1.0),
                                     R=[R_ps[sci]], W=[R_PT[pti]])
                                lastf = (ii == n_i - 1)
                                P.pe(lambda e: e.matmul(ps[oi][:, c_lo:c_hi], lhsT=Vl_fn(i), rhs=PT[pti][:, c_lo:c_hi], start=False, stop=lastf), R=[R_PT[pti], R_V], W=[R_ps[oi]])
                                P.pe(lambda e: e.matmul(ps[si_][:, c_lo:c_hi], lhsT=ones_b[:], rhs=PT[pti][:, c_lo:c_hi], start=False, stop=lastf), R=[R_PT[pti], R_const], W=[R_ps[si_]])

                    for g in range(4):
                        bi = load_w(C_VA + g * 128, 128)
                        proj_tok(bi, 128, lambda st_, g=g: VV[:, st_, g * 128:(g + 1) * 128], R_VV)
                    for qi_ in (0, 1):
                        P.dma("sp", lambda e: e.dma_start(out=QK[qi_][64:67, :], in_=c_qaug), W=[R_QK[qi_]])
                    for h in range(4):
                        Q1, Q2, K1, K2 = QK[0], QK[1], QK[2], QK[3]
                        for kt in (2, 3):
                            P.dma("sp", lambda e: e.dma_start(out=QK[kt][64:67, :], in_=c_kaug[h]), W=[R_QK[kt]])
                        bq = load_w(C_QA + h * 128, 128)
                        bk = load_w(C_KA + h * 128, 128)
                        proj_feat(bq, 0, 64, Q1, R_QK[0], 0, 0.125)
                        proj_feat(bq, 64, 64, Q2, R_QK[1], 0, 0.125)
                        proj_feat(bk, 0, 64, K1, R_QK[2], 0, 1.0)
                        proj_feat(bk, 64, 64, K2, R_QK[3], 0, 1.0)
                        sig = SIG_A[h]
                        for J in range(4):
                            def colr(i, J=J):
                                return (128 * max(0, i - 4 * J), 512)

                            def smm(sci, i, m, c_lo, c_hi, J=J, h=h):
                                isd = i >= 4 * J
                                P.pe(lambda e: e.matmul(ps[sci][:, c_lo:c_hi], lhsT=QK[2 + m][0:67, i * 128:(i + 1) * 128], rhs=QK[m][0:67, J * 512 + c_lo:J * 512 + c_hi], start=True, stop=not isd),
                                     R=[R_QK[2 + m], R_QK[m]], W=[R_ps[sci]])
                                if isd:
                                    a = i - 4 * J
                                    P.pe(lambda e: e.matmul(ps[sci][:, a * 128:(a + 1) * 128], lhsT=ident_b[:], rhs=dmask_b[:, h, :], start=False, stop=True), R=[R_const], W=[R_ps[sci]])
                            attn_block(J, list(range(4 * J + 4)), colr, smm, lambda i, J=J: -sig * (512 * J - 128 * i),
                                       lambda i, h=h: VV[:, i, h * 128:(h + 1) * 128], R_VV, [(0, 1, 0), (2, 3, 1)], None)
                            recip_safe(ft[0], R_ft[0], ps[1], R_ps[1], 0, 128)
                            recip_safe(ft[1], R_ft[1], ps[3], R_ps[3], 0, 128)
                            P.op("dve", lambda e: e.tensor_tensor(out=ft[0][:], in0=ps[0][:, :], in1=ft[0][:], op=ALU.mult), R=[R_ps[0], R_ft[0]], W=[R_ft[0]])
                            P.op("dve", lambda e: e.tensor_tensor(out=ft[1][:], in0=ps[2][:, :], in1=ft[1][:], op=ALU.mult), R=[R_ps[2], R_ft[1]], W=[R_ft[1]])
                            P.op("dve", lambda e: e.scalar_tensor_tensor(out=ft[2][:], in0=ft[1][:], scalar=neglam, in1=ft[0][:], op0=ALU.mult, op1=ALU.add), R=[R_ft[0], R_ft[1], R_sm], W=[R_ft[2]])
                            P.op("pool", lambda e: e.tensor_tensor(out=sqb[:], in0=ft[2][:], in1=ft[2][:], op=ALU.mult), R=[R_ft[2]], W=[R_sqb])
                            P.pe(lambda e: e.matmul(ps[6][:, :], lhsT=ones_b[:], rhs=sqb[:], start=True, stop=True), R=[R_sqb, R_const], W=[R_ps[6]])
                            P.op("dve", lambda e: e.tensor_scalar(out=ft[3][:], in0=ps[6][:, :], scalar1=1.0 / 128, scalar2=EPS, op0=ALU.mult, op1=ALU.add), R=[R_ps[6]], W=[R_ft[3]])
                            P.op("act", lambda e: e.activation(out=ft[3][:], in_=ft[3][:], func=AF.Ln), R=[R_ft[3]], W=[R_ft[3]])
                            P.op("act", lambda e: e.activation(out=ft[3][:], in_=ft[3][:], func=AF.Exp, scale=-0.5), R=[R_ft[3]], W=[R_ft[3]])
                            P.op("dve", lambda e: e.scalar_tensor_tensor(out=mixT[:, h, J * 512:(J + 1) * 512], in0=ft[2][:], scalar=gscale, in1=ft[3][:], op0=ALU.mult, op1=ALU.mult),
                                 R=[R_ft[2], R_ft[3], R_sm], W=[R_mix[h][J]])

                    for g in range(2):
                        bi = load_w(C_VB + g * 128, 128)
                        proj_tok(bi, 128, lambda st_, g=g: VV[:, st_, g * 128:(g + 1) * 128], R_VV)
                    for h in range(4):
                        z0 = 64 if h % 2 == 0 else 0
                        P.op("pool", lambda e: e.memset(QK[2 + h][z0:z0 + 64, :], 0.0), W=[R_QK[2 + h]])
                    for g in range(2):
                        bq = load_w(C_QB + g * 128, 128)
                        bk = load_w(C_KB + g * 128, 128)
                        proj_feat(bq, 0, 128, QK[g], R_QK[g], 0, 0.125)
                        proj_feat_split(bk, QK[2 + 2 * g], R_QK[2 + 2 * g], QK[3 + 2 * g], R_QK[3 + 2 * g], 1.0)
                    for h in range(4):
                        g = h // 2
                        r0 = (h % 2) * 64
                        for J in range(4):
                            i_list = list(range(max(0, 4 * J - 4), 4 * J + 4))

                            def colr(i, J=J):
                                a_lo = max(0, i - 4 * J); a_hi = min(3, i + 4 - 4 * J)
                                return (128 * a_lo, 128 * (a_hi + 1))

                            def smm(sci, i, m, c_lo, c_hi, J=J, h=h, g=g):
                                P.pe(lambda e: e.matmul(ps[sci][:, c_lo:c_hi], lhsT=QK[2 + h][:, i * 128:(i + 1) * 128], rhs=QK[g][:, J * 512 + c_lo:J * 512 + c_hi], start=True, stop=False),
                                     R=[R_QK[2 + h], R_QK[g]], W=[R_ps[sci]])
                                a_lo, a_hi = c_lo // 128, c_hi // 128 - 1
                                for a in range(a_lo, a_hi + 1):
                                    dl = 4 * J + a - i
                                    piece = {0: 0, 1: 1, 2: 2, 3: 2, 4: 3}[dl]
                                    P.pe(lambda e: e.matmul(ps[sci][:, a * 128:(a + 1) * 128], lhsT=ident_b[:], rhs=relb_b[:, h * 4 + piece, :], start=False, stop=(a == a_hi)),
                                         R=[R_const, R_relb], W=[R_ps[sci]])
                            attn_block(J, i_list, colr, smm, lambda i: 0.0, lambda i, g=g: VV[:, i, g * 128:(g + 1) * 128], R_VV, [(0, 1, 0)], None)
                            recip_safe(ft[0], R_ft[0], ps[1], R_ps[1], r0, r0 + 64)
                            P.op("dve", lambda e: e.tensor_tensor(out=mixT[r0:r0 + 64, 4 + g, J * 512:(J + 1) * 512], in0=ps[0][r0:r0 + 64, :], in1=ft[0][r0:r0 + 64, :], op=ALU.mult),
                                 R=[R_ps[0], R_ft[0]], W=[R_mix[4 + g][J]])

                    for g in range(2):
                        bi = load_w(C_VC + g * 128, 128)
                        proj_tok(bi, 128, lambda st_, g=g: VV[:, st_, g * 128:(g + 1) * 128], R_VV)
                    for h in range(4):
                        P.dma("sp", lambda e: e.dma_start(out=QK[h][64:67, :], in_=c_kaug[4 + h]), W=[R_QK[h]])
                        P.dma("sp", lambda e: e.dma_start(out=QK[4 + h][64:67, :], in_=c_qaug), W=[R_QK[4 + h]])
                    for g in range(2):
                        bq = load_w(C_QC + g * 128, 128)
                        bk = load_w(C_KC + g * 128, 128)
                        proj_feat(bq, 0, 64, QK[4 + 2 * g], R_QK[4 + 2 * g], 0, 0.125)
                        proj_feat(bq, 64, 64, QK[5 + 2 * g], R_QK[5 + 2 * g], 0, 0.125)
                        proj_feat(bk, 0, 64, QK[2 * g], R_QK[2 * g], 0, 1.0)
                        proj_feat(bk, 64, 64, QK[2 * g + 1], R_QK[2 * g + 1], 0, 1.0)
                    for g, nh in ((0, 3), (1, 3), (2, 2)):
                        bi = load_w(C_QI + g * 96, 32 * nh)
                        proj_feat(bi, 0, 32 * nh, QK[8 + g], R_QK[8 + g], 0, 1.0)
                    bi = load_w(C_KI, 32, rep=3)
                    proj_feat(bi, 0, 96, QK[11], R_QK[11], 0, 1.0)
                    bi = load_w(C_WI, 8)
                    proj_tok(bi, 8, lambda st_: wtok[:, st_, :], R_wtok)

                    for J in range(4):
                        for a in range(4):
                            j = 4 * J + a
                            L = 128 * (j + 1)
                            if j < 2:
                                P.op("pool", lambda e: e.memset(negmask[:, 0:L], 0.0), W=[R_negm])
                            else:
                                for h8 in range(8):
                                    P.op("dve", lambda e: e.tensor_scalar(out=diag[:, h8, :], in0=ident_f[:], scalar1=wtok[:, j, h8:h8 + 1], scalar2=W_SCALE, op0=ALU.mult, op1=ALU.mult),
                                         R=[R_const, R_wtok], W=[R_diag])
                                nsc = (L + 511) // 512
                                for sc_i in range(nsc):
                                    w_ = min(512, L - 512 * sc_i)
                                    for h8 in range(8):
                                        gq, rr = h8 // 3, h8 % 3
                                        pi = 4 + nxt("pj", 2)
                                        P.pe(lambda e: e.matmul(ps[pi][:, 0:w_], lhsT=QK[8 + gq][32 * rr:32 * rr + 32, j * 128:(j + 1) * 128], rhs=QK[11][32 * rr:32 * rr + 32, sc_i * 512:sc_i * 512 + w_], start=True, stop=True),
                                             R=[R_QK[8 + gq], R_QK[11]], W=[R_ps[pi]])
                                        ri = nxt("relu", 3)
                                        P.op("act", lambda e: e.activation(out=relu_sb[ri][:, 0:w_], in_=ps[pi][:, 0:w_], func=AF.Relu), R=[R_ps[pi]], W=[R_relu[ri]])
                                        P.pe(lambda e: e.matmul(ps[6][:, 0:w_], lhsT=diag[:, h8, :], rhs=relu_sb[ri][:, 0:w_], start=(h8 == 0), stop=(h8 == 7)), R=[R_diag, R_relu[ri]], W=[R_ps[6]])
                                    P.op("dve", lambda e: e.tensor_copy(out=score[:, sc_i * 512:sc_i * 512 + w_], in_=ps[6][:, 0:w_]), R=[R_ps[6]], W=[R_score])
                                P.op("dve", lambda e: e.tensor_reduce(out=sm[:, 8:9], in_=score[:, 0:L], axis=AX.X, op=ALU.min), R=[R_score], W=[R_sm])
                                P.op("pool", lambda e: e.memset(score[0:64, L - 64:L], -1e30), R=[R_sm], W=[R_score])
                                P.op("dve", lambda e: e.reduce_max(out=sm[:, 9:10], in_=score[:, 0:L], axis=AX.X), R=[R_score], W=[R_sm])
                                P.op("dve", lambda e: e.scalar_tensor_tensor(out=sm[:, 10:11], in0=sm[:, 9:10], scalar=1e-20, in1=sm[:, 8:9], op0=ALU.add, op1=ALU.subtract), R=[R_sm], W=[R_sm])
                                P.op("dve", lambda e: e.reciprocal(out=sm[:, 11:12], in_=sm[:, 10:11]), R=[R_sm], W=[R_sm])
                                P.op("dve", lambda e: e.tensor_scalar(out=score[:, 0:L], in0=score[:, 0:L], scalar1=sm[:, 8:9], scalar2=sm[:, 11:12], op0=ALU.subtract, op1=ALU.mult), R=[R_score, R_sm], W=[R_score])
                                P.op("dve", lambda e: e.memset(sm[:, 12:13], 0.5), W=[R_sm])
                                for n in range(NIT):
                                    dlt = 2.0 ** -(n + 2)
                                    P.op("dve", lambda e: e.tensor_scalar(out=negmask[:, 0:L], in0=score[:, 0:L], scalar1=sm[:, 12:13], scalar2=None, op0=ALU.is_gt, op1=ALU.add, accum_out=sm[:, 13:14]),
                                         R=[R_score, R_sm], W=[R_negm, R_sm])
                                    P.op("dve", lambda e: e.tensor_scalar(out=sm[:, 14:15], in0=sm[:, 13:14], scalar1=255.5, scalar2=2.0 * dlt, op0=ALU.is_gt, op1=ALU.mult), R=[R_sm], W=[R_sm])
                                    P.op("dve", lambda e: e.scalar_tensor_tensor(out=sm[:, 12:13], in0=sm[:, 12:13], scalar=-dlt, in1=sm[:, 14:15], op0=ALU.add, op1=ALU.add), R=[R_sm], W=[R_sm])
                                P.op("dve", lambda e: e.tensor_scalar(out=negmask[:, 0:L], in0=score[:, 0:L], scalar1=sm[:, 12:13], scalar2=NEG, op0=ALU.is_le, op1=ALU.mult), R=[R_score, R_sm], W=[R_negm])
                            for i0 in range(0, j + 1, 8):
                                n_ = min(8, j + 1 - i0)
                                for ii in range(n_):
                                    P.pe(lambda e: e.transpose(out=psT[:, ii, :], in_=negmask[:, (i0 + ii) * 128:(i0 + ii + 1) * 128], identity=ident_b[:]), R=[R_negm, R_const], W=[R_psT])
                                P.op("dve", lambda e: e.tensor_copy(out=nmT[:, i0:i0 + n_, a * 128:(a + 1) * 128], in_=psT[:, 0:n_, :]), R=[R_psT], W=[R_nmT])
                        for h in range(4):
                            g = h // 2
                            r0 = (h % 2) * 64
                            sig = SIG_C[h]

                            def colr(i, J=J):
                                return (128 * max(0, i - 4 * J), 512)

                            def smm(sci, i, m, c_lo, c_hi, J=J, h=h):
                                P.pe(lambda e: e.matmul(ps[sci][:, c_lo:c_hi], lhsT=QK[h][0:67, i * 128:(i + 1) * 128], rhs=QK[4 + h][0:67, J * 512 + c_lo:J * 512 + c_hi], start=True, stop=False),
                                     R=[R_QK[h], R_QK[4 + h]], W=[R_ps[sci]])
                                if i >= 4 * J:
                                    a = i - 4 * J
                                    P.pe(lambda e: e.matmul(ps[sci][:, a * 128:(a + 1) * 128], lhsT=ident_b[:], rhs=dmask_b[:, 4 + h, :], start=False, stop=False), R=[R_const], W=[R_ps[sci]])
                                P.pe(lambda e: e.matmul(ps[sci][:, c_lo:c_hi], lhsT=ident_b[:], rhs=nmT[:, i, c_lo:c_hi], start=False, stop=True), R=[R_const, R_nmT], W=[R_ps[sci]])
                            attn_block(J, list(range(4 * J + 4)), colr, smm, lambda i, J=J, sig=sig: -sig * (512 * J - 128 * i),
                                       lambda i, g=g: VV[:, i, g * 128:(g + 1) * 128], R_VV, [(0, 1, 0)], None)
                            recip_safe(ft[0], R_ft[0], ps[1], R_ps[1], r0, r0 + 64)
                            P.op("dve", lambda e: e.tensor_tensor(out=mixT[r0:r0 + 64, 6 + g, J * 512:(J + 1) * 512], in0=ps[0][r0:r0 + 64, :], in1=ft[0][r0:r0 + 64, :], op=ALU.mult),
                                 R=[R_ps[0], R_ft[0]], W=[R_mix[6 + g][J]])
                    P.barrier()

                if dbg == "mix":
                    allmix = [R_mix[c][j] for c in range(8) for j in range(4)]
                    P.dma("sp", lambda e: e.dma_start(out=dbg_out[b], in_=mixT[:]), R=allmix, W=[R_dbg])
                    P.barrier()
                    continue

                with ExitStack() as es:
                    wo_b = sbc(es, "wo_b", [128, 8, D], BF16); R_wo = Res("wo")
                    wst2 = [sbc(es, "wst2_%d" % i, [128, D], F32) for i in range(2)]; R_wst2 = [Res("wst2") for _ in range(2)]
                    g1 = sbc(es, "g1", [128, D], F32); b1 = sbc(es, "b1", [128, D], F32); R_gb = Res("gb")
                    wr_f = sbc(es, "wr_f", [128, 8, NE], F32); br_t = sbc(es, "br_t", [128, NE], F32)
                    xt_ = [sbc(es, "xt%d" % i, [128, D], F32) for i in range(2)]; R_xt = [Res("xt") for _ in range(2)]
                    vt = [sbc(es, "vt%d" % i, [128, D], F32) for i in range(2)]; R_vt = [Res("vt") for _ in range(2)]
                    x1t = [sbc(es, "x1t%d" % i, [128, D], F32) for i in range(2)]; R_x1t = [Res("x1t") for _ in range(2)]
                    x1b = [sbc(es, "x1b%d" % i, [128, D], BF16) for i in range(2)]; R_x1b = [Res("x1b") for _ in range(2)]
                    x1T = sbc(es, "x1T", [128, 8, 128], F32); R_x1T = Res("x1T")
                    junk = sbc(es, "junk", [128, D], BF16); R_junk = Res("junk")
                    lst = sbc(es, "lst", [128, 8], F32); R_lst = Res("lst")
                    lg = sbc(es, "lg", [128, NE], F32); R_lg = Res("lg")
                    mx8 = sbc(es, "mx8", [128, 8], F32); R_mx = Res("mx")
                    rs = sbc(es, "rs", [128, 16], F32); R_rs = Res("rs")
                    maskb = sbc(es, "maskb", [128, NE], BF16); R_maskb = Res("maskb")
                    slotm = sbc(es, "slotm", [128, NE], F32); R_slotm = Res("slotm")
                    oh = sbc(es, "oh", [128, NE], F32); R_oh = Res("oh")
                    for c in range(8):
                        i2 = c % 2
                        P.dma("sp", lambda e: e.dma_start(out=wst2[i2][:], in_=w_out[l, c * 128:(c + 1) * 128, :]), W=[R_wst2[i2]])
                        P.op("pool", lambda e: e.tensor_copy(out=wo_b[:, c, :], in_=wst2[i2][:]), R=[R_wst2[i2]], W=[R_wo])
                    P.dma("sp", lambda e: e.dma_start(out=g1[:], in_=ln1g[l:l + 1, :].to_broadcast([128, D])), W=[R_gb])
                    P.dma("sp", lambda e: e.dma_start(out=b1[:], in_=ln1b[l:l + 1, :].to_broadcast([128, D])), W=[R_gb])
                    P.dma("sp", lambda e: e.dma_start(out=wr_f[:], in_=w_rt[l].rearrange("(k p) c -> p k c", p=128)), W=[R_gb])
                    P.dma("sp", lambda e: e.dma_start(out=br_t[:], in_=b_rt[l:l + 1, :].to_broadcast([128, NE])), W=[R_gb])
                    for tt in range(16):
                        i2 = tt % 2
                        gt_ = b * 16 + tt
                        rows = slice(tok0 + tt * 128, tok0 + (tt + 1) * 128)
                        P.dma("sp", lambda e: e.dma_start(out=xt_[i2][:], in_=xsrc[rows, :]), R=[R_xsrc], W=[R_xt[i2]])
                        for half in range(2):
                            pi = nxt("wo", 2)
                            for c in range(8):
                                P.pe(lambda e: e.matmul(ps[pi][:, :], lhsT=mixT[:, c, tt * 128:(tt + 1) * 128], rhs=wo_b[:, c, half * 512:(half + 1) * 512], start=(c == 0), stop=(c == 7)),
                                     R=[R_mix[c][tt // 4], R_wo], W=[R_ps[pi]])
                            P.op("dve", lambda e: e.scalar_tensor_tensor(out=vt[i2][:, half * 512:(half + 1) * 512], in0=xt_[i2][:, half * 512:(half + 1) * 512], scalar=ALPHA, in1=ps[pi][:, :], op0=ALU.mult, op1=ALU.add),
                                 R=[R_xt[i2], R_ps[pi]], W=[R_vt[i2]])
                        layer_norm(vt[i2], R_vt[i2], g1, b1, R_gb, x1t[i2], R_x1t[i2], lst, R_lst, junk, R_junk)
                        if dbg == "x1":
                            P.dma("pool", lambda e: e.dma_start(out=dbg_out[rows, :], in_=x1t[i2][:]), R=[R_x1t[i2]], W=[R_dbg])
                        P.dma("pool", lambda e: e.dma_start(out=x1s[rows, :], in_=x1t[i2][:]), R=[R_x1t[i2]], W=[R_x1s])
                        P.op("act", lambda e: e.activation(out=x1b[i2][:], in_=x1t[i2][:], func=AF.Copy), R=[R_x1t[i2]], W=[R_x1b[i2]])
                        for hf in range(2):
                            for k4 in range(4):
                                k = hf * 4 + k4
                                P.pe(lambda e: e.transpose(out=ps[2 + hf][:, k4 * 128:(k4 + 1) * 128], in_=x1t[i2][:, k * 128:(k + 1) * 128], identity=ident_f[:]), R=[R_x1t[i2], R_const], W=[R_ps[2 + hf]])
                            P.op("act", lambda e: e.activation(out=x1T[:, hf * 4:(hf + 1) * 4, :], in_=ps[2 + hf][:, :].rearrange("p (a b) -> p a b", a=4), func=AF.Copy), R=[R_ps[2 + hf]], W=[R_x1T])
                        for k in range(8):
                            P.pe(lambda e: e.matmul(ps[4][:, 0:NE], lhsT=x1T[:, k, :], rhs=wr_f[:, k, :], start=(k == 0), stop=(k == 7)), R=[R_x1T, R_gb], W=[R_ps[4]])
                        P.op("dve", lambda e: e.tensor_tensor(out=lg[:], in0=ps[4][:, 0:NE], in1=br_t[:], op=ALU.add), R=[R_ps[4], R_gb], W=[R_lg])
                        P.op("dve", lambda e: e.max(out=mx8[:], in_=lg[:]), R=[R_lg], W=[R_mx])
                        P.op("dve", lambda e: e.tensor_scalar(out=rs[:, 0:1], in0=mx8[:, 0:1], scalar1=-1.0, scalar2=None, op0=ALU.mult), R=[R_mx], W=[R_rs])
                        P.op("act", lambda e: e.activation(out=rs[:, 4:8], in_=mx8[:, 0:4], func=AF.Exp, bias=rs[:, 0:1], scale=1.0, accum_out=rs[:, 1:2]), R=[R_mx, R_rs], W=[R_rs])
                        P.op("dve", lambda e: e.reciprocal(out=rs[:, 2:3], in_=rs[:, 1:2]), R=[R_rs], W=[R_rs])
                        P.op("dve", lambda e: e.tensor_scalar(out=gates[:, gt_ * 4:gt_ * 4 + 4], in0=rs[:, 4:8], scalar1=rs[:, 2:3], scalar2=None, op0=ALU.mult), R=[R_rs], W=[R_gates])
                        P.op("dve", lambda e: e.tensor_scalar(out=maskb[:], in0=lg[:], scalar1=mx8[:, 3:4], scalar2=None, op0=ALU.is_ge), R=[R_lg, R_mx], W=[R_maskb])
                        P.pe(lambda e: e.matmul(ps[5][:, 0:NE], lhsT=ltri_b[:], rhs=maskb[:], start=True, stop=True), R=[R_maskb, R_const], W=[R_ps[5]])
                        P.pe(lambda e: e.matmul(ps[6][:, 0:NE], lhsT=ones_b[:], rhs=maskb[:], start=True, stop=True), R=[R_maskb, R_const], W=[R_ps[6]])
                        P.op("dve", lambda e: e.tensor_tensor(out=slotm[:], in0=ps[5][:, 0:NE], in1=base_cnt[:], op=ALU.add), R=[R_ps[5], R_base], W=[R_slotm])
                        P.op("dve", lambda e: e.tensor_tensor(out=slotm[:], in0=slotm[:], in1=eoff[:], op=ALU.add), R=[R_slotm, R_const], W=[R_slotm])
                        P.op("dve", lambda e: e.tensor_tensor(out=base_cnt[:], in0=ps[6][:, 0:NE], in1=base_cnt[:], op=ALU.add), R=[R_ps[6], R_base], W=[R_base])
                        for k in range(4):
                            P.op("dve", lambda e: e.tensor_scalar(out=oh[:], in0=lg[:], scalar1=mx8[:, k:k + 1], scalar2=None, op0=ALU.is_equal), R=[R_lg, R_mx], W=[R_oh])
                            P.op("dve", lambda e: e.tensor_tensor(out=oh[:], in0=oh[:], in1=slotm[:], op=ALU.mult), R=[R_oh, R_slotm], W=[R_oh])
                            P.op("dve", lambda e: e.reduce_sum(out=slotf[:, gt_ * 4 + k:gt_ * 4 + k + 1], in_=oh[:], axis=AX.X), R=[R_oh], W=[R_slot])
                        P.op("dve", lambda e: e.tensor_copy(out=sloti[:, gt_ * 4:gt_ * 4 + 4], in_=slotf[:, gt_ * 4:gt_ * 4 + 4]), R=[R_slot], W=[R_slot])
                        for k in range(4):
                            P.dma("pool", lambda e: e.indirect_dma_start(out=xg[:, :], out_offset=bass.IndirectOffsetOnAxis(ap=sloti[:, gt_ * 4 + k:gt_ * 4 + k + 1], axis=0), in_=x1b[i2][:, :], in_offset=None),
                                  R=[R_x1b[i2], R_slot], W=[R_xg])
                    P.barrier()
        if dbg in ("mix", "x1"):
            break

        with ExitStack() as es:
            def sbm(name, shape, dt):
                return es.enter_context(nc.sbuf_tensor(name + "_m%d" % l, list(shape), dt))
            wgu_b = [sbm("wgu%d" % i, [128, 8, 2 * D], BF16) for i in range(2)]; R_wgu = [[Res("wgu") for _ in range(8)] for _ in range(2)]
            wdn_b = [sbm("wdn%d" % i, [128, 8, D], BF16) for i in range(2)]; R_wdn = [[Res("wdn") for _ in range(8)] for _ in range(2)]
            stg = [sbm("stg%d" % i, [128, 2 * D], F32) for i in range(3)]; R_stg = [Res("stg") for _ in range(3)]
            xr = [sbm("xr%d" % i, [128, D], BF16) for i in range(3)]; R_xr = [Res("xr") for _ in range(3)]
            xeT = [sbm("xeT%d" % i, [128, 8, CAP], BF16) for i in range(2)]; R_xeT = [Res("xeT") for _ in range(2)]
            GT = sbm("GT", [128, 8, CAP], BF16); R_GT = [Res("GT%d" % c) for c in range(8)]
            et = [sbm("et%d" % i, [128, 512], F32) for i in range(6)]; R_et = [Res("et") for _ in range(6)]
            yt = [sbm("yt%d" % i, [128, D], F32) for i in range(2)]; R_yt = [Res("yt") for _ in range(2)]
            bgT = sbm("bgT", [128, NE * 16], F32); R_bgT = Res("bgT")
            bgs = sbm("bgs", [128, 4, 128], F32); R_bgs = Res("bgs")
            bdb = [sbm("bdb%d" % i, [128, D], F32) for i in range(2)]; R_bdb = [Res("bdb") for _ in range(2)]
            P.dma("sp", lambda e: e.dma_start(out=bgs[:], in_=b_gu[l].rearrange("(a r) p -> r a p", r=128)), W=[R_bgs])
            for a in range(4):
                P.pe(lambda e: e.transpose(out=ps[6][:, a * 128:(a + 1) * 128], in_=bgs[:, a, :], identity=ident_f[:]), R=[R_bgs, R_const], W=[R_ps[6]])
            P.op("dve", lambda e: e.tensor_copy(out=bgT[:], in_=ps[6][:, :]), R=[R_ps[6]], W=[R_bgT])

            def load_expert(e_):
                s2 = e_ % 2
                for k in range(8):
                    si = nxt("stg", 3)
                    P.dma("sp", lambda e: e.dma_start(out=stg[si][:], in_=w_gu[l, e_, k * 128:(k + 1) * 128, :]), W=[R_stg[si]])
                    P.op("pool" if k % 2 == 0 else "dve", lambda e: e.tensor_copy(out=wgu_b[s2][:, k, :], in_=stg[si][:]), R=[R_stg[si]], W=[R_wgu[s2][k]])
                for k in range(8):
                    si = nxt("stg", 3)
                    P.dma("sp", lambda e: e.dma_start(out=stg[si][:, 0:D], in_=w_dn[l, e_, k * 128:(k + 1) * 128, :]), W=[R_stg[si]])
                    P.op("pool" if k % 2 == 0 else "dve", lambda e: e.tensor_copy(out=wdn_b[s2][:, k, :], in_=stg[si][:, 0:D]), R=[R_stg[si]], W=[R_wdn[s2][k]])
                P.dma("sp", lambda e: e.dma_start(out=bdb[s2][:], in_=b_dn[l, e_:e_ + 1, :].to_broadcast([128, D])), W=[R_bdb[s2]])

            load_expert(0)
            for e_ in range(NE):
                s2 = e_ % 2
                if e_ + 1 < NE:
                    load_expert(e_ + 1)
                for st_ in range(CAP // 128):
                    xi = nxt("xr", 3)
                    P.dma("sp", lambda e: e.dma_start(out=xr[xi][:], in_=xg[e_ * CAP + st_ * 128:e_ * CAP + (st_ + 1) * 128, :]), R=[R_xg], W=[R_xr[xi]])
                    for k in range(8):
                        P.pe(lambda e: e.transpose(out=psT[:, k, :], in_=xr[xi][:, k * 128:(k + 1) * 128], identity=ident_b[:]), R=[R_xr[xi], R_const], W=[R_psT])
                    P.op("act", lambda e: e.activation(out=xeT[s2][:, :, st_ * 128:(st_ + 1) * 128], in_=psT[:, :, :], func=AF.Copy), R=[R_psT], W=[R_xeT[s2]])
                for c in range(8):
                    for (s0, sw) in ((0, 512), (512, CAP - 512)):
                        pg = nxt("pg", 2) * 2
                        for k in range(8):
                            P.pe(lambda e: e.matmul(ps[pg][:, 0:sw], lhsT=wgu_b[s2][:, k, c * 128:(c + 1) * 128], rhs=xeT[s2][:, k, s0:s0 + sw], start=(k == 0), stop=(k == 7)),
                                 R=[R_wgu[s2][k], R_xeT[s2]], W=[R_ps[pg]])
                        for k in range(8):
                            P.pe(lambda e: e.matmul(ps[pg + 1][:, 0:sw], lhsT=wgu_b[s2][:, k, D + c * 128:D + (c + 1) * 128], rhs=xeT[s2][:, k, s0:s0 + sw], start=(k == 0), stop=(k == 7)),
                                 R=[R_wgu[s2][k], R_xeT[s2]], W=[R_ps[pg + 1]])
                        ei = nxt("et", 2) * 3
                        bgc = bgT[:, e_ * 16 + c:e_ * 16 + c + 1]
                        buc = bgT[:, e_ * 16 + 8 + c:e_ * 16 + 8 + c + 1]
                        P.op("dve", lambda e: e.tensor_scalar(out=et[ei][:, 0:sw], in0=ps[pg][:, 0:sw], scalar1=bgc, scalar2=7.0, op0=ALU.add, op1=ALU.min), R=[R_ps[pg], R_bgT], W=[R_et[ei]])
                        P.op("act", lambda e: e.activation(out=et[ei + 1][:, 0:sw], in_=et[ei][:, 0:sw], func=AF.Sigmoid, scale=1.702), R=[R_et[ei]], W=[R_et[ei + 1]])
                        P.op("act", lambda e: e.activation(out=et[ei + 2][:, 0:sw], in_=ps[pg + 1][:, 0:sw], func=AF.Identity, bias=buc, scale=1.0), R=[R_ps[pg + 1], R_bgT], W=[R_et[ei + 2]])
                        P.op("pool", lambda e: e.tensor_scalar(out=et[ei + 2][:, 0:sw], in0=et[ei + 2][:, 0:sw], scalar1=7.0, scalar2=-7.0, op0=ALU.min, op1=ALU.max), R=[R_et[ei + 2]], W=[R_et[ei + 2]])
                        P.op("pool", lambda e: e.tensor_tensor(out=et[ei][:, 0:sw], in0=et[ei][:, 0:sw], in1=et[ei + 1][:, 0:sw], op=ALU.mult), R=[R_et[ei], R_et[ei + 1]], W=[R_et[ei]])
                        P.op("dve", lambda e: e.scalar_tensor_tensor(out=GT[:, c, s0:s0 + sw], in0=et[ei + 2][:, 0:sw], scalar=1.0, in1=et[ei][:, 0:sw], op0=ALU.add, op1=ALU.mult), R=[R_et[ei], R_et[ei + 2]], W=[R_GT[c]])
                for st_ in range(CAP // 128):
                    yi = nxt("yt", 2)
                    for half in range(2):
                        pi = 4 + nxt("pd", 2)
                        for c in range(8):
                            P.pe(lambda e: e.matmul(ps[pi][:, :], lhsT=GT[:, c, st_ * 128:(st_ + 1) * 128], rhs=wdn_b[s2][:, c, half * 512:(half + 1) * 512], start=(c == 0), stop=(c == 7)),
                                 R=[R_GT[c], R_wdn[s2][c]], W=[R_ps[pi]])
                        P.op("dve", lambda e: e.tensor_tensor(out=yt[yi][:, half * 512:(half + 1) * 512], in0=ps[pi][:, :], in1=bdb[s2][:, half * 512:(half + 1) * 512], op=ALU.add),
                             R=[R_ps[pi], R_bdb[s2]], W=[R_yt[yi]])
                    P.dma("pool", lambda e: e.dma_start(out=yg[e_ * CAP + st_ * 128:e_ * CAP + (st_ + 1) * 128, :], in_=yt[yi][:]), R=[R_yt[yi]], W=[R_yg])
            P.barrier()

        with ExitStack() as es:
            def sbm(name, shape, dt):
                return es.enter_context(nc.sbuf_tensor(name + "_c%d" % l, list(shape), dt))
            yk = [sbm("yk%d" % i, [128, D], F32) for i in range(8)]; R_yk = [Res("yk") for _ in range(8)]
            xa = [sbm("xa%d" % i, [128, D], F32) for i in range(2)]; R_xa = [Res("xa") for _ in range(2)]
            xo = [sbm("xo%d" % i, [128, D], F32) for i in range(2)]; R_xo = [Res("xo") for _ in range(2)]
            g2 = sbm("g2", [128, D], F32); b2 = sbm("b2", [128, D], F32); R_gb2 = Res("gb2")
            junk2 = sbm("junk2", [128, D], BF16); R_junk2 = Res("junk2")
            lst2 = sbm("lst2", [128, 8], F32); R_lst2 = Res("lst2")
            P.dma("sp", lambda e: e.dma_start(out=g2[:], in_=ln2g[l:l + 1, :].to_broadcast([128, D])), W=[R_gb2])
            P.dma("sp", lambda e: e.dma_start(out=b2[:], in_=ln2b[l:l + 1, :].to_broadcast([128, D])), W=[R_gb2])
            for tt in range(32):
                i2 = tt % 2
                rows = slice(tt * 128, (tt + 1) * 128)
                P.dma("sp", lambda e: e.dma_start(out=xa[i2][:], in_=x1s[rows, :]), R=[R_x1s], W=[R_xa[i2]])
                P.op("act", lambda e: e.activation(out=xa[i2][:], in_=xa[i2][:], func=AF.Copy, scale=ALPHA), R=[R_xa[i2]], W=[R_xa[i2]])
                for k in range(4):
                    yi = i2 * 4 + k
                    P.dma("pool", lambda e: e.indirect_dma_start(out=yk[yi][:, :], out_offset=None, in_=yg[:, :], in_offset=bass.IndirectOffsetOnAxis(ap=sloti[:, tt * 4 + k:tt * 4 + k + 1], axis=0)),
                          R=[R_yg, R_slot], W=[R_yk[yi]])
                    P.op("dve", lambda e: e.scalar_tensor_tensor(out=xa[i2][:], in0=yk[yi][:], scalar=gates[:, tt * 4 + k:tt * 4 + k + 1], in1=xa[i2][:], op0=ALU.mult, op1=ALU.add),
                         R=[R_yk[yi], R_gates, R_xa[i2]], W=[R_xa[i2]])
                layer_norm(xa[i2], R_xa[i2], g2, b2, R_gb2, xo[i2], R_xo[i2], lst2, R_lst2, junk2, R_junk2)
                P.dma("sp", lambda e: e.dma_start(out=xdst[rows, :], in_=xo[i2][:]), R=[R_xo[i2]], W=[R_xdst])
            P.barrier()

    P.finish()
    return nc, P


def _consts():
    bf = ml_dtypes.bfloat16
    ident = np.eye(128, dtype=np.float32)
    ltri = np.triu(np.ones((128, 128), np.float32), 1)
    si = np.arange(128)[:, None]; qi = np.arange(128)[None, :]
    dmask = np.zeros((128, 8, 128), np.float32)
    for h in range(8):
        sg = SIG8[h]
        d = np.where(si <= qi, 0.0, np.where((si // 64) == (qi // 64), -2.0 * sg * (si - qi), NEG))
        dmask[:, h, :] = d
    q = np.arange(S)
    qaug = np.stack([np.ones(S), -(q % 128).astype(np.float64), -(128.0 * ((q // 128) % 4))]).astype(np.float32)
    kaug = np.zeros((8, 3, S), np.float32)
    for h in range(8):
        kaug[h, 0] = SIG8[h] * (q % 128)
        kaug[h, 1] = SIG8[h]
        kaug[h, 2] = SIG8[h]
    eoff = np.tile((np.arange(NE, dtype=np.float32) * CAP)[None, :], (128, 1))
    return {"c_ident": ident.astype(bf), "c_identf": ident, "c_ltri": ltri.astype(bf), "c_dmask": dmask.astype(bf),
            "c_qaug": qaug.astype(bf), "c_kaug": kaug.astype(bf), "c_eoff": eoff}


def _relb_pieces(rel_bias):
    NLn = rel_bias.shape[0]
    si = np.arange(128)[:, None]; qi = np.arange(128)[None, :]
    out = np.empty((NLn, 128, 16, 128), np.float32)
    for h in range(4):
        rel0 = qi - si
        idx0 = np.clip(rel0, -128, 128) + 128
        m0 = ((si // 64) == 1) & ((qi // 64) == 0)
        idx1 = np.clip(128 + qi - si, -128, 128) + 128
        m4 = ((si // 64) == 0) & ((qi // 64) == 1)
        for ln in range(NLn):
            rb = rel_bias[ln, h]
            p0 = rb[idx0].copy(); p0[m0] = NEG
            p1 = rb[idx1]
            p2 = np.broadcast_to(rb[256], (128, 128))
            p4 = np.array(p2); p4[m4] = NEG
            out[ln, :, h * 4 + 0, :] = p0
            out[ln, :, h * 4 + 1, :] = p1
            out[ln, :, h * 4 + 2, :] = p2
            out[ln, :, h * 4 + 3, :] = p4
    return out


_CACHE = {}


def _get_prog(NL, lam_inits, dbg=None):
    key = (NL, tuple(lam_inits), dbg)
    if key not in _CACHE:
        _CACHE[key] = build(NL, lam_inits, dbg)[0]
    return _CACHE[key]


def _layer_inputs(inp, ls):
    f = lambda a: np.ascontiguousarray(a, dtype=np.float32)
    d = {
        "w_in": f(inp["w_in"][ls]),
        "lamv": f(np.stack([inp["lam_q1"][ls], inp["lam_k1"][ls], inp["lam_q2"][ls], inp["lam_k2"][ls]], axis=1)),
        "subln_g": f(inp["subln_g"][ls]),
        "relb": _relb_pieces(np.asarray(inp["rel_bias"][ls], np.float32)),
        "w_out": f(inp["w_out"][ls]),
        "ln1_g": f(inp["ln1_g"][ls]), "ln1_b": f(inp["ln1_b"][ls]),
        "w_router": f(inp["w_router"][ls]), "b_router": f(inp["b_router"][ls]),
        "w_gu": f(inp["w_gu"][ls]), "b_gu": f(inp["b_gu"][ls]).reshape(len(range(*ls.indices(DEPTH))), NE * 16, 128),
        "w_down": f(inp["w_down"][ls]), "b_down": f(inp["b_down"][ls]),
        "ln2_g": f(inp["ln2_g"][ls]), "ln2_b": f(inp["ln2_b"][ls]),
    }
    return d


FUSED = False


def kernel(**inp):
    x = np.ascontiguousarray(inp["x"], dtype=np.float32)
    consts = _consts()
    lam_inits_all = [0.8 - 0.6 * math.exp(-0.3 * l) for l in range(DEPTH)]
    xs = [x[c * NBL:(c + 1) * NBL].reshape(T, D) for c in range(NCORES)]
    if FUSED:
        nc = _get_prog(DEPTH, lam_inits_all)
        li = _layer_inputs(inp, slice(0, DEPTH))
        in_maps = [dict(li, x=xs[c], **consts) for c in range(NCORES)]
        res = run_bass_kernel_spmd(nc, in_maps, core_ids=list(range(NCORES)))
        xs = [res.results[c]["y"] for c in range(NCORES)]
    else:
        for l in range(DEPTH):
            nc = _get_prog(1, [lam_inits_all[l]])
            li = _layer_inputs(inp, slice(l, l + 1))
            in_maps = [dict(li, x=xs[c], **consts) for c in range(NCORES)]
            res = run_bass_kernel_spmd(nc, in_maps, core_ids=list(range(NCORES)))
            xs = [np.asarray(res.results[c]["y"]) for c in range(NCORES)]
    out = np.stack([xs[c].reshape(NBL, S, D) for c in range(NCORES)], axis=0).reshape(NCORES * NBL, S, D)
    return out.astype(np.float32)
```

```python
import math
from contextlib import ExitStack
import numpy as np
import ml_dtypes
import concourse.bass as bass
import concourse.mybir as mybir
from concourse.bass_utils import run_bass_kernel_spmd

F32 = mybir.dt.float32
BF16 = mybir.dt.bfloat16
I32 = mybir.dt.int32
AF = mybir.ActivationFunctionType
ALU = mybir.AluOpType
AX = mybir.AxisListType

NCORES = 8
DEPTH = 4
S = 2048
D = 1024
NBL = 2
T = NBL * S
DIN = 3368
NE = 32
CAP = 768
NSLOT = NE * CAP
ALPHA = (2 * DEPTH) ** 0.25
EPS = 1e-5
NEG = -30000.0
SIG_A = [2.0 ** -1, 2.0 ** -3, 2.0 ** -5, 2.0 ** -7]
SIG_C = [2.0 ** -2, 2.0 ** -4, 2.0 ** -6, 2.0 ** -8]
SIG8 = SIG_A + SIG_C
W_SCALE = (8 ** -0.5) * (32 ** -0.5)
NIT = 20
C_QA, C_KA, C_VA, C_QB, C_KB, C_VB, C_QC, C_KC, C_VC, C_QI, C_KI, C_WI = (
    0, 512, 1024, 1536, 1792, 2048, 2304, 2560, 2816, 3072, 3328, 3360)


class Res:
    __slots__ = ("name", "w", "r", "multi")

    def __init__(self, name, multi=False):
        self.name = name
        self.w = {}
        self.r = {}
        self.multi = multi


class Prog:
    def __init__(self, nc, ndma=(("sp", 40), ("pool", 40), ("act", 8))):
        self.nc = nc
        self.E = {"pe": nc.tensor, "act": nc.scalar, "dve": nc.vector, "pool": nc.gpsimd, "sp": nc.sync}
        self.esem = {k: nc.alloc_semaphore("e_" + k) for k in self.E}
        self.ecnt = {k: 0 for k in self.E}
        self.waited = {k: {} for k in self.E}
        self.dsem = {}
        for k, n in ndma:
            self.dsem[k] = [[nc.alloc_semaphore("d_%s%d" % (k, i)), 0, "d_%s%d" % (k, i)] for i in range(n)]
        self.dnext = {k: 0 for k in self.dsem}
        self.nins = 0

    def _wait(self, eng, ev):
        key, sem, val = ev
        if self.waited[eng].get(key, 0) >= val:
            return
        self.E[eng].wait_ge(sem, val)
        self.waited[eng][key] = val
        self.nins += 1

    def _deps(self, eng, R, W, skip_self):
        for r in R:
            for ev in r.w.values():
                if not (skip_self and ev[0] == eng):
                    self._wait(eng, ev)
        for w in W:
            if not w.multi:
                for ev in w.w.values():
                    if not (skip_self and ev[0] == eng):
                        self._wait(eng, ev)
            for ev in w.r.values():
                if not (skip_self and ev[0] == eng):
                    self._wait(eng, ev)

    def _record(self, ev, R, W):
        for r in R:
            r.r[ev[0]] = ev
        for w in W:
            if w.multi:
                w.w[ev[0]] = ev
            else:
                w.w = {ev[0]: ev}
            w.r = {}

    def op(self, eng, fn, R=(), W=(), skip_self=False):
        self._deps(eng, R, W, skip_self)
        ins = fn(self.E[eng])
        self.ecnt[eng] += 1
        ins.then_inc(self.esem[eng], 1)
        ev = (eng, self.esem[eng], self.ecnt[eng])
        self._record(ev, R, W)
        self.nins += 1
        return ev

    def pe(self, fn, R=(), W=()):
        return self.op("pe", fn, R, W, skip_self=True)

    def dma(self, eng, fn, R=(), W=()):
        self._deps(eng, R, W, False)
        pool = self.dsem[eng]
        slot = pool[self.dnext[eng] % len(pool)]
        self.dnext[eng] += 1
        if slot[1] > 0:
            self._wait(eng, (slot[2], slot[0], slot[1]))
        ins = fn(self.E[eng])
        slot[1] += 16
        ins.then_inc(slot[0], 16)
        ev = (slot[2], slot[0], slot[1])
        self._record(ev, R, W)
        self.nins += 1
        return ev

    def barrier(self):
        evs = [(k, self.esem[k], self.ecnt[k]) for k in self.E if self.ecnt[k] > 0]
        for k in self.dsem:
            for s in self.dsem[k]:
                if s[1] > 0:
                    evs.append((s[2], s[0], s[1]))
        for e in self.E:
            for ev in evs:
                self._wait(e, ev)

    def finish(self):
        self.barrier()


def build(NL, lam_inits, dbg=None):
    nc = bass.Bass("TRN2", target_bir_lowering=False)
    P = Prog(nc)

    def din(name, shape, dt=F32):
        return nc.dram_tensor(name, list(shape), dt, kind="ExternalInput").ap()

    x_in = din("x", [T, D])
    w_in = din("w_in", [NL, D, DIN])
    lamv = din("lamv", [NL, 4, 64])
    subg = din("subln_g", [NL, 128])
    relb = din("relb", [NL, 128, 16, 128])
    w_out = din("w_out", [NL, D, D])
    ln1g = din("ln1_g", [NL, D]); ln1b = din("ln1_b", [NL, D])
    w_rt = din("w_router", [NL, D, NE]); b_rt = din("b_router", [NL, NE])
    if dbg is None or dbg == "moe":
        w_gu = din("w_gu", [NL, NE, D, 2 * D]); w_dn = din("w_down", [NL, NE, D, D])
    b_gu = din("b_gu", [NL, NE * 16, 128]); b_dn = din("b_down", [NL, NE, D])
    ln2g = din("ln2_g", [NL, D]); ln2b = din("ln2_b", [NL, D])
    c_ident = din("c_ident", [128, 128], BF16)
    c_identf = din("c_identf", [128, 128], F32)
    c_ltri = din("c_ltri", [128, 128], BF16)
    c_dmask = din("c_dmask", [128, 8, 128], BF16)
    c_qaug = din("c_qaug", [3, S], BF16)
    c_kaug = din("c_kaug", [8, 3, S], BF16)
    c_eoff = din("c_eoff", [128, NE], F32)
    y_out = nc.dram_tensor("y", [T, D], F32, kind="ExternalOutput").ap()
    dbg_out = None
    if dbg == "mix":
        dbg_out = nc.dram_tensor("dbg", [NBL, 128, 8, S], BF16, kind="ExternalOutput").ap()
    if dbg == "x1":
        dbg_out = nc.dram_tensor("dbg", [T, D], F32, kind="ExternalOutput").ap()
    xcur = nc.dram_tensor("xcur", [T, D], F32).ap()
    x1s = nc.dram_tensor("x1s", [T, D], F32).ap()
    xg = nc.dram_tensor("xg", [NSLOT, D], BF16).ap()
    yg = nc.dram_tensor("yg", [NSLOT, D], F32).ap()
    R_xin = Res("xin", True); R_xcur = Res("xcur", True); R_x1s = Res("x1s", True)
    R_xg = Res("xg", True); R_yg = Res("yg", True); R_y = Res("y", True); R_dbg = Res("dbg", True)
    R_const = Res("const", True)

    def sb(name, shape, dt):
        return nc.alloc_sbuf_tensor(name, list(shape), dt)

    ident_b = sb("ident_b", [128, 128], BF16)
    ident_f = sb("ident_f", [128, 128], F32)
    ltri_b = sb("ltri_b", [128, 128], BF16)
    ones_b = sb("ones_b", [128, 128], BF16)
    zeros_b = sb("zeros_b", [128, 512], BF16)
    dmask_b = sb("dmask_b", [128, 8, 128], BF16)
    eoff = sb("eoff", [128, NE], F32)
    base_cnt = sb("base_cnt", [128, NE], F32)
    slotf = sb("slotf", [128, 32 * 4], F32)
    sloti = sb("sloti", [128, 32 * 4], I32)
    gates = sb("gates", [128, 32 * 4], F32)
    R_slot = Res("slot"); R_gates = Res("gates"); R_base = Res("base")
    for t_, src in ((ident_b, c_ident), (ident_f, c_identf), (ltri_b, c_ltri), (dmask_b, c_dmask), (eoff, c_eoff)):
        P.dma("sp", lambda e, t_=t_, src=src: e.dma_start(out=t_[:], in_=src), W=[R_const])
    P.op("pool", lambda e: e.memset(ones_b[:], 1.0), W=[R_const])
    P.op("pool", lambda e: e.memset(zeros_b[:], 0.0), W=[R_const])

    ps = [nc.alloc_psum_tensor("ps%d" % i, [128, 512], F32) for i in range(7)]
    R_ps = [Res("ps%d" % i) for i in range(7)]
    psT = nc.alloc_psum_tensor("psT", [128, 8, 128], BF16)
    R_psT = Res("psT")

    rot = {}

    def nxt(key, n):
        rot[key] = (rot.get(key, -1) + 1) % n
        return rot[key]

    def layer_norm(v, R_v, g_t, b_t, R_gb, out_t, R_out, st, R_st, junk, R_junk):
        P.op("dve", lambda e: e.reduce_sum(out=st[:, 0:1], in_=v[:], axis=AX.X), R=[R_v], W=[R_st])
        P.op("dve", lambda e: e.tensor_scalar(out=st[:, 1:2], in0=st[:, 0:1], scalar1=-1.0 / D, scalar2=None, op0=ALU.mult), R=[R_st], W=[R_st])
        P.op("act", lambda e: e.activation(out=junk[:], in_=v[:], func=AF.Square, bias=st[:, 1:2], scale=1.0, accum_out=st[:, 2:3]), R=[R_v, R_st], W=[R_junk, R_st])
        P.op("dve", lambda e: e.tensor_scalar(out=st[:, 3:4], in0=st[:, 2:3], scalar1=1.0 / D, scalar2=EPS, op0=ALU.mult, op1=ALU.add), R=[R_st], W=[R_st])
        P.op("act", lambda e: e.activation(out=st[:, 4:5], in_=st[:, 3:4], func=AF.Ln), R=[R_st], W=[R_st])
        P.op("act", lambda e: e.activation(out=st[:, 5:6], in_=st[:, 4:5], func=AF.Exp, scale=-0.5), R=[R_st], W=[R_st])
        P.op("dve", lambda e: e.tensor_scalar(out=v[:], in0=v[:], scalar1=st[:, 1:2], scalar2=st[:, 5:6], op0=ALU.add, op1=ALU.mult), R=[R_v, R_st], W=[R_v])
        P.op("pool", lambda e: e.tensor_tensor(out=v[:], in0=v[:], in1=g_t[:], op=ALU.mult), R=[R_v, R_gb], W=[R_v])
        P.op("dve", lambda e: e.tensor_tensor(out=out_t[:], in0=v[:], in1=b_t[:], op=ALU.add), R=[R_v, R_gb], W=[R_out])

    for l in range(NL):
        lam_init = lam_inits[l]
        xsrc, R_xsrc = (x_in, R_xin) if l == 0 else (xcur, R_xcur)
        last = (l == NL - 1)
        xdst, R_xdst = (y_out, R_y) if last else (xcur, R_xcur)
        P.op("pool", lambda e: e.memset(base_cnt[:], 0.0), W=[R_base])

        for b in range(NBL):
            tok0 = b * S
            with ExitStack() as es_b:
                def sbc(es, name, shape, dt):
                    return es.enter_context(nc.sbuf_tensor(name + "_%d_%d" % (l, b), list(shape), dt))
                xT = sbc(es_b, "xT", [128, 8, S], BF16); R_xT = [Res("xT%d" % i) for i in range(4)]
                mixT = sbc(es_b, "mixT", [128, 8, S], BF16)
                R_mix = [[Res("mix%d_%d" % (c, j)) for j in range(4)] for c in range(8)]

                with ExitStack() as es:
                    xst = [sbc(es, "xst%d" % i, [128, D], F32) for i in range(2)]; R_xst = [Res("xst") for _ in range(2)]
                    xbs = [sbc(es, "xbs%d" % i, [128, D], BF16) for i in range(2)]; R_xbs = [Res("xbs") for _ in range(2)]
                    for tt in range(16):
                        i2 = tt % 2
                        P.dma("sp", lambda e: e.dma_start(out=xst[i2][:], in_=xsrc[tok0 + tt * 128: tok0 + (tt + 1) * 128, :]), R=[R_xsrc], W=[R_xst[i2]])
                        P.op("act", lambda e: e.activation(out=xbs[i2][:], in_=xst[i2][:], func=AF.Copy), R=[R_xst[i2]], W=[R_xbs[i2]])
                        for k in range(8):
                            P.pe(lambda e: e.transpose(out=psT[:, k, :], in_=xbs[i2][:, k * 128:(k + 1) * 128], identity=ident_b[:]), R=[R_xbs[i2], R_const], W=[R_psT])
                        P.op("dve", lambda e: e.tensor_copy(out=xT[:, :, tt * 128:(tt + 1) * 128], in_=psT[:, :, :]), R=[R_psT], W=[R_xT[tt // 4]])
                    P.barrier()

                with ExitStack() as es:
                    QK = [sbc(es, "qk%d" % i, [128, S], BF16) for i in range(12)]
                    R_QK = [Res("qk%d" % i) for i in range(12)]
                    VV = sbc(es, "vv", [128, 16, 512], BF16); R_VV = Res("vv")
                    wst = [sbc(es, "wst%d" % i, [128, 8, 128], F32) for i in range(2)]; R_wst = [Res("wst") for _ in range(2)]
                    wbf = [sbc(es, "wbf%d" % i, [128, 8, 128], BF16) for i in range(4)]; R_wbf = [Res("wbf") for _ in range(4)]
                    PT = [sbc(es, "pt%d" % i, [128, 512], BF16) for i in range(4)]; R_PT = [Res("pt") for _ in range(4)]
                    relb_b = sbc(es, "relb_b", [128, 16, 128], BF16); R_relb = Res("relb")
                    score = sbc(es, "score", [128, S], F32); R_score = Res("score")
                    relu_sb = [sbc(es, "relu%d" % i, [128, 512], BF16) for i in range(3)]; R_relu = [Res("relu") for _ in range(3)]
                    negmask = sbc(es, "negmask", [128, S], BF16); R_negm = Res("negm")
                    nmT = sbc(es, "nmT", [128, 16, 512], BF16); R_nmT = Res("nmT")
                    diag = sbc(es, "diag", [128, 8, 128], BF16); R_diag = Res("diag")
                    wtok = sbc(es, "wtok", [128, 16, 8], F32); R_wtok = Res("wtok")
                    ft = [sbc(es, "ft%d" % i, [128, 512], F32) for i in range(4)]; R_ft = [Res("ft%d" % i) for i in range(4)]
                    sqb = sbc(es, "sqb", [128, 512], BF16); R_sqb = Res("sqb")
                    sm = sbc(es, "sm", [128, 16], F32); R_sm = Res("sm")
                    lamt = sbc(es, "lamt", [128, 4, 64], F32); R_lamt = Res("lamt")
                    gsc = sbc(es, "gsc", [128, 2], F32)

                    P.dma("sp", lambda e: e.dma_start(out=lamt[:], in_=lamv[l:l + 1, :, :].to_broadcast([128, 4, 64])), W=[R_lamt])
                    P.dma("sp", lambda e: e.dma_start(out=gsc[:, 0:1], in_=subg[l, :].rearrange("(p o) -> p o", o=1)), W=[R_sm])
                    P.op("dve", lambda e: e.tensor_tensor(out=lamt[:, 0, :], in0=lamt[:, 0, :], in1=lamt[:, 1, :], op=ALU.mult), R=[R_lamt], W=[R_lamt])
                    P.op("dve", lambda e: e.tensor_tensor(out=lamt[:, 2, :], in0=lamt[:, 2, :], in1=lamt[:, 3, :], op=ALU.mult), R=[R_lamt], W=[R_lamt])
                    P.op("dve", lambda e: e.reduce_sum(out=sm[:, 0:1], in_=lamt[:, 0, :], axis=AX.X), R=[R_lamt], W=[R_sm])
                    P.op("dve", lambda e: e.reduce_sum(out=sm[:, 1:2], in_=lamt[:, 2, :], axis=AX.X), R=[R_lamt], W=[R_sm])
                    P.op("act", lambda e: e.activation(out=sm[:, 2:4], in_=sm[:, 0:2], func=AF.Exp), R=[R_sm], W=[R_sm])
                    P.op("dve", lambda e: e.tensor_tensor(out=sm[:, 4:5], in0=sm[:, 3:4], in1=sm[:, 2:3], op=ALU.subtract), R=[R_sm], W=[R_sm])
                    P.op("dve", lambda e: e.tensor_scalar(out=sm[:, 5:6], in0=sm[:, 4:5], scalar1=-lam_init, scalar2=None, op0=ALU.add), R=[R_sm], W=[R_sm])
                    P.op("dve", lambda e: e.tensor_scalar(out=gsc[:, 1:2], in0=gsc[:, 0:1], scalar1=1.0 - lam_init, scalar2=None, op0=ALU.mult), R=[R_sm], W=[R_sm])
                    neglam = sm[:, 5:6]
                    gscale = gsc[:, 1:2]

                    P.dma("sp", lambda e: e.dma_start(out=score[:, :].rearrange("p (a b) -> p a b", a=16), in_=relb[l]), W=[R_score])
                    P.op("pool", lambda e: e.tensor_copy(out=relb_b[:], in_=score[:, :].rearrange("p (a b) -> p a b", a=16)), R=[R_score], W=[R_relb])

                    def load_w(c0, ncol, rep=1):
                        si = nxt("wst", 2); bi = nxt("wbf", 4)
                        P.dma("sp", lambda e: e.dma_start(out=wst[si][:, :, 0:ncol], in_=w_in[l, :, c0:c0 + ncol].rearrange("(k p) c -> p k c", p=128)), W=[R_wst[si]])
                        for r in range(rep):
                            P.op("pool", lambda e: e.tensor_copy(out=wbf[bi][:, :, r * ncol:(r + 1) * ncol], in_=wst[si][:, :, 0:ncol]), R=[R_wst[si]], W=[R_wbf[bi]])
                        return bi

                    def proj_feat(bi, c_lo, m, dst, R_dst, p_lo, scale):
                        for tb in range(4):
                            pi = 4 + nxt("pj", 2)
                            for k in range(8):
                                P.pe(lambda e: e.matmul(ps[pi][0:m, :], lhsT=wbf[bi][:, k, c_lo:c_lo + m], rhs=xT[:, k, tb * 512:(tb + 1) * 512], start=(k == 0), stop=(k == 7)),
                                     R=[R_wbf[bi], R_xT[tb]], W=[R_ps[pi]])
                            P.op("act", lambda e: e.activation(out=dst[p_lo:p_lo + m, tb * 512:(tb + 1) * 512], in_=ps[pi][p_lo:p_lo + m, :], func=AF.Copy, scale=scale),
                                 R=[R_ps[pi]], W=[R_dst])

                    def proj_feat_split(bi, dst0, R0, dst1, R1, scale):
                        for tb in range(4):
                            pi = 4 + nxt("pj", 2)
                            for k in range(8):
                                P.pe(lambda e: e.matmul(ps[pi][:, :], lhsT=wbf[bi][:, k, :], rhs=xT[:, k, tb * 512:(tb + 1) * 512], start=(k == 0), stop=(k == 7)),
                                     R=[R_wbf[bi], R_xT[tb]], W=[R_ps[pi]])
                            P.op("act", lambda e: e.activation(out=dst0[0:64, tb * 512:(tb + 1) * 512], in_=ps[pi][0:64, :], func=AF.Copy, scale=scale), R=[R_ps[pi]], W=[R0])
                            P.op("act", lambda e: e.activation(out=dst1[64:128, tb * 512:(tb + 1) * 512], in_=ps[pi][64:128, :], func=AF.Copy, scale=scale), R=[R_ps[pi]], W=[R1])

                    def proj_tok(bi, ncol, dst_fn, R_dst, act_eng="act"):
                        for st_ in range(16):
                            pi = 4 + nxt("pj", 2)
                            for k in range(8):
                                P.pe(lambda e: e.matmul(ps[pi][:, 0:ncol], lhsT=xT[:, k, st_ * 128:(st_ + 1) * 128], rhs=wbf[bi][:, k, 0:ncol], start=(k == 0), stop=(k == 7)),
                                     R=[R_wbf[bi], R_xT[st_ // 4]], W=[R_ps[pi]])
                            P.op("act", lambda e: e.activation(out=dst_fn(st_), in_=ps[pi][:, 0:ncol], func=AF.Copy), R=[R_ps[pi]], W=[R_dst])

                    def recip_safe(dst, R_dst, src_ps, R_src, p0, p1):
                        P.op("dve", lambda e: e.tensor_scalar(out=dst[p0:p1, :], in0=src_ps[p0:p1, :], scalar1=1e-30, scalar2=None, op0=ALU.max), R=[R_src], W=[R_dst])
                        P.op("dve", lambda e: e.reciprocal(out=dst[p0:p1, :], in_=dst[p0:p1, :]), R=[R_dst], W=[R_dst])

                    def attn_block(J, i_list, colrange_fn, score_mms, cfn, Vl_fn, R_V, accs, R_extra):
                        for (oi, si_, m) in accs:
                            for bi_ in (oi, si_):
                                P.pe(lambda e: e.matmul(ps[bi_][:, :], lhsT=zeros_b[:, 0:128], rhs=zeros_b[:, :], start=True, stop=False), R=[R_const], W=[R_ps[bi_]])
                        n_i = len(i_list)
                        for ii, i in enumerate(i_list):
                            c_lo, c_hi = colrange_fn(i)
                            for (oi, si_, m) in accs:
                                sci = 4 + nxt("scb", 3)
                                score_mms(sci, i, m, c_lo, c_hi)
                                pti = nxt("pt", 4)
                                P.op("act", lambda e: e.activation(out=PT[pti][:, c_lo:c_hi], in_=ps[sci][:, c_lo:c_hi], func=AF.Exp, bias=float(cfn(i)), scale=1.0),
                                     R=[R_ps[sci]], W=[R_PT[pti]])
                                lastf = (ii == n_i - 1)
                                P.pe(lambda e: e.matmul(ps[oi][:, c_lo:c_hi], lhsT=Vl_fn(i), rhs=PT[pti][:, c_lo:c_hi], start=False, stop=lastf), R=[R_PT[pti], R_V], W=[R_ps[oi]])
                                P.pe(lambda e: e.matmul(ps[si_][:, c_lo:c_hi], lhsT=ones_b[:], rhs=PT[pti][:, c_lo:c_hi], start=False, stop=lastf), R=[R_PT[pti], R_const], W=[R_ps[si_]])

                    for g in range(4):
                        bi = load_w(C_VA + g * 128, 128)
                        proj_tok(bi, 128, lambda st_, g=g: VV[:, st_, g * 128:(g + 1) * 128], R_VV)
                    for qi_ in (0, 1):
                        P.dma("sp", lambda e: e.dma_start(out=QK[qi_][64:67, :], in_=c_qaug), W=[R_QK[qi_]])
                    for h in range(4):
                        Q1, Q2, K1, K2 = QK[0], QK[1], QK[2], QK[3]
                        for kt in (2, 3):
                            P.dma("sp", lambda e: e.dma_start(out=QK[kt][64:67, :], in_=c_kaug[h]), W=[R_QK[kt]])
                        bq = load_w(C_QA + h * 128, 128)
                        bk = load_w(C_KA + h * 128, 128)
                        proj_feat(bq, 0, 64, Q1, R_QK[0], 0, 0.125)
                        proj_feat(bq, 64, 64, Q2, R_QK[1], 0, 0.125)
                        proj_feat(bk, 0, 64, K1, R_QK[2], 0, 1.0)
                        proj_feat(bk, 64, 64, K2, R_QK[3], 0, 1.0)
                        sig = SIG_A[h]
                        for J in range(4):
                            def colr(i, J=J):
                                return (128 * max(0, i - 4 * J), 512)

                            def smm(sci, i, m, c_lo, c_hi, J=J, h=h):
                                isd = i >= 4 * J
                                P.pe(lambda e: e.matmul(ps[sci][:, c_lo:c_hi], lhsT=QK[2 + m][0:67, i * 128:(i + 1) * 128], rhs=QK[m][0:67, J * 512 + c_lo:J * 512 + c_hi], start=True, stop=not isd),
                                     R=[R_QK[2 + m], R_QK[m]], W=[R_ps[sci]])
                                if isd:
                                    a = i - 4 * J
                                    P.pe(lambda e: e.matmul(ps[sci][:, a * 128:(a + 1) * 128], lhsT=ident_b[:], rhs=dmask_b[:, h, :], start=False, stop=True), R=[R_const], W=[R_ps[sci]])
                            attn_block(J, list(range(4 * J + 4)), colr, smm, lambda i, J=J: -sig * (512 * J - 128 * i),
                                       lambda i, h=h: VV[:, i, h * 128:(h + 1) * 128], R_VV, [(0, 1, 0), (2, 3, 1)], None)
                            recip_safe(ft[0], R_ft[0], ps[1], R_ps[1], 0, 128)
                            recip_safe(ft[1], R_ft[1], ps[3], R_ps[3], 0, 128)
                            P.op("dve", lambda e: e.tensor_tensor(out=ft[0][:], in0=ps[0][:, :], in1=ft[0][:], op=ALU.mult), R=[R_ps[0], R_ft[0]], W=[R_ft[0]])
                            P.op("dve", lambda e: e.tensor_tensor(out=ft[1][:], in0=ps[2][:, :], in1=ft[1][:], op=ALU.mult), R=[R_ps[2], R_ft[1]], W=[R_ft[1]])
                            P.op("dve", lambda e: e.scalar_tensor_tensor(out=ft[2][:], in0=ft[1][:], scalar=neglam, in1=ft[0][:], op0=ALU.mult, op1=ALU.add), R=[R_ft[0], R_ft[1], R_sm], W=[R_ft[2]])
                            P.op("pool", lambda e: e.tensor_tensor(out=sqb[:], in0=ft[2][:], in1=ft[2][:], op=ALU.mult), R=[R_ft[2]], W=[R_sqb])
                            P.pe(lambda e: e.matmul(ps[6][:, :], lhsT=ones_b[:], rhs=sqb[:], start=True, stop=True), R=[R_sqb, R_const], W=[R_ps[6]])
                            P.op("dve", lambda e: e.tensor_scalar(out=ft[3][:], in0=ps[6][:, :], scalar1=1.0 / 128, scalar2=EPS, op0=ALU.mult, op1=ALU.add), R=[R_ps[6]], W=[R_ft[3]])
                            P.op("act", lambda e: e.activation(out=ft[3][:], in_=ft[3][:], func=AF.Ln), R=[R_ft[3]], W=[R_ft[3]])
                            P.op("act", lambda e: e.activation(out=ft[3][:], in_=ft[3][:], func=AF.Exp, scale=-0.5), R=[R_ft[3]], W=[R_ft[3]])
                            P.op("dve", lambda e: e.scalar_tensor_tensor(out=mixT[:, h, J * 512:(J + 1) * 512], in0=ft[2][:], scalar=gscale, in1=ft[3][:], op0=ALU.mult, op1=ALU.mult),
                                 R=[R_ft[2], R_ft[3], R_sm], W=[R_mix[h][J]])

                    for g in range(2):
                        bi = load_w(C_VB + g * 128, 128)
                        proj_tok(bi, 128, lambda st_, g=g: VV[:, st_, g * 128:(g + 1) * 128], R_VV)
                    for h in range(4):
                        z0 = 64 if h % 2 == 0 else 0
                        P.op("pool", lambda e: e.memset(QK[2 + h][z0:z0 + 64, :], 0.0), W=[R_QK[2 + h]])
                    for g in range(2):
                        bq = load_w(C_QB + g * 128, 128)
                        bk = load_w(C_KB + g * 128, 128)
                        proj_feat(bq, 0, 128, QK[g], R_QK[g], 0, 0.125)
                        proj_feat_split(bk, QK[2 + 2 * g], R_QK[2 + 2 * g], QK[3 + 2 * g], R_QK[3 + 2 * g], 1.0)
                    for h in range(4):
                        g = h // 2
                        r0 = (h % 2) * 64
                        for J in range(4):
                            i_list = list(range(max(0, 4 * J - 4), 4 * J + 4))

                            def colr(i, J=J):
                                a_lo = max(0, i - 4 * J); a_hi = min(3, i + 4 - 4 * J)
                                return (128 * a_lo, 128 * (a_hi + 1))

                            def smm(sci, i, m, c_lo, c_hi, J=J, h=h, g=g):
                                P.pe(lambda e: e.matmul(ps[sci][:, c_lo:c_hi], lhsT=QK[2 + h][:, i * 128:(i + 1) * 128], rhs=QK[g][:, J * 512 + c_lo:J * 512 + c_hi], start=True, stop=False),
                                     R=[R_QK[2 + h], R_QK[g]], W=[R_ps[sci]])
                                a_lo, a_hi = c_lo // 128, c_hi // 128 - 1
                                for a in range(a_lo, a_hi + 1):
                                    dl = 4 * J + a - i
                                    piece = {0: 0, 1: 1, 2: 2, 3: 2, 4: 3}[dl]
                                    P.pe(lambda e: e.matmul(ps[sci][:, a * 128:(a + 1) * 128], lhsT=ident_b[:], rhs=relb_b[:, h * 4 + piece, :], start=False, stop=(a == a_hi)),
                                         R=[R_const, R_relb], W=[R_ps[sci]])
                            attn_block(J, i_list, colr, smm, lambda i: 0.0, lambda i, g=g: VV[:, i, g * 128:(g + 1) * 128], R_VV, [(0, 1, 0)], None)
                            recip_safe(ft[0], R_ft[0], ps[1], R_ps[1], r0, r0 + 64)
                            P.op("dve", lambda e: e.tensor_tensor(out=mixT[r0:r0 + 64, 4 + g, J * 512:(J + 1) * 512], in0=ps[0][r0:r0 + 64, :], in1=ft[0][r0:r0 + 64, :], op=ALU.mult),
                                 R=[R_ps[0], R_ft[0]], W=[R_mix[4 + g][J]])

                    for g in range(2):
                        bi = load_w(C_VC + g * 128, 128)
                        proj_tok(bi, 128, lambda st_, g=g: VV[:, st_, g * 128:(g + 1) * 128], R_VV)
                    for h in range(4):
                        P.dma("sp", lambda e: e.dma_start(out=QK[h][64:67, :], in_=c_kaug[4 + h]), W=[R_QK[h]])
                        P.dma("sp", lambda e: e.dma_start(out=QK[4 + h][64:67, :], in_=c_qaug), W=[R_QK[4 + h]])
                    for g in range(2):
                        bq = load_w(C_QC + g * 128, 128)
                        bk = load_w(C_KC + g * 128, 128)
                        proj_feat(bq, 0, 64, QK[4 + 2 * g], R_QK[4 + 2 * g], 0, 0.125)
                        proj_feat(bq, 64, 64, QK[5 + 2 * g], R_QK[5 + 2 * g], 0, 0.125)
                        proj_feat(bk, 0, 64, QK[2 * g], R_QK[2 * g], 0, 1.0)
                        proj_feat(bk, 64, 64, QK[2 * g + 1], R_QK[2 * g + 1], 0, 1.0)
                    for g, nh in ((0, 3), (1, 3), (2, 2)):
                        bi = load_w(C_QI + g * 96, 32 * nh)
                        proj_feat(bi, 0, 32 * nh, QK[8 + g], R_QK[8 + g], 0, 1.0)
                    bi = load_w(C_KI, 32, rep=3)
                    proj_feat(bi, 0, 96, QK[11], R_QK[11], 0, 1.0)
                    bi = load_w(C_WI, 8)
                    proj_tok(bi, 8, lambda st_: wtok[:, st_, :], R_wtok)

                    for J in range(4):
                        for a in range(4):
                            j = 4 * J + a
                            L = 128 * (j + 1)
                            if j < 2:
                                P.op("pool", lambda e: e.memset(negmask[:, 0:L], 0.0), W=[R_negm])
                            else:
                                for h8 in range(8):
                                    P.op("dve", lambda e: e.tensor_scalar(out=diag[:, h8, :], in0=ident_f[:], scalar1=wtok[:, j, h8:h8 + 1], scalar2=W_SCALE, op0=ALU.mult, op1=ALU.mult),
                                         R=[R_const, R_wtok], W=[R_diag])
                                nsc = (L + 511) // 512
                                for sc_i in range(nsc):
                                    w_ = min(512, L - 512 * sc_i)
                                    for h8 in range(8):
                                        gq, rr = h8 // 3, h8 % 3
                                        pi = 4 + nxt("pj", 2)
                                        P.pe(lambda e: e.matmul(ps[pi][:, 0:w_], lhsT=QK[8 + gq][32 * rr:32 * rr + 32, j * 128:(j + 1) * 128], rhs=QK[11][32 * rr:32 * rr + 32, sc_i * 512:sc_i * 512 + w_], start=True, stop=True),
                                             R=[R_QK[8 + gq], R_QK[11]], W=[R_ps[pi]])
                                        ri = nxt("relu", 3)
                                        P.op("act", lambda e: e.activation(out=relu_sb[ri][:, 0:w_], in_=ps[pi][:, 0:w_], func=AF.Relu), R=[R_ps[pi]], W=[R_relu[ri]])
                                        P.pe(lambda e: e.matmul(ps[6][:, 0:w_], lhsT=diag[:, h8, :], rhs=relu_sb[ri][:, 0:w_], start=(h8 == 0), stop=(h8 == 7)), R=[R_diag, R_relu[ri]], W=[R_ps[6]])
                                    P.op("dve", lambda e: e.tensor_copy(out=score[:, sc_i * 512:sc_i * 512 + w_], in_=ps[6][:, 0:w_]), R=[R_ps[6]], W=[R_score])
                                P.op("dve", lambda e: e.tensor_reduce(out=sm[:, 8:9], in_=score[:, 0:L], axis=AX.X, op=ALU.min), R=[R_score], W=[R_sm])
                                P.op("pool", lambda e: e.memset(score[0:64, L - 64:L], -1e30), R=[R_sm], W=[R_score])
                                P.op("dve", lambda e: e.reduce_max(out=sm[:, 9:10], in_=score[:, 0:L], axis=AX.X), R=[R_score], W=[R_sm])
                                P.op("dve", lambda e: e.scalar_tensor_tensor(out=sm[:, 10:11], in0=sm[:, 9:10], scalar=1e-20, in1=sm[:, 8:9], op0=ALU.add, op1=ALU.subtract), R=[R_sm], W=[R_sm])
                                P.op("dve", lambda e: e.reciprocal(out=sm[:, 11:12], in_=sm[:, 10:11]), R=[R_sm], W=[R_sm])
                                P.op("dve", lambda e: e.tensor_scalar(out=score[:, 0:L], in0=score[:, 0:L], scalar1=sm[:, 8:9], scalar2=sm[:, 11:12], op0=ALU.subtract, op1=ALU.mult), R=[R_score, R_sm], W=[R_score])
                                P.op("dve", lambda e: e.memset(sm[:, 12:13], 0.5), W=[R_sm])
                                for n in range(NIT):
                                    dlt = 2.0 ** -(n + 2)
                                    P.op("dve", lambda e: e.tensor_scalar(out=negmask[:, 0:L], in0=score[:, 0:L], scalar1=sm[:, 12:13], scalar2=None, op0=ALU.is_gt, op1=ALU.add, accum_out=sm[:, 13:14]),
                                         R=[R_score, R_sm], W=[R_negm, R_sm])
                                    P.op("dve", lambda e: e.tensor_scalar(out=sm[:, 14:15], in0=sm[:, 13:14], scalar1=255.5, scalar2=2.0 * dlt, op0=ALU.is_gt, op1=ALU.mult), R=[R_sm], W=[R_sm])
                                    P.op("dve", lambda e: e.scalar_tensor_tensor(out=sm[:, 12:13], in0=sm[:, 12:13], scalar=-dlt, in1=sm[:, 14:15], op0=ALU.add, op1=ALU.add), R=[R_sm], W=[R_sm])
                                P.op("dve", lambda e: e.tensor_scalar(out=negmask[:, 0:L], in0=score[:, 0:L], scalar1=sm[:, 12:13], scalar2=NEG, op0=ALU.is_le, op1=ALU.mult), R=[R_score, R_sm], W=[R_negm])
                            for i0 in range(0, j + 1, 8):
                                n_ = min(8, j + 1 - i0)
                                for ii in range(n_):
                                    P.pe(lambda e: e.transpose(out=psT[:, ii, :], in_=negmask[:, (i0 + ii) * 128:(i0 + ii + 1) * 128], identity=ident_b[:]), R=[R_negm, R_const], W=[R_psT])
                                P.op("dve", lambda e: e.tensor_copy(out=nmT[:, i0:i0 + n_, a * 128:(a + 1) * 128], in_=psT[:, 0:n_, :]), R=[R_psT], W=[R_nmT])
                        for h in range(4):
                            g = h // 2
                            r0 = (h % 2) * 64
                            sig = SIG_C[h]

                            def colr(i, J=J):
                                return (128 * max(0, i - 4 * J), 512)

                            def smm(sci, i, m, c_lo, c_hi, J=J, h=h):
                                P.pe(lambda e: e.matmul(ps[sci][:, c_lo:c_hi], lhsT=QK[h][0:67, i * 128:(i + 1) * 128], rhs=QK[4 + h][0:67, J * 512 + c_lo:J * 512 + c_hi], start=True, stop=False),
                                     R=[R_QK[h], R_QK[4 + h]], W=[R_ps[sci]])
                                if i >= 4 * J:
                                    a = i - 4 * J
                                    P.pe(lambda e: e.matmul(ps[sci][:, a * 128:(a + 1) * 128], lhsT=ident_b[:], rhs=dmask_b[:, 4 + h, :], start=False, stop=False), R=[R_const], W=[R_ps[sci]])
                                P.pe(lambda e: e.matmul(ps[sci][:, c_lo:c_hi], lhsT=ident_b[:], rhs=nmT[:, i, c_lo:c_hi], start=False, stop=True), R=[R_const, R_nmT], W=[R_ps[sci]])
                            attn_block(J, list(range(4 * J + 4)), colr, smm, lambda i, J=J, sig=sig: -sig * (512 * J - 128 * i),
                                       lambda i, g=g: VV[:, i, g * 128:(g + 1) * 128], R_VV, [(0, 1, 0)], None)
                            recip_safe(ft[0], R_ft[0], ps[1], R_ps[1], r0, r0 + 64)
                            P.op("dve", lambda e: e.tensor_tensor(out=mixT[r0:r0 + 64, 6 + g, J * 512:(J + 1) * 512], in0=ps[0][r0:r0 + 64, :], in1=ft[0][r0:r0 + 64, :], op=ALU.mult),
                                 R=[R_ps[0], R_ft[0]], W=[R_mix[6 + g][J]])
                    P.barrier()

                if dbg == "mix":
                    allmix = [R_mix[c][j] for c in range(8) for j in range(4)]
                    P.dma("sp", lambda e: e.dma_start(out=dbg_out[b], in_=mixT[:]), R=allmix, W=[R_dbg])
                    P.barrier()
                    continue

                with ExitStack() as es:
                    wo_b = sbc(es, "wo_b", [128, 8, D], BF16); R_wo = Res("wo")
                    wst2 = [sbc(es, "wst2_%d" % i, [128, D], F32) for i in range(2)]; R_wst2 = [Res("wst2") for _ in range(2)]
                    g1 = sbc(es, "g1", [128, D], F32); b1 = sbc(es, "b1", [128, D], F32); R_gb = Res("gb")
                    wr_f = sbc(es, "wr_f", [128, 8, NE], F32); br_t = sbc(es, "br_t", [128, NE], F32)
                    xt_ = [sbc(es, "xt%d" % i, [128, D], F32) for i in range(2)]; R_xt = [Res("xt") for _ in range(2)]
                    vt = [sbc(es, "vt%d" % i, [128, D], F32) for i in range(2)]; R_vt = [Res("vt") for _ in range(2)]
                    x1t = [sbc(es, "x1t%d" % i, [128, D], F32) for i in range(2)]; R_x1t = [Res("x1t") for _ in range(2)]
                    x1b = [sbc(es, "x1b%d" % i, [128, D], BF16) for i in range(2)]; R_x1b = [Res("x1b") for _ in range(2)]
                    x1T = sbc(es, "x1T", [128, 8, 128], F32); R_x1T = Res("x1T")
                    junk = sbc(es, "junk", [128, D], BF16); R_junk = Res("junk")
                    lst = sbc(es, "lst", [128, 8], F32); R_lst = Res("lst")
                    lg = sbc(es, "lg", [128, NE], F32); R_lg = Res("lg")
                    mx8 = sbc(es, "mx8", [128, 8], F32); R_mx = Res("mx")
                    rs = sbc(es, "rs", [128, 16], F32); R_rs = Res("rs")
                    maskb = sbc(es, "maskb", [128, NE], BF16); R_maskb = Res("maskb")
                    slotm = sbc(es, "slotm", [128, NE], F32); R_slotm = Res("slotm")
                    oh = sbc(es, "oh", [128, NE], F32); R_oh = Res("oh")
                    for c in range(8):
                        i2 = c % 2
                        P.dma("sp", lambda e: e.dma_start(out=wst2[i2][:], in_=w_out[l, c * 128:(c + 1) * 128, :]), W=[R_wst2[i2]])
                        P.op("pool", lambda e: e.tensor_copy(out=wo_b[:, c, :], in_=wst2[i2][:]), R=[R_wst2[i2]], W=[R_wo])
                    P.dma("sp", lambda e: e.dma_start(out=g1[:], in_=ln1g[l:l + 1, :].to_broadcast([128, D])), W=[R_gb])
                    P.dma("sp", lambda e: e.dma_start(out=b1[:], in_=ln1b[l:l + 1, :].to_broadcast([128, D])), W=[R_gb])
                    P.dma("sp", lambda e: e.dma_start(out=wr_f[:], in_=w_rt[l].rearrange("(k p) c -> p k c", p=128)), W=[R_gb])
                    P.dma("sp", lambda e: e.dma_start(out=br_t[:], in_=b_rt[l:l + 1, :].to_broadcast([128, NE])), W=[R_gb])
                    for tt in range(16):
                        i2 = tt % 2
                        gt_ = b * 16 + tt
                        rows = slice(tok0 + tt * 128, tok0 + (tt + 1) * 128)
                        P.dma("sp", lambda e: e.dma_start(out=xt_[i2][:], in_=xsrc[rows, :]), R=[R_xsrc], W=[R_xt[i2]])
                        for half in range(2):
                            pi = nxt("wo", 2)
                            for c in range(8):
                                P.pe(lambda e: e.matmul(ps[pi][:, :], lhsT=mixT[:, c, tt * 128:(tt + 1) * 128], rhs=wo_b[:, c, half * 512:(half + 1) * 512], start=(c == 0), stop=(c == 7)),
                                     R=[R_mix[c][tt // 4], R_wo], W=[R_ps[pi]])
                            P.op("dve", lambda e: e.scalar_tensor_tensor(out=vt[i2][:, half * 512:(half + 1) * 512], in0=xt_[i2][:, half * 512:(half + 1) * 512], scalar=ALPHA, in1=ps[pi][:, :], op0=ALU.mult, op1=ALU.add),
                                 R=[R_xt[i2], R_ps[pi]], W=[R_vt[i2]])
                        layer_norm(vt[i2], R_vt[i2], g1, b1, R_gb, x1t[i2], R_x1t[i2], lst, R_lst, junk, R_junk)
                        if dbg == "x1":
                            P.dma("pool", lambda e: e.dma_start(out=dbg_out[rows, :], in_=x1t[i2][:]), R=[R_x1t[i2]], W=[R_dbg])
                        P.dma("pool", lambda e: e.dma_start(out=x1s[rows, :], in_=x1t[i2][:]), R=[R_x1t[i2]], W=[R_x1s])
                        P.op("act", lambda e: e.activation(out=x1b[i2][:], in_=x1t[i2][:], func=AF.Copy), R=[R_x1t[i2]], W=[R_x1b[i2]])
                        for hf in range(2):
                            for k4 in range(4):
                                k = hf * 4 + k4
                                P.pe(lambda e: e.transpose(out=ps[2 + hf][:, k4 * 128:(k4 + 1) * 128], in_=x1t[i2][:, k * 128:(k + 1) * 128], identity=ident_f[:]), R=[R_x1t[i2], R_const], W=[R_ps[2 + hf]])
                            P.op("act", lambda e: e.activation(out=x1T[:, hf * 4:(hf + 1) * 4, :], in_=ps[2 + hf][:, :].rearrange("p (a b) -> p a b", a=4), func=AF.Copy), R=[R_ps[2 + hf]], W=[R_x1T])
                        for k in range(8):
                            P.pe(lambda e: e.matmul(ps[4][:, 0:NE], lhsT=x1T[:, k, :], rhs=wr_f[:, k, :], start=(k == 0), stop=(k == 7)), R=[R_x1T, R_gb], W=[R_ps[4]])
                        P.op("dve", lambda e: e.tensor_tensor(out=lg[:], in0=ps[4][:, 0:NE], in1=br_t[:], op=ALU.add), R=[R_ps[4], R_gb], W=[R_lg])
                        P.op("dve", lambda e: e.max(out=mx8[:], in_=lg[:]), R=[R_lg], W=[R_mx])
                        P.op("dve", lambda e: e.tensor_scalar(out=rs[:, 0:1], in0=mx8[:, 0:1], scalar1=-1.0, scalar2=None, op0=ALU.mult), R=[R_mx], W=[R_rs])
                        P.op("act", lambda e: e.activation(out=rs[:, 4:8], in_=mx8[:, 0:4], func=AF.Exp, bias=rs[:, 0:1], scale=1.0, accum_out=rs[:, 1:2]), R=[R_mx, R_rs], W=[R_rs])
                        P.op("dve", lambda e: e.reciprocal(out=rs[:, 2:3], in_=rs[:, 1:2]), R=[R_rs], W=[R_rs])
                        P.op("dve", lambda e: e.tensor_scalar(out=gates[:, gt_ * 4:gt_ * 4 + 4], in0=rs[:, 4:8], scalar1=rs[:, 2:3], scalar2=None, op0=ALU.mult), R=[R_rs], W=[R_gates])
                        P.op("dve", lambda e: e.tensor_scalar(out=maskb[:], in0=lg[:], scalar1=mx8[:, 3:4], scalar2=None, op0=ALU.is_ge), R=[R_lg, R_mx], W=[R_maskb])
                        P.pe(lambda e: e.matmul(ps[5][:, 0:NE], lhsT=ltri_b[:], rhs=maskb[:], start=True, stop=True), R=[R_maskb, R_const], W=[R_ps[5]])
                        P.pe(lambda e: e.matmul(ps[6][:, 0:NE], lhsT=ones_b[:], rhs=maskb[:], start=True, stop=True), R=[R_maskb, R_const], W=[R_ps[6]])
                        P.op("dve", lambda e: e.tensor_tensor(out=slotm[:], in0=ps[5][:, 0:NE], in1=base_cnt[:], op=ALU.add), R=[R_ps[5], R_base], W=[R_slotm])
                        P.op("dve", lambda e: e.tensor_tensor(out=slotm[:], in0=slotm[:], in1=eoff[:], op=ALU.add), R=[R_slotm, R_const], W=[R_slotm])
                        P.op("dve", lambda e: e.tensor_tensor(out=base_cnt[:], in0=ps[6][:, 0:NE], in1=base_cnt[:], op=ALU.add), R=[R_ps[6], R_base], W=[R_base])
                        for k in range(4):
                            P.op("dve", lambda e: e.tensor_scalar(out=oh[:], in0=lg[:], scalar1=mx8[:, k:k + 1], scalar2=None, op0=ALU.is_equal), R=[R_lg, R_mx], W=[R_oh])
                            P.op("dve", lambda e: e.tensor_tensor(out=oh[:], in0=oh[:], in1=slotm[:], op=ALU.mult), R=[R_oh, R_slotm], W=[R_oh])
                            P.op("dve", lambda e: e.reduce_sum(out=slotf[:, gt_ * 4 + k:gt_ * 4 + k + 1], in_=oh[:], axis=AX.X), R=[R_oh], W=[R_slot])
                        P.op("dve", lambda e: e.tensor_copy(out=sloti[:, gt_ * 4:gt_ * 4 + 4], in_=slotf[:, gt_ * 4:gt_ * 4 + 4]), R=[R_slot], W=[R_slot])
                        for k in range(4):
                            P.dma("pool", lambda e: e.indirect_dma_start(out=xg[:, :], out_offset=bass.IndirectOffsetOnAxis(ap=sloti[:, gt_ * 4 + k:gt_ * 4 + k + 1], axis=0), in_=x1b[i2][:, :], in_offset=None),
                                  R=[R_x1b[i2], R_slot], W=[R_xg])
                    P.barrier()
        if dbg in ("mix", "x1"):
            break

        with ExitStack() as es:
            def sbm(name, shape, dt):
                return es.enter_context(nc.sbuf_tensor(name + "_m%d" % l, list(shape), dt))
            wgu_b = [sbm("wgu%d" % i, [128, 8, 2 * D], BF16) for i in range(2)]; R_wgu = [[Res("wgu") for _ in range(8)] for _ in range(2)]
            wdn_b = [sbm("wdn%d" % i, [128, 8, D], BF16) for i in range(2)]; R_wdn = [[Res("wdn") for _ in range(8)] for _ in range(2)]
            stg = [sbm("stg%d" % i, [128, 2 * D], F32) for i in range(3)]; R_stg = [Res("stg") for _ in range(3)]
            xr = [sbm("xr%d" % i, [128, D], BF16) for i in range(3)]; R_xr = [Res("xr") for _ in range(3)]
            xeT = [sbm("xeT%d" % i, [128, 8, CAP], BF16) for i in range(2)]; R_xeT = [Res("xeT") for _ in range(2)]
            GT = sbm("GT", [128, 8, CAP], BF16); R_GT = [Res("GT%d" % c) for c in range(8)]
            et = [sbm("et%d" % i, [128, 512], F32) for i in range(6)]; R_et = [Res("et") for _ in range(6)]
            yt = [sbm("yt%d" % i, [128, D], F32) for i in range(2)]; R_yt = [Res("yt") for _ in range(2)]
            bgT = sbm("bgT", [128, NE * 16], F32); R_bgT = Res("bgT")
            bgs = sbm("bgs", [128, 4, 128], F32); R_bgs = Res("bgs")
            bdb = [sbm("bdb%d" % i, [128, D], F32) for i in range(2)]; R_bdb = [Res("bdb") for _ in range(2)]
            P.dma("sp", lambda e: e.dma_start(out=bgs[:], in_=b_gu[l].rearrange("(a r) p -> r a p", r=128)), W=[R_bgs])
            for a in range(4):
                P.pe(lambda e: e.transpose(out=ps[6][:, a * 128:(a + 1) * 128], in_=bgs[:, a, :], identity=ident_f[:]), R=[R_bgs, R_const], W=[R_ps[6]])
            P.op("dve", lambda e: e.tensor_copy(out=bgT[:], in_=ps[6][:, :]), R=[R_ps[6]], W=[R_bgT])

            def load_expert(e_):
                s2 = e_ % 2
                for k in range(8):
                    si = nxt("stg", 3)
                    P.dma("sp", lambda e: e.dma_start(out=stg[si][:], in_=w_gu[l, e_, k * 128:(k + 1) * 128, :]), W=[R_stg[si]])
                    P.op("pool" if k % 2 == 0 else "dve", lambda e: e.tensor_copy(out=wgu_b[s2][:, k, :], in_=stg[si][:]), R=[R_stg[si]], W=[R_wgu[s2][k]])
                for k in range(8):
                    si = nxt("stg", 3)
                    P.dma("sp", lambda e: e.dma_start(out=stg[si][:, 0:D], in_=w_dn[l, e_, k * 128:(k + 1) * 128, :]), W=[R_stg[si]])
                    P.op("pool" if k % 2 == 0 else "dve", lambda e: e.tensor_copy(out=wdn_b[s2][:, k, :], in_=stg[si][:, 0:D]), R=[R_stg[si]], W=[R_wdn[s2][k]])
                P.dma("sp", lambda e: e.dma_start(out=bdb[s2][:], in_=b_dn[l, e_:e_ + 1, :].to_broadcast([128, D])), W=[R_bdb[s2]])

            load_expert(0)
            for e_ in range(NE):
                s2 = e_ % 2
                if e_ + 1 < NE:
                    load_expert(e_ + 1)
                for st_ in range(CAP // 128):
                    xi = nxt("xr", 3)
                    P.dma("sp", lambda e: e.dma_start(out=xr[xi][:], in_=xg[e_ * CAP + st_ * 128:e_ * CAP + (st_ + 1) * 128, :]), R=[R_xg], W=[R_xr[xi]])
                    for k in range(8):
                        P.pe(lambda e: e.transpose(out=psT[:, k, :], in_=xr[xi][:, k * 128:(k + 1) * 128], identity=ident_b[:]), R=[R_xr[xi], R_const], W=[R_psT])
                    P.op("act", lambda e: e.activation(out=xeT[s2][:, :, st_ * 128:(st_ + 1) * 128], in_=psT[:, :, :], func=AF.Copy), R=[R_psT], W=[R_xeT[s2]])
                for c in range(8):
                    for (s0, sw) in ((0, 512), (512, CAP - 512)):
                        pg = nxt("pg", 2) * 2
                        for k in range(8):
                            P.pe(lambda e: e.matmul(ps[pg][:, 0:sw], lhsT=wgu_b[s2][:, k, c * 128:(c + 1) * 128], rhs=xeT[s2][:, k, s0:s0 + sw], start=(k == 0), stop=(k == 7)),
                                 R=[R_wgu[s2][k], R_xeT[s2]], W=[R_ps[pg]])
                        for k in range(8):
                            P.pe(lambda e: e.matmul(ps[pg + 1][:, 0:sw], lhsT=wgu_b[s2][:, k, D + c * 128:D + (c + 1) * 128], rhs=xeT[s2][:, k, s0:s0 + sw], start=(k == 0), stop=(k == 7)),
                                 R=[R_wgu[s2][k], R_xeT[s2]], W=[R_ps[pg + 1]])
                        ei = nxt("et", 2) * 3
                        bgc = bgT[:, e_ * 16 + c:e_ * 16 + c + 1]
                        buc = bgT[:, e_ * 16 + 8 + c:e_ * 16 + 8 + c + 1]
                        P.op("dve", lambda e: e.tensor_scalar(out=et[ei][:, 0:sw], in0=ps[pg][:, 0:sw], scalar1=bgc, scalar2=7.0, op0=ALU.add, op1=ALU.min), R=[R_ps[pg], R_bgT], W=[R_et[ei]])
                        P.op("act", lambda e: e.activation(out=et[ei + 1][:, 0:sw], in_=et[ei][:, 0:sw], func=AF.Sigmoid, scale=1.702), R=[R_et[ei]], W=[R_et[ei + 1]])
                        P.op("act", lambda e: e.activation(out=et[ei + 2][:, 0:sw], in_=ps[pg + 1][:, 0:sw], func=AF.Identity, bias=buc, scale=1.0), R=[R_ps[pg + 1], R_bgT], W=[R_et[ei + 2]])
                        P.op("pool", lambda e: e.tensor_scalar(out=et[ei + 2][:, 0:sw], in0=et[ei + 2][:, 0:sw], scalar1=7.0, scalar2=-7.0, op0=ALU.min, op1=ALU.max), R=[R_et[ei + 2]], W=[R_et[ei + 2]])
                        P.op("pool", lambda e: e.tensor_tensor(out=et[ei][:, 0:sw], in0=et[ei][:, 0:sw], in1=et[ei + 1][:, 0:sw], op=ALU.mult), R=[R_et[ei], R_et[ei + 1]], W=[R_et[ei]])
                        P.op("dve", lambda e: e.scalar_tensor_tensor(out=GT[:, c, s0:s0 + sw], in0=et[ei + 2][:, 0:sw], scalar=1.0, in1=et[ei][:, 0:sw], op0=ALU.add, op1=ALU.mult), R=[R_et[ei], R_et[ei + 2]], W=[R_GT[c]])
                for st_ in range(CAP // 128):
                    yi = nxt("yt", 2)
                    for half in range(2):
                        pi = 4 + nxt("pd", 2)
                        for c in range(8):
                            P.pe(lambda e: e.matmul(ps[pi][:, :], lhsT=GT[:, c, st_ * 128:(st_ + 1) * 128], rhs=wdn_b[s2][:, c, half * 512:(half + 1) * 512], start=(c == 0), stop=(c == 7)),
                                 R=[R_GT[c], R_wdn[s2][c]], W=[R_ps[pi]])
                        P.op("dve", lambda e: e.tensor_tensor(out=yt[yi][:, half * 512:(half + 1) * 512], in0=ps[pi][:, :], in1=bdb[s2][:, half * 512:(half + 1) * 512], op=ALU.add),
                             R=[R_ps[pi], R_bdb[s2]], W=[R_yt[yi]])
                    P.dma("pool", lambda e: e.dma_start(out=yg[e_ * CAP + st_ * 128:e_ * CAP + (st_ + 1) * 128, :], in_=yt[yi][:]), R=[R_yt[yi]], W=[R_yg])
            P.barrier()

        with ExitStack() as es:
            def sbm(name, shape, dt):
                return es.enter_context(nc.sbuf_tensor(name + "_c%d" % l, list(shape), dt))
            yk = [sbm("yk%d" % i, [128, D], F32) for i in range(8)]; R_yk = [Res("yk") for _ in range(8)]
            xa = [sbm("xa%d" % i, [128, D], F32) for i in range(2)]; R_xa = [Res("xa") for _ in range(2)]
            xo = [sbm("xo%d" % i, [128, D], F32) for i in range(2)]; R_xo = [Res("xo") for _ in range(2)]
            g2 = sbm("g2", [128, D], F32); b2 = sbm("b2", [128, D], F32); R_gb2 = Res("gb2")
            junk2 = sbm("junk2", [128, D], BF16); R_junk2 = Res("junk2")
            lst2 = sbm("lst2", [128, 8], F32); R_lst2 = Res("lst2")
            P.dma("sp", lambda e: e.dma_start(out=g2[:], in_=ln2g[l:l + 1, :].to_broadcast([128, D])), W=[R_gb2])
            P.dma("sp", lambda e: e.dma_start(out=b2[:], in_=ln2b[l:l + 1, :].to_broadcast([128, D])), W=[R_gb2])
            for tt in range(32):
                i2 = tt % 2
                rows = slice(tt * 128, (tt + 1) * 128)
                P.dma("sp", lambda e: e.dma_start(out=xa[i2][:], in_=x1s[rows, :]), R=[R_x1s], W=[R_xa[i2]])
                P.op("act", lambda e: e.activation(out=xa[i2][:], in_=xa[i2][:], func=AF.Copy, scale=ALPHA), R=[R_xa[i2]], W=[R_xa[i2]])
                for k in range(4):
                    yi = i2 * 4 + k
                    P.dma("pool", lambda e: e.indirect_dma_start(out=yk[yi][:, :], out_offset=None, in_=yg[:, :], in_offset=bass.IndirectOffsetOnAxis(ap=sloti[:, tt * 4 + k:tt * 4 + k + 1], axis=0)),
                          R=[R_yg, R_slot], W=[R_yk[yi]])
                    P.op("dve", lambda e: e.scalar_tensor_tensor(out=xa[i2][:], in0=yk[yi][:], scalar=gates[:, tt * 4 + k:tt * 4 + k + 1], in1=xa[i2][:], op0=ALU.mult, op1=ALU.add),
                         R=[R_yk[yi], R_gates, R_xa[i2]], W=[R_xa[i2]])
                layer_norm(xa[i2], R_xa[i2], g2, b2, R_gb2, xo[i2], R_xo[i2], lst2, R_lst2, junk2, R_junk2)
                P.dma("sp", lambda e: e.dma_start(out=xdst[rows, :], in_=xo[i2][:]), R=[R_xo[i2]], W=[R_xdst])
            P.barrier()

    P.finish()
    return nc, P


def _consts():
    bf = ml_dtypes.bfloat16
    ident = np.eye(128, dtype=np.float32)
    ltri = np.triu(np.ones((128, 128), np.float32), 1)
    si = np.arange(128)[:, None]; qi = np.arange(128)[None, :]
    dmask = np.zeros((128, 8, 128), np.float32)
    for h in range(8):
        sg = SIG8[h]
        d = np.where(si <= qi, 0.0, np.where((si // 64) == (qi // 64), -2.0 * sg * (si - qi), NEG))
        dmask[:, h, :] = d
    q = np.arange(S)
    qaug = np.stack([np.ones(S), -(q % 128).astype(np.float64), -(128.0 * ((q // 128) % 4))]).astype(np.float32)
    kaug = np.zeros((8, 3, S), np.float32)
    for h in range(8):
        kaug[h, 0] = SIG8[h] * (q % 128)
        kaug[h, 1] = SIG8[h]
        kaug[h, 2] = SIG8[h]
    eoff = np.tile((np.arange(NE, dtype=np.float32) * CAP)[None, :], (128, 1))
    return {"c_ident": ident.astype(bf), "c_identf": ident, "c_ltri": ltri.astype(bf), "c_dmask": dmask.astype(bf),
            "c_qaug": qaug.astype(bf), "c_kaug": kaug.astype(bf), "c_eoff": eoff}


def _relb_pieces(rel_bias):
    NLn = rel_bias.shape[0]
    si = np.arange(128)[:, None]; qi = np.arange(128)[None, :]
    out = np.empty((NLn, 128, 16, 128), np.float32)
    for h in range(4):
        rel0 = qi - si
        idx0 = np.clip(rel0, -128, 128) + 128
        m0 = ((si // 64) == 1) & ((qi // 64) == 0)
        idx1 = np.clip(128 + qi - si, -128, 128) + 128
        m4 = ((si // 64) == 0) & ((qi // 64) == 1)
        for ln in range(NLn):
            rb = rel_bias[ln, h]
            p0 = rb[idx0].copy(); p0[m0] = NEG
            p1 = rb[idx1]
            p2 = np.broadcast_to(rb[256], (128, 128))
            p4 = np.array(p2); p4[m4] = NEG
            out[ln, :, h * 4 + 0, :] = p0
            out[ln, :, h * 4 + 1, :] = p1
            out[ln, :, h * 4 + 2, :] = p2
            out[ln, :, h * 4 + 3, :] = p4
    return out


_CACHE = {}


def _get_prog(NL, lam_inits, dbg=None):
    key = (NL, tuple(lam_inits), dbg)
    if key not in _CACHE:
        _CACHE[key] = build(NL, lam_inits, dbg)[0]
    return _CACHE[key]


def _layer_inputs(inp, ls):
    f = lambda a: np.ascontiguousarray(a, dtype=np.float32)
    d = {
        "w_in": f(inp["w_in"][ls]),
        "lamv": f(np.stack([inp["lam_q1"][ls], inp["lam_k1"][ls], inp["lam_q2"][ls], inp["lam_k2"][ls]], axis=1)),
        "subln_g": f(inp["subln_g"][ls]),
        "relb": _relb_pieces(np.asarray(inp["rel_bias"][ls], np.float32)),
        "w_out": f(inp["w_out"][ls]),
        "ln1_g": f(inp["ln1_g"][ls]), "ln1_b": f(inp["ln1_b"][ls]),
        "w_router": f(inp["w_router"][ls]), "b_router": f(inp["b_router"][ls]),
        "w_gu": f(inp["w_gu"][ls]), "b_gu": f(inp["b_gu"][ls]).reshape(len(range(*ls.indices(DEPTH))), NE * 16, 128),
        "w_down": f(inp["w_down"][ls]), "b_down": f(inp["b_down"][ls]),
        "ln2_g": f(inp["ln2_g"][ls]), "ln2_b": f(inp["ln2_b"][ls]),
    }
    return d


FUSED = True


def kernel(**inp):
    x = np.ascontiguousarray(inp["x"], dtype=np.float32)
    consts = _consts()
    lam_inits_all = [0.8 - 0.6 * math.exp(-0.3 * l) for l in range(DEPTH)]
    xs = [x[c * NBL:(c + 1) * NBL].reshape(T, D) for c in range(NCORES)]
    if FUSED:
        nc = _get_prog(DEPTH, lam_inits_all)
        li = _layer_inputs(inp, slice(0, DEPTH))
        in_maps = [dict(li, x=xs[c], **consts) for c in range(NCORES)]
        res = run_bass_kernel_spmd(nc, in_maps, core_ids=list(range(NCORES)))
        xs = [res.results[c]["y"] for c in range(NCORES)]
    else:
        for l in range(DEPTH):
            nc = _get_prog(1, [lam_inits_all[l]])
            li = _layer_inputs(inp, slice(l, l + 1))
            in_maps = [dict(li, x=xs[c], **consts) for c in range(NCORES)]
            res = run_bass_kernel_spmd(nc, in_maps, core_ids=list(range(NCORES)))
            xs = [np.asarray(res.results[c]["y"]) for c in range(NCORES)]
    out = np.stack([xs[c].reshape(NBL, S, D) for c in range(NCORES)], axis=0).reshape(NCORES * NBL, S, D)
    return out.astype(np.float32)
```

```python
import math
from contextlib import ExitStack
import numpy as np
import ml_dtypes
import concourse.bass as bass
import concourse.mybir as mybir
from concourse.bass_utils import run_bass_kernel_spmd

F32 = mybir.dt.float32
BF16 = mybir.dt.bfloat16
I32 = mybir.dt.int32
AF = mybir.ActivationFunctionType
ALU = mybir.AluOpType
AX = mybir.AxisListType

NCORES = 8
DEPTH = 4
S = 2048
D = 1024
NBL = 2
T = NBL * S
DIN = 3368
NE = 32
CAP = 768
NSLOT = NE * CAP
ALPHA = (2 * DEPTH) ** 0.25
EPS = 1e-5
NEG = -30000.0
SIG_A = [2.0 ** -1, 2.0 ** -3, 2.0 ** -5, 2.0 ** -7]
SIG_C = [2.0 ** -2, 2.0 ** -4, 2.0 ** -6, 2.0 ** -8]
SIG8 = SIG_A + SIG_C
W_SCALE = (8 ** -0.5) * (32 ** -0.5)
NIT = 16
C_QA, C_KA, C_VA, C_QB, C_KB, C_VB, C_QC, C_KC, C_VC, C_QI, C_KI, C_WI = (
    0, 512, 1024, 1536, 1792, 2048, 2304, 2560, 2816, 3072, 3328, 3360)


class Res:
    __slots__ = ("name", "w", "r", "multi")

    def __init__(self, name, multi=False):
        self.name = name
        self.w = {}
        self.r = {}
        self.multi = multi


class Prog:
    def __init__(self, nc, ndma=(("sp", 40), ("pool", 40), ("act", 8))):
        self.nc = nc
        self.E = {"pe": nc.tensor, "act": nc.scalar, "dve": nc.vector, "pool": nc.gpsimd, "sp": nc.sync}
        self.esem = {k: nc.alloc_semaphore("e_" + k) for k in self.E}
        self.ecnt = {k: 0 for k in self.E}
        self.waited = {k: {} for k in self.E}
        self.dsem = {}
        for k, n in ndma:
            self.dsem[k] = [[nc.alloc_semaphore("d_%s%d" % (k, i)), 0, "d_%s%d" % (k, i)] for i in range(n)]
        self.dnext = {k: 0 for k in self.dsem}
        self.nins = 0

    def _wait(self, eng, ev):
        key, sem, val = ev
        if self.waited[eng].get(key, 0) >= val:
            return
        self.E[eng].wait_ge(sem, val)
        self.waited[eng][key] = val
        self.nins += 1

    def _deps(self, eng, R, W, skip_self):
        for r in R:
            for ev in r.w.values():
                if not (skip_self and ev[0] == eng):
                    self._wait(eng, ev)
        for w in W:
            if not w.multi:
                for ev in w.w.values():
                    if not (skip_self and ev[0] == eng):
                        self._wait(eng, ev)
            for ev in w.r.values():
                if not (skip_self and ev[0] == eng):
                    self._wait(eng, ev)

    def _record(self, ev, R, W):
        for r in R:
            r.r[ev[0]] = ev
        for w in W:
            if w.multi:
                w.w[ev[0]] = ev
            else:
                w.w = {ev[0]: ev}
            w.r = {}

    def op(self, eng, fn, R=(), W=(), skip_self=False):
        self._deps(eng, R, W, skip_self)
        ins = fn(self.E[eng])
        self.ecnt[eng] += 1
        ins.then_inc(self.esem[eng], 1)
        ev = (eng, self.esem[eng], self.ecnt[eng])
        self._record(ev, R, W)
        self.nins += 1
        return ev

    def pe(self, fn, R=(), W=()):
        return self.op("pe", fn, R, W, skip_self=True)

    def dma(self, eng, fn, R=(), W=()):
        self._deps(eng, R, W, False)
        pool = self.dsem[eng]
        slot = pool[self.dnext[eng] % len(pool)]
        self.dnext[eng] += 1
        if slot[1] > 0:
            self._wait(eng, (slot[2], slot[0], slot[1]))
        ins = fn(self.E[eng])
        slot[1] += 16
        ins.then_inc(slot[0], 16)
        ev = (slot[2], slot[0], slot[1])
        self._record(ev, R, W)
        self.nins += 1
        return ev

    def barrier(self):
        evs = [(k, self.esem[k], self.ecnt[k]) for k in self.E if self.ecnt[k] > 0]
        for k in self.dsem:
            for s in self.dsem[k]:
                if s[1] > 0:
                    evs.append((s[2], s[0], s[1]))
        for e in self.E:
            for ev in evs:
                self._wait(e, ev)

    def finish(self):
        self.barrier()


def build(NL, lam_inits, dbg=None):
    nc = bass.Bass("TRN2", target_bir_lowering=False)
    P = Prog(nc)

    def din(name, shape, dt=F32):
        return nc.dram_tensor(name, list(shape), dt, kind="ExternalInput").ap()

    x_in = din("x", [T, D])
    w_in = din("w_in", [NL, D, DIN])
    lamv = din("lamv", [NL, 4, 64])
    subg = din("subln_g", [NL, 128])
    relb = din("relb", [NL, 128, 16, 128])
    w_out = din("w_out", [NL, D, D])
    ln1g = din("ln1_g", [NL, D]); ln1b = din("ln1_b", [NL, D])
    w_rt = din("w_router", [NL, D, NE]); b_rt = din("b_router", [NL, NE])
    if dbg is None or dbg == "moe":
        w_gu = din("w_gu", [NL, NE, D, 2 * D]); w_dn = din("w_down", [NL, NE, D, D])
    b_gu = din("b_gu", [NL, NE * 16, 128]); b_dn = din("b_down", [NL, NE, D])
    ln2g = din("ln2_g", [NL, D]); ln2b = din("ln2_b", [NL, D])
    c_ident = din("c_ident", [128, 128], BF16)
    c_identf = din("c_identf", [128, 128], F32)
    c_ltri = din("c_ltri", [128, 128], BF16)
    c_dmask = din("c_dmask", [128, 8, 128], BF16)
    c_qaug = din("c_qaug", [3, S], BF16)
    c_kaug = din("c_kaug", [8, 3, S], BF16)
    c_eoff = din("c_eoff", [128, NE], F32)
    y_out = nc.dram_tensor("y", [T, D], F32, kind="ExternalOutput").ap()
    dbg_out = None
    if dbg == "mix":
        dbg_out = nc.dram_tensor("dbg", [NBL, 128, 8, S], BF16, kind="ExternalOutput").ap()
    if dbg == "x1":
        dbg_out = nc.dram_tensor("dbg", [T, D], F32, kind="ExternalOutput").ap()
    xcur = nc.dram_tensor("xcur", [T, D], F32).ap()
    x1s = nc.dram_tensor("x1s", [T, D], F32).ap()
    xg = nc.dram_tensor("xg", [NSLOT, D], BF16).ap()
    yg = nc.dram_tensor("yg", [NSLOT, D], F32).ap()
    R_xin = Res("xin", True); R_xcur = Res("xcur", True); R_x1s = Res("x1s", True)
    R_xg = Res("xg", True); R_yg = Res("yg", True); R_y = Res("y", True); R_dbg = Res("dbg", True)
    R_const = Res("const", True)

    def sb(name, shape, dt):
        return nc.alloc_sbuf_tensor(name, list(shape), dt)

    ident_b = sb("ident_b", [128, 128], BF16)
    ident_f = sb("ident_f", [128, 128], F32)
    ltri_b = sb("ltri_b", [128, 128], BF16)
    ones_b = sb("ones_b", [128, 128], BF16)
    zeros_b = sb("zeros_b", [128, 512], BF16)
    dmask_b = sb("dmask_b", [128, 8, 128], BF16)
    eoff = sb("eoff", [128, NE], F32)
    base_cnt = sb("base_cnt", [128, NE], F32)
    slotf = sb("slotf", [128, 32 * 4], F32)
    sloti = sb("sloti", [128, 32 * 4], I32)
    gates = sb("gates", [128, 32 * 4], F32)
    R_slot = Res("slot"); R_gates = Res("gates"); R_base = Res("base")
    for t_, src in ((ident_b, c_ident), (ident_f, c_identf), (ltri_b, c_ltri), (dmask_b, c_dmask), (eoff, c_eoff)):
        P.dma("sp", lambda e, t_=t_, src=src: e.dma_start(out=t_[:], in_=src), W=[R_const])
    P.op("pool", lambda e: e.memset(ones_b[:], 1.0), W=[R_const])
    P.op("pool", lambda e: e.memset(zeros_b[:], 0.0), W=[R_const])

    ps = [nc.alloc_psum_tensor("ps%d" % i, [128, 512], F32) for i in range(7)]
    R_ps = [Res("ps%d" % i) for i in range(7)]
    psT = nc.alloc_psum_tensor("psT", [128, 8, 128], BF16)
    R_psT = Res("psT")

    rot = {}

    def nxt(key, n):
        rot[key] = (rot.get(key, -1) + 1) % n
        return rot[key]

    def layer_norm(v, R_v, g_t, b_t, R_gb, out_t, R_out, st, R_st, junk, R_junk):
        P.op("dve", lambda e: e.reduce_sum(out=st[:, 0:1], in_=v[:], axis=AX.X), R=[R_v], W=[R_st])
        P.op("dve", lambda e: e.tensor_scalar(out=st[:, 1:2], in0=st[:, 0:1], scalar1=-1.0 / D, scalar2=None, op0=ALU.mult), R=[R_st], W=[R_st])
        P.op("act", lambda e: e.activation(out=junk[:], in_=v[:], func=AF.Square, bias=st[:, 1:2], scale=1.0, accum_out=st[:, 2:3]), R=[R_v, R_st], W=[R_junk, R_st])
        P.op("dve", lambda e: e.tensor_scalar(out=st[:, 3:4], in0=st[:, 2:3], scalar1=1.0 / D, scalar2=EPS, op0=ALU.mult, op1=ALU.add), R=[R_st], W=[R_st])
        P.op("act", lambda e: e.activation(out=st[:, 4:5], in_=st[:, 3:4], func=AF.Ln), R=[R_st], W=[R_st])
        P.op("act", lambda e: e.activation(out=st[:, 5:6], in_=st[:, 4:5], func=AF.Exp, scale=-0.5), R=[R_st], W=[R_st])
        P.op("dve", lambda e: e.tensor_scalar(out=v[:], in0=v[:], scalar1=st[:, 1:2], scalar2=st[:, 5:6], op0=ALU.add, op1=ALU.mult), R=[R_v, R_st], W=[R_v])
        P.op("pool", lambda e: e.tensor_tensor(out=v[:], in0=v[:], in1=g_t[:], op=ALU.mult), R=[R_v, R_gb], W=[R_v])
        P.op("dve", lambda e: e.tensor_tensor(out=out_t[:], in0=v[:], in1=b_t[:], op=ALU.add), R=[R_v, R_gb], W=[R_out])

    for l in range(NL):
        lam_init = lam_inits[l]
        xsrc, R_xsrc = (x_in, R_xin) if l == 0 else (xcur, R_xcur)
        last = (l == NL - 1)
        xdst, R_xdst = (y_out, R_y) if last else (xcur, R_xcur)
        P.op("pool", lambda e: e.memset(base_cnt[:], 0.0), W=[R_base])

        for b in range(NBL):
            tok0 = b * S
            with ExitStack() as es_b:
                def sbc(es, name, shape, dt):
                    return es.enter_context(nc.sbuf_tensor(name + "_%d_%d" % (l, b), list(shape), dt))
                xT = sbc(es_b, "xT", [128, 8, S], BF16); R_xT = [Res("xT%d" % i) for i in range(4)]
                mixT = sbc(es_b, "mixT", [128, 8, S], BF16)
                R_mix = [[Res("mix%d_%d" % (c, j)) for j in range(4)] for c in range(8)]

                with ExitStack() as es:
                    xbs = [sbc(es, "xbs%d" % i, [128, D], BF16) for i in range(3)]; R_xbs = [Res("xbs") for _ in range(3)]
                    for tt in range(16):
                        i2 = tt % 3
                        P.dma("pool", lambda e: e.dma_start(out=xbs[i2][:], in_=xsrc[tok0 + tt * 128: tok0 + (tt + 1) * 128, :]), R=[R_xsrc], W=[R_xbs[i2]])
                        for k in range(8):
                            P.pe(lambda e: e.transpose(out=psT[:, k, :], in_=xbs[i2][:, k * 128:(k + 1) * 128], identity=ident_b[:]), R=[R_xbs[i2], R_const], W=[R_psT])
                        P.op("dve", lambda e: e.tensor_copy(out=xT[:, :, tt * 128:(tt + 1) * 128], in_=psT[:, :, :]), R=[R_psT], W=[R_xT[tt // 4]])
                    P.barrier()

                with ExitStack() as es:
                    QK = [sbc(es, "qk%d" % i, [128, S], BF16) for i in range(12)]
                    R_QK = [Res("qk%d" % i) for i in range(12)]
                    VV = sbc(es, "vv", [128, 16, 512], BF16); R_VV = Res("vv")
                    wbf = [sbc(es, "wbf%d" % i, [128, 8, 128], BF16) for i in range(4)]; R_wbf = [Res("wbf") for _ in range(4)]
                    PT = [sbc(es, "pt%d" % i, [128, 512], BF16) for i in range(4)]; R_PT = [Res("pt") for _ in range(4)]
                    relb_b = sbc(es, "relb_b", [128, 16, 128], BF16); R_relb = Res("relb")
                    scoreb = [sbc(es, "score%d" % i, [128, S], F32) for i in range(2)]; R_scoreb = [Res("score%d" % i) for i in range(2)]
                    score = scoreb[0]; R_score = R_scoreb[0]
                    relu_sb = [sbc(es, "relu%d" % i, [128, 512], BF16) for i in range(3)]; R_relu = [Res("relu") for _ in range(3)]
                    negmask = sbc(es, "negmask", [128, S], BF16); R_negm = Res("negm")
                    nmT = sbc(es, "nmT", [128, 16, 512], BF16); R_nmT = Res("nmT")
                    diag = [sbc(es, "diag%d" % i, [128, 8, 128], BF16) for i in range(2)]; R_diag = [Res("diag%d" % i) for i in range(2)]
                    wtok = sbc(es, "wtok", [128, 16, 8], F32); R_wtok = Res("wtok")
                    ft = [sbc(es, "ft%d" % i, [128, 512], F32) for i in range(4)]; R_ft = [Res("ft%d" % i) for i in range(4)]
                    sqb = sbc(es, "sqb", [128, 512], BF16); R_sqb = Res("sqb")
                    sm = sbc(es, "sm", [128, 16], F32); R_sm = Res("sm")
                    lamt = sbc(es, "lamt", [128, 4, 64], F32); R_lamt = Res("lamt")
                    gsc = sbc(es, "gsc", [128, 2], F32)

                    P.dma("sp", lambda e: e.dma_start(out=lamt[:], in_=lamv[l:l + 1, :, :].to_broadcast([128, 4, 64])), W=[R_lamt])
                    P.dma("sp", lambda e: e.dma_start(out=gsc[:, 0:1], in_=subg[l, :].rearrange("(p o) -> p o", o=1)), W=[R_sm])
                    P.op("dve", lambda e: e.tensor_tensor(out=lamt[:, 0, :], in0=lamt[:, 0, :], in1=lamt[:, 1, :], op=ALU.mult), R=[R_lamt], W=[R_lamt])
                    P.op("dve", lambda e: e.tensor_tensor(out=lamt[:, 2, :], in0=lamt[:, 2, :], in1=lamt[:, 3, :], op=ALU.mult), R=[R_lamt], W=[R_lamt])
                    P.op("dve", lambda e: e.reduce_sum(out=sm[:, 0:1], in_=lamt[:, 0, :], axis=AX.X), R=[R_lamt], W=[R_sm])
                    P.op("dve", lambda e: e.reduce_sum(out=sm[:, 1:2], in_=lamt[:, 2, :], axis=AX.X), R=[R_lamt], W=[R_sm])
                    P.op("act", lambda e: e.activation(out=sm[:, 2:4], in_=sm[:, 0:2], func=AF.Exp), R=[R_sm], W=[R_sm])
                    P.op("dve", lambda e: e.tensor_tensor(out=sm[:, 4:5], in0=sm[:, 3:4], in1=sm[:, 2:3], op=ALU.subtract), R=[R_sm], W=[R_sm])
                    P.op("dve", lambda e: e.tensor_scalar(out=sm[:, 5:6], in0=sm[:, 4:5], scalar1=-lam_init, scalar2=None, op0=ALU.add), R=[R_sm], W=[R_sm])
                    P.op("dve", lambda e: e.tensor_scalar(out=gsc[:, 1:2], in0=gsc[:, 0:1], scalar1=1.0 - lam_init, scalar2=None, op0=ALU.mult), R=[R_sm], W=[R_sm])
                    neglam = sm[:, 5:6]
                    gscale = gsc[:, 1:2]

                    P.dma("sp", lambda e: e.dma_start(out=score[:, :].rearrange("p (a b) -> p a b", a=16), in_=relb[l]), W=[R_score])
                    P.op("pool", lambda e: e.tensor_copy(out=relb_b[:], in_=score[:, :].rearrange("p (a b) -> p a b", a=16)), R=[R_score], W=[R_relb])

                    def load_w(c0, ncol, rep=1):
                        bi = nxt("wbf", 4)
                        for r in range(rep):
                            P.dma("pool", lambda e: e.dma_start(out=wbf[bi][:, :, r * ncol:(r + 1) * ncol], in_=w_in[l, :, c0:c0 + ncol].rearrange("(k p) c -> p k c", p=128)), W=[R_wbf[bi]])
                        return bi

                    def proj_feat(bi, c_lo, m, dst, R_dst, p_lo, scale):
                        for tb in range(4):
                            pi = 4 + nxt("pj", 2)
                            for k in range(8):
                                P.pe(lambda e: e.matmul(ps[pi][0:m, :], lhsT=wbf[bi][:, k, c_lo:c_lo + m], rhs=xT[:, k, tb * 512:(tb + 1) * 512], start=(k == 0), stop=(k == 7)),
                                     R=[R_wbf[bi], R_xT[tb]], W=[R_ps[pi]])
                            P.op("act", lambda e: e.activation(out=dst[p_lo:p_lo + m, tb * 512:(tb + 1) * 512], in_=ps[pi][p_lo:p_lo + m, :], func=AF.Copy, scale=scale),
                                 R=[R_ps[pi]], W=[R_dst])

                    def proj_feat_split(bi, dst0, R0, dst1, R1, scale):
                        for tb in range(4):
                            pi = 4 + nxt("pj", 2)
                            for k in range(8):
                                P.pe(lambda e: e.matmul(ps[pi][:, :], lhsT=wbf[bi][:, k, :], rhs=xT[:, k, tb * 512:(tb + 1) * 512], start=(k == 0), stop=(k == 7)),
                                     R=[R_wbf[bi], R_xT[tb]], W=[R_ps[pi]])
                            P.op("act", lambda e: e.activation(out=dst0[0:64, tb * 512:(tb + 1) * 512], in_=ps[pi][0:64, :], func=AF.Copy, scale=scale), R=[R_ps[pi]], W=[R0])
                            P.op("act", lambda e: e.activation(out=dst1[64:128, tb * 512:(tb + 1) * 512], in_=ps[pi][64:128, :], func=AF.Copy, scale=scale), R=[R_ps[pi]], W=[R1])

                    def proj_tok(bi, ncol, dst_fn, R_dst, act_eng="act"):
                        for st_ in range(16):
                            pi = 4 + nxt("pj", 2)
                            for k in range(8):
                                P.pe(lambda e: e.matmul(ps[pi][:, 0:ncol], lhsT=xT[:, k, st_ * 128:(st_ + 1) * 128], rhs=wbf[bi][:, k, 0:ncol], start=(k == 0), stop=(k == 7)),
                                     R=[R_wbf[bi], R_xT[st_ // 4]], W=[R_ps[pi]])
                            P.op("act", lambda e: e.activation(out=dst_fn(st_), in_=ps[pi][:, 0:ncol], func=AF.Copy), R=[R_ps[pi]], W=[R_dst])

                    def recip_safe(dst, R_dst, src_ps, R_src, p0, p1):
                        P.op("dve", lambda e: e.tensor_scalar(out=dst[p0:p1, :], in0=src_ps[p0:p1, :], scalar1=1e-30, scalar2=None, op0=ALU.max), R=[R_src], W=[R_dst])
                        P.op("dve", lambda e: e.reciprocal(out=dst[p0:p1, :], in_=dst[p0:p1, :]), R=[R_dst], W=[R_dst])

                    def attn_block(J, i_list, colrange_fn, score_mms, cfn, Vl_fn, R_V, accs, deferred=None):
                        for (oi, si_, m) in accs:
                            for bi_ in (oi, si_):
                                P.pe(lambda e: e.matmul(ps[bi_][:, :], lhsT=zeros_b[:, 0:128], rhs=zeros_b[:, :], start=True, stop=False), R=[R_const], W=[R_ps[bi_]])
                        n_i = len(i_list)
                        units = [(ii, i, acc) for ii, i in enumerate(i_list) for acc in accs]
                        n_u = len(units)
                        stt = {}

                        def S_(u):
                            ii, i, (oi, si_, m) = units[u]
                            c_lo, c_hi = colrange_fn(i)
                            sci = 4 + nxt("scb", 3)
                            score_mms(sci, i, m, c_lo, c_hi)
                            stt[u] = sci

                        def E_(u):
                            ii, i, (oi, si_, m) = units[u]
                            c_lo, c_hi = colrange_fn(i)
                            sci = stt[u]
                            pti = nxt("pt", 4)
                            P.op("act", lambda e: e.activation(out=PT[pti][:, c_lo:c_hi], in_=ps[sci][:, c_lo:c_hi], func=AF.Exp, bias=float(cfn(i)), scale=1.0),
                                 R=[R_ps[sci]], W=[R_PT[pti]])
                            stt[u] = pti

                        def V_(u):
                            ii, i, (oi, si_, m) = units[u]
                            c_lo, c_hi = colrange_fn(i)
                            pti = stt[u]
                            lastf = (ii == n_i - 1)
                            P.pe(lambda e: e.matmul(ps[oi][:, c_lo:c_hi], lhsT=Vl_fn(i), rhs=PT[pti][:, c_lo:c_hi], start=False, stop=lastf), R=[R_PT[pti], R_V], W=[R_ps[oi]])
                            P.pe(lambda e: e.matmul(ps[si_][:, c_lo:c_hi], lhsT=ones_b[:], rhs=PT[pti][:, c_lo:c_hi], start=False, stop=lastf), R=[R_PT[pti], R_const], W=[R_ps[si_]])
                        S_(0)
                        if n_u > 1:
                            S_(1)
                        for u in range(n_u):
                            E_(u)
                            if u + 2 < n_u:
                                S_(u + 2)
                            V_(u)
                            if deferred is not None and u == min(3, n_u - 1):
                                deferred()

                    pend = []
                    for g in range(4):
                        bi = load_w(C_VA + g * 128, 128)
                        proj_tok(bi, 128, lambda st_, g=g: VV[:, st_, g * 128:(g + 1) * 128], R_VV)
                    for qi_ in (0, 1):
                        P.dma("sp", lambda e: e.dma_start(out=QK[qi_][64:67, :], in_=c_qaug), W=[R_QK[qi_]])
                    for h in range(4):
                        Q1, Q2, K1, K2 = QK[0], QK[1], QK[2], QK[3]
                        for kt in (2, 3):
                            P.dma("sp", lambda e: e.dma_start(out=QK[kt][64:67, :], in_=c_kaug[h]), W=[R_QK[kt]])
                        bq = load_w(C_QA + h * 128, 128)
                        bk = load_w(C_KA + h * 128, 128)
                        proj_feat(bq, 0, 64, Q1, R_QK[0], 0, 0.125)
                        proj_feat(bq, 64, 64, Q2, R_QK[1], 0, 0.125)
                        proj_feat(bk, 0, 64, K1, R_QK[2], 0, 1.0)
                        proj_feat(bk, 64, 64, K2, R_QK[3], 0, 1.0)
                        sig = SIG_A[h]
                        for J in range(4):
                            def colr(i, J=J):
                                return (128 * max(0, i - 4 * J), 512)

                            def smm(sci, i, m, c_lo, c_hi, J=J, h=h):
                                isd = i >= 4 * J
                                P.pe(lambda e: e.matmul(ps[sci][:, c_lo:c_hi], lhsT=QK[2 + m][0:67, i * 128:(i + 1) * 128], rhs=QK[m][0:67, J * 512 + c_lo:J * 512 + c_hi], start=True, stop=not isd),
                                     R=[R_QK[2 + m], R_QK[m]], W=[R_ps[sci]])
                                if isd:
                                    a = i - 4 * J
                                    P.pe(lambda e: e.matmul(ps[sci][:, a * 128:(a + 1) * 128], lhsT=ident_b[:], rhs=dmask_b[:, h, :], start=False, stop=True), R=[R_const], W=[R_ps[sci]])
                            attn_block(J, list(range(4 * J + 4)), colr, smm, lambda i, J=J: -sig * (512 * J - 128 * i),
                                       lambda i, h=h: VV[:, i, h * 128:(h + 1) * 128], R_VV, [(0, 1, 0), (2, 3, 1)], deferred=pend.pop() if pend else None)
                            recip_safe(ft[0], R_ft[0], ps[1], R_ps[1], 0, 128)
                            recip_safe(ft[1], R_ft[1], ps[3], R_ps[3], 0, 128)
                            P.op("dve", lambda e: e.tensor_tensor(out=ft[0][:], in0=ps[0][:, :], in1=ft[0][:], op=ALU.mult), R=[R_ps[0], R_ft[0]], W=[R_ft[0]])
                            P.op("dve", lambda e: e.tensor_tensor(out=ft[1][:], in0=ps[2][:, :], in1=ft[1][:], op=ALU.mult), R=[R_ps[2], R_ft[1]], W=[R_ft[1]])
                            P.op("dve", lambda e: e.scalar_tensor_tensor(out=ft[2][:], in0=ft[1][:], scalar=neglam, in1=ft[0][:], op0=ALU.mult, op1=ALU.add), R=[R_ft[0], R_ft[1], R_sm], W=[R_ft[2]])

                            def tail(h=h, J=J):
                                P.op("pool", lambda e: e.tensor_tensor(out=sqb[:], in0=ft[2][:], in1=ft[2][:], op=ALU.mult), R=[R_ft[2]], W=[R_sqb])
                                tb_ = 4 + nxt("scb", 3)
                                P.pe(lambda e: e.matmul(ps[tb_][:, :], lhsT=ones_b[:], rhs=sqb[:], start=True, stop=True), R=[R_sqb, R_const], W=[R_ps[tb_]])
                                P.op("dve", lambda e: e.tensor_scalar(out=ft[3][:], in0=ps[tb_][:, :], scalar1=1.0 / 128, scalar2=EPS, op0=ALU.mult, op1=ALU.add), R=[R_ps[tb_]], W=[R_ft[3]])
                                P.op("act", lambda e: e.activation(out=ft[3][:], in_=ft[3][:], func=AF.Ln), R=[R_ft[3]], W=[R_ft[3]])
                                P.op("act", lambda e: e.activation(out=ft[3][:], in_=ft[3][:], func=AF.Exp, scale=-0.5), R=[R_ft[3]], W=[R_ft[3]])
                                P.op("dve", lambda e: e.scalar_tensor_tensor(out=mixT[:, h, J * 512:(J + 1) * 512], in0=ft[2][:], scalar=gscale, in1=ft[3][:], op0=ALU.mult, op1=ALU.mult),
                                     R=[R_ft[2], R_ft[3], R_sm], W=[R_mix[h][J]])
                            pend.append(tail)
                    while pend:
                        pend.pop()()

                    for g in range(2):
                        bi = load_w(C_VB + g * 128, 128)
                        proj_tok(bi, 128, lambda st_, g=g: VV[:, st_, g * 128:(g + 1) * 128], R_VV)
                    for h in range(4):
                        z0 = 64 if h % 2 == 0 else 0
                        P.op("pool", lambda e: e.memset(QK[2 + h][z0:z0 + 64, :], 0.0), W=[R_QK[2 + h]])
                    for g in range(2):
                        bq = load_w(C_QB + g * 128, 128)
                        bk = load_w(C_KB + g * 128, 128)
                        proj_feat(bq, 0, 128, QK[g], R_QK[g], 0, 0.125)
                        proj_feat_split(bk, QK[2 + 2 * g], R_QK[2 + 2 * g], QK[3 + 2 * g], R_QK[3 + 2 * g], 1.0)
                    for h in range(4):
                        g = h // 2
                        r0 = (h % 2) * 64
                        for J in range(4):
                            i_list = list(range(max(0, 4 * J - 4), 4 * J + 4))

                            def colr(i, J=J):
                                a_lo = max(0, i - 4 * J); a_hi = min(3, i + 4 - 4 * J)
                                return (128 * a_lo, 128 * (a_hi + 1))

                            def smm(sci, i, m, c_lo, c_hi, J=J, h=h, g=g):
                                P.pe(lambda e: e.matmul(ps[sci][:, c_lo:c_hi], lhsT=QK[2 + h][:, i * 128:(i + 1) * 128], rhs=QK[g][:, J * 512 + c_lo:J * 512 + c_hi], start=True, stop=False),
                                     R=[R_QK[2 + h], R_QK[g]], W=[R_ps[sci]])
                                a_lo, a_hi = c_lo // 128, c_hi // 128 - 1
                                for a in range(a_lo, a_hi + 1):
                                    dl = 4 * J + a - i
                                    piece = {0: 0, 1: 1, 2: 2, 3: 2, 4: 3}[dl]
                                    P.pe(lambda e: e.matmul(ps[sci][:, a * 128:(a + 1) * 128], lhsT=ident_b[:], rhs=relb_b[:, h * 4 + piece, :], start=False, stop=(a == a_hi)),
                                         R=[R_const, R_relb], W=[R_ps[sci]])
                            attn_block(J, i_list, colr, smm, lambda i: 0.0, lambda i, g=g: VV[:, i, g * 128:(g + 1) * 128], R_VV, [(0, 1, 0)])
                            recip_safe(ft[0], R_ft[0], ps[1], R_ps[1], r0, r0 + 64)
                            P.op("dve", lambda e: e.tensor_tensor(out=mixT[r0:r0 + 64, 4 + g, J * 512:(J + 1) * 512], in0=ps[0][r0:r0 + 64, :], in1=ft[0][r0:r0 + 64, :], op=ALU.mult),
                                 R=[R_ps[0], R_ft[0]], W=[R_mix[4 + g][J]])

                    for g in range(2):
                        bi = load_w(C_VC + g * 128, 128)
                        proj_tok(bi, 128, lambda st_, g=g: VV[:, st_, g * 128:(g + 1) * 128], R_VV)
                    for h in range(4):
                        P.dma("sp", lambda e: e.dma_start(out=QK[h][64:67, :], in_=c_kaug[4 + h]), W=[R_QK[h]])
                        P.dma("sp", lambda e: e.dma_start(out=QK[4 + h][64:67, :], in_=c_qaug), W=[R_QK[4 + h]])
                    for g in range(2):
                        bq = load_w(C_QC + g * 128, 128)
                        bk = load_w(C_KC + g * 128, 128)
                        proj_feat(bq, 0, 64, QK[4 + 2 * g], R_QK[4 + 2 * g], 0, 0.125)
                        proj_feat(bq, 64, 64, QK[5 + 2 * g], R_QK[5 + 2 * g], 0, 0.125)
                        proj_feat(bk, 0, 64, QK[2 * g], R_QK[2 * g], 0, 1.0)
                        proj_feat(bk, 64, 64, QK[2 * g + 1], R_QK[2 * g + 1], 0, 1.0)
                    for g, nh in ((0, 3), (1, 3), (2, 2)):
                        bi = load_w(C_QI + g * 96, 32 * nh)
                        proj_feat(bi, 0, 32 * nh, QK[8 + g], R_QK[8 + g], 0, 1.0)
                    bi = load_w(C_KI, 32, rep=3)
                    proj_feat(bi, 0, 96, QK[11], R_QK[11], 0, 1.0)
                    bi = load_w(C_WI, 8)
                    proj_tok(bi, 8, lambda st_: wtok[:, st_, :], R_wtok)

                    def indexer(j):
                        L = 128 * (j + 1)
                        sb_ = scoreb[j % 2]; Rsb = R_scoreb[j % 2]; dg = diag[j % 2]; Rdg = R_diag[j % 2]
                        for h8 in range(8):
                            P.op("pool", lambda e: e.tensor_scalar(out=dg[:, h8, :], in0=ident_f[:], scalar1=wtok[:, j, h8:h8 + 1], scalar2=W_SCALE, op0=ALU.mult, op1=ALU.mult),
                                 R=[R_const, R_wtok], W=[Rdg])
                        nsc = (L + 511) // 512
                        units = [(sc_i, h8) for sc_i in range(nsc) for h8 in range(8)]
                        gbank = [4 + nxt("gb", 2) for _ in range(nsc)]
                        stt = {}

                        def R_(u):
                            sc_i, h8 = units[u]
                            w_ = min(512, L - 512 * sc_i)
                            gq, rr = h8 // 3, h8 % 3
                            rb = nxt("rb", 4)
                            P.pe(lambda e: e.matmul(ps[rb][:, 0:w_], lhsT=QK[8 + gq][32 * rr:32 * rr + 32, j * 128:(j + 1) * 128], rhs=QK[11][32 * rr:32 * rr + 32, sc_i * 512:sc_i * 512 + w_], start=True, stop=True),
                                 R=[R_QK[8 + gq], R_QK[11]], W=[R_ps[rb]])
                            stt[u] = rb

                        def L_(u):
                            sc_i, h8 = units[u]
                            w_ = min(512, L - 512 * sc_i)
                            rb = stt[u]
                            ri = nxt("relu", 3)
                            P.op("act", lambda e: e.activation(out=relu_sb[ri][:, 0:w_], in_=ps[rb][:, 0:w_], func=AF.Relu), R=[R_ps[rb]], W=[R_relu[ri]])
                            stt[u] = ri

                        def G_(u):
                            sc_i, h8 = units[u]
                            w_ = min(512, L - 512 * sc_i)
                            ri = stt[u]
                            gb = gbank[sc_i]
                            P.pe(lambda e: e.matmul(ps[gb][:, 0:w_], lhsT=dg[:, h8, :], rhs=relu_sb[ri][:, 0:w_], start=(h8 == 0), stop=(h8 == 7)), R=[Rdg, R_relu[ri]], W=[R_ps[gb]])
                            if h8 == 7:
                                P.op("act", lambda e: e.activation(out=sb_[:, sc_i * 512:sc_i * 512 + w_], in_=ps[gb][:, 0:w_], func=AF.Copy), R=[R_ps[gb]], W=[Rsb])
                        n_u = len(units)
                        R_(0); R_(1)
                        for u in range(n_u):
                            L_(u)
                            if u + 2 < n_u:
                                R_(u + 2)
                            G_(u)

                    def select(j):
                        L = 128 * (j + 1)
                        a = j % 4
                        sb_ = scoreb[j % 2]; Rsb = R_scoreb[j % 2]
                        if j < 2:
                            P.op("pool", lambda e: e.memset(negmask[:, 0:L], 0.0), W=[R_negm])
                        else:
                            P.op("dve", lambda e: e.tensor_reduce(out=sm[:, 8:9], in_=sb_[:, 0:L], axis=AX.X, op=ALU.min), R=[Rsb], W=[R_sm])
                            P.op("pool", lambda e: e.memset(sb_[0:64, L - 64:L], -1e30), R=[R_sm], W=[Rsb])
                            P.op("dve", lambda e: e.reduce_max(out=sm[:, 9:10], in_=sb_[:, 0:L], axis=AX.X), R=[Rsb], W=[R_sm])
                            P.op("dve", lambda e: e.scalar_tensor_tensor(out=sm[:, 10:11], in0=sm[:, 9:10], scalar=1e-20, in1=sm[:, 8:9], op0=ALU.add, op1=ALU.subtract), R=[R_sm], W=[R_sm])
                            P.op("dve", lambda e: e.reciprocal(out=sm[:, 11:12], in_=sm[:, 10:11]), R=[R_sm], W=[R_sm])
                            P.op("dve", lambda e: e.tensor_scalar(out=sb_[:, 0:L], in0=sb_[:, 0:L], scalar1=sm[:, 8:9], scalar2=sm[:, 11:12], op0=ALU.subtract, op1=ALU.mult), R=[Rsb, R_sm], W=[Rsb])
                            P.op("dve", lambda e: e.memset(sm[:, 12:13], 0.5), W=[R_sm])
                            for n in range(NIT):
                                dlt = 2.0 ** -(n + 2)
                                P.op("dve", lambda e: e.tensor_scalar(out=negmask[:, 0:L], in0=sb_[:, 0:L], scalar1=sm[:, 12:13], scalar2=None, op0=ALU.is_gt, op1=ALU.add, accum_out=sm[:, 13:14]),
                                     R=[Rsb, R_sm], W=[R_negm, R_sm])
                                P.op("dve", lambda e: e.tensor_scalar(out=sm[:, 14:15], in0=sm[:, 13:14], scalar1=255.5, scalar2=2.0 * dlt, op0=ALU.is_gt, op1=ALU.mult), R=[R_sm], W=[R_sm])
                                P.op("dve", lambda e: e.scalar_tensor_tensor(out=sm[:, 12:13], in0=sm[:, 12:13], scalar=-dlt, in1=sm[:, 14:15], op0=ALU.add, op1=ALU.add), R=[R_sm], W=[R_sm])
                            P.op("dve", lambda e: e.tensor_scalar(out=negmask[:, 0:L], in0=sb_[:, 0:L], scalar1=sm[:, 12:13], scalar2=NEG, op0=ALU.is_le, op1=ALU.mult), R=[Rsb, R_sm], W=[R_negm])
                        for i0 in range(0, j + 1, 8):
                            n_ = min(8, j + 1 - i0)
                            for ii in range(n_):
                                P.pe(lambda e: e.transpose(out=psT[:, ii, :], in_=negmask[:, (i0 + ii) * 128:(i0 + ii + 1) * 128], identity=ident_b[:]), R=[R_negm, R_const], W=[R_psT])
                            P.op("act", lambda e: e.activation(out=nmT[:, i0:i0 + n_, a * 128:(a + 1) * 128], in_=psT[:, 0:n_, :], func=AF.Copy), R=[R_psT], W=[R_nmT])

                    for J in range(4):
                        for a in range(4):
                            j = 4 * J + a
                            if 2 <= j + 1 < 16:
                                indexer(j + 1)
                            select(j)
                        for h in range(4):
                            g = h // 2
                            r0 = (h % 2) * 64
                            sig = SIG_C[h]

                            def colr(i, J=J):
                                return (128 * max(0, i - 4 * J), 512)

                            def smm(sci, i, m, c_lo, c_hi, J=J, h=h):
                                P.pe(lambda e: e.matmul(ps[sci][:, c_lo:c_hi], lhsT=QK[h][0:67, i * 128:(i + 1) * 128], rhs=QK[4 + h][0:67, J * 512 + c_lo:J * 512 + c_hi], start=True, stop=False),
                                     R=[R_QK[h], R_QK[4 + h]], W=[R_ps[sci]])
                                if i >= 4 * J:
                                    a = i - 4 * J
                                    P.pe(lambda e: e.matmul(ps[sci][:, a * 128:(a + 1) * 128], lhsT=ident_b[:], rhs=dmask_b[:, 4 + h, :], start=False, stop=False), R=[R_const], W=[R_ps[sci]])
                                P.pe(lambda e: e.matmul(ps[sci][:, c_lo:c_hi], lhsT=ident_b[:], rhs=nmT[:, i, c_lo:c_hi], start=False, stop=True), R=[R_const, R_nmT], W=[R_ps[sci]])
                            attn_block(J, list(range(4 * J + 4)), colr, smm, lambda i, J=J, sig=sig: -sig * (512 * J - 128 * i),
                                       lambda i, g=g: VV[:, i, g * 128:(g + 1) * 128], R_VV, [(0, 1, 0)])
                            recip_safe(ft[0], R_ft[0], ps[1], R_ps[1], r0, r0 + 64)
                            P.op("dve", lambda e: e.tensor_tensor(out=mixT[r0:r0 + 64, 6 + g, J * 512:(J + 1) * 512], in0=ps[0][r0:r0 + 64, :], in1=ft[0][r0:r0 + 64, :], op=ALU.mult),
                                 R=[R_ps[0], R_ft[0]], W=[R_mix[6 + g][J]])
                    P.barrier()

                if dbg == "mix":
                    allmix = [R_mix[c][j] for c in range(8) for j in range(4)]
                    P.dma("sp", lambda e: e.dma_start(out=dbg_out[b], in_=mixT[:]), R=allmix, W=[R_dbg])
                    P.barrier()
                    continue

                with ExitStack() as es:
                    wo_b = sbc(es, "wo_b", [128, 8, D], BF16); R_wo = Res("wo")
                    g1 = sbc(es, "g1", [128, D], F32); b1 = sbc(es, "b1", [128, D], F32); R_gb = Res("gb")
                    wr_f = sbc(es, "wr_f", [128, 8, NE], F32); br_t = sbc(es, "br_t", [128, NE], F32)
                    xt_ = [sbc(es, "xt%d" % i, [128, D], F32) for i in range(2)]; R_xt = [Res("xt") for _ in range(2)]
                    vt = [sbc(es, "vt%d" % i, [128, D], F32) for i in range(2)]; R_vt = [Res("vt") for _ in range(2)]
                    x1t = [sbc(es, "x1t%d" % i, [128, D], F32) for i in range(2)]; R_x1t = [Res("x1t") for _ in range(2)]
                    x1b = [sbc(es, "x1b%d" % i, [128, D], BF16) for i in range(2)]; R_x1b = [Res("x1b") for _ in range(2)]
                    x1T = sbc(es, "x1T", [128, 8, 128], F32); R_x1T = Res("x1T")
                    junk = sbc(es, "junk", [128, D], BF16); R_junk = Res("junk")
                    lst = sbc(es, "lst", [128, 8], F32); R_lst = Res("lst")
                    lg = sbc(es, "lg", [128, NE], F32); R_lg = Res("lg")
                    mx8 = sbc(es, "mx8", [128, 8], F32); R_mx = Res("mx")
                    rs = sbc(es, "rs", [128, 16], F32); R_rs = Res("rs")
                    maskb = sbc(es, "maskb", [128, NE], BF16); R_maskb = Res("maskb")
                    slotm = sbc(es, "slotm", [128, NE], F32); R_slotm = Res("slotm")
                    oh = sbc(es, "oh", [128, NE], F32); R_oh = Res("oh")
                    for c in range(8):
                        P.dma("pool", lambda e: e.dma_start(out=wo_b[:, c, :], in_=w_out[l, c * 128:(c + 1) * 128, :]), W=[R_wo])
                    P.dma("sp", lambda e: e.dma_start(out=g1[:], in_=ln1g[l:l + 1, :].to_broadcast([128, D])), W=[R_gb])
                    P.dma("sp", lambda e: e.dma_start(out=b1[:], in_=ln1b[l:l + 1, :].to_broadcast([128, D])), W=[R_gb])
                    P.dma("sp", lambda e: e.dma_start(out=wr_f[:], in_=w_rt[l].rearrange("(k p) c -> p k c", p=128)), W=[R_gb])
                    P.dma("sp", lambda e: e.dma_start(out=br_t[:], in_=b_rt[l:l + 1, :].to_broadcast([128, NE])), W=[R_gb])
                    for tt in range(16):
                        i2 = tt % 2
                        gt_ = b * 16 + tt
                        rows = slice(tok0 + tt * 128, tok0 + (tt + 1) * 128)
                        P.dma("sp", lambda e: e.dma_start(out=xt_[i2][:], in_=xsrc[rows, :]), R=[R_xsrc], W=[R_xt[i2]])
                        for half in range(2):
                            pi = nxt("wo", 2)
                            for c in range(8):
                                P.pe(lambda e: e.matmul(ps[pi][:, :], lhsT=mixT[:, c, tt * 128:(tt + 1) * 128], rhs=wo_b[:, c, half * 512:(half + 1) * 512], start=(c == 0), stop=(c == 7)),
                                     R=[R_mix[c][tt // 4], R_wo], W=[R_ps[pi]])
                            P.op("dve", lambda e: e.scalar_tensor_tensor(out=vt[i2][:, half * 512:(half + 1) * 512], in0=xt_[i2][:, half * 512:(half + 1) * 512], scalar=ALPHA, in1=ps[pi][:, :], op0=ALU.mult, op1=ALU.add),
                                 R=[R_xt[i2], R_ps[pi]], W=[R_vt[i2]])
                        layer_norm(vt[i2], R_vt[i2], g1, b1, R_gb, x1t[i2], R_x1t[i2], lst, R_lst, junk, R_junk)
                        if dbg == "x1":
                            P.dma("pool", lambda e: e.dma_start(out=dbg_out[rows, :], in_=x1t[i2][:]), R=[R_x1t[i2]], W=[R_dbg])
                        P.dma("pool", lambda e: e.dma_start(out=x1s[rows, :], in_=x1t[i2][:]), R=[R_x1t[i2]], W=[R_x1s])
                        P.op("act", lambda e: e.activation(out=x1b[i2][:], in_=x1t[i2][:], func=AF.Copy), R=[R_x1t[i2]], W=[R_x1b[i2]])
                        for hf in range(2):
                            for k4 in range(4):
                                k = hf * 4 + k4
                                P.pe(lambda e: e.transpose(out=ps[2 + hf][:, k4 * 128:(k4 + 1) * 128], in_=x1t[i2][:, k * 128:(k + 1) * 128], identity=ident_f[:]), R=[R_x1t[i2], R_const], W=[R_ps[2 + hf]])
                            P.op("act", lambda e: e.activation(out=x1T[:, hf * 4:(hf + 1) * 4, :], in_=ps[2 + hf][:, :].rearrange("p (a b) -> p a b", a=4), func=AF.Copy), R=[R_ps[2 + hf]], W=[R_x1T])
                        for k in range(8):
                            P.pe(lambda e: e.matmul(ps[4][:, 0:NE], lhsT=x1T[:, k, :], rhs=wr_f[:, k, :], start=(k == 0), stop=(k == 7)), R=[R_x1T, R_gb], W=[R_ps[4]])
                        P.op("dve", lambda e: e.tensor_tensor(out=lg[:], in0=ps[4][:, 0:NE], in1=br_t[:], op=ALU.add), R=[R_ps[4], R_gb], W=[R_lg])
                        P.op("dve", lambda e: e.max(out=mx8[:], in_=lg[:]), R=[R_lg], W=[R_mx])
                        P.op("dve", lambda e: e.tensor_scalar(out=rs[:, 0:1], in0=mx8[:, 0:1], scalar1=-1.0, scalar2=None, op0=ALU.mult), R=[R_mx], W=[R_rs])
                        P.op("act", lambda e: e.activation(out=rs[:, 4:8], in_=mx8[:, 0:4], func=AF.Exp, bias=rs[:, 0:1], scale=1.0, accum_out=rs[:, 1:2]), R=[R_mx, R_rs], W=[R_rs])
                        P.op("dve", lambda e: e.reciprocal(out=rs[:, 2:3], in_=rs[:, 1:2]), R=[R_rs], W=[R_rs])
                        P.op("dve", lambda e: e.tensor_scalar(out=gates[:, gt_ * 4:gt_ * 4 + 4], in0=rs[:, 4:8], scalar1=rs[:, 2:3], scalar2=None, op0=ALU.mult), R=[R_rs], W=[R_gates])
                        P.op("dve", lambda e: e.tensor_scalar(out=maskb[:], in0=lg[:], scalar1=mx8[:, 3:4], scalar2=None, op0=ALU.is_ge), R=[R_lg, R_mx], W=[R_maskb])
                        P.pe(lambda e: e.matmul(ps[5][:, 0:NE], lhsT=ltri_b[:], rhs=maskb[:], start=True, stop=True), R=[R_maskb, R_const], W=[R_ps[5]])
                        P.pe(lambda e: e.matmul(ps[6][:, 0:NE], lhsT=ones_b[:], rhs=maskb[:], start=True, stop=True), R=[R_maskb, R_const], W=[R_ps[6]])
                        P.op("dve", lambda e: e.tensor_tensor(out=slotm[:], in0=ps[5][:, 0:NE], in1=base_cnt[:], op=ALU.add), R=[R_ps[5], R_base], W=[R_slotm])
                        P.op("dve", lambda e: e.tensor_tensor(out=slotm[:], in0=slotm[:], in1=eoff[:], op=ALU.add), R=[R_slotm, R_const], W=[R_slotm])
                        P.op("dve", lambda e: e.tensor_tensor(out=base_cnt[:], in0=ps[6][:, 0:NE], in1=base_cnt[:], op=ALU.add), R=[R_ps[6], R_base], W=[R_base])
                        for k in range(4):
                            P.op("dve", lambda e: e.tensor_scalar(out=oh[:], in0=lg[:], scalar1=mx8[:, k:k + 1], scalar2=None, op0=ALU.is_equal), R=[R_lg, R_mx], W=[R_oh])
                            P.op("dve", lambda e: e.tensor_tensor(out=oh[:], in0=oh[:], in1=slotm[:], op=ALU.mult), R=[R_oh, R_slotm], W=[R_oh])
                            P.op("dve", lambda e: e.reduce_sum(out=slotf[:, gt_ * 4 + k:gt_ * 4 + k + 1], in_=oh[:], axis=AX.X), R=[R_oh], W=[R_slot])
                        P.op("dve", lambda e: e.tensor_copy(out=sloti[:, gt_ * 4:gt_ * 4 + 4], in_=slotf[:, gt_ * 4:gt_ * 4 + 4]), R=[R_slot], W=[R_slot])
                        for k in range(4):
                            P.dma("pool", lambda e: e.indirect_dma_start(out=xg[:, :], out_offset=bass.IndirectOffsetOnAxis(ap=sloti[:, gt_ * 4 + k:gt_ * 4 + k + 1], axis=0), in_=x1b[i2][:, :], in_offset=None),
                                  R=[R_x1b[i2], R_slot], W=[R_xg])
                    P.barrier()
        if dbg in ("mix", "x1"):
            break

        with ExitStack() as es:
            def sbm(name, shape, dt):
                return es.enter_context(nc.sbuf_tensor(name + "_m%d" % l, list(shape), dt))
            wgu_b = [sbm("wgu%d" % i, [128, 8, 2 * D], BF16) for i in range(2)]; R_wgu = [[Res("wgu") for _ in range(8)] for _ in range(2)]
            wdn_b = [sbm("wdn%d" % i, [128, 8, D], BF16) for i in range(2)]; R_wdn = [[Res("wdn") for _ in range(8)] for _ in range(2)]
            xr = [sbm("xr%d" % i, [128, D], BF16) for i in range(3)]; R_xr = [Res("xr") for _ in range(3)]
            xeT = [sbm("xeT%d" % i, [128, 8, CAP], BF16) for i in range(2)]; R_xeT = [Res("xeT") for _ in range(2)]
            GT = sbm("GT", [128, 8, CAP], BF16); R_GT = [Res("GT%d" % c) for c in range(8)]
            et = [sbm("et%d" % i, [128, 512], F32) for i in range(9)]; R_et = [Res("et") for _ in range(9)]
            yt = [sbm("yt%d" % i, [128, D], F32) for i in range(3)]; R_yt = [Res("yt") for _ in range(3)]
            bgT = sbm("bgT", [128, NE * 16], F32); R_bgT = Res("bgT")
            bgs = sbm("bgs", [128, 4, 128], F32); R_bgs = Res("bgs")
            bdb = [sbm("bdb%d" % i, [128, D], F32) for i in range(2)]; R_bdb = [Res("bdb") for _ in range(2)]
            P.dma("sp", lambda e: e.dma_start(out=bgs[:], in_=b_gu[l].rearrange("(a r) p -> r a p", r=128)), W=[R_bgs])
            for a in range(4):
                P.pe(lambda e: e.transpose(out=ps[6][:, a * 128:(a + 1) * 128], in_=bgs[:, a, :], identity=ident_f[:]), R=[R_bgs, R_const], W=[R_ps[6]])
            P.op("dve", lambda e: e.tensor_copy(out=bgT[:], in_=ps[6][:, :]), R=[R_ps[6]], W=[R_bgT])

            def load_expert(e_):
                s2 = e_ % 2
                for k in range(8):
                    P.dma("pool", lambda e: e.dma_start(out=wgu_b[s2][:, k, :], in_=w_gu[l, e_, k * 128:(k + 1) * 128, :]), W=[R_wgu[s2][k]])
                for k in range(8):
                    P.dma("pool", lambda e: e.dma_start(out=wdn_b[s2][:, k, :], in_=w_dn[l, e_, k * 128:(k + 1) * 128, :]), W=[R_wdn[s2][k]])
                P.dma("sp", lambda e: e.dma_start(out=bdb[s2][:], in_=b_dn[l, e_:e_ + 1, :].to_broadcast([128, D])), W=[R_bdb[s2]])

            def build_x(e_):
                s2 = e_ % 2
                for st_ in range(CAP // 128):
                    xi = nxt("xr", 3)
                    P.dma("sp", lambda e: e.dma_start(out=xr[xi][:], in_=xg[e_ * CAP + st_ * 128:e_ * CAP + (st_ + 1) * 128, :]), R=[R_xg], W=[R_xr[xi]])
                    for k in range(8):
                        P.pe(lambda e: e.transpose(out=psT[:, k, :], in_=xr[xi][:, k * 128:(k + 1) * 128], identity=ident_b[:]), R=[R_xr[xi], R_const], W=[R_psT])
                    P.op("act", lambda e: e.activation(out=xeT[s2][:, :, st_ * 128:(st_ + 1) * 128], in_=psT[:, :, :], func=AF.Copy), R=[R_psT], W=[R_xeT[s2]])

            load_expert(0)
            build_x(0)
            for e_ in range(NE):
                s2 = e_ % 2
                if e_ + 1 < NE:
                    load_expert(e_ + 1)
                for c in range(8):
                    for (s0, sw) in ((0, 512), (512, CAP - 512)):
                        pg = nxt("pg", 2) * 2
                        for k in range(8):
                            P.pe(lambda e: e.matmul(ps[pg][:, 0:sw], lhsT=wgu_b[s2][:, k, c * 128:(c + 1) * 128], rhs=xeT[s2][:, k, s0:s0 + sw], start=(k == 0), stop=(k == 7)),
                                 R=[R_wgu[s2][k], R_xeT[s2]], W=[R_ps[pg]])
                        for k in range(8):
                            P.pe(lambda e: e.matmul(ps[pg + 1][:, 0:sw], lhsT=wgu_b[s2][:, k, D + c * 128:D + (c + 1) * 128], rhs=xeT[s2][:, k, s0:s0 + sw], start=(k == 0), stop=(k == 7)),
                                 R=[R_wgu[s2][k], R_xeT[s2]], W=[R_ps[pg + 1]])
                        ei = nxt("et", 3) * 3
                        bgc = bgT[:, e_ * 16 + c:e_ * 16 + c + 1]
                        buc = bgT[:, e_ * 16 + 8 + c:e_ * 16 + 8 + c + 1]
                        P.op("dve", lambda e: e.tensor_scalar(out=et[ei][:, 0:sw], in0=ps[pg][:, 0:sw], scalar1=bgc, scalar2=7.0, op0=ALU.add, op1=ALU.min), R=[R_ps[pg], R_bgT], W=[R_et[ei]])
                        P.op("act", lambda e: e.activation(out=et[ei + 1][:, 0:sw], in_=et[ei][:, 0:sw], func=AF.Sigmoid, scale=1.702), R=[R_et[ei]], W=[R_et[ei + 1]])
                        P.op("act", lambda e: e.activation(out=et[ei + 2][:, 0:sw], in_=ps[pg + 1][:, 0:sw], func=AF.Identity, bias=buc, scale=1.0), R=[R_ps[pg + 1], R_bgT], W=[R_et[ei + 2]])
                        P.op("dve", lambda e: e.tensor_scalar(out=et[ei + 2][:, 0:sw], in0=et[ei + 2][:, 0:sw], scalar1=7.0, scalar2=-7.0, op0=ALU.min, op1=ALU.max), R=[R_et[ei + 2]], W=[R_et[ei + 2]])
                        P.op("dve", lambda e: e.tensor_tensor(out=et[ei][:, 0:sw], in0=et[ei][:, 0:sw], in1=et[ei + 1][:, 0:sw], op=ALU.mult), R=[R_et[ei], R_et[ei + 1]], W=[R_et[ei]])
                        P.op("dve", lambda e: e.scalar_tensor_tensor(out=GT[:, c, s0:s0 + sw], in0=et[ei + 2][:, 0:sw], scalar=1.0, in1=et[ei][:, 0:sw], op0=ALU.add, op1=ALU.mult), R=[R_et[ei], R_et[ei + 2]], W=[R_GT[c]])
                if e_ + 1 < NE:
                    build_x(e_ + 1)
                for st_ in range(CAP // 128):
                    yi = nxt("yt", 3)
                    for half in range(2):
                        pi = 4 + nxt("pd", 2)
                        for c in range(8):
                            P.pe(lambda e: e.matmul(ps[pi][:, :], lhsT=GT[:, c, st_ * 128:(st_ + 1) * 128], rhs=wdn_b[s2][:, c, half * 512:(half + 1) * 512], start=(c == 0), stop=(c == 7)),
                                 R=[R_GT[c], R_wdn[s2][c]], W=[R_ps[pi]])
                        P.op("dve", lambda e: e.tensor_tensor(out=yt[yi][:, half * 512:(half + 1) * 512], in0=ps[pi][:, :], in1=bdb[s2][:, half * 512:(half + 1) * 512], op=ALU.add),
                             R=[R_ps[pi], R_bdb[s2]], W=[R_yt[yi]])
                    P.dma("sp", lambda e: e.dma_start(out=yg[e_ * CAP + st_ * 128:e_ * CAP + (st_ + 1) * 128, :], in_=yt[yi][:]), R=[R_yt[yi]], W=[R_yg])
            P.barrier()

        with ExitStack() as es:
            def sbm(name, shape, dt):
                return es.enter_context(nc.sbuf_tensor(name + "_c%d" % l, list(shape), dt))
            yk = [sbm("yk%d" % i, [128, D], F32) for i in range(8)]; R_yk = [Res("yk") for _ in range(8)]
            xa = [sbm("xa%d" % i, [128, D], F32) for i in range(2)]; R_xa = [Res("xa") for _ in range(2)]
            xo = [sbm("xo%d" % i, [128, D], F32) for i in range(2)]; R_xo = [Res("xo") for _ in range(2)]
            g2 = sbm("g2", [128, D], F32); b2 = sbm("b2", [128, D], F32); R_gb2 = Res("gb2")
            junk2 = sbm("junk2", [128, D], BF16); R_junk2 = Res("junk2")
            lst2 = sbm("lst2", [128, 8], F32); R_lst2 = Res("lst2")
            P.dma("sp", lambda e: e.dma_start(out=g2[:], in_=ln2g[l:l + 1, :].to_broadcast([128, D])), W=[R_gb2])
            P.dma("sp", lambda e: e.dma_start(out=b2[:], in_=ln2b[l:l + 1, :].to_broadcast([128, D])), W=[R_gb2])
            for tt in range(32):
                i2 = tt % 2
                rows = slice(tt * 128, (tt + 1) * 128)
                P.dma("sp", lambda e: e.dma_start(out=xa[i2][:], in_=x1s[rows, :]), R=[R_x1s], W=[R_xa[i2]])
                P.op("act", lambda e: e.activation(out=xa[i2][:], in_=xa[i2][:], func=AF.Copy, scale=ALPHA), R=[R_xa[i2]], W=[R_xa[i2]])
                for k in range(4):
                    yi = i2 * 4 + k
                    P.dma("pool", lambda e: e.indirect_dma_start(out=yk[yi][:, :], out_offset=None, in_=yg[:, :], in_offset=bass.IndirectOffsetOnAxis(ap=sloti[:, tt * 4 + k:tt * 4 + k + 1], axis=0)),
                          R=[R_yg, R_slot], W=[R_yk[yi]])
                    P.op("dve", lambda e: e.scalar_tensor_tensor(out=xa[i2][:], in0=yk[yi][:], scalar=gates[:, tt * 4 + k:tt * 4 + k + 1], in1=xa[i2][:], op0=ALU.mult, op1=ALU.add),
                         R=[R_yk[yi], R_gates, R_xa[i2]], W=[R_xa[i2]])
                layer_norm(xa[i2], R_xa[i2], g2, b2, R_gb2, xo[i2], R_xo[i2], lst2, R_lst2, junk2, R_junk2)
                P.dma("sp", lambda e: e.dma_start(out=xdst[rows, :], in_=xo[i2][:]), R=[R_xo[i2]], W=[R_xdst])
            P.barrier()

    P.finish()
    return nc, P


def _consts():
    bf = ml_dtypes.bfloat16
    ident = np.eye(128, dtype=np.float32)
    ltri = np.triu(np.ones((128, 128), np.float32), 1)
    si = np.arange(128)[:, None]; qi = np.arange(128)[None, :]
    dmask = np.zeros((128, 8, 128), np.float32)
    for h in range(8):
        sg = SIG8[h]
        d = np.where(si <= qi, 0.0, np.where((si // 64) == (qi // 64), -2.0 * sg * (si - qi), NEG))
        dmask[:, h, :] = d
    q = np.arange(S)
    qaug = np.stack([np.ones(S), -(q % 128).astype(np.float64), -(128.0 * ((q // 128) % 4))]).astype(np.float32)
    kaug = np.zeros((8, 3, S), np.float32)
    for h in range(8):
        kaug[h, 0] = SIG8[h] * (q % 128)
        kaug[h, 1] = SIG8[h]
        kaug[h, 2] = SIG8[h]
    eoff = np.tile((np.arange(NE, dtype=np.float32) * CAP)[None, :], (128, 1))
    return {"c_ident": ident.astype(bf), "c_identf": ident, "c_ltri": ltri.astype(bf), "c_dmask": dmask.astype(bf),
            "c_qaug": qaug.astype(bf), "c_kaug": kaug.astype(bf), "c_eoff": eoff}


def _relb_pieces(rel_bias):
    NLn = rel_bias.shape[0]
    si = np.arange(128)[:, None]; qi = np.arange(128)[None, :]
    out = np.empty((NLn, 128, 16, 128), np.float32)
    for h in range(4):
        rel0 = qi - si
        idx0 = np.clip(rel0, -128, 128) + 128
        m0 = ((si // 64) == 1) & ((qi // 64) == 0)
        idx1 = np.clip(128 + qi - si, -128, 128) + 128
        m4 = ((si // 64) == 0) & ((qi // 64) == 1)
        for ln in range(NLn):
            rb = rel_bias[ln, h]
            p0 = rb[idx0].copy(); p0[m0] = NEG
            p1 = rb[idx1]
            p2 = np.broadcast_to(rb[256], (128, 128))
            p4 = np.array(p2); p4[m4] = NEG
            out[ln, :, h * 4 + 0, :] = p0
            out[ln, :, h * 4 + 1, :] = p1
            out[ln, :, h * 4 + 2, :] = p2
            out[ln, :, h * 4 + 3, :] = p4
    return out


_CACHE = {}


def _get_prog(NL, lam_inits, dbg=None):
    key = (NL, tuple(lam_inits), dbg)
    if key not in _CACHE:
        _CACHE[key] = build(NL, lam_inits, dbg)[0]
    return _CACHE[key]


def _layer_inputs(inp, ls):
    f = lambda a: np.ascontiguousarray(a, dtype=np.float32)
    d = {
        "w_in": f(inp["w_in"][ls]),
        "lamv": f(np.stack([inp["lam_q1"][ls], inp["lam_k1"][ls], inp["lam_q2"][ls], inp["lam_k2"][ls]], axis=1)),
        "subln_g": f(inp["subln_g"][ls]),
        "relb": _relb_pieces(np.asarray(inp["rel_bias"][ls], np.float32)),
        "w_out": f(inp["w_out"][ls]),
        "ln1_g": f(inp["ln1_g"][ls]), "ln1_b": f(inp["ln1_b"][ls]),
        "w_router": f(inp["w_router"][ls]), "b_router": f(inp["b_router"][ls]),
        "w_gu": f(inp["w_gu"][ls]), "b_gu": f(inp["b_gu"][ls]).reshape(len(range(*ls.indices(DEPTH))), NE * 16, 128),
        "w_down": f(inp["w_down"][ls]), "b_down": f(inp["b_down"][ls]),
        "ln2_g": f(inp["ln2_g"][ls]), "ln2_b": f(inp["ln2_b"][ls]),
    }
    return d


FUSED = True


def kernel(**inp):
    x = np.ascontiguousarray(inp["x"], dtype=np.float32)
    consts = _consts()
    lam_inits_all = [0.8 - 0.6 * math.exp(-0.3 * l) for l in range(DEPTH)]
    xs = [x[c * NBL:(c + 1) * NBL].reshape(T, D) for c in range(NCORES)]
    if FUSED:
        nc = _get_prog(DEPTH, lam_inits_all)
        li = _layer_inputs(inp, slice(0, DEPTH))
        in_maps = [dict(li, x=xs[c], **consts) for c in range(NCORES)]
        res = run_bass_kernel_spmd(nc, in_maps, core_ids=list(range(NCORES)))
        xs = [res.results[c]["y"] for c in range(NCORES)]
    else:
        for l in range(DEPTH):
            nc = _get_prog(1, [lam_inits_all[l]])
            li = _layer_inputs(inp, slice(l, l + 1))
            in_maps = [dict(li, x=xs[c], **consts) for c in range(NCORES)]
            res = run_bass_kernel_spmd(nc, in_maps, core_ids=list(range(NCORES)))
            xs = [np.asarray(res.results[c]["y"]) for c in range(NCORES)]
    out = np.stack([xs[c].reshape(NBL, S, D) for c in range(NCORES)], axis=0).reshape(NCORES * NBL, S, D)
    return out.astype(np.float32)
```

```python
import math
from contextlib import ExitStack
import numpy as np
import ml_dtypes
import concourse.bass as bass
import concourse.mybir as mybir
from concourse.bass_utils import run_bass_kernel_spmd

F32 = mybir.dt.float32
BF16 = mybir.dt.bfloat16
I32 = mybir.dt.int32
AF = mybir.ActivationFunctionType
ALU = mybir.AluOpType
AX = mybir.AxisListType

NCORES = 8
DEPTH = 4
S = 2048
D = 1024
NBL = 2
T = NBL * S
DIN = 3368
NE = 32
CAP = 768
NSLOT = NE * CAP
ALPHA = (2 * DEPTH) ** 0.25
EPS = 1e-5
NEG = -30000.0
SIG_A = [2.0 ** -1, 2.0 ** -3, 2.0 ** -5, 2.0 ** -7]
SIG_C = [2.0 ** -2, 2.0 ** -4, 2.0 ** -6, 2.0 ** -8]
SIG8 = SIG_A + SIG_C
W_SCALE = (8 ** -0.5) * (32 ** -0.5)
NIT = 12
C_QA, C_KA, C_VA, C_QB, C_KB, C_VB, C_QC, C_KC, C_VC, C_QI, C_KI, C_WI = (
    0, 512, 1024, 1536, 1792, 2048, 2304, 2560, 2816, 3072, 3328, 3360)


class Res:
    __slots__ = ("name", "w", "r", "multi")

    def __init__(self, name, multi=False):
        self.name = name
        self.w = {}
        self.r = {}
        self.multi = multi


class Prog:
    def __init__(self, nc, ndma=(("sp", 40), ("pool", 40), ("act", 8))):
        self.nc = nc
        self.E = {"pe": nc.tensor, "act": nc.scalar, "dve": nc.vector, "pool": nc.gpsimd, "sp": nc.sync}
        self.esem = {k: nc.alloc_semaphore("e_" + k) for k in self.E}
        self.ecnt = {k: 0 for k in self.E}
        self.waited = {k: {} for k in self.E}
        self.dsem = {}
        for k, n in ndma:
            self.dsem[k] = [[nc.alloc_semaphore("d_%s%d" % (k, i)), 0, "d_%s%d" % (k, i)] for i in range(n)]
        self.dnext = {k: 0 for k in self.dsem}
        self.nins = 0

    def _wait(self, eng, ev):
        key, sem, val = ev
        if self.waited[eng].get(key, 0) >= val:
            return
        self.E[eng].wait_ge(sem, val)
        self.waited[eng][key] = val
        self.nins += 1

    def _deps(self, eng, R, W, skip_self):
        for r in R:
            for ev in r.w.values():
                if not (skip_self and ev[0] == eng):
                    self._wait(eng, ev)
        for w in W:
            if not w.multi:
                for ev in w.w.values():
                    if not (skip_self and ev[0] == eng):
                        self._wait(eng, ev)
            for ev in w.r.values():
                if not (skip_self and ev[0] == eng):
                    self._wait(eng, ev)

    def _record(self, ev, R, W):
        for r in R:
            r.r[ev[0]] = ev
        for w in W:
            if w.multi:
                w.w[ev[0]] = ev
            else:
                w.w = {ev[0]: ev}
            w.r = {}

    def op(self, eng, fn, R=(), W=(), skip_self=False):
        self._deps(eng, R, W, skip_self)
        ins = fn(self.E[eng])
        self.ecnt[eng] += 1
        ins.then_inc(self.esem[eng], 1)
        ev = (eng, self.esem[eng], self.ecnt[eng])
        self._record(ev, R, W)
        self.nins += 1
        return ev

    def pe(self, fn, R=(), W=()):
        return self.op("pe", fn, R, W, skip_self=True)

    def dma(self, eng, fn, R=(), W=()):
        self._deps(eng, R, W, False)
        pool = self.dsem[eng]
        slot = pool[self.dnext[eng] % len(pool)]
        self.dnext[eng] += 1
        if slot[1] > 0:
            self._wait(eng, (slot[2], slot[0], slot[1]))
        ins = fn(self.E[eng])
        slot[1] += 16
        ins.then_inc(slot[0], 16)
        ev = (slot[2], slot[0], slot[1])
        self._record(ev, R, W)
        self.nins += 1
        return ev

    def barrier(self):
        evs = [(k, self.esem[k], self.ecnt[k]) for k in self.E if self.ecnt[k] > 0]
        for k in self.dsem:
            for s in self.dsem[k]:
                if s[1] > 0:
                    evs.append((s[2], s[0], s[1]))
        for e in self.E:
            for ev in evs:
                self._wait(e, ev)

    def finish(self):
        self.barrier()


def build(NL, lam_inits, dbg=None):
    nc = bass.Bass("TRN2", target_bir_lowering=False)
    P = Prog(nc)

    def din(name, shape, dt=F32):
        return nc.dram_tensor(name, list(shape), dt, kind="ExternalInput").ap()

    x_in = din("x", [T, D])
    w_in = din("w_in", [NL, D, DIN])
    lamv = din("lamv", [NL, 4, 64])
    subg = din("subln_g", [NL, 128])
    relb = din("relb", [NL, 128, 16, 128])
    w_out = din("w_out", [NL, D, D])
    ln1g = din("ln1_g", [NL, D]); ln1b = din("ln1_b", [NL, D])
    w_rt = din("w_router", [NL, D, NE]); b_rt = din("b_router", [NL, NE])
    if dbg is None or dbg == "moe":
        w_gu = din("w_gu", [NL, NE, D, 2 * D]); w_dn = din("w_down", [NL, NE, D, D])
    b_gu = din("b_gu", [NL, NE * 16, 128]); b_dn = din("b_down", [NL, NE, D])
    ln2g = din("ln2_g", [NL, D]); ln2b = din("ln2_b", [NL, D])
    c_ident = din("c_ident", [128, 128], BF16)
    c_identf = din("c_identf", [128, 128], F32)
    c_ltri = din("c_ltri", [128, 128], BF16)
    c_dmask = din("c_dmask", [128, 8, 128], BF16)
    c_qaug = din("c_qaug", [3, S], BF16)
    c_kaug = din("c_kaug", [8, 3, S], BF16)
    c_eoff = din("c_eoff", [128, NE], F32)
    y_out = nc.dram_tensor("y", [T, D], F32, kind="ExternalOutput").ap()
    dbg_out = None
    if dbg == "mix":
        dbg_out = nc.dram_tensor("dbg", [NBL, 128, 8, S], BF16, kind="ExternalOutput").ap()
    if dbg == "x1":
        dbg_out = nc.dram_tensor("dbg", [T, D], F32, kind="ExternalOutput").ap()
    xcur = nc.dram_tensor("xcur", [T, D], F32).ap()
    x1s = nc.dram_tensor("x1s", [T, D], F32).ap()
    xg = nc.dram_tensor("xg", [NSLOT, D], BF16).ap()
    yg = nc.dram_tensor("yg", [NSLOT, D], F32).ap()
    R_xin = Res("xin", True); R_xcur = Res("xcur", True); R_x1s = Res("x1s", True)
    R_xg = Res("xg", True); R_yg = Res("yg", True); R_y = Res("y", True); R_dbg = Res("dbg", True)
    R_const = Res("const", True)

    def sb(name, shape, dt):
        return nc.alloc_sbuf_tensor(name, list(shape), dt)

    ident_b = sb("ident_b", [128, 128], BF16)
    ident_f = sb("ident_f", [128, 128], F32)
    ltri_b = sb("ltri_b", [128, 128], BF16)
    ones_b = sb("ones_b", [128, 128], BF16)
    zeros_b = sb("zeros_b", [128, 512], BF16)
    dmask_b = sb("dmask_b", [128, 8, 128], BF16)
    eoff = sb("eoff", [128, NE], F32)
    base_cnt = sb("base_cnt", [128, NE], F32)
    slotf = sb("slotf", [128, 32 * 4], F32)
    sloti = sb("sloti", [128, 32 * 4], I32)
    gates = sb("gates", [128, 32 * 4], F32)
    R_slot = Res("slot"); R_gates = Res("gates"); R_base = Res("base")
    for t_, src in ((ident_b, c_ident), (ident_f, c_identf), (ltri_b, c_ltri), (dmask_b, c_dmask), (eoff, c_eoff)):
        P.dma("sp", lambda e, t_=t_, src=src: e.dma_start(out=t_[:], in_=src), W=[R_const])
    P.op("pool", lambda e: e.memset(ones_b[:], 1.0), W=[R_const])
    P.op("pool", lambda e: e.memset(zeros_b[:], 0.0), W=[R_const])

    ps = [nc.alloc_psum_tensor("ps%d" % i, [128, 512], F32) for i in range(7)]
    R_ps = [Res("ps%d" % i) for i in range(7)]
    psT = nc.alloc_psum_tensor("psT", [128, 8, 128], BF16)
    R_psT = Res("psT")
    psT2 = ps[6][:, :].bitcast(BF16).rearrange("p (a b) -> p a b", a=8)

    rot = {}

    def nxt(key, n):
        rot[key] = (rot.get(key, -1) + 1) % n
        return rot[key]

    def run_window(gen_list, width):
        active = []
        it = iter(gen_list)
        while True:
            while len(active) < width:
                g_ = next(it, None)
                if g_ is None:
                    break
                active.append(g_)
            if not active:
                break
            for g_ in list(active):
                try:
                    next(g_)
                except StopIteration:
                    active.remove(g_)

    def ln_gen(v, R_v, g_t, b_t, R_gb, out_t, R_out, st, R_st, junk, R_junk):
        P.op("dve", lambda e: e.tensor_scalar(out=st[:, 1:2], in0=st[:, 0:1], scalar1=-1.0 / D, scalar2=None, op0=ALU.mult), R=[R_st], W=[R_st])
        yield
        P.op("act", lambda e: e.activation(out=junk[:], in_=v[:], func=AF.Square, bias=st[:, 1:2], scale=1.0, accum_out=st[:, 2:3]), R=[R_v, R_st], W=[R_junk, R_st])
        yield
        P.op("dve", lambda e: e.tensor_scalar(out=st[:, 3:4], in0=st[:, 2:3], scalar1=1.0 / D, scalar2=EPS, op0=ALU.mult, op1=ALU.add), R=[R_st], W=[R_st])
        yield
        P.op("act", lambda e: e.activation(out=st[:, 4:5], in_=st[:, 3:4], func=AF.Ln), R=[R_st], W=[R_st])
        P.op("act", lambda e: e.activation(out=st[:, 5:6], in_=st[:, 4:5], func=AF.Exp, scale=-0.5), R=[R_st], W=[R_st])
        yield
        P.op("dve", lambda e: e.tensor_tensor(out=st[:, 6:7], in0=st[:, 1:2], in1=st[:, 5:6], op=ALU.mult), R=[R_st], W=[R_st])
        yield
        P.op("act", lambda e: e.activation(out=v[:], in_=v[:], func=AF.Identity, bias=st[:, 6:7], scale=st[:, 5:6]), R=[R_v, R_st], W=[R_v])
        yield
        P.op("dve", lambda e: e.tensor_tensor(out=v[:], in0=v[:], in1=g_t[:], op=ALU.mult), R=[R_v, R_gb], W=[R_v])
        yield
        P.op("dve", lambda e: e.tensor_tensor(out=out_t[:], in0=v[:], in1=b_t[:], op=ALU.add), R=[R_v, R_gb], W=[R_out])
        yield

    for l in range(NL):
        lam_init = lam_inits[l]
        xsrc, R_xsrc = (x_in, R_xin) if l == 0 else (xcur, R_xcur)
        last = (l == NL - 1)
        xdst, R_xdst = (y_out, R_y) if last else (xcur, R_xcur)
        P.op("pool", lambda e: e.memset(base_cnt[:], 0.0), W=[R_base])

        for b in range(NBL):
            tok0 = b * S
            with ExitStack() as es_b:
                def sbc(es, name, shape, dt):
                    return es.enter_context(nc.sbuf_tensor(name + "_%d_%d" % (l, b), list(shape), dt))
                xT = sbc(es_b, "xT", [128, 8, S], BF16); R_xT = [Res("xT%d" % i) for i in range(4)]
                mixT = sbc(es_b, "mixT", [128, 8, S], BF16)
                R_mix = [[Res("mix%d_%d" % (c, j)) for j in range(4)] for c in range(8)]

                with ExitStack() as es:
                    xbs = [sbc(es, "xbs%d" % i, [128, D], BF16) for i in range(3)]; R_xbs = [Res("xbs") for _ in range(3)]
                    for tt in range(16):
                        i2 = tt % 3
                        P.dma("pool", lambda e: e.dma_start(out=xbs[i2][:], in_=xsrc[tok0 + tt * 128: tok0 + (tt + 1) * 128, :]), R=[R_xsrc], W=[R_xbs[i2]])
                        for k in range(8):
                            P.pe(lambda e: e.transpose(out=psT[:, k, :], in_=xbs[i2][:, k * 128:(k + 1) * 128], identity=ident_b[:]), R=[R_xbs[i2], R_const], W=[R_psT])
                        P.op("dve", lambda e: e.tensor_copy(out=xT[:, :, tt * 128:(tt + 1) * 128], in_=psT[:, :, :]), R=[R_psT], W=[R_xT[tt // 4]])
                    P.barrier()

                with ExitStack() as es:
                    QK = [sbc(es, "qk%d" % i, [128, S], BF16) for i in range(12)]
                    R_QK = [Res("qk%d" % i) for i in range(12)]
                    VV = sbc(es, "vv", [128, 16, 512], BF16); R_VV = Res("vv")
                    wbf = [sbc(es, "wbf%d" % i, [128, 8, 128], BF16) for i in range(4)]; R_wbf = [Res("wbf") for _ in range(4)]
                    PT = [sbc(es, "pt%d" % i, [128, 512], BF16) for i in range(4)]; R_PT = [Res("pt") for _ in range(4)]
                    relb_b = sbc(es, "relb_b", [128, 16, 128], BF16); R_relb = Res("relb")
                    scoreb = [sbc(es, "score%d" % i, [128, S], F32) for i in range(2)]; R_scoreb = [Res("score%d" % i) for i in range(2)]
                    score = scoreb[0]; R_score = R_scoreb[0]
                    relu_sb = [sbc(es, "relu%d" % i, [128, 512], BF16) for i in range(3)]; R_relu = [Res("relu") for _ in range(3)]
                    negmask = sbc(es, "negmask", [128, S], BF16); R_negm = Res("negm")
                    nmT = sbc(es, "nmT", [128, 16, 512], BF16); R_nmT = Res("nmT")
                    diag = [sbc(es, "diag%d" % i, [128, 8, 128], BF16) for i in range(2)]; R_diag = [Res("diag%d" % i) for i in range(2)]
                    wtok = sbc(es, "wtok", [128, 16, 8], F32); R_wtok = Res("wtok")
                    ft = [sbc(es, "ft%d" % i, [128, 512], F32) for i in range(4)]; R_ft = [Res("ft%d" % i) for i in range(4)]
                    sqb = sbc(es, "sqb", [128, 512], BF16); R_sqb = Res("sqb")
                    sm = sbc(es, "sm", [128, 16], F32); R_sm = Res("sm")
                    lamt = sbc(es, "lamt", [128, 4, 64], F32); R_lamt = Res("lamt")
                    gsc = sbc(es, "gsc", [128, 2], F32)

                    P.dma("sp", lambda e: e.dma_start(out=lamt[:], in_=lamv[l:l + 1, :, :].to_broadcast([128, 4, 64])), W=[R_lamt])
                    P.dma("sp", lambda e: e.dma_start(out=gsc[:, 0:1], in_=subg[l, :].rearrange("(p o) -> p o", o=1)), W=[R_sm])
                    P.op("dve", lambda e: e.tensor_tensor(out=lamt[:, 0, :], in0=lamt[:, 0, :], in1=lamt[:, 1, :], op=ALU.mult), R=[R_lamt], W=[R_lamt])
                    P.op("dve", lambda e: e.tensor_tensor(out=lamt[:, 2, :], in0=lamt[:, 2, :], in1=lamt[:, 3, :], op=ALU.mult), R=[R_lamt], W=[R_lamt])
                    P.op("dve", lambda e: e.reduce_sum(out=sm[:, 0:1], in_=lamt[:, 0, :], axis=AX.X), R=[R_lamt], W=[R_sm])
                    P.op("dve", lambda e: e.reduce_sum(out=sm[:, 1:2], in_=lamt[:, 2, :], axis=AX.X), R=[R_lamt], W=[R_sm])
                    P.op("act", lambda e: e.activation(out=sm[:, 2:4], in_=sm[:, 0:2], func=AF.Exp), R=[R_sm], W=[R_sm])
                    P.op("dve", lambda e: e.tensor_tensor(out=sm[:, 4:5], in0=sm[:, 3:4], in1=sm[:, 2:3], op=ALU.subtract), R=[R_sm], W=[R_sm])
                    P.op("dve", lambda e: e.tensor_scalar(out=sm[:, 5:6], in0=sm[:, 4:5], scalar1=-lam_init, scalar2=None, op0=ALU.add), R=[R_sm], W=[R_sm])
                    P.op("dve", lambda e: e.tensor_scalar(out=gsc[:, 1:2], in0=gsc[:, 0:1], scalar1=1.0 - lam_init, scalar2=None, op0=ALU.mult), R=[R_sm], W=[R_sm])
                    neglam = sm[:, 5:6]
                    gscale = gsc[:, 1:2]

                    P.dma("sp", lambda e: e.dma_start(out=score[:, :].rearrange("p (a b) -> p a b", a=16), in_=relb[l]), W=[R_score])
                    P.op("pool", lambda e: e.tensor_copy(out=relb_b[:], in_=score[:, :].rearrange("p (a b) -> p a b", a=16)), R=[R_score], W=[R_relb])

                    def load_w(c0, ncol, rep=1):
                        bi = nxt("wbf", 4)
                        for r in range(rep):
                            P.dma("pool", lambda e: e.dma_start(out=wbf[bi][:, :, r * ncol:(r + 1) * ncol], in_=w_in[l, :, c0:c0 + ncol].rearrange("(k p) c -> p k c", p=128)), W=[R_wbf[bi]])
                        return bi

                    def proj_feat(bi, c_lo, m, dst, R_dst, p_lo, scale):
                        for tb in range(4):
                            pi = 4 + nxt("pj", 2)
                            for k in range(8):
                                P.pe(lambda e: e.matmul(ps[pi][0:m, :], lhsT=wbf[bi][:, k, c_lo:c_lo + m], rhs=xT[:, k, tb * 512:(tb + 1) * 512], start=(k == 0), stop=(k == 7)),
                                     R=[R_wbf[bi], R_xT[tb]], W=[R_ps[pi]])
                            P.op("act", lambda e: e.activation(out=dst[p_lo:p_lo + m, tb * 512:(tb + 1) * 512], in_=ps[pi][p_lo:p_lo + m, :], func=AF.Copy, scale=scale),
                                 R=[R_ps[pi]], W=[R_dst])

                    def proj_feat_split(bi, dst0, R0, dst1, R1, scale):
                        for tb in range(4):
                            pi = 4 + nxt("pj", 2)
                            for k in range(8):
                                P.pe(lambda e: e.matmul(ps[pi][:, :], lhsT=wbf[bi][:, k, :], rhs=xT[:, k, tb * 512:(tb + 1) * 512], start=(k == 0), stop=(k == 7)),
                                     R=[R_wbf[bi], R_xT[tb]], W=[R_ps[pi]])
                            P.op("act", lambda e: e.activation(out=dst0[0:64, tb * 512:(tb + 1) * 512], in_=ps[pi][0:64, :], func=AF.Copy, scale=scale), R=[R_ps[pi]], W=[R0])
                            P.op("act", lambda e: e.activation(out=dst1[64:128, tb * 512:(tb + 1) * 512], in_=ps[pi][64:128, :], func=AF.Copy, scale=scale), R=[R_ps[pi]], W=[R1])

                    def proj_tok(bi, ncol, dst_fn, R_dst, act_eng="act"):
                        for st_ in range(16):
                            pi = 4 + nxt("pj", 2)
                            for k in range(8):
                                P.pe(lambda e: e.matmul(ps[pi][:, 0:ncol], lhsT=xT[:, k, st_ * 128:(st_ + 1) * 128], rhs=wbf[bi][:, k, 0:ncol], start=(k == 0), stop=(k == 7)),
                                     R=[R_wbf[bi], R_xT[st_ // 4]], W=[R_ps[pi]])
                            P.op("act", lambda e: e.activation(out=dst_fn(st_), in_=ps[pi][:, 0:ncol], func=AF.Copy), R=[R_ps[pi]], W=[R_dst])

                    def recip_safe(dst, R_dst, src_ps, R_src, p0, p1):
                        P.op("dve", lambda e: e.tensor_scalar(out=dst[p0:p1, :], in0=src_ps[p0:p1, :], scalar1=1e-30, scalar2=None, op0=ALU.max), R=[R_src], W=[R_dst])
                        P.op("dve", lambda e: e.reciprocal(out=dst[p0:p1, :], in_=dst[p0:p1, :]), R=[R_dst], W=[R_dst])

                    def attn_block(J, i_list, colrange_fn, score_mms, cfn, Vl_fn, R_V, accs, deferred=None):
                        for (oi, si_, m) in accs:
                            for bi_ in (oi, si_):
                                P.pe(lambda e: e.matmul(ps[bi_][:, :], lhsT=zeros_b[:, 0:128], rhs=zeros_b[:, :], start=True, stop=False), R=[R_const], W=[R_ps[bi_]])
                        n_i = len(i_list)
                        units = [(ii, i, acc) for ii, i in enumerate(i_list) for acc in accs]
                        n_u = len(units)
                        stt = {}

                        def S_(u):
                            ii, i, (oi, si_, m) = units[u]
                            c_lo, c_hi = colrange_fn(i)
                            sci = 4 + nxt("scb", 3)
                            score_mms(sci, i, m, c_lo, c_hi)
                            stt[u] = sci

                        def E_(u):
                            ii, i, (oi, si_, m) = units[u]
                            c_lo, c_hi = colrange_fn(i)
                            sci = stt[u]
                            pti = nxt("pt", 4)
                            P.op("act", lambda e: e.activation(out=PT[pti][:, c_lo:c_hi], in_=ps[sci][:, c_lo:c_hi], func=AF.Exp, bias=float(cfn(i)), scale=1.0),
                                 R=[R_ps[sci]], W=[R_PT[pti]])
                            stt[u] = pti

                        def V_(u):
                            ii, i, (oi, si_, m) = units[u]
                            c_lo, c_hi = colrange_fn(i)
                            pti = stt[u]
                            lastf = (ii == n_i - 1)
                            P.pe(lambda e: e.matmul(ps[oi][:, c_lo:c_hi], lhsT=Vl_fn(i), rhs=PT[pti][:, c_lo:c_hi], start=False, stop=lastf), R=[R_PT[pti], R_V], W=[R_ps[oi]])
                            P.pe(lambda e: e.matmul(ps[si_][:, c_lo:c_hi], lhsT=ones_b[:], rhs=PT[pti][:, c_lo:c_hi], start=False, stop=lastf), R=[R_PT[pti], R_const], W=[R_ps[si_]])
                        S_(0)
                        if n_u > 1:
                            S_(1)
                        for u in range(n_u):
                            E_(u)
                            if u + 2 < n_u:
                                S_(u + 2)
                            V_(u)
                            if deferred is not None and u == min(3, n_u - 1):
                                deferred()

                    pend = []
                    for g in range(4):
                        bi = load_w(C_VA + g * 128, 128)
                        proj_tok(bi, 128, lambda st_, g=g: VV[:, st_, g * 128:(g + 1) * 128], R_VV)
                    for qi_ in (0, 1):
                        P.dma("sp", lambda e: e.dma_start(out=QK[qi_][64:67, :], in_=c_qaug), W=[R_QK[qi_]])
                    for h in range(4):
                        Q1, Q2, K1, K2 = QK[0], QK[1], QK[2], QK[3]
                        for kt in (2, 3):
                            P.dma("sp", lambda e: e.dma_start(out=QK[kt][64:67, :], in_=c_kaug[h]), W=[R_QK[kt]])
                        bq = load_w(C_QA + h * 128, 128)
                        bk = load_w(C_KA + h * 128, 128)
                        proj_feat(bq, 0, 64, Q1, R_QK[0], 0, 0.125)
                        proj_feat(bq, 64, 64, Q2, R_QK[1], 0, 0.125)
                        proj_feat(bk, 0, 64, K1, R_QK[2], 0, 1.0)
                        proj_feat(bk, 64, 64, K2, R_QK[3], 0, 1.0)
                        sig = SIG_A[h]
                        for J in range(4):
                            def colr(i, J=J):
                                return (128 * max(0, i - 4 * J), 512)

                            def smm(sci, i, m, c_lo, c_hi, J=J, h=h):
                                isd = i >= 4 * J
                                P.pe(lambda e: e.matmul(ps[sci][:, c_lo:c_hi], lhsT=QK[2 + m][0:67, i * 128:(i + 1) * 128], rhs=QK[m][0:67, J * 512 + c_lo:J * 512 + c_hi], start=True, stop=not isd),
                                     R=[R_QK[2 + m], R_QK[m]], W=[R_ps[sci]])
                                if isd:
                                    a = i - 4 * J
                                    P.pe(lambda e: e.matmul(ps[sci][:, a * 128:(a + 1) * 128], lhsT=ident_b[:], rhs=dmask_b[:, h, :], start=False, stop=True), R=[R_const], W=[R_ps[sci]])
                            attn_block(J, list(range(4 * J + 4)), colr, smm, lambda i, J=J: -sig * (512 * J - 128 * i),
                                       lambda i, h=h: VV[:, i, h * 128:(h + 1) * 128], R_VV, [(0, 1, 0), (2, 3, 1)], deferred=pend.pop() if pend else None)
                            recip_safe(ft[0], R_ft[0], ps[1], R_ps[1], 0, 128)
                            recip_safe(ft[1], R_ft[1], ps[3], R_ps[3], 0, 128)
                            P.op("dve", lambda e: e.tensor_tensor(out=ft[0][:], in0=ps[0][:, :], in1=ft[0][:], op=ALU.mult), R=[R_ps[0], R_ft[0]], W=[R_ft[0]])
                            P.op("dve", lambda e: e.tensor_tensor(out=ft[1][:], in0=ps[2][:, :], in1=ft[1][:], op=ALU.mult), R=[R_ps[2], R_ft[1]], W=[R_ft[1]])
                            P.op("dve", lambda e: e.scalar_tensor_tensor(out=ft[2][:], in0=ft[1][:], scalar=neglam, in1=ft[0][:], op0=ALU.mult, op1=ALU.add), R=[R_ft[0], R_ft[1], R_sm], W=[R_ft[2]])

                            def tail(h=h, J=J):
                                P.op("pool", lambda e: e.tensor_tensor(out=sqb[:], in0=ft[2][:], in1=ft[2][:], op=ALU.mult), R=[R_ft[2]], W=[R_sqb])
                                tb_ = 4 + nxt("scb", 3)
                                P.pe(lambda e: e.matmul(ps[tb_][:, :], lhsT=ones_b[:], rhs=sqb[:], start=True, stop=True), R=[R_sqb, R_const], W=[R_ps[tb_]])
                                P.op("dve", lambda e: e.tensor_scalar(out=ft[3][:], in0=ps[tb_][:, :], scalar1=1.0 / 128, scalar2=EPS, op0=ALU.mult, op1=ALU.add), R=[R_ps[tb_]], W=[R_ft[3]])
                                P.op("act", lambda e: e.activation(out=ft[3][:], in_=ft[3][:], func=AF.Ln), R=[R_ft[3]], W=[R_ft[3]])
                                P.op("act", lambda e: e.activation(out=ft[3][:], in_=ft[3][:], func=AF.Exp, scale=-0.5), R=[R_ft[3]], W=[R_ft[3]])
                                P.op("dve", lambda e: e.scalar_tensor_tensor(out=mixT[:, h, J * 512:(J + 1) * 512], in0=ft[2][:], scalar=gscale, in1=ft[3][:], op0=ALU.mult, op1=ALU.mult),
                                     R=[R_ft[2], R_ft[3], R_sm], W=[R_mix[h][J]])
                            pend.append(tail)
                    while pend:
                        pend.pop()()

                    for g in range(2):
                        bi = load_w(C_VB + g * 128, 128)
                        proj_tok(bi, 128, lambda st_, g=g: VV[:, st_, g * 128:(g + 1) * 128], R_VV)
                    for h in range(4):
                        z0 = 64 if h % 2 == 0 else 0
                        P.op("pool", lambda e: e.memset(QK[2 + h][z0:z0 + 64, :], 0.0), W=[R_QK[2 + h]])
                    for g in range(2):
                        bq = load_w(C_QB + g * 128, 128)
                        bk = load_w(C_KB + g * 128, 128)
                        proj_feat(bq, 0, 128, QK[g], R_QK[g], 0, 0.125)
                        proj_feat_split(bk, QK[2 + 2 * g], R_QK[2 + 2 * g], QK[3 + 2 * g], R_QK[3 + 2 * g], 1.0)
                    for h in range(4):
                        g = h // 2
                        r0 = (h % 2) * 64
                        for J in range(4):
                            i_list = list(range(max(0, 4 * J - 4), 4 * J + 4))

                            def colr(i, J=J):
                                a_lo = max(0, i - 4 * J); a_hi = min(3, i + 4 - 4 * J)
                                return (128 * a_lo, 128 * (a_hi + 1))

                            def smm(sci, i, m, c_lo, c_hi, J=J, h=h, g=g):
                                P.pe(lambda e: e.matmul(ps[sci][:, c_lo:c_hi], lhsT=QK[2 + h][:, i * 128:(i + 1) * 128], rhs=QK[g][:, J * 512 + c_lo:J * 512 + c_hi], start=True, stop=False),
                                     R=[R_QK[2 + h], R_QK[g]], W=[R_ps[sci]])
                                a_lo, a_hi = c_lo // 128, c_hi // 128 - 1
                                for a in range(a_lo, a_hi + 1):
                                    dl = 4 * J + a - i
                                    piece = {0: 0, 1: 1, 2: 2, 3: 2, 4: 3}[dl]
                                    P.pe(lambda e: e.matmul(ps[sci][:, a * 128:(a + 1) * 128], lhsT=ident_b[:], rhs=relb_b[:, h * 4 + piece, :], start=False, stop=(a == a_hi)),
                                         R=[R_const, R_relb], W=[R_ps[sci]])
                            attn_block(J, i_list, colr, smm, lambda i: 0.0, lambda i, g=g: VV[:, i, g * 128:(g + 1) * 128], R_VV, [(0, 1, 0)])
                            recip_safe(ft[0], R_ft[0], ps[1], R_ps[1], r0, r0 + 64)
                            P.op("dve", lambda e: e.tensor_tensor(out=mixT[r0:r0 + 64, 4 + g, J * 512:(J + 1) * 512], in0=ps[0][r0:r0 + 64, :], in1=ft[0][r0:r0 + 64, :], op=ALU.mult),
                                 R=[R_ps[0], R_ft[0]], W=[R_mix[4 + g][J]])

                    for g in range(2):
                        bi = load_w(C_VC + g * 128, 128)
                        proj_tok(bi, 128, lambda st_, g=g: VV[:, st_, g * 128:(g + 1) * 128], R_VV)
                    for h in range(4):
                        P.dma("sp", lambda e: e.dma_start(out=QK[h][64:67, :], in_=c_kaug[4 + h]), W=[R_QK[h]])
                        P.dma("sp", lambda e: e.dma_start(out=QK[4 + h][64:67, :], in_=c_qaug), W=[R_QK[4 + h]])
                    for g in range(2):
                        bq = load_w(C_QC + g * 128, 128)
                        bk = load_w(C_KC + g * 128, 128)
                        proj_feat(bq, 0, 64, QK[4 + 2 * g], R_QK[4 + 2 * g], 0, 0.125)
                        proj_feat(bq, 64, 64, QK[5 + 2 * g], R_QK[5 + 2 * g], 0, 0.125)
                        proj_feat(bk, 0, 64, QK[2 * g], R_QK[2 * g], 0, 1.0)
                        proj_feat(bk, 64, 64, QK[2 * g + 1], R_QK[2 * g + 1], 0, 1.0)
                    for g, nh in ((0, 3), (1, 3), (2, 2)):
                        bi = load_w(C_QI + g * 96, 32 * nh)
                        proj_feat(bi, 0, 32 * nh, QK[8 + g], R_QK[8 + g], 0, 1.0)
                    bi = load_w(C_KI, 32, rep=3)
                    proj_feat(bi, 0, 96, QK[11], R_QK[11], 0, 1.0)
                    bi = load_w(C_WI, 8)
                    proj_tok(bi, 8, lambda st_: wtok[:, st_, :], R_wtok)

                    def indexer(j):
                        L = 128 * (j + 1)
                        sb_ = scoreb[j % 2]; Rsb = R_scoreb[j % 2]; dg = diag[j % 2]; Rdg = R_diag[j % 2]
                        for h8 in range(8):
                            P.op("pool", lambda e: e.tensor_scalar(out=dg[:, h8, :], in0=ident_f[:], scalar1=wtok[:, j, h8:h8 + 1], scalar2=W_SCALE, op0=ALU.mult, op1=ALU.mult),
                                 R=[R_const, R_wtok], W=[Rdg])
                        nsc = (L + 511) // 512
                        units = [(sc_i, h8) for sc_i in range(nsc) for h8 in range(8)]
                        gbank = [4 + nxt("gb", 2) for _ in range(nsc)]
                        stt = {}

                        def R_(u):
                            sc_i, h8 = units[u]
                            w_ = min(512, L - 512 * sc_i)
                            gq, rr = h8 // 3, h8 % 3
                            rb = nxt("rb", 4)
                            P.pe(lambda e: e.matmul(ps[rb][:, 0:w_], lhsT=QK[8 + gq][32 * rr:32 * rr + 32, j * 128:(j + 1) * 128], rhs=QK[11][32 * rr:32 * rr + 32, sc_i * 512:sc_i * 512 + w_], start=True, stop=True),
                                 R=[R_QK[8 + gq], R_QK[11]], W=[R_ps[rb]])
                            stt[u] = rb

                        def L_(u):
                            sc_i, h8 = units[u]
                            w_ = min(512, L - 512 * sc_i)
                            rb = stt[u]
                            ri = nxt("relu", 3)
                            P.op("act", lambda e: e.activation(out=relu_sb[ri][:, 0:w_], in_=ps[rb][:, 0:w_], func=AF.Relu), R=[R_ps[rb]], W=[R_relu[ri]])
                            stt[u] = ri

                        def G_(u):
                            sc_i, h8 = units[u]
                            w_ = min(512, L - 512 * sc_i)
                            ri = stt[u]
                            gb = gbank[sc_i]
                            P.pe(lambda e: e.matmul(ps[gb][:, 0:w_], lhsT=dg[:, h8, :], rhs=relu_sb[ri][:, 0:w_], start=(h8 == 0), stop=(h8 == 7)), R=[Rdg, R_relu[ri]], W=[R_ps[gb]])
                            if h8 == 7:
                                P.op("act", lambda e: e.activation(out=sb_[:, sc_i * 512:sc_i * 512 + w_], in_=ps[gb][:, 0:w_], func=AF.Copy), R=[R_ps[gb]], W=[Rsb])
                        n_u = len(units)
                        R_(0); R_(1)
                        for u in range(n_u):
                            L_(u)
                            if u + 2 < n_u:
                                R_(u + 2)
                            G_(u)

                    def select(j):
                        L = 128 * (j + 1)
                        a = j % 4
                        sb_ = scoreb[j % 2]; Rsb = R_scoreb[j % 2]
                        if j < 2:
                            P.op("pool", lambda e: e.memset(negmask[:, 0:L], 0.0), W=[R_negm])
                        else:
                            P.op("dve", lambda e: e.tensor_reduce(out=sm[:, 8:9], in_=sb_[:, 0:L], axis=AX.X, op=ALU.min), R=[Rsb], W=[R_sm])
                            P.op("pool", lambda e: e.memset(sb_[0:64, L - 64:L], -1e30), R=[R_sm], W=[Rsb])
                            P.op("dve", lambda e: e.reduce_max(out=sm[:, 9:10], in_=sb_[:, 0:L], axis=AX.X), R=[Rsb], W=[R_sm])
                            P.op("dve", lambda e: e.scalar_tensor_tensor(out=sm[:, 10:11], in0=sm[:, 9:10], scalar=1e-20, in1=sm[:, 8:9], op0=ALU.add, op1=ALU.subtract), R=[R_sm], W=[R_sm])
                            P.op("dve", lambda e: e.reciprocal(out=sm[:, 11:12], in_=sm[:, 10:11]), R=[R_sm], W=[R_sm])
                            P.op("dve", lambda e: e.tensor_scalar(out=sb_[:, 0:L], in0=sb_[:, 0:L], scalar1=sm[:, 8:9], scalar2=sm[:, 11:12], op0=ALU.subtract, op1=ALU.mult), R=[Rsb, R_sm], W=[Rsb])
                            P.op("dve", lambda e: e.memset(sm[:, 12:13], 0.5), W=[R_sm])
                            for n in range(NIT):
                                dlt = 2.0 ** -(n + 2)
                                P.op("dve", lambda e: e.tensor_scalar(out=negmask[:, 0:L], in0=sb_[:, 0:L], scalar1=sm[:, 12:13], scalar2=None, op0=ALU.is_gt, op1=ALU.add, accum_out=sm[:, 13:14]),
                                     R=[Rsb, R_sm], W=[R_negm, R_sm])
                                P.op("dve", lambda e: e.tensor_scalar(out=sm[:, 14:15], in0=sm[:, 13:14], scalar1=255.5, scalar2=2.0 * dlt, op0=ALU.is_gt, op1=ALU.mult), R=[R_sm], W=[R_sm])
                                P.op("dve", lambda e: e.scalar_tensor_tensor(out=sm[:, 12:13], in0=sm[:, 12:13], scalar=-dlt, in1=sm[:, 14:15], op0=ALU.add, op1=ALU.add), R=[R_sm], W=[R_sm])
                            P.op("dve", lambda e: e.tensor_scalar(out=negmask[:, 0:L], in0=sb_[:, 0:L], scalar1=sm[:, 12:13], scalar2=NEG, op0=ALU.is_le, op1=ALU.mult), R=[Rsb, R_sm], W=[R_negm])
                        for i0 in range(0, j + 1, 8):
                            n_ = min(8, j + 1 - i0)
                            for ii in range(n_):
                                P.pe(lambda e: e.transpose(out=psT[:, ii, :], in_=negmask[:, (i0 + ii) * 128:(i0 + ii + 1) * 128], identity=ident_b[:]), R=[R_negm, R_const], W=[R_psT])
                            P.op("act", lambda e: e.activation(out=nmT[:, i0:i0 + n_, a * 128:(a + 1) * 128], in_=psT[:, 0:n_, :], func=AF.Copy), R=[R_psT], W=[R_nmT])

                    for J in range(4):
                        for a in range(4):
                            j = 4 * J + a
                            if 2 <= j + 1 < 16:
                                indexer(j + 1)
                            select(j)
                        for h in range(4):
                            g = h // 2
                            r0 = (h % 2) * 64
                            sig = SIG_C[h]

                            def colr(i, J=J):
                                return (128 * max(0, i - 4 * J), 512)

                            def smm(sci, i, m, c_lo, c_hi, J=J, h=h):
                                P.pe(lambda e: e.matmul(ps[sci][:, c_lo:c_hi], lhsT=QK[h][0:67, i * 128:(i + 1) * 128], rhs=QK[4 + h][0:67, J * 512 + c_lo:J * 512 + c_hi], start=True, stop=False),
                                     R=[R_QK[h], R_QK[4 + h]], W=[R_ps[sci]])
                                if i >= 4 * J:
                                    a = i - 4 * J
                                    P.pe(lambda e: e.matmul(ps[sci][:, a * 128:(a + 1) * 128], lhsT=ident_b[:], rhs=dmask_b[:, 4 + h, :], start=False, stop=False), R=[R_const], W=[R_ps[sci]])
                                P.pe(lambda e: e.matmul(ps[sci][:, c_lo:c_hi], lhsT=ident_b[:], rhs=nmT[:, i, c_lo:c_hi], start=False, stop=True), R=[R_const, R_nmT], W=[R_ps[sci]])
                            attn_block(J, list(range(4 * J + 4)), colr, smm, lambda i, J=J, sig=sig: -sig * (512 * J - 128 * i),
                                       lambda i, g=g: VV[:, i, g * 128:(g + 1) * 128], R_VV, [(0, 1, 0)])
                            recip_safe(ft[0], R_ft[0], ps[1], R_ps[1], r0, r0 + 64)
                            P.op("dve", lambda e: e.tensor_tensor(out=mixT[r0:r0 + 64, 6 + g, J * 512:(J + 1) * 512], in0=ps[0][r0:r0 + 64, :], in1=ft[0][r0:r0 + 64, :], op=ALU.mult),
                                 R=[R_ps[0], R_ft[0]], W=[R_mix[6 + g][J]])
                    P.barrier()

                if dbg == "mix":
                    allmix = [R_mix[c][j] for c in range(8) for j in range(4)]
                    P.dma("sp", lambda e: e.dma_start(out=dbg_out[b], in_=mixT[:]), R=allmix, W=[R_dbg])
                    P.barrier()
                    continue

                with ExitStack() as es:
                    wo_b = sbc(es, "wo_b", [128, 8, D], BF16); R_wo = Res("wo")
                    g1 = sbc(es, "g1", [128, D], F32); b1 = sbc(es, "b1", [128, D], F32); R_gb = Res("gb")
                    wr_f = sbc(es, "wr_f", [128, 8, NE], F32); br_t = sbc(es, "br_t", [128, NE], F32)
                    xt_ = [sbc(es, "xt%d" % i, [128, D], F32) for i in range(2)]; R_xt = [Res("xt") for _ in range(2)]
                    vt = [sbc(es, "vt%d" % i, [128, D], F32) for i in range(2)]; R_vt = [Res("vt") for _ in range(2)]
                    x1t = [sbc(es, "x1t%d" % i, [128, D], F32) for i in range(2)]; R_x1t = [Res("x1t") for _ in range(2)]
                    x1b = [sbc(es, "x1b%d" % i, [128, D], BF16) for i in range(2)]; R_x1b = [Res("x1b") for _ in range(2)]
                    x1T = [sbc(es, "x1T%d" % i, [128, 8, 128], F32) for i in range(2)]; R_x1T = [Res("x1T") for _ in range(2)]
                    junk = [sbc(es, "junk%d" % i, [128, D], BF16) for i in range(2)]; R_junk = [Res("junk") for _ in range(2)]
                    lst = [sbc(es, "lst%d" % i, [128, 12], F32) for i in range(2)]; R_lst = [Res("lst") for _ in range(2)]
                    lg = [sbc(es, "lg%d" % i, [128, NE], F32) for i in range(2)]; R_lg = [Res("lg") for _ in range(2)]
                    mx8 = [sbc(es, "mx8%d" % i, [128, 8], F32) for i in range(2)]; R_mx = [Res("mx") for _ in range(2)]
                    rs = [sbc(es, "rs%d" % i, [128, 16], F32) for i in range(2)]; R_rs = [Res("rs") for _ in range(2)]
                    maskb = [sbc(es, "maskb%d" % i, [128, NE], BF16) for i in range(2)]; R_maskb = [Res("maskb") for _ in range(2)]
                    slotm = [sbc(es, "slotm%d" % i, [128, NE], F32) for i in range(2)]; R_slotm = [Res("slotm") for _ in range(2)]
                    oh = [sbc(es, "oh%d" % i, [128, NE], F32) for i in range(2)]; R_oh = [Res("oh") for _ in range(2)]
                    R_p6 = [Res("p6_%d" % i) for i in range(2)]
                    for c in range(8):
                        P.dma("pool", lambda e: e.dma_start(out=wo_b[:, c, :], in_=w_out[l, c * 128:(c + 1) * 128, :]), W=[R_wo])
                    P.dma("sp", lambda e: e.dma_start(out=g1[:], in_=ln1g[l:l + 1, :].to_broadcast([128, D])), W=[R_gb])
                    P.dma("sp", lambda e: e.dma_start(out=b1[:], in_=ln1b[l:l + 1, :].to_broadcast([128, D])), W=[R_gb])
                    P.dma("sp", lambda e: e.dma_start(out=wr_f[:], in_=w_rt[l].rearrange("(k p) c -> p k c", p=128)), W=[R_gb])
                    P.dma("sp", lambda e: e.dma_start(out=br_t[:], in_=b_rt[l:l + 1, :].to_broadcast([128, NE])), W=[R_gb])
                    P.op("dve", lambda e: e.memset(lst[0][:, 0:1], 0.0), R=[R_ps[6]], W=[R_lst[0], R_p6[0], R_p6[1]])

                    def ln1_tile(tt):
                        p = tt % 2
                        gt_ = b * 16 + tt
                        rows = slice(tok0 + tt * 128, tok0 + (tt + 1) * 128)
                        c6 = p * 128
                        P.dma("sp", lambda e: e.dma_start(out=xt_[p][:], in_=xsrc[rows, :]), R=[R_xsrc], W=[R_xt[p]])
                        yield
                        for half in range(2):
                            pi = 2 * p + half
                            for c in range(8):
                                P.pe(lambda e: e.matmul(ps[pi][:, :], lhsT=mixT[:, c, tt * 128:(tt + 1) * 128], rhs=wo_b[:, c, half * 512:(half + 1) * 512], start=(c == 0), stop=(c == 7)),
                                     R=[R_mix[c][tt // 4], R_wo], W=[R_ps[pi]])
                            P.op("dve", lambda e: e.scalar_tensor_tensor(out=vt[p][:, half * 512:(half + 1) * 512], in0=xt_[p][:, half * 512:(half + 1) * 512], scalar=ALPHA, in1=ps[pi][:, :], op0=ALU.mult, op1=ALU.add, accum_out=lst[p][:, 8 + half:9 + half]),
                                 R=[R_xt[p], R_ps[pi]], W=[R_vt[p], R_lst[p]])
                            yield
                        P.op("dve", lambda e: e.tensor_tensor(out=lst[p][:, 0:1], in0=lst[p][:, 8:9], in1=lst[p][:, 9:10], op=ALU.add), R=[R_lst[p]], W=[R_lst[p]])
                        yield
                        yield from ln_gen(vt[p], R_vt[p], g1, b1, R_gb, x1t[p], R_x1t[p], lst[p], R_lst[p], junk[p], R_junk[p])
                        if dbg == "x1":
                            P.dma("sp", lambda e: e.dma_start(out=dbg_out[rows, :], in_=x1t[p][:]), R=[R_x1t[p]], W=[R_dbg])
                        P.dma("sp", lambda e: e.dma_start(out=x1s[rows, :], in_=x1t[p][:]), R=[R_x1t[p]], W=[R_x1s])
                        P.op("act", lambda e: e.activation(out=x1b[p][:], in_=x1t[p][:], func=AF.Copy), R=[R_x1t[p]], W=[R_x1b[p]])
                        yield
                        for hf in range(2):
                            for k4 in range(4):
                                k = hf * 4 + k4
                                P.pe(lambda e: e.transpose(out=ps[4 + p][:, k4 * 128:(k4 + 1) * 128], in_=x1t[p][:, k * 128:(k + 1) * 128], identity=ident_f[:]), R=[R_x1t[p], R_const], W=[R_ps[4 + p]])
                            P.op("act", lambda e: e.activation(out=x1T[p][:, hf * 4:(hf + 1) * 4, :], in_=ps[4 + p][:, :].rearrange("p (a b) -> p a b", a=4), func=AF.Copy), R=[R_ps[4 + p]], W=[R_x1T[p]])
                            yield
                        for k in range(8):
                            P.pe(lambda e: e.matmul(ps[6][:, c6:c6 + NE], lhsT=x1T[p][:, k, :], rhs=wr_f[:, k, :], start=(k == 0), stop=(k == 7)), R=[R_x1T[p], R_gb], W=[R_p6[p]])
                        P.op("dve", lambda e: e.tensor_tensor(out=lg[p][:], in0=ps[6][:, c6:c6 + NE], in1=br_t[:], op=ALU.add), R=[R_p6[p], R_gb], W=[R_lg[p]])
                        yield
                        P.op("dve", lambda e: e.max(out=mx8[p][:], in_=lg[p][:]), R=[R_lg[p]], W=[R_mx[p]])
                        yield
                        P.op("dve", lambda e: e.tensor_scalar(out=rs[p][:, 0:1], in0=mx8[p][:, 0:1], scalar1=-1.0, scalar2=None, op0=ALU.mult), R=[R_mx[p]], W=[R_rs[p]])
                        P.op("dve", lambda e: e.tensor_scalar(out=maskb[p][:], in0=lg[p][:], scalar1=mx8[p][:, 3:4], scalar2=None, op0=ALU.is_ge), R=[R_lg[p], R_mx[p]], W=[R_maskb[p]])
                        yield
                        P.op("act", lambda e: e.activation(out=rs[p][:, 4:8], in_=mx8[p][:, 0:4], func=AF.Exp, bias=rs[p][:, 0:1], scale=1.0, accum_out=rs[p][:, 1:2]), R=[R_mx[p], R_rs[p]], W=[R_rs[p]])
                        P.pe(lambda e: e.matmul(ps[6][:, c6 + 32:c6 + 64], lhsT=ltri_b[:], rhs=maskb[p][:], start=True, stop=True), R=[R_maskb[p], R_const], W=[R_p6[p]])
                        P.pe(lambda e: e.matmul(ps[6][:, c6 + 64:c6 + 96], lhsT=ones_b[:], rhs=maskb[p][:], start=True, stop=True), R=[R_maskb[p], R_const], W=[R_p6[p]])
                        yield
                        P.op("dve", lambda e: e.tensor_tensor(out=slotm[p][:], in0=ps[6][:, c6 + 32:c6 + 64], in1=base_cnt[:], op=ALU.add), R=[R_p6[p], R_base], W=[R_slotm[p]])
                        P.op("dve", lambda e: e.tensor_tensor(out=base_cnt[:], in0=ps[6][:, c6 + 64:c6 + 96], in1=base_cnt[:], op=ALU.add), R=[R_p6[p], R_base], W=[R_base])
                        yield
                        P.op("dve", lambda e: e.tensor_tensor(out=slotm[p][:], in0=slotm[p][:], in1=eoff[:], op=ALU.add), R=[R_slotm[p], R_const], W=[R_slotm[p]])
                        P.op("dve", lambda e: e.reciprocal(out=rs[p][:, 2:3], in_=rs[p][:, 1:2]), R=[R_rs[p]], W=[R_rs[p]])
                        yield
                        P.op("dve", lambda e: e.tensor_scalar(out=gates[:, gt_ * 4:gt_ * 4 + 4], in0=rs[p][:, 4:8], scalar1=rs[p][:, 2:3], scalar2=None, op0=ALU.mult), R=[R_rs[p]], W=[R_gates])
                        yield
                        for k in range(4):
                            P.op("dve", lambda e: e.scalar_tensor_tensor(out=oh[p][:], in0=lg[p][:], scalar=mx8[p][:, k:k + 1], in1=slotm[p][:], op0=ALU.is_equal, op1=ALU.mult, accum_out=slotf[:, gt_ * 4 + k:gt_ * 4 + k + 1]),
                                 R=[R_lg[p], R_mx[p], R_slotm[p]], W=[R_oh[p], R_slot])
                            yield
                        P.op("dve", lambda e: e.tensor_copy(out=sloti[:, gt_ * 4:gt_ * 4 + 4], in_=slotf[:, gt_ * 4:gt_ * 4 + 4]), R=[R_slot], W=[R_slot])
                        yield
                        for k in range(4):
                            P.dma("pool", lambda e: e.indirect_dma_start(out=xg[:, :], out_offset=bass.IndirectOffsetOnAxis(ap=sloti[:, gt_ * 4 + k:gt_ * 4 + k + 1], axis=0), in_=x1b[p][:, :], in_offset=None),
                                  R=[R_x1b[p], R_slot], W=[R_xg])
                        yield

                    run_window([ln1_tile(tt) for tt in range(16)], 2)
                    P.barrier()
        if dbg in ("mix", "x1"):
            break

        with ExitStack() as es:
            def sbm(name, shape, dt):
                return es.enter_context(nc.sbuf_tensor(name + "_m%d" % l, list(shape), dt))
            wgu_b = [sbm("wgu%d" % i, [128, 8, 2 * D], BF16) for i in range(2)]; R_wgu = [[Res("wgu") for _ in range(8)] for _ in range(2)]
            wdn_b = [sbm("wdn%d" % i, [128, 8, D], BF16) for i in range(2)]; R_wdn = [[Res("wdn") for _ in range(8)] for _ in range(2)]
            xr = [sbm("xr%d" % i, [128, D], BF16) for i in range(4)]; R_xr = [Res("xr") for _ in range(4)]
            xeT = [sbm("xeT%d" % i, [128, 8, CAP], BF16) for i in range(2)]; R_xeT = [Res("xeT") for _ in range(2)]
            GT = sbm("GT", [128, 8, CAP], BF16); R_GT = [Res("GT%d" % c) for c in range(8)]
            et = [sbm("et%d" % i, [128, 512], F32) for i in range(9)]; R_et = [Res("et") for _ in range(9)]
            yt = [sbm("yt%d" % i, [128, D], F32) for i in range(3)]; R_yt = [Res("yt") for _ in range(3)]
            bgT = sbm("bgT", [128, NE * 16], F32); R_bgT = Res("bgT")
            bgs = sbm("bgs", [128, 4, 128], F32); R_bgs = Res("bgs")
            bdb = [sbm("bdb%d" % i, [128, D], F32) for i in range(2)]; R_bdb = [Res("bdb") for _ in range(2)]
            P.dma("sp", lambda e: e.dma_start(out=bgs[:], in_=b_gu[l].rearrange("(a r) p -> r a p", r=128)), W=[R_bgs])
            for a in range(4):
                P.pe(lambda e: e.transpose(out=ps[6][:, a * 128:(a + 1) * 128], in_=bgs[:, a, :], identity=ident_f[:]), R=[R_bgs, R_const], W=[R_ps[6]])
            P.op("dve", lambda e: e.tensor_copy(out=bgT[:], in_=ps[6][:, :]), R=[R_ps[6]], W=[R_bgT])

            def load_expert(e_):
                s2 = e_ % 2
                for k in range(8):
                    P.dma("pool", lambda e: e.dma_start(out=wgu_b[s2][:, k, :], in_=w_gu[l, e_, k * 128:(k + 1) * 128, :]), W=[R_wgu[s2][k]])
                for k in range(8):
                    P.dma("pool", lambda e: e.dma_start(out=wdn_b[s2][:, k, :], in_=w_dn[l, e_, k * 128:(k + 1) * 128, :]), W=[R_wdn[s2][k]])
                P.dma("sp", lambda e: e.dma_start(out=bdb[s2][:], in_=b_dn[l, e_:e_ + 1, :].to_broadcast([128, D])), W=[R_bdb[s2]])

            def build_x(e_):
                s2 = e_ % 2
                for st_ in range(CAP // 128):
                    xi = nxt("xr", 4)
                    P.dma("sp", lambda e: e.dma_start(out=xr[xi][:], in_=xg[e_ * CAP + st_ * 128:e_ * CAP + (st_ + 1) * 128, :]), R=[R_xg], W=[R_xr[xi]])
                    tv, R_tv = (psT, R_psT) if st_ % 2 == 0 else (psT2, R_ps[6])
                    for k in range(8):
                        P.pe(lambda e: e.transpose(out=tv[:, k, :], in_=xr[xi][:, k * 128:(k + 1) * 128], identity=ident_b[:]), R=[R_xr[xi], R_const], W=[R_tv])
                    P.op("act", lambda e: e.activation(out=xeT[s2][:, :, st_ * 128:(st_ + 1) * 128], in_=tv[:, :, :], func=AF.Copy), R=[R_tv], W=[R_xeT[s2]])

            load_expert(0)
            build_x(0)
            for e_ in range(NE):
                s2 = e_ % 2
                if e_ + 1 < NE:
                    load_expert(e_ + 1)
                for c in range(8):
                    for (s0, sw) in ((0, 512), (512, CAP - 512)):
                        pg = nxt("pg", 2) * 2
                        for k in range(8):
                            P.pe(lambda e: e.matmul(ps[pg][:, 0:sw], lhsT=wgu_b[s2][:, k, c * 128:(c + 1) * 128], rhs=xeT[s2][:, k, s0:s0 + sw], start=(k == 0), stop=(k == 7)),
                                 R=[R_wgu[s2][k], R_xeT[s2]], W=[R_ps[pg]])
                        for k in range(8):
                            P.pe(lambda e: e.matmul(ps[pg + 1][:, 0:sw], lhsT=wgu_b[s2][:, k, D + c * 128:D + (c + 1) * 128], rhs=xeT[s2][:, k, s0:s0 + sw], start=(k == 0), stop=(k == 7)),
                                 R=[R_wgu[s2][k], R_xeT[s2]], W=[R_ps[pg + 1]])
                        ei = nxt("et", 3) * 3
                        bgc = bgT[:, e_ * 16 + c:e_ * 16 + c + 1]
                        buc = bgT[:, e_ * 16 + 8 + c:e_ * 16 + 8 + c + 1]
                        P.op("dve", lambda e: e.tensor_scalar(out=et[ei][:, 0:sw], in0=ps[pg][:, 0:sw], scalar1=bgc, scalar2=7.0, op0=ALU.add, op1=ALU.min), R=[R_ps[pg], R_bgT], W=[R_et[ei]])
                        P.op("act", lambda e: e.activation(out=et[ei + 1][:, 0:sw], in_=et[ei][:, 0:sw], func=AF.Sigmoid, scale=1.702), R=[R_et[ei]], W=[R_et[ei + 1]])
                        P.op("act", lambda e: e.activation(out=et[ei + 2][:, 0:sw], in_=ps[pg + 1][:, 0:sw], func=AF.Identity, bias=buc, scale=1.0), R=[R_ps[pg + 1], R_bgT], W=[R_et[ei + 2]])
                        P.op("dve", lambda e: e.tensor_scalar(out=et[ei + 2][:, 0:sw], in0=et[ei + 2][:, 0:sw], scalar1=7.0, scalar2=-7.0, op0=ALU.min, op1=ALU.max), R=[R_et[ei + 2]], W=[R_et[ei + 2]])
                        P.op("dve", lambda e: e.tensor_tensor(out=et[ei][:, 0:sw], in0=et[ei][:, 0:sw], in1=et[ei + 1][:, 0:sw], op=ALU.mult), R=[R_et[ei], R_et[ei + 1]], W=[R_et[ei]])
                        P.op("dve", lambda e: e.scalar_tensor_tensor(out=GT[:, c, s0:s0 + sw], in0=et[ei + 2][:, 0:sw], scalar=1.0, in1=et[ei][:, 0:sw], op0=ALU.add, op1=ALU.mult), R=[R_et[ei], R_et[ei + 2]], W=[R_GT[c]])
                if e_ + 1 < NE:
                    build_x(e_ + 1)
                for st_ in range(CAP // 128):
                    yi = nxt("yt", 3)
                    for half in range(2):
                        pi = 4 + nxt("pd", 2)
                        for c in range(8):
                            P.pe(lambda e: e.matmul(ps[pi][:, :], lhsT=GT[:, c, st_ * 128:(st_ + 1) * 128], rhs=wdn_b[s2][:, c, half * 512:(half + 1) * 512], start=(c == 0), stop=(c == 7)),
                                 R=[R_GT[c], R_wdn[s2][c]], W=[R_ps[pi]])
                        P.op("dve", lambda e: e.tensor_tensor(out=yt[yi][:, half * 512:(half + 1) * 512], in0=ps[pi][:, :], in1=bdb[s2][:, half * 512:(half + 1) * 512], op=ALU.add),
                             R=[R_ps[pi], R_bdb[s2]], W=[R_yt[yi]])
                    P.dma("sp", lambda e: e.dma_start(out=yg[e_ * CAP + st_ * 128:e_ * CAP + (st_ + 1) * 128, :], in_=yt[yi][:]), R=[R_yt[yi]], W=[R_yg])
            P.barrier()

        with ExitStack() as es:
            def sbm(name, shape, dt):
                return es.enter_context(nc.sbuf_tensor(name + "_c%d" % l, list(shape), dt))
            WC = 3
            yk = [sbm("yk%d" % i, [128, D], F32) for i in range(4 * WC)]; R_yk = [Res("yk") for _ in range(4 * WC)]
            xa = [sbm("xa%d" % i, [128, D], F32) for i in range(WC)]; R_xa = [Res("xa") for _ in range(WC)]
            xo = [sbm("xo%d" % i, [128, D], F32) for i in range(WC)]; R_xo = [Res("xo") for _ in range(WC)]
            g2 = sbm("g2", [128, D], F32); b2 = sbm("b2", [128, D], F32); R_gb2 = Res("gb2")
            junk2 = [sbm("junk2_%d" % i, [128, D], BF16) for i in range(WC)]; R_junk2 = [Res("junk2") for _ in range(WC)]
            lst2 = [sbm("lst2_%d" % i, [128, 12], F32) for i in range(WC)]; R_lst2 = [Res("lst2") for _ in range(WC)]
            P.dma("sp", lambda e: e.dma_start(out=g2[:], in_=ln2g[l:l + 1, :].to_broadcast([128, D])), W=[R_gb2])
            P.dma("sp", lambda e: e.dma_start(out=b2[:], in_=ln2b[l:l + 1, :].to_broadcast([128, D])), W=[R_gb2])

            def comb_tile(tt):
                p = tt % WC
                rows = slice(tt * 128, (tt + 1) * 128)
                P.dma("sp", lambda e: e.dma_start(out=xa[p][:], in_=x1s[rows, :]), R=[R_x1s], W=[R_xa[p]])
                for k in range(4):
                    yi = p * 4 + k
                    P.dma("pool", lambda e: e.indirect_dma_start(out=yk[yi][:, :], out_offset=None, in_=yg[:, :], in_offset=bass.IndirectOffsetOnAxis(ap=sloti[:, tt * 4 + k:tt * 4 + k + 1], axis=0)),
                          R=[R_yg, R_slot], W=[R_yk[yi]])
                yield
                P.op("act", lambda e: e.activation(out=xa[p][:], in_=xa[p][:], func=AF.Copy, scale=ALPHA), R=[R_xa[p]], W=[R_xa[p]])
                yield
                for k in range(4):
                    yi = p * 4 + k
                    if k < 3:
                        P.op("dve", lambda e: e.scalar_tensor_tensor(out=xa[p][:], in0=yk[yi][:], scalar=gates[:, tt * 4 + k:tt * 4 + k + 1], in1=xa[p][:], op0=ALU.mult, op1=ALU.add),
                             R=[R_yk[yi], R_gates, R_xa[p]], W=[R_xa[p]])
                    else:
                        P.op("dve", lambda e: e.scalar_tensor_tensor(out=xa[p][:], in0=yk[yi][:], scalar=gates[:, tt * 4 + k:tt * 4 + k + 1], in1=xa[p][:], op0=ALU.mult, op1=ALU.add, accum_out=lst2[p][:, 0:1]),
                             R=[R_yk[yi], R_gates, R_xa[p]], W=[R_xa[p], R_lst2[p]])
                    yield
                yield from ln_gen(xa[p], R_xa[p], g2, b2, R_gb2, xo[p], R_xo[p], lst2[p], R_lst2[p], junk2[p], R_junk2[p])
                P.dma("sp", lambda e: e.dma_start(out=xdst[rows, :], in_=xo[p][:]), R=[R_xo[p]], W=[R_xdst])
                yield

            run_window([comb_tile(tt) for tt in range(32)], WC)
            P.barrier()

    P.finish()
    return nc, P


def _consts():
    bf = ml_dtypes.bfloat16
    ident = np.eye(128, dtype=np.float32)
    ltri = np.triu(np.ones((128, 128), np.float32), 1)
    si = np.arange(128)[:, None]; qi = np.arange(128)[None, :]
    dmask = np.zeros((128, 8, 128), np.float32)
    for h in range(8):
        sg = SIG8[h]
        d = np.where(si <= qi, 0.0, np.where((si // 64) == (qi // 64), -2.0 * sg * (si - qi), NEG))
        dmask[:, h, :] = d
    q = np.arange(S)
    qaug = np.stack([np.ones(S), -(q % 128).astype(np.float64), -(128.0 * ((q // 128) % 4))]).astype(np.float32)
    kaug = np.zeros((8, 3, S), np.float32)
    for h in range(8):
        kaug[h, 0] = SIG8[h] * (q % 128)
        kaug[h, 1] = SIG8[h]
        kaug[h, 2] = SIG8[h]
    eoff = np.tile((np.arange(NE, dtype=np.float32) * CAP)[None, :], (128, 1))
    return {"c_ident": ident.astype(bf), "c_identf": ident, "c_ltri": ltri.astype(bf), "c_dmask": dmask.astype(bf),
            "c_qaug": qaug.astype(bf), "c_kaug": kaug.astype(bf), "c_eoff": eoff}


def _relb_pieces(rel_bias):
    NLn = rel_bias.shape[0]
    si = np.arange(128)[:, None]; qi = np.arange(128)[None, :]
    out = np.empty((NLn, 128, 16, 128), np.float32)
    for h in range(4):
        rel0 = qi - si
        idx0 = np.clip(rel0, -128, 128) + 128
        m0 = ((si // 64) == 1) & ((qi // 64) == 0)
        idx1 = np.clip(128 + qi - si, -128, 128) + 128
        m4 = ((si // 64) == 0) & ((qi // 64) == 1)
        for ln in range(NLn):
            rb = rel_bias[ln, h]
            p0 = rb[idx0].copy(); p0[m0] = NEG
            p1 = rb[idx1]
            p2 = np.broadcast_to(rb[256], (128, 128))
            p4 = np.array(p2); p4[m4] = NEG
            out[ln, :, h * 4 + 0, :] = p0
            out[ln, :, h * 4 + 1, :] = p1
            out[ln, :, h * 4 + 2, :] = p2
            out[ln, :, h * 4 + 3, :] = p4
    return out


_CACHE = {}


def _get_prog(NL, lam_inits, dbg=None):
    key = (NL, tuple(lam_inits), dbg)
    if key not in _CACHE:
        _CACHE[key] = build(NL, lam_inits, dbg)[0]
    return _CACHE[key]


def _layer_inputs(inp, ls):
    f = lambda a: np.ascontiguousarray(a, dtype=np.float32)
    d = {
        "w_in": f(inp["w_in"][ls]),
        "lamv": f(np.stack([inp["lam_q1"][ls], inp["lam_k1"][ls], inp["lam_q2"][ls], inp["lam_k2"][ls]], axis=1)),
        "subln_g": f(inp["subln_g"][ls]),
        "relb": _relb_pieces(np.asarray(inp["rel_bias"][ls], np.float32)),
        "w_out": f(inp["w_out"][ls]),
        "ln1_g": f(inp["ln1_g"][ls]), "ln1_b": f(inp["ln1_b"][ls]),
        "w_router": f(inp["w_router"][ls]), "b_router": f(inp["b_router"][ls]),
        "w_gu": f(inp["w_gu"][ls]), "b_gu": f(inp["b_gu"][ls]).reshape(len(range(*ls.indices(DEPTH))), NE * 16, 128),
        "w_down": f(inp["w_down"][ls]), "b_down": f(inp["b_down"][ls]),
        "ln2_g": f(inp["ln2_g"][ls]), "ln2_b": f(inp["ln2_b"][ls]),
    }
    return d


FUSED = True


def kernel(**inp):
    x = np.ascontiguousarray(inp["x"], dtype=np.float32)
    consts = _consts()
    lam_inits_all = [0.8 - 0.6 * math.exp(-0.3 * l) for l in range(DEPTH)]
    xs = [x[c * NBL:(c + 1) * NBL].reshape(T, D) for c in range(NCORES)]
    if FUSED:
        nc = _get_prog(DEPTH, lam_inits_all)
        li = _layer_inputs(inp, slice(0, DEPTH))
        in_maps = [dict(li, x=xs[c], **consts) for c in range(NCORES)]
        res = run_bass_kernel_spmd(nc, in_maps, core_ids=list(range(NCORES)))
        xs = [res.results[c]["y"] for c in range(NCORES)]
    else:
        for l in range(DEPTH):
            nc = _get_prog(1, [lam_inits_all[l]])
            li = _layer_inputs(inp, slice(l, l + 1))
            in_maps = [dict(li, x=xs[c], **consts) for c in range(NCORES)]
            res = run_bass_kernel_spmd(nc, in_maps, core_ids=list(range(NCORES)))
            xs = [np.asarray(res.results[c]["y"]) for c in range(NCORES)]
    out = np.stack([xs[c].reshape(NBL, S, D) for c in range(NCORES)], axis=0).reshape(NCORES * NBL, S, D)
    return out.astype(np.float32)
```

```python
import math
from contextlib import ExitStack
import numpy as np
import ml_dtypes
import concourse.bass as bass
import concourse.mybir as mybir
from concourse.bass_utils import run_bass_kernel_spmd

F32 = mybir.dt.float32
BF16 = mybir.dt.bfloat16
I32 = mybir.dt.int32
AF = mybir.ActivationFunctionType
ALU = mybir.AluOpType
AX = mybir.AxisListType

NCORES = 8
DEPTH = 4
S = 2048
D = 1024
NBL = 2
T = NBL * S
DIN = 3368
NE = 32
CAP = 768
NSLOT = NE * CAP
ALPHA = (2 * DEPTH) ** 0.25
EPS = 1e-5
NEG = -30000.0
SIG_A = [2.0 ** -1, 2.0 ** -3, 2.0 ** -5, 2.0 ** -7]
SIG_C = [2.0 ** -2, 2.0 ** -4, 2.0 ** -6, 2.0 ** -8]
SIG8 = SIG_A + SIG_C
W_SCALE = (8 ** -0.5) * (32 ** -0.5)
NIT = 12
C_QA, C_KA, C_VA, C_QB, C_KB, C_VB, C_QC, C_KC, C_VC, C_QI, C_KI, C_WI = (
    0, 512, 1024, 1536, 1792, 2048, 2304, 2560, 2816, 3072, 3328, 3360)


class Res:
    __slots__ = ("name", "w", "r", "multi")

    def __init__(self, name, multi=False):
        self.name = name
        self.w = {}
        self.r = {}
        self.multi = multi


class Prog:
    def __init__(self, nc, ndma=(("sp", 40), ("pool", 40), ("act", 8))):
        self.nc = nc
        self.E = {"pe": nc.tensor, "act": nc.scalar, "dve": nc.vector, "pool": nc.gpsimd, "sp": nc.sync}
        self.esem = {k: nc.alloc_semaphore("e_" + k) for k in self.E}
        self.ecnt = {k: 0 for k in self.E}
        self.waited = {k: {} for k in self.E}
        self.dsem = {}
        for k, n in ndma:
            self.dsem[k] = [[nc.alloc_semaphore("d_%s%d" % (k, i)), 0, "d_%s%d" % (k, i)] for i in range(n)]
        self.dnext = {k: 0 for k in self.dsem}
        self.nins = 0

    def _wait(self, eng, ev):
        key, sem, val = ev
        if self.waited[eng].get(key, 0) >= val:
            return
        self.E[eng].wait_ge(sem, val)
        self.waited[eng][key] = val
        self.nins += 1

    def _deps(self, eng, R, W, skip_self):
        for r in R:
            for ev in r.w.values():
                if not (skip_self and ev[0] == eng):
                    self._wait(eng, ev)
        for w in W:
            if not w.multi:
                for ev in w.w.values():
                    if not (skip_self and ev[0] == eng):
                        self._wait(eng, ev)
            for ev in w.r.values():
                if not (skip_self and ev[0] == eng):
                    self._wait(eng, ev)

    def _record(self, ev, R, W):
        for r in R:
            r.r[ev[0]] = ev
        for w in W:
            if w.multi:
                w.w[ev[0]] = ev
            else:
                w.w = {ev[0]: ev}
            w.r = {}

    def op(self, eng, fn, R=(), W=(), skip_self=False):
        self._deps(eng, R, W, skip_self)
        ins = fn(self.E[eng])
        self.ecnt[eng] += 1
        ins.then_inc(self.esem[eng], 1)
        ev = (eng, self.esem[eng], self.ecnt[eng])
        self._record(ev, R, W)
        self.nins += 1
        return ev

    def pe(self, fn, R=(), W=()):
        return self.op("pe", fn, R, W, skip_self=True)

    def dma(self, eng, fn, R=(), W=()):
        self._deps(eng, R, W, False)
        pool = self.dsem[eng]
        slot = pool[self.dnext[eng] % len(pool)]
        self.dnext[eng] += 1
        if slot[1] > 0:
            self._wait(eng, (slot[2], slot[0], slot[1]))
        ins = fn(self.E[eng])
        slot[1] += 16
        ins.then_inc(slot[0], 16)
        ev = (slot[2], slot[0], slot[1])
        self._record(ev, R, W)
        self.nins += 1
        return ev

    def barrier(self):
        evs = [(k, self.esem[k], self.ecnt[k]) for k in self.E if self.ecnt[k] > 0]
        for k in self.dsem:
            for s in self.dsem[k]:
                if s[1] > 0:
                    evs.append((s[2], s[0], s[1]))
        for e in self.E:
            for ev in evs:
                self._wait(e, ev)

    def finish(self):
        self.barrier()


def build(NL, lam_inits, dbg=None):
    nc = bass.Bass("TRN2", target_bir_lowering=False)
    P = Prog(nc)

    def din(name, shape, dt=F32):
        return nc.dram_tensor(name, list(shape), dt, kind="ExternalInput").ap()

    x_in = din("x", [T, D])
    w_in = din("w_in", [NL, D, DIN])
    lamv = din("lamv", [NL, 4, 64])
    subg = din("subln_g", [NL, 128])
    relb = din("relb", [NL, 128, 16, 128])
    w_out = din("w_out", [NL, D, D])
    ln1g = din("ln1_g", [NL, D]); ln1b = din("ln1_b", [NL, D])
    w_rt = din("w_router", [NL, D, NE]); b_rt = din("b_router", [NL, NE])
    if dbg is None or dbg == "moe":
        w_gu = din("w_gu", [NL, NE, D, 2 * D]); w_dn = din("w_down", [NL, NE, D, D])
    b_gu = din("b_gu", [NL, NE * 16, 128]); b_dn = din("b_down", [NL, NE, D])
    ln2g = din("ln2_g", [NL, D]); ln2b = din("ln2_b", [NL, D])
    c_ident = din("c_ident", [128, 128], BF16)
    c_identf = din("c_identf", [128, 128], F32)
    c_ltri = din("c_ltri", [128, 128], BF16)
    c_dmask = din("c_dmask", [128, 8, 128], BF16)
    c_qaug = din("c_qaug", [3, S], BF16)
    c_kaug = din("c_kaug", [8, 3, S], BF16)
    c_eoff = din("c_eoff", [128, NE], F32)
    y_out = nc.dram_tensor("y", [T, D], F32, kind="ExternalOutput").ap()
    dbg_out = None
    if dbg == "mix":
        dbg_out = nc.dram_tensor("dbg", [NBL, 128, 8, S], BF16, kind="ExternalOutput").ap()
    if dbg == "x1":
        dbg_out = nc.dram_tensor("dbg", [T, D], F32, kind="ExternalOutput").ap()
    xcur = nc.dram_tensor("xcur", [T, D], F32).ap()
    x1s = nc.dram_tensor("x1s", [T, D], F32).ap()
    xg = nc.dram_tensor("xg", [NSLOT, D], BF16).ap()
    yg = nc.dram_tensor("yg", [NSLOT, D], F32).ap()
    R_xin = Res("xin", True); R_xcur = Res("xcur", True); R_x1s = Res("x1s", True)
    R_xg = Res("xg", True); R_yg = Res("yg", True); R_y = Res("y", True); R_dbg = Res("dbg", True)
    R_const = Res("const", True)

    def sb(name, shape, dt):
        return nc.alloc_sbuf_tensor(name, list(shape), dt)

    ident_b = sb("ident_b", [128, 128], BF16)
    ident_f = sb("ident_f", [128, 128], F32)
    ltri_b = sb("ltri_b", [128, 128], BF16)
    ones_b = sb("ones_b", [128, 128], BF16)
    zeros_b = sb("zeros_b", [128, 512], BF16)
    dmask_b = sb("dmask_b", [128, 8, 128], BF16)
    eoff = sb("eoff", [128, NE], F32)
    base_cnt = sb("base_cnt", [128, NE], F32)
    slotf = sb("slotf", [128, 32 * 4], F32)
    sloti = sb("sloti", [128, 32 * 4], I32)
    gates = sb("gates", [128, 32 * 4], F32)
    R_slot = Res("slot"); R_gates = Res("gates"); R_base = Res("base")
    for t_, src in ((ident_b, c_ident), (ident_f, c_identf), (ltri_b, c_ltri), (dmask_b, c_dmask), (eoff, c_eoff)):
        P.dma("sp", lambda e, t_=t_, src=src: e.dma_start(out=t_[:], in_=src), W=[R_const])
    P.op("pool", lambda e: e.memset(ones_b[:], 1.0), W=[R_const])
    P.op("pool", lambda e: e.memset(zeros_b[:], 0.0), W=[R_const])

    ps = [nc.alloc_psum_tensor("ps%d" % i, [128, 512], F32) for i in range(7)]
    R_ps = [Res("ps%d" % i) for i in range(7)]
    psT = nc.alloc_psum_tensor("psT", [128, 8, 128], BF16)
    R_psT = Res("psT")
    psT2 = ps[6][:, :].bitcast(BF16).rearrange("p (a b) -> p a b", a=8)

    rot = {}

    def nxt(key, n):
        rot[key] = (rot.get(key, -1) + 1) % n
        return rot[key]

    def run_window(gen_list, width):
        active = []
        it = iter(gen_list)
        while True:
            while len(active) < width:
                g_ = next(it, None)
                if g_ is None:
                    break
                active.append(g_)
            if not active:
                break
            for g_ in list(active):
                try:
                    next(g_)
                except StopIteration:
                    active.remove(g_)

    def ln_gen(v, R_v, g_t, b_t, R_gb, out_t, R_out, st, R_st, junk, R_junk):
        P.op("dve", lambda e: e.tensor_scalar(out=st[:, 1:2], in0=st[:, 0:1], scalar1=-1.0 / D, scalar2=None, op0=ALU.mult), R=[R_st], W=[R_st])
        yield
        P.op("act", lambda e: e.activation(out=junk[:], in_=v[:], func=AF.Square, bias=st[:, 1:2], scale=1.0, accum_out=st[:, 2:3]), R=[R_v, R_st], W=[R_junk, R_st])
        yield
        P.op("dve", lambda e: e.tensor_scalar(out=st[:, 3:4], in0=st[:, 2:3], scalar1=1.0 / D, scalar2=EPS, op0=ALU.mult, op1=ALU.add), R=[R_st], W=[R_st])
        yield
        P.op("act", lambda e: e.activation(out=st[:, 4:5], in_=st[:, 3:4], func=AF.Ln), R=[R_st], W=[R_st])
        P.op("act", lambda e: e.activation(out=st[:, 5:6], in_=st[:, 4:5], func=AF.Exp, scale=-0.5), R=[R_st], W=[R_st])
        yield
        P.op("dve", lambda e: e.tensor_tensor(out=st[:, 6:7], in0=st[:, 1:2], in1=st[:, 5:6], op=ALU.mult), R=[R_st], W=[R_st])
        yield
        P.op("act", lambda e: e.activation(out=v[:], in_=v[:], func=AF.Identity, bias=st[:, 6:7], scale=st[:, 5:6]), R=[R_v, R_st], W=[R_v])
        yield
        P.op("dve", lambda e: e.tensor_tensor(out=v[:], in0=v[:], in1=g_t[:], op=ALU.mult), R=[R_v, R_gb], W=[R_v])
        yield
        P.op("dve", lambda e: e.tensor_tensor(out=out_t[:], in0=v[:], in1=b_t[:], op=ALU.add), R=[R_v, R_gb], W=[R_out])
        yield

    for l in range(NL):
        lam_init = lam_inits[l]
        xsrc, R_xsrc = (x_in, R_xin) if l == 0 else (xcur, R_xcur)
        last = (l == NL - 1)
        xdst, R_xdst = (y_out, R_y) if last else (xcur, R_xcur)
        P.op("pool", lambda e: e.memset(base_cnt[:], 0.0), W=[R_base])

        for b in range(NBL):
            tok0 = b * S
            with ExitStack() as es_b:
                def sbc(es, name, shape, dt):
                    return es.enter_context(nc.sbuf_tensor(name + "_%d_%d" % (l, b), list(shape), dt))
                xT = sbc(es_b, "xT", [128, 8, S], BF16); R_xT = [Res("xT%d" % i) for i in range(4)]
                mixT = sbc(es_b, "mixT", [128, 8, S], BF16)
                R_mix = [[Res("mix%d_%d" % (c, j)) for j in range(4)] for c in range(8)]

                with ExitStack() as es:
                    xbs = [sbc(es, "xbs%d" % i, [128, D], BF16) for i in range(3)]; R_xbs = [Res("xbs") for _ in range(3)]
                    for tt in range(16):
                        i2 = tt % 3
                        P.dma("pool", lambda e: e.dma_start(out=xbs[i2][:], in_=xsrc[tok0 + tt * 128: tok0 + (tt + 1) * 128, :]), R=[R_xsrc], W=[R_xbs[i2]])
                        for k in range(8):
                            P.pe(lambda e: e.transpose(out=psT[:, k, :], in_=xbs[i2][:, k * 128:(k + 1) * 128], identity=ident_b[:]), R=[R_xbs[i2], R_const], W=[R_psT])
                        P.op("dve", lambda e: e.tensor_copy(out=xT[:, :, tt * 128:(tt + 1) * 128], in_=psT[:, :, :]), R=[R_psT], W=[R_xT[tt // 4]])
                    P.barrier()

                with ExitStack() as es:
                    QK = [sbc(es, "qk%d" % i, [128, S], BF16) for i in range(12)]
                    R_QK = [Res("qk%d" % i) for i in range(12)]
                    VV = sbc(es, "vv", [128, 16, 512], BF16); R_VV = Res("vv")
                    wbf = [sbc(es, "wbf%d" % i, [128, 8, 128], BF16) for i in range(4)]; R_wbf = [Res("wbf") for _ in range(4)]
                    PT = [sbc(es, "pt%d" % i, [128, 512], BF16) for i in range(4)]; R_PT = [Res("pt") for _ in range(4)]
                    relb_b = sbc(es, "relb_b", [128, 16, 128], BF16); R_relb = Res("relb")
                    scoreb = [sbc(es, "score%d" % i, [128, S], F32) for i in range(2)]; R_scoreb = [Res("score%d" % i) for i in range(2)]
                    score = scoreb[0]; R_score = R_scoreb[0]
                    relu_sb = [sbc(es, "relu%d" % i, [128, 512], BF16) for i in range(3)]; R_relu = [Res("relu") for _ in range(3)]
                    negmask = sbc(es, "negmask", [128, S], BF16); R_negm = Res("negm")
                    nmT = sbc(es, "nmT", [128, 16, 512], BF16); R_nmT = Res("nmT")
                    diag = [sbc(es, "diag%d" % i, [128, 8, 128], BF16) for i in range(2)]; R_diag = [Res("diag%d" % i) for i in range(2)]
                    wtok = sbc(es, "wtok", [128, 16, 8], F32); R_wtok = Res("wtok")
                    ft = [sbc(es, "ft%d" % i, [128, 512], F32) for i in range(4)]; R_ft = [Res("ft%d" % i) for i in range(4)]
                    sqb = sbc(es, "sqb", [128, 512], BF16); R_sqb = Res("sqb")
                    sm = sbc(es, "sm", [128, 16], F32); R_sm = Res("sm")
                    lamt = sbc(es, "lamt", [128, 4, 64], F32); R_lamt = Res("lamt")
                    gsc = sbc(es, "gsc", [128, 2], F32)

                    P.dma("sp", lambda e: e.dma_start(out=lamt[:], in_=lamv[l:l + 1, :, :].to_broadcast([128, 4, 64])), W=[R_lamt])
                    P.dma("sp", lambda e: e.dma_start(out=gsc[:, 0:1], in_=subg[l, :].rearrange("(p o) -> p o", o=1)), W=[R_sm])
                    P.op("dve", lambda e: e.tensor_tensor(out=lamt[:, 0, :], in0=lamt[:, 0, :], in1=lamt[:, 1, :], op=ALU.mult), R=[R_lamt], W=[R_lamt])
                    P.op("dve", lambda e: e.tensor_tensor(out=lamt[:, 2, :], in0=lamt[:, 2, :], in1=lamt[:, 3, :], op=ALU.mult), R=[R_lamt], W=[R_lamt])
                    P.op("dve", lambda e: e.reduce_sum(out=sm[:, 0:1], in_=lamt[:, 0, :], axis=AX.X), R=[R_lamt], W=[R_sm])
                    P.op("dve", lambda e: e.reduce_sum(out=sm[:, 1:2], in_=lamt[:, 2, :], axis=AX.X), R=[R_lamt], W=[R_sm])
                    P.op("act", lambda e: e.activation(out=sm[:, 2:4], in_=sm[:, 0:2], func=AF.Exp), R=[R_sm], W=[R_sm])
                    P.op("dve", lambda e: e.tensor_tensor(out=sm[:, 4:5], in0=sm[:, 3:4], in1=sm[:, 2:3], op=ALU.subtract), R=[R_sm], W=[R_sm])
                    P.op("dve", lambda e: e.tensor_scalar(out=sm[:, 5:6], in0=sm[:, 4:5], scalar1=-lam_init, scalar2=None, op0=ALU.add), R=[R_sm], W=[R_sm])
                    P.op("dve", lambda e: e.tensor_scalar(out=gsc[:, 1:2], in0=gsc[:, 0:1], scalar1=1.0 - lam_init, scalar2=None, op0=ALU.mult), R=[R_sm], W=[R_sm])
                    neglam = sm[:, 5:6]
                    gscale = gsc[:, 1:2]

                    P.dma("sp", lambda e: e.dma_start(out=score[:, :].rearrange("p (a b) -> p a b", a=16), in_=relb[l]), W=[R_score])
                    P.op("pool", lambda e: e.tensor_copy(out=relb_b[:], in_=score[:, :].rearrange("p (a b) -> p a b", a=16)), R=[R_score], W=[R_relb])

                    def load_w(c0, ncol, rep=1):
                        bi = nxt("wbf", 4)
                        for r in range(rep):
                            P.dma("pool", lambda e: e.dma_start(out=wbf[bi][:, :, r * ncol:(r + 1) * ncol], in_=w_in[l, :, c0:c0 + ncol].rearrange("(k p) c -> p k c", p=128)), W=[R_wbf[bi]])
                        return bi

                    def proj_feat(bi, c_lo, m, dst, R_dst, p_lo, scale):
                        for tb in range(4):
                            pi = 4 + nxt("pj", 2)
                            for k in range(8):
                                P.pe(lambda e: e.matmul(ps[pi][0:m, :], lhsT=wbf[bi][:, k, c_lo:c_lo + m], rhs=xT[:, k, tb * 512:(tb + 1) * 512], start=(k == 0), stop=(k == 7)),
                                     R=[R_wbf[bi], R_xT[tb]], W=[R_ps[pi]])
                            P.op("act", lambda e: e.activation(out=dst[p_lo:p_lo + m, tb * 512:(tb + 1) * 512], in_=ps[pi][p_lo:p_lo + m, :], func=AF.Copy, scale=scale),
                                 R=[R_ps[pi]], W=[R_dst])

                    def proj_feat_split(bi, dst0, R0, dst1, R1, scale):
                        for tb in range(4):
                            pi = 4 + nxt("pj", 2)
                            for k in range(8):
                                P.pe(lambda e: e.matmul(ps[pi][:, :], lhsT=wbf[bi][:, k, :], rhs=xT[:, k, tb * 512:(tb + 1) * 512], start=(k == 0), stop=(k == 7)),
                                     R=[R_wbf[bi], R_xT[tb]], W=[R_ps[pi]])
                            P.op("act", lambda e: e.activation(out=dst0[0:64, tb * 512:(tb + 1) * 512], in_=ps[pi][0:64, :], func=AF.Copy, scale=scale), R=[R_ps[pi]], W=[R0])
                            P.op("act", lambda e: e.activation(out=dst1[64:128, tb * 512:(tb + 1) * 512], in_=ps[pi][64:128, :], func=AF.Copy, scale=scale), R=[R_ps[pi]], W=[R1])

                    def proj_tok(bi, ncol, dst_fn, R_dst, act_eng="act"):
                        for st_ in range(16):
                            pi = 4 + nxt("pj", 2)
                            for k in range(8):
                                P.pe(lambda e: e.matmul(ps[pi][:, 0:ncol], lhsT=xT[:, k, st_ * 128:(st_ + 1) * 128], rhs=wbf[bi][:, k, 0:ncol], start=(k == 0), stop=(k == 7)),
                                     R=[R_wbf[bi], R_xT[st_ // 4]], W=[R_ps[pi]])
                            P.op("act", lambda e: e.activation(out=dst_fn(st_), in_=ps[pi][:, 0:ncol], func=AF.Copy), R=[R_ps[pi]], W=[R_dst])

                    def recip_safe(dst, R_dst, src_ps, R_src, p0, p1):
                        P.op("dve", lambda e: e.tensor_scalar(out=dst[p0:p1, :], in0=src_ps[p0:p1, :], scalar1=1e-30, scalar2=None, op0=ALU.max), R=[R_src], W=[R_dst])
                        P.op("dve", lambda e: e.reciprocal(out=dst[p0:p1, :], in_=dst[p0:p1, :]), R=[R_dst], W=[R_dst])

                    pend = []

                    def attn_block(J, i_list, colrange_fn, score_mms, cfn, Vl_fn, R_V, accs):
                        for (oi, si_, m) in accs:
                            for bi_ in (oi, si_):
                                P.pe(lambda e: e.matmul(ps[bi_][:, :], lhsT=zeros_b[:, 0:128], rhs=zeros_b[:, :], start=True, stop=False), R=[R_const], W=[R_ps[bi_]])
                        n_i = len(i_list)
                        units = [(ii, i, acc) for ii, i in enumerate(i_list) for acc in accs]
                        n_u = len(units)
                        stt = {}

                        def S_(u):
                            ii, i, (oi, si_, m) = units[u]
                            c_lo, c_hi = colrange_fn(i)
                            sci = 4 + nxt("scb", 3)
                            score_mms(sci, i, m, c_lo, c_hi)
                            stt[u] = sci

                        def E_(u):
                            ii, i, (oi, si_, m) = units[u]
                            c_lo, c_hi = colrange_fn(i)
                            sci = stt[u]
                            pti = nxt("pt", 4)
                            P.op("act", lambda e: e.activation(out=PT[pti][:, c_lo:c_hi], in_=ps[sci][:, c_lo:c_hi], func=AF.Exp, bias=float(cfn(i)), scale=1.0),
                                 R=[R_ps[sci]], W=[R_PT[pti]])
                            stt[u] = pti

                        def V_(u):
                            ii, i, (oi, si_, m) = units[u]
                            c_lo, c_hi = colrange_fn(i)
                            pti = stt[u]
                            lastf = (ii == n_i - 1)
                            P.pe(lambda e: e.matmul(ps[oi][:, c_lo:c_hi], lhsT=Vl_fn(i), rhs=PT[pti][:, c_lo:c_hi], start=False, stop=lastf), R=[R_PT[pti], R_V], W=[R_ps[oi]])
                            P.pe(lambda e: e.matmul(ps[si_][:, c_lo:c_hi], lhsT=ones_b[:], rhs=PT[pti][:, c_lo:c_hi], start=False, stop=lastf), R=[R_PT[pti], R_const], W=[R_ps[si_]])
                        S_(0)
                        if n_u > 1:
                            S_(1)
                        for u in range(n_u):
                            E_(u)
                            if u + 2 < n_u:
                                S_(u + 2)
                            V_(u)
                            if pend and (u == min(3, n_u - 1) or u == min(13, n_u - 1)):
                                pend.pop(0)()

                    for g in range(4):
                        bi = load_w(C_VA + g * 128, 128)
                        proj_tok(bi, 128, lambda st_, g=g: VV[:, st_, g * 128:(g + 1) * 128], R_VV)
                    for qi_ in (0, 1):
                        P.dma("sp", lambda e: e.dma_start(out=QK[qi_][64:67, :], in_=c_qaug), W=[R_QK[qi_]])
                    for h in range(4):
                        Q1, Q2, K1, K2 = QK[0], QK[1], QK[2], QK[3]
                        for kt in (2, 3):
                            P.dma("sp", lambda e: e.dma_start(out=QK[kt][64:67, :], in_=c_kaug[h]), W=[R_QK[kt]])
                        bq = load_w(C_QA + h * 128, 128)
                        bk = load_w(C_KA + h * 128, 128)
                        proj_feat(bq, 0, 64, Q1, R_QK[0], 0, 0.125)
                        proj_feat(bq, 64, 64, Q2, R_QK[1], 0, 0.125)
                        proj_feat(bk, 0, 64, K1, R_QK[2], 0, 1.0)
                        proj_feat(bk, 64, 64, K2, R_QK[3], 0, 1.0)
                        sig = SIG_A[h]
                        for J in range(4):
                            def colr(i, J=J):
                                return (128 * max(0, i - 4 * J), 512)

                            def smm(sci, i, m, c_lo, c_hi, J=J, h=h):
                                isd = i >= 4 * J
                                P.pe(lambda e: e.matmul(ps[sci][:, c_lo:c_hi], lhsT=QK[2 + m][0:67, i * 128:(i + 1) * 128], rhs=QK[m][0:67, J * 512 + c_lo:J * 512 + c_hi], start=True, stop=not isd),
                                     R=[R_QK[2 + m], R_QK[m]], W=[R_ps[sci]])
                                if isd:
                                    a = i - 4 * J
                                    P.pe(lambda e: e.matmul(ps[sci][:, a * 128:(a + 1) * 128], lhsT=ident_b[:], rhs=dmask_b[:, h, :], start=False, stop=True), R=[R_const], W=[R_ps[sci]])
                            attn_block(J, list(range(4 * J + 4)), colr, smm, lambda i, J=J: -sig * (512 * J - 128 * i),
                                       lambda i, h=h: VV[:, i, h * 128:(h + 1) * 128], R_VV, [(0, 1, 0), (2, 3, 1)])
                            P.op("act", lambda e: e.activation(out=ft[0][:], in_=ps[0][:, :], func=AF.Copy), R=[R_ps[0]], W=[R_ft[0]])
                            P.op("dve", lambda e: e.tensor_copy(out=ft[1][:], in_=ps[1][:, :]), R=[R_ps[1]], W=[R_ft[1]])
                            P.op("act", lambda e: e.activation(out=ft[2][:], in_=ps[2][:, :], func=AF.Copy), R=[R_ps[2]], W=[R_ft[2]])
                            P.op("dve", lambda e: e.tensor_copy(out=ft[3][:], in_=ps[3][:, :]), R=[R_ps[3]], W=[R_ft[3]])

                            def fin1(h=h, J=J):
                                for (oi_, si2) in ((0, 1), (2, 3)):
                                    P.op("dve", lambda e: e.tensor_scalar(out=ft[si2][:], in0=ft[si2][:], scalar1=1e-30, scalar2=None, op0=ALU.max), R=[R_ft[si2]], W=[R_ft[si2]])
                                    P.op("dve", lambda e: e.reciprocal(out=ft[si2][:], in_=ft[si2][:]), R=[R_ft[si2]], W=[R_ft[si2]])
                                    P.op("dve", lambda e: e.tensor_tensor(out=ft[oi_][:], in0=ft[oi_][:], in1=ft[si2][:], op=ALU.mult), R=[R_ft[oi_], R_ft[si2]], W=[R_ft[oi_]])
                                P.op("dve", lambda e: e.scalar_tensor_tensor(out=ft[2][:], in0=ft[2][:], scalar=neglam, in1=ft[0][:], op0=ALU.mult, op1=ALU.add), R=[R_ft[0], R_ft[2], R_sm], W=[R_ft[2]])
                                P.op("pool", lambda e: e.tensor_tensor(out=sqb[:], in0=ft[2][:], in1=ft[2][:], op=ALU.mult), R=[R_ft[2]], W=[R_sqb])

                            def fin2(h=h, J=J):
                                tb_ = 4 + nxt("scb", 3)
                                P.pe(lambda e: e.matmul(ps[tb_][:, :], lhsT=ones_b[:], rhs=sqb[:], start=True, stop=True), R=[R_sqb, R_const], W=[R_ps[tb_]])
                                P.op("dve", lambda e: e.tensor_scalar(out=ft[3][:], in0=ps[tb_][:, :], scalar1=1.0 / 128, scalar2=EPS, op0=ALU.mult, op1=ALU.add), R=[R_ps[tb_]], W=[R_ft[3]])
                                P.op("act", lambda e: e.activation(out=ft[3][:], in_=ft[3][:], func=AF.Ln), R=[R_ft[3]], W=[R_ft[3]])
                                P.op("act", lambda e: e.activation(out=ft[3][:], in_=ft[3][:], func=AF.Exp, scale=-0.5), R=[R_ft[3]], W=[R_ft[3]])
                                P.op("dve", lambda e: e.scalar_tensor_tensor(out=mixT[:, h, J * 512:(J + 1) * 512], in0=ft[2][:], scalar=gscale, in1=ft[3][:], op0=ALU.mult, op1=ALU.mult),
                                     R=[R_ft[2], R_ft[3], R_sm], W=[R_mix[h][J]])
                            pend.append(fin1)
                            pend.append(fin2)
                    while pend:
                        pend.pop(0)()

                    for g in range(2):
                        bi = load_w(C_VB + g * 128, 128)
                        proj_tok(bi, 128, lambda st_, g=g: VV[:, st_, g * 128:(g + 1) * 128], R_VV)
                    for h in range(4):
                        z0 = 64 if h % 2 == 0 else 0
                        P.op("pool", lambda e: e.memset(QK[2 + h][z0:z0 + 64, :], 0.0), W=[R_QK[2 + h]])
                    for g in range(2):
                        bq = load_w(C_QB + g * 128, 128)
                        bk = load_w(C_KB + g * 128, 128)
                        proj_feat(bq, 0, 128, QK[g], R_QK[g], 0, 0.125)
                        proj_feat_split(bk, QK[2 + 2 * g], R_QK[2 + 2 * g], QK[3 + 2 * g], R_QK[3 + 2 * g], 1.0)
                    for h in range(4):
                        g = h // 2
                        r0 = (h % 2) * 64
                        for J in range(4):
                            i_list = list(range(max(0, 4 * J - 4), 4 * J + 4))

                            def colr(i, J=J):
                                a_lo = max(0, i - 4 * J); a_hi = min(3, i + 4 - 4 * J)
                                return (128 * a_lo, 128 * (a_hi + 1))

                            def smm(sci, i, m, c_lo, c_hi, J=J, h=h, g=g):
                                P.pe(lambda e: e.matmul(ps[sci][:, c_lo:c_hi], lhsT=QK[2 + h][:, i * 128:(i + 1) * 128], rhs=QK[g][:, J * 512 + c_lo:J * 512 + c_hi], start=True, stop=False),
                                     R=[R_QK[2 + h], R_QK[g]], W=[R_ps[sci]])
                                a_lo, a_hi = c_lo // 128, c_hi // 128 - 1
                                for a in range(a_lo, a_hi + 1):
                                    dl = 4 * J + a - i
                                    piece = {0: 0, 1: 1, 2: 2, 3: 2, 4: 3}[dl]
                                    P.pe(lambda e: e.matmul(ps[sci][:, a * 128:(a + 1) * 128], lhsT=ident_b[:], rhs=relb_b[:, h * 4 + piece, :], start=False, stop=(a == a_hi)),
                                         R=[R_const, R_relb], W=[R_ps[sci]])
                            attn_block(J, i_list, colr, smm, lambda i: 0.0, lambda i, g=g: VV[:, i, g * 128:(g + 1) * 128], R_VV, [(0, 1, 0)])
                            P.op("act", lambda e: e.activation(out=ft[0][r0:r0 + 64, :], in_=ps[0][r0:r0 + 64, :], func=AF.Copy), R=[R_ps[0]], W=[R_ft[0]])
                            P.op("dve", lambda e: e.tensor_copy(out=ft[1][r0:r0 + 64, :], in_=ps[1][r0:r0 + 64, :]), R=[R_ps[1]], W=[R_ft[1]])

                            def finB(r0=r0, cc=4 + g, J=J):
                                P.op("dve", lambda e: e.tensor_scalar(out=ft[1][r0:r0 + 64, :], in0=ft[1][r0:r0 + 64, :], scalar1=1e-30, scalar2=None, op0=ALU.max), R=[R_ft[1]], W=[R_ft[1]])
                                P.op("dve", lambda e: e.reciprocal(out=ft[1][r0:r0 + 64, :], in_=ft[1][r0:r0 + 64, :]), R=[R_ft[1]], W=[R_ft[1]])
                                P.op("dve", lambda e: e.tensor_tensor(out=mixT[r0:r0 + 64, cc, J * 512:(J + 1) * 512], in0=ft[0][r0:r0 + 64, :], in1=ft[1][r0:r0 + 64, :], op=ALU.mult),
                                     R=[R_ft[0], R_ft[1]], W=[R_mix[cc][J]])
                            pend.append(finB)

                    while pend:
                        pend.pop(0)()
                    for g in range(2):
                        bi = load_w(C_VC + g * 128, 128)
                        proj_tok(bi, 128, lambda st_, g=g: VV[:, st_, g * 128:(g + 1) * 128], R_VV)
                    for h in range(4):
                        P.dma("sp", lambda e: e.dma_start(out=QK[h][64:67, :], in_=c_kaug[4 + h]), W=[R_QK[h]])
                        P.dma("sp", lambda e: e.dma_start(out=QK[4 + h][64:67, :], in_=c_qaug), W=[R_QK[4 + h]])
                    for g in range(2):
                        bq = load_w(C_QC + g * 128, 128)
                        bk = load_w(C_KC + g * 128, 128)
                        proj_feat(bq, 0, 64, QK[4 + 2 * g], R_QK[4 + 2 * g], 0, 0.125)
                        proj_feat(bq, 64, 64, QK[5 + 2 * g], R_QK[5 + 2 * g], 0, 0.125)
                        proj_feat(bk, 0, 64, QK[2 * g], R_QK[2 * g], 0, 1.0)
                        proj_feat(bk, 64, 64, QK[2 * g + 1], R_QK[2 * g + 1], 0, 1.0)
                    for g, nh in ((0, 3), (1, 3), (2, 2)):
                        bi = load_w(C_QI + g * 96, 32 * nh)
                        proj_feat(bi, 0, 32 * nh, QK[8 + g], R_QK[8 + g], 0, 1.0)
                    bi = load_w(C_KI, 32, rep=3)
                    proj_feat(bi, 0, 96, QK[11], R_QK[11], 0, 1.0)
                    bi = load_w(C_WI, 8)
                    proj_tok(bi, 8, lambda st_: wtok[:, st_, :], R_wtok)

                    def indexer(j):
                        L = 128 * (j + 1)
                        sb_ = scoreb[j % 2]; Rsb = R_scoreb[j % 2]; dg = diag[j % 2]; Rdg = R_diag[j % 2]
                        for h8 in range(8):
                            P.op("pool", lambda e: e.tensor_scalar(out=dg[:, h8, :], in0=ident_f[:], scalar1=wtok[:, j, h8:h8 + 1], scalar2=W_SCALE, op0=ALU.mult, op1=ALU.mult),
                                 R=[R_const, R_wtok], W=[Rdg])
                        nsc = (L + 511) // 512
                        units = [(sc_i, h8) for sc_i in range(nsc) for h8 in range(8)]
                        gbank = [4 + nxt("gb", 2) for _ in range(nsc)]
                        stt = {}

                        def R_(u):
                            sc_i, h8 = units[u]
                            w_ = min(512, L - 512 * sc_i)
                            gq, rr = h8 // 3, h8 % 3
                            rb = nxt("rb", 4)
                            P.pe(lambda e: e.matmul(ps[rb][:, 0:w_], lhsT=QK[8 + gq][32 * rr:32 * rr + 32, j * 128:(j + 1) * 128], rhs=QK[11][32 * rr:32 * rr + 32, sc_i * 512:sc_i * 512 + w_], start=True, stop=True),
                                 R=[R_QK[8 + gq], R_QK[11]], W=[R_ps[rb]])
                            stt[u] = rb

                        def L_(u):
                            sc_i, h8 = units[u]
                            w_ = min(512, L - 512 * sc_i)
                            rb = stt[u]
                            ri = nxt("relu", 3)
                            P.op("act", lambda e: e.activation(out=relu_sb[ri][:, 0:w_], in_=ps[rb][:, 0:w_], func=AF.Relu), R=[R_ps[rb]], W=[R_relu[ri]])
                            stt[u] = ri

                        def G_(u):
                            sc_i, h8 = units[u]
                            w_ = min(512, L - 512 * sc_i)
                            ri = stt[u]
                            gb = gbank[sc_i]
                            P.pe(lambda e: e.matmul(ps[gb][:, 0:w_], lhsT=dg[:, h8, :], rhs=relu_sb[ri][:, 0:w_], start=(h8 == 0), stop=(h8 == 7)), R=[Rdg, R_relu[ri]], W=[R_ps[gb]])
                            if h8 == 7:
                                P.op("act", lambda e: e.activation(out=sb_[:, sc_i * 512:sc_i * 512 + w_], in_=ps[gb][:, 0:w_], func=AF.Copy), R=[R_ps[gb]], W=[Rsb])
                        n_u = len(units)
                        R_(0); R_(1)
                        for u in range(n_u):
                            L_(u)
                            if u + 2 < n_u:
                                R_(u + 2)
                            G_(u)

                    def select(j):
                        L = 128 * (j + 1)
                        a = j % 4
                        sb_ = scoreb[j % 2]; Rsb = R_scoreb[j % 2]
                        if j < 2:
                            P.op("pool", lambda e: e.memset(negmask[:, 0:L], 0.0), W=[R_negm])
                        else:
                            P.op("dve", lambda e: e.tensor_reduce(out=sm[:, 8:9], in_=sb_[:, 0:L], axis=AX.X, op=ALU.min), R=[Rsb], W=[R_sm])
                            P.op("pool", lambda e: e.memset(sb_[0:64, L - 64:L], -1e30), R=[R_sm], W=[Rsb])
                            P.op("dve", lambda e: e.reduce_max(out=sm[:, 9:10], in_=sb_[:, 0:L], axis=AX.X), R=[Rsb], W=[R_sm])
                            P.op("dve", lambda e: e.scalar_tensor_tensor(out=sm[:, 10:11], in0=sm[:, 9:10], scalar=1e-20, in1=sm[:, 8:9], op0=ALU.add, op1=ALU.subtract), R=[R_sm], W=[R_sm])
                            P.op("dve", lambda e: e.reciprocal(out=sm[:, 11:12], in_=sm[:, 10:11]), R=[R_sm], W=[R_sm])
                            P.op("dve", lambda e: e.tensor_scalar(out=sb_[:, 0:L], in0=sb_[:, 0:L], scalar1=sm[:, 8:9], scalar2=sm[:, 11:12], op0=ALU.subtract, op1=ALU.mult), R=[Rsb, R_sm], W=[Rsb])
                            P.op("dve", lambda e: e.memset(sm[:, 12:13], 0.5), W=[R_sm])
                            for n in range(NIT):
                                dlt = 2.0 ** -(n + 2)
                                P.op("dve", lambda e: e.tensor_scalar(out=negmask[:, 0:L], in0=sb_[:, 0:L], scalar1=sm[:, 12:13], scalar2=None, op0=ALU.is_gt, op1=ALU.add, accum_out=sm[:, 13:14]),
                                     R=[Rsb, R_sm], W=[R_negm, R_sm])
                                P.op("dve", lambda e: e.tensor_scalar(out=sm[:, 14:15], in0=sm[:, 13:14], scalar1=255.5, scalar2=2.0 * dlt, op0=ALU.is_gt, op1=ALU.mult), R=[R_sm], W=[R_sm])
                                P.op("dve", lambda e: e.scalar_tensor_tensor(out=sm[:, 12:13], in0=sm[:, 12:13], scalar=-dlt, in1=sm[:, 14:15], op0=ALU.add, op1=ALU.add), R=[R_sm], W=[R_sm])
                            P.op("dve", lambda e: e.tensor_scalar(out=negmask[:, 0:L], in0=sb_[:, 0:L], scalar1=sm[:, 12:13], scalar2=NEG, op0=ALU.is_le, op1=ALU.mult), R=[Rsb, R_sm], W=[R_negm])
                        for i0 in range(0, j + 1, 8):
                            n_ = min(8, j + 1 - i0)
                            for ii in range(n_):
                                P.pe(lambda e: e.transpose(out=psT[:, ii, :], in_=negmask[:, (i0 + ii) * 128:(i0 + ii + 1) * 128], identity=ident_b[:]), R=[R_negm, R_const], W=[R_psT])
                            P.op("act", lambda e: e.activation(out=nmT[:, i0:i0 + n_, a * 128:(a + 1) * 128], in_=psT[:, 0:n_, :], func=AF.Copy), R=[R_psT], W=[R_nmT])

                    for J in range(4):
                        for a in range(4):
                            j = 4 * J + a
                            if 2 <= j + 1 < 16:
                                indexer(j + 1)
                            select(j)
                        for h in range(4):
                            g = h // 2
                            r0 = (h % 2) * 64
                            sig = SIG_C[h]

                            def colr(i, J=J):
                                return (128 * max(0, i - 4 * J), 512)

                            def smm(sci, i, m, c_lo, c_hi, J=J, h=h):
                                P.pe(lambda e: e.matmul(ps[sci][:, c_lo:c_hi], lhsT=QK[h][0:67, i * 128:(i + 1) * 128], rhs=QK[4 + h][0:67, J * 512 + c_lo:J * 512 + c_hi], start=True, stop=False),
                                     R=[R_QK[h], R_QK[4 + h]], W=[R_ps[sci]])
                                if i >= 4 * J:
                                    a = i - 4 * J
                                    P.pe(lambda e: e.matmul(ps[sci][:, a * 128:(a + 1) * 128], lhsT=ident_b[:], rhs=dmask_b[:, 4 + h, :], start=False, stop=False), R=[R_const], W=[R_ps[sci]])
                                P.pe(lambda e: e.matmul(ps[sci][:, c_lo:c_hi], lhsT=ident_b[:], rhs=nmT[:, i, c_lo:c_hi], start=False, stop=True), R=[R_const, R_nmT], W=[R_ps[sci]])
                            attn_block(J, list(range(4 * J + 4)), colr, smm, lambda i, J=J, sig=sig: -sig * (512 * J - 128 * i),
                                       lambda i, g=g: VV[:, i, g * 128:(g + 1) * 128], R_VV, [(0, 1, 0)])
                            P.op("act", lambda e: e.activation(out=ft[0][r0:r0 + 64, :], in_=ps[0][r0:r0 + 64, :], func=AF.Copy), R=[R_ps[0]], W=[R_ft[0]])
                            P.op("dve", lambda e: e.tensor_copy(out=ft[1][r0:r0 + 64, :], in_=ps[1][r0:r0 + 64, :]), R=[R_ps[1]], W=[R_ft[1]])

                            def finB(r0=r0, cc=6 + g, J=J):
                                P.op("dve", lambda e: e.tensor_scalar(out=ft[1][r0:r0 + 64, :], in0=ft[1][r0:r0 + 64, :], scalar1=1e-30, scalar2=None, op0=ALU.max), R=[R_ft[1]], W=[R_ft[1]])
                                P.op("dve", lambda e: e.reciprocal(out=ft[1][r0:r0 + 64, :], in_=ft[1][r0:r0 + 64, :]), R=[R_ft[1]], W=[R_ft[1]])
                                P.op("dve", lambda e: e.tensor_tensor(out=mixT[r0:r0 + 64, cc, J * 512:(J + 1) * 512], in0=ft[0][r0:r0 + 64, :], in1=ft[1][r0:r0 + 64, :], op=ALU.mult),
                                     R=[R_ft[0], R_ft[1]], W=[R_mix[cc][J]])
                            pend.append(finB)
                    while pend:
                        pend.pop(0)()
                    P.barrier()

                if dbg == "mix":
                    allmix = [R_mix[c][j] for c in range(8) for j in range(4)]
                    P.dma("sp", lambda e: e.dma_start(out=dbg_out[b], in_=mixT[:]), R=allmix, W=[R_dbg])
                    P.barrier()
                    continue

                with ExitStack() as es:
                    WL = 3
                    wo_b = sbc(es, "wo_b", [128, 8, D], BF16); R_wo = Res("wo")
                    g1 = sbc(es, "g1", [128, D], F32); b1 = sbc(es, "b1", [128, D], F32); R_gb = Res("gb")
                    wr_f = sbc(es, "wr_f", [128, 8, NE], F32); br_t = sbc(es, "br_t", [128, NE], F32)
                    xt_ = [sbc(es, "xt%d" % i, [128, D], F32) for i in range(WL)]; R_xt = [Res("xt") for _ in range(WL)]
                    vt = [sbc(es, "vt%d" % i, [128, D], F32) for i in range(WL)]; R_vt = [Res("vt") for _ in range(WL)]
                    x1t = [sbc(es, "x1t%d" % i, [128, D], F32) for i in range(WL)]; R_x1t = [Res("x1t") for _ in range(WL)]
                    x1b = [sbc(es, "x1b%d" % i, [128, D], BF16) for i in range(WL)]; R_x1b = [Res("x1b") for _ in range(WL)]
                    x1T = [sbc(es, "x1T%d" % i, [128, 8, 128], F32) for i in range(WL)]; R_x1T = [Res("x1T") for _ in range(WL)]
                    junk = [sbc(es, "junk%d" % i, [128, D], BF16) for i in range(WL)]; R_junk = [Res("junk") for _ in range(WL)]
                    lst = [sbc(es, "lst%d" % i, [128, 12], F32) for i in range(WL)]; R_lst = [Res("lst") for _ in range(WL)]
                    lg = [sbc(es, "lg%d" % i, [128, NE], F32) for i in range(WL)]; R_lg = [Res("lg") for _ in range(WL)]
                    mx8 = [sbc(es, "mx8%d" % i, [128, 8], F32) for i in range(WL)]; R_mx = [Res("mx") for _ in range(WL)]
                    rs = [sbc(es, "rs%d" % i, [128, 16], F32) for i in range(WL)]; R_rs = [Res("rs") for _ in range(WL)]
                    maskb = [sbc(es, "maskb%d" % i, [128, NE], BF16) for i in range(WL)]; R_maskb = [Res("maskb") for _ in range(WL)]
                    slotm = [sbc(es, "slotm%d" % i, [128, NE], F32) for i in range(WL)]; R_slotm = [Res("slotm") for _ in range(WL)]
                    oh = [sbc(es, "oh%d" % i, [128, NE], F32) for i in range(WL)]; R_oh = [Res("oh") for _ in range(WL)]
                    R_p6 = [R_ps[6]] * WL
                    for c in range(8):
                        P.dma("pool", lambda e: e.dma_start(out=wo_b[:, c, :], in_=w_out[l, c * 128:(c + 1) * 128, :]), W=[R_wo])
                    P.dma("sp", lambda e: e.dma_start(out=g1[:], in_=ln1g[l:l + 1, :].to_broadcast([128, D])), W=[R_gb])
                    P.dma("sp", lambda e: e.dma_start(out=b1[:], in_=ln1b[l:l + 1, :].to_broadcast([128, D])), W=[R_gb])
                    P.dma("sp", lambda e: e.dma_start(out=wr_f[:], in_=w_rt[l].rearrange("(k p) c -> p k c", p=128)), W=[R_gb])
                    P.dma("sp", lambda e: e.dma_start(out=br_t[:], in_=b_rt[l:l + 1, :].to_broadcast([128, NE])), W=[R_gb])
                    P.op("dve", lambda e: e.memset(lst[0][:, 0:1], 0.0), R=[R_ps[6]], W=[R_lst[0]])

                    def ln1_tile(tt):
                        p = tt % WL
                        gt_ = b * 16 + tt
                        rows = slice(tok0 + tt * 128, tok0 + (tt + 1) * 128)
                        c6 = p * 128
                        P.dma("sp", lambda e: e.dma_start(out=xt_[p][:], in_=xsrc[rows, :]), R=[R_xsrc], W=[R_xt[p]])
                        yield
                        for half in range(2):
                            pi = p
                            for c in range(8):
                                P.pe(lambda e: e.matmul(ps[pi][:, :], lhsT=mixT[:, c, tt * 128:(tt + 1) * 128], rhs=wo_b[:, c, half * 512:(half + 1) * 512], start=(c == 0), stop=(c == 7)),
                                     R=[R_mix[c][tt // 4], R_wo], W=[R_ps[pi]])
                            P.op("dve", lambda e: e.scalar_tensor_tensor(out=vt[p][:, half * 512:(half + 1) * 512], in0=xt_[p][:, half * 512:(half + 1) * 512], scalar=ALPHA, in1=ps[pi][:, :], op0=ALU.mult, op1=ALU.add, accum_out=lst[p][:, 8 + half:9 + half]),
                                 R=[R_xt[p], R_ps[pi]], W=[R_vt[p], R_lst[p]])
                            yield
                        P.op("dve", lambda e: e.tensor_tensor(out=lst[p][:, 0:1], in0=lst[p][:, 8:9], in1=lst[p][:, 9:10], op=ALU.add), R=[R_lst[p]], W=[R_lst[p]])
                        yield
                        yield from ln_gen(vt[p], R_vt[p], g1, b1, R_gb, x1t[p], R_x1t[p], lst[p], R_lst[p], junk[p], R_junk[p])
                        if dbg == "x1":
                            P.dma("sp", lambda e: e.dma_start(out=dbg_out[rows, :], in_=x1t[p][:]), R=[R_x1t[p]], W=[R_dbg])
                        P.dma("sp", lambda e: e.dma_start(out=x1s[rows, :], in_=x1t[p][:]), R=[R_x1t[p]], W=[R_x1s])
                        P.op("act", lambda e: e.activation(out=x1b[p][:], in_=x1t[p][:], func=AF.Copy), R=[R_x1t[p]], W=[R_x1b[p]])
                        yield
                        for hf in range(2):
                            for k4 in range(4):
                                k = hf * 4 + k4
                                P.pe(lambda e: e.transpose(out=ps[3 + p][:, k4 * 128:(k4 + 1) * 128], in_=x1t[p][:, k * 128:(k + 1) * 128], identity=ident_f[:]), R=[R_x1t[p], R_const], W=[R_ps[3 + p]])
                            P.op("act", lambda e: e.activation(out=x1T[p][:, hf * 4:(hf + 1) * 4, :], in_=ps[3 + p][:, :].rearrange("p (a b) -> p a b", a=4), func=AF.Copy), R=[R_ps[3 + p]], W=[R_x1T[p]])
                            yield
                        for k in range(8):
                            P.pe(lambda e: e.matmul(ps[6][:, c6:c6 + NE], lhsT=x1T[p][:, k, :], rhs=wr_f[:, k, :], start=(k == 0), stop=(k == 7)), R=[R_x1T[p], R_gb], W=[R_p6[p]])
                        P.op("dve", lambda e: e.tensor_tensor(out=lg[p][:], in0=ps[6][:, c6:c6 + NE], in1=br_t[:], op=ALU.add), R=[R_p6[p], R_gb], W=[R_lg[p]])
                        yield
                        P.op("dve", lambda e: e.max(out=mx8[p][:], in_=lg[p][:]), R=[R_lg[p]], W=[R_mx[p]])
                        yield
                        P.op("dve", lambda e: e.tensor_scalar(out=rs[p][:, 0:1], in0=mx8[p][:, 0:1], scalar1=-1.0, scalar2=None, op0=ALU.mult), R=[R_mx[p]], W=[R_rs[p]])
                        P.op("dve", lambda e: e.tensor_scalar(out=maskb[p][:], in0=lg[p][:], scalar1=mx8[p][:, 3:4], scalar2=None, op0=ALU.is_ge), R=[R_lg[p], R_mx[p]], W=[R_maskb[p]])
                        yield
                        P.op("act", lambda e: e.activation(out=rs[p][:, 4:8], in_=mx8[p][:, 0:4], func=AF.Exp, bias=rs[p][:, 0:1], scale=1.0, accum_out=rs[p][:, 1:2]), R=[R_mx[p], R_rs[p]], W=[R_rs[p]])
                        P.pe(lambda e: e.matmul(ps[6][:, c6 + 32:c6 + 64], lhsT=ltri_b[:], rhs=maskb[p][:], start=True, stop=True), R=[R_maskb[p], R_const], W=[R_p6[p]])
                        P.pe(lambda e: e.matmul(ps[6][:, c6 + 64:c6 + 96], lhsT=ones_b[:], rhs=maskb[p][:], start=True, stop=True), R=[R_maskb[p], R_const], W=[R_p6[p]])
                        yield
                        P.op("dve", lambda e: e.tensor_tensor(out=slotm[p][:], in0=ps[6][:, c6 + 32:c6 + 64], in1=base_cnt[:], op=ALU.add), R=[R_p6[p], R_base], W=[R_slotm[p]])
                        P.op("dve", lambda e: e.tensor_tensor(out=base_cnt[:], in0=ps[6][:, c6 + 64:c6 + 96], in1=base_cnt[:], op=ALU.add), R=[R_p6[p], R_base], W=[R_base])
                        yield
                        P.op("dve", lambda e: e.tensor_tensor(out=slotm[p][:], in0=slotm[p][:], in1=eoff[:], op=ALU.add), R=[R_slotm[p], R_const], W=[R_slotm[p]])
                        P.op("dve", lambda e: e.reciprocal(out=rs[p][:, 2:3], in_=rs[p][:, 1:2]), R=[R_rs[p]], W=[R_rs[p]])
                        yield
                        P.op("dve", lambda e: e.tensor_scalar(out=gates[:, gt_ * 4:gt_ * 4 + 4], in0=rs[p][:, 4:8], scalar1=rs[p][:, 2:3], scalar2=None, op0=ALU.mult), R=[R_rs[p]], W=[R_gates])
                        yield
                        for k in range(4):
                            P.op("dve", lambda e: e.scalar_tensor_tensor(out=oh[p][:], in0=lg[p][:], scalar=mx8[p][:, k:k + 1], in1=slotm[p][:], op0=ALU.is_equal, op1=ALU.mult, accum_out=slotf[:, gt_ * 4 + k:gt_ * 4 + k + 1]),
                                 R=[R_lg[p], R_mx[p], R_slotm[p]], W=[R_oh[p], R_slot])
                            yield
                        P.op("dve", lambda e: e.tensor_copy(out=sloti[:, gt_ * 4:gt_ * 4 + 4], in_=slotf[:, gt_ * 4:gt_ * 4 + 4]), R=[R_slot], W=[R_slot])
                        yield
                        for k in range(4):
                            P.dma("pool", lambda e: e.indirect_dma_start(out=xg[:, :], out_offset=bass.IndirectOffsetOnAxis(ap=sloti[:, gt_ * 4 + k:gt_ * 4 + k + 1], axis=0), in_=x1b[p][:, :], in_offset=None),
                                  R=[R_x1b[p], R_slot], W=[R_xg])
                        yield

                    run_window([ln1_tile(tt) for tt in range(16)], WL)
                    P.barrier()
        if dbg in ("mix", "x1"):
            break

        with ExitStack() as es:
            def sbm(name, shape, dt):
                return es.enter_context(nc.sbuf_tensor(name + "_m%d" % l, list(shape), dt))
            wgu_b = [sbm("wgu%d" % i, [128, 8, 2 * D], BF16) for i in range(2)]; R_wgu = [[Res("wgu") for _ in range(8)] for _ in range(2)]
            wdn_b = [sbm("wdn%d" % i, [128, 8, D], BF16) for i in range(2)]; R_wdn = [[Res("wdn") for _ in range(8)] for _ in range(2)]
            xr = [sbm("xr%d" % i, [128, D], BF16) for i in range(4)]; R_xr = [Res("xr") for _ in range(4)]
            xeT = [sbm("xeT%d" % i, [128, 8, CAP], BF16) for i in range(2)]; R_xeT = [Res("xeT") for _ in range(2)]
            GT = sbm("GT", [128, 8, CAP], BF16); R_GT = [Res("GT%d" % c) for c in range(8)]
            et = [sbm("et%d" % i, [128, 512], F32) for i in range(9)]; R_et = [Res("et") for _ in range(9)]
            yt = [sbm("yt%d" % i, [128, D], F32) for i in range(3)]; R_yt = [Res("yt") for _ in range(3)]
            bgT = sbm("bgT", [128, NE * 16], F32); R_bgT = Res("bgT")
            bgs = sbm("bgs", [128, 4, 128], F32); R_bgs = Res("bgs")
            bdb = [sbm("bdb%d" % i, [128, D], F32) for i in range(2)]; R_bdb = [Res("bdb") for _ in range(2)]
            P.dma("sp", lambda e: e.dma_start(out=bgs[:], in_=b_gu[l].rearrange("(a r) p -> r a p", r=128)), W=[R_bgs])
            for a in range(4):
                P.pe(lambda e: e.transpose(out=ps[6][:, a * 128:(a + 1) * 128], in_=bgs[:, a, :], identity=ident_f[:]), R=[R_bgs, R_const], W=[R_ps[6]])
            P.op("dve", lambda e: e.tensor_copy(out=bgT[:], in_=ps[6][:, :]), R=[R_ps[6]], W=[R_bgT])

            def load_expert(e_):
                s2 = e_ % 2
                for k in range(8):
                    P.dma("pool", lambda e: e.dma_start(out=wgu_b[s2][:, k, :], in_=w_gu[l, e_, k * 128:(k + 1) * 128, :]), W=[R_wgu[s2][k]])
                for k in range(8):
                    P.dma("pool", lambda e: e.dma_start(out=wdn_b[s2][:, k, :], in_=w_dn[l, e_, k * 128:(k + 1) * 128, :]), W=[R_wdn[s2][k]])
                P.dma("sp", lambda e: e.dma_start(out=bdb[s2][:], in_=b_dn[l, e_:e_ + 1, :].to_broadcast([128, D])), W=[R_bdb[s2]])

            def build_x(e_):
                s2 = e_ % 2
                for st_ in range(CAP // 128):
                    xi = nxt("xr", 4)
                    P.dma("sp", lambda e: e.dma_start(out=xr[xi][:], in_=xg[e_ * CAP + st_ * 128:e_ * CAP + (st_ + 1) * 128, :]), R=[R_xg], W=[R_xr[xi]])
                    tv, R_tv = (psT, R_psT) if st_ % 2 == 0 else (psT2, R_ps[6])
                    for k in range(8):
                        P.pe(lambda e: e.transpose(out=tv[:, k, :], in_=xr[xi][:, k * 128:(k + 1) * 128], identity=ident_b[:]), R=[R_xr[xi], R_const], W=[R_tv])
                    P.op("act", lambda e: e.activation(out=xeT[s2][:, :, st_ * 128:(st_ + 1) * 128], in_=tv[:, :, :], func=AF.Copy), R=[R_tv], W=[R_xeT[s2]])

            load_expert(0)
            build_x(0)
            for e_ in range(NE):
                s2 = e_ % 2
                if e_ + 1 < NE:
                    load_expert(e_ + 1)
                for c in range(8):
                    for (s0, sw) in ((0, 512), (512, CAP - 512)):
                        pg = nxt("pg", 2) * 2
                        for k in range(8):
                            P.pe(lambda e: e.matmul(ps[pg][:, 0:sw], lhsT=wgu_b[s2][:, k, c * 128:(c + 1) * 128], rhs=xeT[s2][:, k, s0:s0 + sw], start=(k == 0), stop=(k == 7)),
                                 R=[R_wgu[s2][k], R_xeT[s2]], W=[R_ps[pg]])
                        for k in range(8):
                            P.pe(lambda e: e.matmul(ps[pg + 1][:, 0:sw], lhsT=wgu_b[s2][:, k, D + c * 128:D + (c + 1) * 128], rhs=xeT[s2][:, k, s0:s0 + sw], start=(k == 0), stop=(k == 7)),
                                 R=[R_wgu[s2][k], R_xeT[s2]], W=[R_ps[pg + 1]])
                        ei = nxt("et", 3) * 3
                        bgc = bgT[:, e_ * 16 + c:e_ * 16 + c + 1]
                        buc = bgT[:, e_ * 16 + 8 + c:e_ * 16 + 8 + c + 1]
                        P.op("dve", lambda e: e.tensor_scalar(out=et[ei][:, 0:sw], in0=ps[pg][:, 0:sw], scalar1=bgc, scalar2=7.0, op0=ALU.add, op1=ALU.min), R=[R_ps[pg], R_bgT], W=[R_et[ei]])
                        P.op("act", lambda e: e.activation(out=et[ei + 1][:, 0:sw], in_=et[ei][:, 0:sw], func=AF.Sigmoid, scale=1.702), R=[R_et[ei]], W=[R_et[ei + 1]])
                        P.op("act", lambda e: e.activation(out=et[ei + 2][:, 0:sw], in_=ps[pg + 1][:, 0:sw], func=AF.Identity, bias=buc, scale=1.0), R=[R_ps[pg + 1], R_bgT], W=[R_et[ei + 2]])
                        P.op("dve", lambda e: e.tensor_scalar(out=et[ei + 2][:, 0:sw], in0=et[ei + 2][:, 0:sw], scalar1=7.0, scalar2=-7.0, op0=ALU.min, op1=ALU.max), R=[R_et[ei + 2]], W=[R_et[ei + 2]])
                        P.op("dve", lambda e: e.tensor_tensor(out=et[ei][:, 0:sw], in0=et[ei][:, 0:sw], in1=et[ei + 1][:, 0:sw], op=ALU.mult), R=[R_et[ei], R_et[ei + 1]], W=[R_et[ei]])
                        P.op("dve", lambda e: e.scalar_tensor_tensor(out=GT[:, c, s0:s0 + sw], in0=et[ei + 2][:, 0:sw], scalar=1.0, in1=et[ei][:, 0:sw], op0=ALU.add, op1=ALU.mult), R=[R_et[ei], R_et[ei + 2]], W=[R_GT[c]])
                if e_ + 1 < NE:
                    build_x(e_ + 1)
                for st_ in range(CAP // 128):
                    yi = nxt("yt", 3)
                    for half in range(2):
                        pi = 4 + nxt("pd", 2)
                        for c in range(8):
                            P.pe(lambda e: e.matmul(ps[pi][:, :], lhsT=GT[:, c, st_ * 128:(st_ + 1) * 128], rhs=wdn_b[s2][:, c, half * 512:(half + 1) * 512], start=(c == 0), stop=(c == 7)),
                                 R=[R_GT[c], R_wdn[s2][c]], W=[R_ps[pi]])
                        P.op("dve", lambda e: e.tensor_tensor(out=yt[yi][:, half * 512:(half + 1) * 512], in0=ps[pi][:, :], in1=bdb[s2][:, half * 512:(half + 1) * 512], op=ALU.add),
                             R=[R_ps[pi], R_bdb[s2]], W=[R_yt[yi]])
                    P.dma("sp", lambda e: e.dma_start(out=yg[e_ * CAP + st_ * 128:e_ * CAP + (st_ + 1) * 128, :], in_=yt[yi][:]), R=[R_yt[yi]], W=[R_yg])
            P.barrier()

        with ExitStack() as es:
            def sbm(name, shape, dt):
                return es.enter_context(nc.sbuf_tensor(name + "_c%d" % l, list(shape), dt))
            WC = 3
            yk = [sbm("yk%d" % i, [128, D], F32) for i in range(4 * WC)]; R_yk = [Res("yk") for _ in range(4 * WC)]
            xa = [sbm("xa%d" % i, [128, D], F32) for i in range(WC)]; R_xa = [Res("xa") for _ in range(WC)]
            xo = [sbm("xo%d" % i, [128, D], F32) for i in range(WC)]; R_xo = [Res("xo") for _ in range(WC)]
            g2 = sbm("g2", [128, D], F32); b2 = sbm("b2", [128, D], F32); R_gb2 = Res("gb2")
            junk2 = [sbm("junk2_%d" % i, [128, D], BF16) for i in range(WC)]; R_junk2 = [Res("junk2") for _ in range(WC)]
            lst2 = [sbm("lst2_%d" % i, [128, 12], F32) for i in range(WC)]; R_lst2 = [Res("lst2") for _ in range(WC)]
            P.dma("sp", lambda e: e.dma_start(out=g2[:], in_=ln2g[l:l + 1, :].to_broadcast([128, D])), W=[R_gb2])
            P.dma("sp", lambda e: e.dma_start(out=b2[:], in_=ln2b[l:l + 1, :].to_broadcast([128, D])), W=[R_gb2])

            def comb_tile(tt):
                p = tt % WC
                rows = slice(tt * 128, (tt + 1) * 128)
                P.dma("sp", lambda e: e.dma_start(out=xa[p][:], in_=x1s[rows, :]), R=[R_x1s], W=[R_xa[p]])
                for k in range(4):
                    yi = p * 4 + k
                    P.dma("pool", lambda e: e.indirect_dma_start(out=yk[yi][:, :], out_offset=None, in_=yg[:, :], in_offset=bass.IndirectOffsetOnAxis(ap=sloti[:, tt * 4 + k:tt * 4 + k + 1], axis=0)),
                          R=[R_yg, R_slot], W=[R_yk[yi]])
                yield
                P.op("act", lambda e: e.activation(out=xa[p][:], in_=xa[p][:], func=AF.Copy, scale=ALPHA), R=[R_xa[p]], W=[R_xa[p]])
                yield
                for k in range(4):
                    yi = p * 4 + k
                    if k < 3:
                        P.op("dve", lambda e: e.scalar_tensor_tensor(out=xa[p][:], in0=yk[yi][:], scalar=gates[:, tt * 4 + k:tt * 4 + k + 1], in1=xa[p][:], op0=ALU.mult, op1=ALU.add),
                             R=[R_yk[yi], R_gates, R_xa[p]], W=[R_xa[p]])
                    else:
                        P.op("dve", lambda e: e.scalar_tensor_tensor(out=xa[p][:], in0=yk[yi][:], scalar=gates[:, tt * 4 + k:tt * 4 + k + 1], in1=xa[p][:], op0=ALU.mult, op1=ALU.add, accum_out=lst2[p][:, 0:1]),
                             R=[R_yk[yi], R_gates, R_xa[p]], W=[R_xa[p], R_lst2[p]])
                    yield
                yield from ln_gen(xa[p], R_xa[p], g2, b2, R_gb2, xo[p], R_xo[p], lst2[p], R_lst2[p], junk2[p], R_junk2[p])
                P.dma("sp", lambda e: e.dma_start(out=xdst[rows, :], in_=xo[p][:]), R=[R_xo[p]], W=[R_xdst])
                yield

            run_window([comb_tile(tt) for tt in range(32)], WC)
            P.barrier()

    P.finish()
    return nc, P


def _consts():
    bf = ml_dtypes.bfloat16
    ident = np.eye(128, dtype=np.float32)
    ltri = np.triu(np.ones((128, 128), np.float32), 1)
    si = np.arange(128)[:, None]; qi = np.arange(128)[None, :]
    dmask = np.zeros((128, 8, 128), np.float32)
    for h in range(8):
        sg = SIG8[h]
        d = np.where(si <= qi, 0.0, np.where((si // 64) == (qi // 64), -2.0 * sg * (si - qi), NEG))
        dmask[:, h, :] = d
    q = np.arange(S)
    qaug = np.stack([np.ones(S), -(q % 128).astype(np.float64), -(128.0 * ((q // 128) % 4))]).astype(np.float32)
    kaug = np.zeros((8, 3, S), np.float32)
    for h in range(8):
        kaug[h, 0] = SIG8[h] * (q % 128)
        kaug[h, 1] = SIG8[h]
        kaug[h, 2] = SIG8[h]
    eoff = np.tile((np.arange(NE, dtype=np.float32) * CAP)[None, :], (128, 1))
    return {"c_ident": ident.astype(bf), "c_identf": ident, "c_ltri": ltri.astype(bf), "c_dmask": dmask.astype(bf),
            "c_qaug": qaug.astype(bf), "c_kaug": kaug.astype(bf), "c_eoff": eoff}


def _relb_pieces(rel_bias):
    NLn = rel_bias.shape[0]
    si = np.arange(128)[:, None]; qi = np.arange(128)[None, :]
    out = np.empty((NLn, 128, 16, 128), np.float32)
    for h in range(4):
        rel0 = qi - si
        idx0 = np.clip(rel0, -128, 128) + 128
        m0 = ((si // 64) == 1) & ((qi // 64) == 0)
        idx1 = np.clip(128 + qi - si, -128, 128) + 128
        m4 = ((si // 64) == 0) & ((qi // 64) == 1)
        for ln in range(NLn):
            rb = rel_bias[ln, h]
            p0 = rb[idx0].copy(); p0[m0] = NEG
            p1 = rb[idx1]
            p2 = np.broadcast_to(rb[256], (128, 128))
            p4 = np.array(p2); p4[m4] = NEG
            out[ln, :, h * 4 + 0, :] = p0
            out[ln, :, h * 4 + 1, :] = p1
            out[ln, :, h * 4 + 2, :] = p2
            out[ln, :, h * 4 + 3, :] = p4
    return out


_CACHE = {}


def _get_prog(NL, lam_inits, dbg=None):
    key = (NL, tuple(lam_inits), dbg)
    if key not in _CACHE:
        _CACHE[key] = build(NL, lam_inits, dbg)[0]
    return _CACHE[key]


def _layer_inputs(inp, ls):
    f = lambda a: np.ascontiguousarray(a, dtype=np.float32)
    d = {
        "w_in": f(inp["w_in"][ls]),
        "lamv": f(np.stack([inp["lam_q1"][ls], inp["lam_k1"][ls], inp["lam_q2"][ls], inp["lam_k2"][ls]], axis=1)),
        "subln_g": f(inp["subln_g"][ls]),
        "relb": _relb_pieces(np.asarray(inp["rel_bias"][ls], np.float32)),
        "w_out": f(inp["w_out"][ls]),
        "ln1_g": f(inp["ln1_g"][ls]), "ln1_b": f(inp["ln1_b"][ls]),
        "w_router": f(inp["w_router"][ls]), "b_router": f(inp["b_router"][ls]),
        "w_gu": f(inp["w_gu"][ls]), "b_gu": f(inp["b_gu"][ls]).reshape(len(range(*ls.indices(DEPTH))), NE * 16, 128),
        "w_down": f(inp["w_down"][ls]), "b_down": f(inp["b_down"][ls]),
        "ln2_g": f(inp["ln2_g"][ls]), "ln2_b": f(inp["ln2_b"][ls]),
    }
    return d


FUSED = True


def kernel(**inp):
    x = np.ascontiguousarray(inp["x"], dtype=np.float32)
    consts = _consts()
    lam_inits_all = [0.8 - 0.6 * math.exp(-0.3 * l) for l in range(DEPTH)]
    xs = [x[c * NBL:(c + 1) * NBL].reshape(T, D) for c in range(NCORES)]
    if FUSED:
        nc = _get_prog(DEPTH, lam_inits_all)
        li = _layer_inputs(inp, slice(0, DEPTH))
        in_maps = [dict(li, x=xs[c], **consts) for c in range(NCORES)]
        res = run_bass_kernel_spmd(nc, in_maps, core_ids=list(range(NCORES)))
        xs = [res.results[c]["y"] for c in range(NCORES)]
    else:
        for l in range(DEPTH):
            nc = _get_prog(1, [lam_inits_all[l]])
            li = _layer_inputs(inp, slice(l, l + 1))
            in_maps = [dict(li, x=xs[c], **consts) for c in range(NCORES)]
            res = run_bass_kernel_spmd(nc, in_maps, core_ids=list(range(NCORES)))
            xs = [np.asarray(res.results[c]["y"]) for c in range(NCORES)]
    out = np.stack([xs[c].reshape(NBL, S, D) for c in range(NCORES)], axis=0).reshape(NCORES * NBL, S, D)
    return out.astype(np.float32)
```

```python
import math
from contextlib import ExitStack
import numpy as np
import ml_dtypes
import concourse.bass as bass
import concourse.mybir as mybir
from concourse.bass_utils import run_bass_kernel_spmd

F32 = mybir.dt.float32
BF16 = mybir.dt.bfloat16
I32 = mybir.dt.int32
AF = mybir.ActivationFunctionType
ALU = mybir.AluOpType
AX = mybir.AxisListType

NCORES = 8
DEPTH = 4
S = 2048
D = 1024
NBL = 2
T = NBL * S
DIN = 3368
NE = 32
CAP = 768
NSLOT = NE * CAP
ALPHA = (2 * DEPTH) ** 0.25
EPS = 1e-5
NEG = -30000.0
SIG_A = [2.0 ** -1, 2.0 ** -3, 2.0 ** -5, 2.0 ** -7]
SIG_C = [2.0 ** -2, 2.0 ** -4, 2.0 ** -6, 2.0 ** -8]
SIG8 = SIG_A + SIG_C
W_SCALE = (8 ** -0.5) * (32 ** -0.5)
NIT = 12
C_QA, C_KA, C_VA, C_QB, C_KB, C_VB, C_QC, C_KC, C_VC, C_QI, C_KI, C_WI = (
    0, 512, 1024, 1536, 1792, 2048, 2304, 2560, 2816, 3072, 3328, 3360)


class Res:
    __slots__ = ("name", "w", "r", "multi")

    def __init__(self, name, multi=False):
        self.name = name
        self.w = {}
        self.r = {}
        self.multi = multi


class Prog:
    def __init__(self, nc, ndma=(("sp", 40), ("pool", 40), ("act", 8))):
        self.nc = nc
        self.E = {"pe": nc.tensor, "act": nc.scalar, "dve": nc.vector, "pool": nc.gpsimd, "sp": nc.sync}
        self.esem = {k: nc.alloc_semaphore("e_" + k) for k in self.E}
        self.ecnt = {k: 0 for k in self.E}
        self.waited = {k: {} for k in self.E}
        self.dsem = {}
        for k, n in ndma:
            self.dsem[k] = [[nc.alloc_semaphore("d_%s%d" % (k, i)), 0, "d_%s%d" % (k, i)] for i in range(n)]
        self.dnext = {k: 0 for k in self.dsem}
        self.nins = 0

    def _wait(self, eng, ev):
        key, sem, val = ev
        if self.waited[eng].get(key, 0) >= val:
            return
        self.E[eng].wait_ge(sem, val)
        self.waited[eng][key] = val
        self.nins += 1

    def _deps(self, eng, R, W, skip_self):
        for r in R:
            for ev in r.w.values():
                if not (skip_self and ev[0] == eng):
                    self._wait(eng, ev)
        for w in W:
            if not w.multi:
                for ev in w.w.values():
                    if not (skip_self and ev[0] == eng):
                        self._wait(eng, ev)
            for ev in w.r.values():
                if not (skip_self and ev[0] == eng):
                    self._wait(eng, ev)

    def _record(self, ev, R, W):
        for r in R:
            r.r[ev[0]] = ev
        for w in W:
            if w.multi:
                w.w[ev[0]] = ev
            else:
                w.w = {ev[0]: ev}
            w.r = {}

    def op(self, eng, fn, R=(), W=(), skip_self=False):
        self._deps(eng, R, W, skip_self)
        ins = fn(self.E[eng])
        self.ecnt[eng] += 1
        ins.then_inc(self.esem[eng], 1)
        ev = (eng, self.esem[eng], self.ecnt[eng])
        self._record(ev, R, W)
        self.nins += 1
        return ev

    def pe(self, fn, R=(), W=()):
        return self.op("pe", fn, R, W, skip_self=True)

    def dma(self, eng, fn, R=(), W=()):
        self._deps(eng, R, W, False)
        pool = self.dsem[eng]
        slot = pool[self.dnext[eng] % len(pool)]
        self.dnext[eng] += 1
        if slot[1] > 0:
            self._wait(eng, (slot[2], slot[0], slot[1]))
        ins = fn(self.E[eng])
        slot[1] += 16
        ins.then_inc(slot[0], 16)
        ev = (slot[2], slot[0], slot[1])
        self._record(ev, R, W)
        self.nins += 1
        return ev

    def barrier(self):
        evs = [(k, self.esem[k], self.ecnt[k]) for k in self.E if self.ecnt[k] > 0]
        for k in self.dsem:
            for s in self.dsem[k]:
                if s[1] > 0:
                    evs.append((s[2], s[0], s[1]))
        for e in self.E:
            for ev in evs:
                self._wait(e, ev)

    def finish(self):
        self.barrier()


def build(NL, lam_inits, dbg=None):
    nc = bass.Bass("TRN2", target_bir_lowering=False)
    P = Prog(nc)

    def din(name, shape, dt=F32):
        return nc.dram_tensor(name, list(shape), dt, kind="ExternalInput").ap()

    x_in = din("x", [T, D])
    w_in = din("w_in", [NL, D, DIN])
    lamv = din("lamv", [NL, 4, 64])
    subg = din("subln_g", [NL, 128])
    relb = din("relb", [NL, 128, 16, 128])
    w_out = din("w_out", [NL, D, D])
    ln1g = din("ln1_g", [NL, D]); ln1b = din("ln1_b", [NL, D])
    w_rt = din("w_router", [NL, D, NE]); b_rt = din("b_router", [NL, NE])
    if dbg is None or dbg == "moe":
        w_gu = din("w_gu", [NL, NE, D, 2 * D]); w_dn = din("w_down", [NL, NE, D, D])
    b_gu = din("b_gu", [NL, NE * 16, 128]); b_dn = din("b_down", [NL, NE, D])
    ln2g = din("ln2_g", [NL, D]); ln2b = din("ln2_b", [NL, D])
    c_ident = din("c_ident", [128, 128], BF16)
    c_identf = din("c_identf", [128, 128], F32)
    c_ltri = din("c_ltri", [128, 128], BF16)
    c_dmask = din("c_dmask", [128, 8, 128], BF16)
    c_qaug = din("c_qaug", [3, S], BF16)
    c_kaug = din("c_kaug", [8, 3, S], BF16)
    c_eoff = din("c_eoff", [128, NE], F32)
    y_out = nc.dram_tensor("y", [T, D], F32, kind="ExternalOutput").ap()
    dbg_out = None
    if dbg == "mix":
        dbg_out = nc.dram_tensor("dbg", [NBL, 128, 8, S], BF16, kind="ExternalOutput").ap()
    if dbg == "x1":
        dbg_out = nc.dram_tensor("dbg", [T, D], F32, kind="ExternalOutput").ap()
    xcur = nc.dram_tensor("xcur", [T, D], F32).ap()
    x1s = nc.dram_tensor("x1s", [T, D], F32).ap()
    xg = nc.dram_tensor("xg", [NSLOT, D], BF16).ap()
    yg = nc.dram_tensor("yg", [NSLOT, D], F32).ap()
    R_xin = Res("xin", True); R_xcur = Res("xcur", True); R_x1s = Res("x1s", True)
    R_xg = Res("xg", True); R_yg = Res("yg", True); R_y = Res("y", True); R_dbg = Res("dbg", True)
    R_const = Res("const", True)

    def sb(name, shape, dt):
        return nc.alloc_sbuf_tensor(name, list(shape), dt)

    ident_b = sb("ident_b", [128, 128], BF16)
    ident_f = sb("ident_f", [128, 128], F32)
    ltri_b = sb("ltri_b", [128, 128], BF16)
    ones_b = sb("ones_b", [128, 128], BF16)
    zeros_b = sb("zeros_b", [128, 512], BF16)
    dmask_b = sb("dmask_b", [128, 8, 128], BF16)
    eoff = sb("eoff", [128, NE], F32)
    base_cnt = sb("base_cnt", [128, NE], F32)
    slotf = sb("slotf", [128, 32 * 4], F32)
    sloti = sb("sloti", [128, 32 * 4], I32)
    gates = sb("gates", [128, 32 * 4], F32)
    R_slot = Res("slot"); R_gates = Res("gates"); R_base = Res("base")
    for t_, src in ((ident_b, c_ident), (ident_f, c_identf), (ltri_b, c_ltri), (dmask_b, c_dmask), (eoff, c_eoff)):
        P.dma("sp", lambda e, t_=t_, src=src: e.dma_start(out=t_[:], in_=src), W=[R_const])
    P.op("pool", lambda e: e.memset(ones_b[:], 1.0), W=[R_const])
    P.op("pool", lambda e: e.memset(zeros_b[:], 0.0), W=[R_const])

    ps = [nc.alloc_psum_tensor("ps%d" % i, [128, 512], F32) for i in range(7)]
    R_ps = [Res("ps%d" % i) for i in range(7)]
    psT = nc.alloc_psum_tensor("psT", [128, 8, 128], BF16)
    R_psT = Res("psT")
    psT2 = ps[6][:, :].bitcast(BF16).rearrange("p (a b) -> p a b", a=8)

    rot = {}

    def nxt(key, n):
        rot[key] = (rot.get(key, -1) + 1) % n
        return rot[key]

    def run_window(gen_list, width, stagger=0):
        active = []
        it = iter(gen_list)
        exhausted = False
        since = stagger
        while True:
            if not exhausted and len(active) < width and since >= stagger:
                g_ = next(it, None)
                if g_ is None:
                    exhausted = True
                else:
                    active.append(g_)
                    since = 0
            if not active:
                if exhausted:
                    break
                since = stagger
                continue
            for g_ in list(active):
                try:
                    next(g_)
                except StopIteration:
                    active.remove(g_)
            since += 1

    def ln_gen(v, R_v, g_t, b_t, R_gb, out_t, R_out, st, R_st, junk, R_junk):
        P.op("dve", lambda e: e.tensor_scalar(out=st[:, 1:2], in0=st[:, 0:1], scalar1=-1.0 / D, scalar2=None, op0=ALU.mult), R=[R_st], W=[R_st])
        yield
        P.op("act", lambda e: e.activation(out=junk[:], in_=v[:], func=AF.Square, bias=st[:, 1:2], scale=1.0, accum_out=st[:, 2:3]), R=[R_v, R_st], W=[R_junk, R_st])
        yield
        P.op("dve", lambda e: e.tensor_scalar(out=st[:, 3:4], in0=st[:, 2:3], scalar1=1.0 / D, scalar2=EPS, op0=ALU.mult, op1=ALU.add), R=[R_st], W=[R_st])
        yield
        P.op("act", lambda e: e.activation(out=st[:, 4:5], in_=st[:, 3:4], func=AF.Ln), R=[R_st], W=[R_st])
        P.op("act", lambda e: e.activation(out=st[:, 5:6], in_=st[:, 4:5], func=AF.Exp, scale=-0.5), R=[R_st], W=[R_st])
        yield
        P.op("dve", lambda e: e.tensor_tensor(out=st[:, 6:7], in0=st[:, 1:2], in1=st[:, 5:6], op=ALU.mult), R=[R_st], W=[R_st])
        yield
        P.op("act", lambda e: e.activation(out=v[:], in_=v[:], func=AF.Identity, bias=st[:, 6:7], scale=st[:, 5:6]), R=[R_v, R_st], W=[R_v])
        yield
        P.op("dve", lambda e: e.tensor_tensor(out=v[:], in0=v[:], in1=g_t[:], op=ALU.mult), R=[R_v, R_gb], W=[R_v])
        yield
        P.op("dve", lambda e: e.tensor_tensor(out=out_t[:], in0=v[:], in1=b_t[:], op=ALU.add), R=[R_v, R_gb], W=[R_out])
        yield

    for l in range(NL):
        lam_init = lam_inits[l]
        xsrc, R_xsrc = (x_in, R_xin) if l == 0 else (xcur, R_xcur)
        last = (l == NL - 1)
        xdst, R_xdst = (y_out, R_y) if last else (xcur, R_xcur)
        P.op("pool", lambda e: e.memset(base_cnt[:], 0.0), W=[R_base])

        for b in range(NBL):
            tok0 = b * S
            with ExitStack() as es_b:
                def sbc(es, name, shape, dt):
                    return es.enter_context(nc.sbuf_tensor(name + "_%d_%d" % (l, b), list(shape), dt))
                xT = sbc(es_b, "xT", [128, 8, S], BF16); R_xT = [Res("xT%d" % i) for i in range(4)]
                mixT = sbc(es_b, "mixT", [128, 8, S], BF16)
                R_mix = [[Res("mix%d_%d" % (c, j)) for j in range(4)] for c in range(8)]

                with ExitStack() as es:
                    xbs = [sbc(es, "xbs%d" % i, [128, D], BF16) for i in range(3)]; R_xbs = [Res("xbs") for _ in range(3)]
                    for tt in range(16):
                        i2 = tt % 3
                        P.dma("pool", lambda e: e.dma_start(out=xbs[i2][:], in_=xsrc[tok0 + tt * 128: tok0 + (tt + 1) * 128, :]), R=[R_xsrc], W=[R_xbs[i2]])
                        for k in range(8):
                            P.pe(lambda e: e.transpose(out=psT[:, k, :], in_=xbs[i2][:, k * 128:(k + 1) * 128], identity=ident_b[:]), R=[R_xbs[i2], R_const], W=[R_psT])
                        P.op("dve", lambda e: e.tensor_copy(out=xT[:, :, tt * 128:(tt + 1) * 128], in_=psT[:, :, :]), R=[R_psT], W=[R_xT[tt // 4]])
                    P.barrier()

                with ExitStack() as es:
                    QK = [sbc(es, "qk%d" % i, [128, S], BF16) for i in range(12)]
                    R_QK = [Res("qk%d" % i) for i in range(12)]
                    VV = sbc(es, "vv", [128, 16, 512], BF16); R_VV = Res("vv")
                    wbf = [sbc(es, "wbf%d" % i, [128, 8, 128], BF16) for i in range(4)]; R_wbf = [Res("wbf") for _ in range(4)]
                    PT = [sbc(es, "pt%d" % i, [128, 512], BF16) for i in range(4)]; R_PT = [Res("pt") for _ in range(4)]
                    relb_b = sbc(es, "relb_b", [128, 16, 128], BF16); R_relb = Res("relb")
                    scoreb = [sbc(es, "score%d" % i, [128, S], F32) for i in range(2)]; R_scoreb = [Res("score%d" % i) for i in range(2)]
                    score = scoreb[0]; R_score = R_scoreb[0]
                    relu_sb = [sbc(es, "relu%d" % i, [128, 512], BF16) for i in range(3)]; R_relu = [Res("relu") for _ in range(3)]
                    negmask = sbc(es, "negmask", [128, S], BF16); R_negm = Res("negm")
                    nmT = sbc(es, "nmT", [128, 16, 512], BF16); R_nmT = Res("nmT")
                    diag = [sbc(es, "diag%d" % i, [128, 8, 128], BF16) for i in range(2)]; R_diag = [Res("diag%d" % i) for i in range(2)]
                    wtok = sbc(es, "wtok", [128, 16, 8], F32); R_wtok = Res("wtok")
                    ft = [sbc(es, "ft%d" % i, [128, 512], F32) for i in range(4)]; R_ft = [Res("ft%d" % i) for i in range(4)]
                    sqb = sbc(es, "sqb", [128, 512], BF16); R_sqb = Res("sqb")
                    sm = sbc(es, "sm", [128, 16], F32); R_sm = Res("sm")
                    lamt = sbc(es, "lamt", [128, 4, 64], F32); R_lamt = Res("lamt")
                    gsc = sbc(es, "gsc", [128, 2], F32)

                    P.dma("sp", lambda e: e.dma_start(out=lamt[:], in_=lamv[l:l + 1, :, :].to_broadcast([128, 4, 64])), W=[R_lamt])
                    P.dma("sp", lambda e: e.dma_start(out=gsc[:, 0:1], in_=subg[l, :].rearrange("(p o) -> p o", o=1)), W=[R_sm])
                    P.op("dve", lambda e: e.tensor_tensor(out=lamt[:, 0, :], in0=lamt[:, 0, :], in1=lamt[:, 1, :], op=ALU.mult), R=[R_lamt], W=[R_lamt])
                    P.op("dve", lambda e: e.tensor_tensor(out=lamt[:, 2, :], in0=lamt[:, 2, :], in1=lamt[:, 3, :], op=ALU.mult), R=[R_lamt], W=[R_lamt])
                    P.op("dve", lambda e: e.reduce_sum(out=sm[:, 0:1], in_=lamt[:, 0, :], axis=AX.X), R=[R_lamt], W=[R_sm])
                    P.op("dve", lambda e: e.reduce_sum(out=sm[:, 1:2], in_=lamt[:, 2, :], axis=AX.X), R=[R_lamt], W=[R_sm])
                    P.op("act", lambda e: e.activation(out=sm[:, 2:4], in_=sm[:, 0:2], func=AF.Exp), R=[R_sm], W=[R_sm])
                    P.op("dve", lambda e: e.tensor_tensor(out=sm[:, 4:5], in0=sm[:, 3:4], in1=sm[:, 2:3], op=ALU.subtract), R=[R_sm], W=[R_sm])
                    P.op("dve", lambda e: e.tensor_scalar(out=sm[:, 5:6], in0=sm[:, 4:5], scalar1=-lam_init, scalar2=None, op0=ALU.add), R=[R_sm], W=[R_sm])
                    P.op("dve", lambda e: e.tensor_scalar(out=gsc[:, 1:2], in0=gsc[:, 0:1], scalar1=1.0 - lam_init, scalar2=None, op0=ALU.mult), R=[R_sm], W=[R_sm])
                    neglam = sm[:, 5:6]
                    gscale = gsc[:, 1:2]

                    P.dma("sp", lambda e: e.dma_start(out=score[:, :].rearrange("p (a b) -> p a b", a=16), in_=relb[l]), W=[R_score])
                    P.op("pool", lambda e: e.tensor_copy(out=relb_b[:], in_=score[:, :].rearrange("p (a b) -> p a b", a=16)), R=[R_score], W=[R_relb])

                    def load_w(c0, ncol, rep=1):
                        bi = nxt("wbf", 4)
                        for r in range(rep):
                            P.dma("pool", lambda e: e.dma_start(out=wbf[bi][:, :, r * ncol:(r + 1) * ncol], in_=w_in[l, :, c0:c0 + ncol].rearrange("(k p) c -> p k c", p=128)), W=[R_wbf[bi]])
                        return bi

                    def proj_feat(bi, c_lo, m, dst, R_dst, p_lo, scale):
                        for tb in range(4):
                            pi = 4 + nxt("pj", 2)
                            for k in range(8):
                                P.pe(lambda e: e.matmul(ps[pi][0:m, :], lhsT=wbf[bi][:, k, c_lo:c_lo + m], rhs=xT[:, k, tb * 512:(tb + 1) * 512], start=(k == 0), stop=(k == 7)),
                                     R=[R_wbf[bi], R_xT[tb]], W=[R_ps[pi]])
                            P.op("act", lambda e: e.activation(out=dst[p_lo:p_lo + m, tb * 512:(tb + 1) * 512], in_=ps[pi][p_lo:p_lo + m, :], func=AF.Copy, scale=scale),
                                 R=[R_ps[pi]], W=[R_dst])

                    def proj_feat_split(bi, dst0, R0, dst1, R1, scale):
                        for tb in range(4):
                            pi = 4 + nxt("pj", 2)
                            for k in range(8):
                                P.pe(lambda e: e.matmul(ps[pi][:, :], lhsT=wbf[bi][:, k, :], rhs=xT[:, k, tb * 512:(tb + 1) * 512], start=(k == 0), stop=(k == 7)),
                                     R=[R_wbf[bi], R_xT[tb]], W=[R_ps[pi]])
                            P.op("act", lambda e: e.activation(out=dst0[0:64, tb * 512:(tb + 1) * 512], in_=ps[pi][0:64, :], func=AF.Copy, scale=scale), R=[R_ps[pi]], W=[R0])
                            P.op("act", lambda e: e.activation(out=dst1[64:128, tb * 512:(tb + 1) * 512], in_=ps[pi][64:128, :], func=AF.Copy, scale=scale), R=[R_ps[pi]], W=[R1])

                    def proj_tok(bi, ncol, dst_fn, R_dst, act_eng="act"):
                        for st_ in range(16):
                            pi = 4 + nxt("pj", 2)
                            for k in range(8):
                                P.pe(lambda e: e.matmul(ps[pi][:, 0:ncol], lhsT=xT[:, k, st_ * 128:(st_ + 1) * 128], rhs=wbf[bi][:, k, 0:ncol], start=(k == 0), stop=(k == 7)),
                                     R=[R_wbf[bi], R_xT[st_ // 4]], W=[R_ps[pi]])
                            P.op("act", lambda e: e.activation(out=dst_fn(st_), in_=ps[pi][:, 0:ncol], func=AF.Copy), R=[R_ps[pi]], W=[R_dst])

                    def recip_safe(dst, R_dst, src_ps, R_src, p0, p1):
                        P.op("dve", lambda e: e.tensor_scalar(out=dst[p0:p1, :], in0=src_ps[p0:p1, :], scalar1=1e-30, scalar2=None, op0=ALU.max), R=[R_src], W=[R_dst])
                        P.op("dve", lambda e: e.reciprocal(out=dst[p0:p1, :], in_=dst[p0:p1, :]), R=[R_dst], W=[R_dst])

                    pend = []

                    def attn_block(J, i_list, colrange_fn, score_mms, cfn, Vl_fn, R_V, accs):
                        for (oi, si_, m) in accs:
                            for bi_ in (oi, si_):
                                P.pe(lambda e: e.matmul(ps[bi_][:, :], lhsT=zeros_b[:, 0:128], rhs=zeros_b[:, :], start=True, stop=False), R=[R_const], W=[R_ps[bi_]])
                        n_i = len(i_list)
                        units = [(ii, i, acc) for ii, i in enumerate(i_list) for acc in accs]
                        n_u = len(units)
                        stt = {}

                        def S_(u):
                            ii, i, (oi, si_, m) = units[u]
                            c_lo, c_hi = colrange_fn(i)
                            sci = 4 + nxt("scb", 3)
                            score_mms(sci, i, m, c_lo, c_hi)
                            stt[u] = sci

                        def E_(u):
                            ii, i, (oi, si_, m) = units[u]
                            c_lo, c_hi = colrange_fn(i)
                            sci = stt[u]
                            pti = nxt("pt", 4)
                            P.op("act", lambda e: e.activation(out=PT[pti][:, c_lo:c_hi], in_=ps[sci][:, c_lo:c_hi], func=AF.Exp, bias=float(cfn(i)), scale=1.0),
                                 R=[R_ps[sci]], W=[R_PT[pti]])
                            stt[u] = pti

                        def V_(u):
                            ii, i, (oi, si_, m) = units[u]
                            c_lo, c_hi = colrange_fn(i)
                            pti = stt[u]
                            lastf = (ii == n_i - 1)
                            P.pe(lambda e: e.matmul(ps[oi][:, c_lo:c_hi], lhsT=Vl_fn(i), rhs=PT[pti][:, c_lo:c_hi], start=False, stop=lastf), R=[R_PT[pti], R_V], W=[R_ps[oi]])
                            P.pe(lambda e: e.matmul(ps[si_][:, c_lo:c_hi], lhsT=ones_b[:], rhs=PT[pti][:, c_lo:c_hi], start=False, stop=lastf), R=[R_PT[pti], R_const], W=[R_ps[si_]])
                        S_(0)
                        if n_u > 1:
                            S_(1)
                        for u in range(n_u):
                            E_(u)
                            if u + 2 < n_u:
                                S_(u + 2)
                            V_(u)
                            if pend and (u == min(3, n_u - 1) or u == min(13, n_u - 1)):
                                pend.pop(0)()

                    for g in range(4):
                        bi = load_w(C_VA + g * 128, 128)
                        proj_tok(bi, 128, lambda st_, g=g: VV[:, st_, g * 128:(g + 1) * 128], R_VV)
                    for qi_ in (0, 1):
                        P.dma("sp", lambda e: e.dma_start(out=QK[qi_][64:67, :], in_=c_qaug), W=[R_QK[qi_]])
                    for h in range(4):
                        Q1, Q2, K1, K2 = QK[0], QK[1], QK[2], QK[3]
                        for kt in (2, 3):
                            P.dma("sp", lambda e: e.dma_start(out=QK[kt][64:67, :], in_=c_kaug[h]), W=[R_QK[kt]])
                        bq = load_w(C_QA + h * 128, 128)
                        bk = load_w(C_KA + h * 128, 128)
                        proj_feat(bq, 0, 64, Q1, R_QK[0], 0, 0.125)
                        proj_feat(bq, 64, 64, Q2, R_QK[1], 0, 0.125)
                        proj_feat(bk, 0, 64, K1, R_QK[2], 0, 1.0)
                        proj_feat(bk, 64, 64, K2, R_QK[3], 0, 1.0)
                        sig = SIG_A[h]
                        for J in range(4):
                            def colr(i, J=J):
                                return (128 * max(0, i - 4 * J), 512)

                            def smm(sci, i, m, c_lo, c_hi, J=J, h=h):
                                isd = i >= 4 * J
                                P.pe(lambda e: e.matmul(ps[sci][:, c_lo:c_hi], lhsT=QK[2 + m][0:67, i * 128:(i + 1) * 128], rhs=QK[m][0:67, J * 512 + c_lo:J * 512 + c_hi], start=True, stop=not isd),
                                     R=[R_QK[2 + m], R_QK[m]], W=[R_ps[sci]])
                                if isd:
                                    a = i - 4 * J
                                    P.pe(lambda e: e.matmul(ps[sci][:, a * 128:(a + 1) * 128], lhsT=ident_b[:], rhs=dmask_b[:, h, :], start=False, stop=True), R=[R_const], W=[R_ps[sci]])
                            attn_block(J, list(range(4 * J + 4)), colr, smm, lambda i, J=J: -sig * (512 * J - 128 * i),
                                       lambda i, h=h: VV[:, i, h * 128:(h + 1) * 128], R_VV, [(0, 1, 0), (2, 3, 1)])
                            P.op("act", lambda e: e.activation(out=ft[0][:], in_=ps[0][:, :], func=AF.Copy), R=[R_ps[0]], W=[R_ft[0]])
                            P.op("dve", lambda e: e.tensor_copy(out=ft[1][:], in_=ps[1][:, :]), R=[R_ps[1]], W=[R_ft[1]])
                            P.op("act", lambda e: e.activation(out=ft[2][:], in_=ps[2][:, :], func=AF.Copy), R=[R_ps[2]], W=[R_ft[2]])
                            P.op("dve", lambda e: e.tensor_copy(out=ft[3][:], in_=ps[3][:, :]), R=[R_ps[3]], W=[R_ft[3]])

                            def fin1(h=h, J=J):
                                for (oi_, si2) in ((0, 1), (2, 3)):
                                    P.op("dve", lambda e: e.tensor_scalar(out=ft[si2][:], in0=ft[si2][:], scalar1=1e-30, scalar2=None, op0=ALU.max), R=[R_ft[si2]], W=[R_ft[si2]])
                                    P.op("dve", lambda e: e.reciprocal(out=ft[si2][:], in_=ft[si2][:]), R=[R_ft[si2]], W=[R_ft[si2]])
                                    P.op("dve", lambda e: e.tensor_tensor(out=ft[oi_][:], in0=ft[oi_][:], in1=ft[si2][:], op=ALU.mult), R=[R_ft[oi_], R_ft[si2]], W=[R_ft[oi_]])
                                P.op("dve", lambda e: e.scalar_tensor_tensor(out=ft[2][:], in0=ft[2][:], scalar=neglam, in1=ft[0][:], op0=ALU.mult, op1=ALU.add), R=[R_ft[0], R_ft[2], R_sm], W=[R_ft[2]])
                                P.op("pool", lambda e: e.tensor_tensor(out=sqb[:], in0=ft[2][:], in1=ft[2][:], op=ALU.mult), R=[R_ft[2]], W=[R_sqb])

                            def fin2(h=h, J=J):
                                tb_ = 4 + nxt("scb", 3)
                                P.pe(lambda e: e.matmul(ps[tb_][:, :], lhsT=ones_b[:], rhs=sqb[:], start=True, stop=True), R=[R_sqb, R_const], W=[R_ps[tb_]])
                                P.op("dve", lambda e: e.tensor_scalar(out=ft[3][:], in0=ps[tb_][:, :], scalar1=1.0 / 128, scalar2=EPS, op0=ALU.mult, op1=ALU.add), R=[R_ps[tb_]], W=[R_ft[3]])
                                P.op("act", lambda e: e.activation(out=ft[3][:], in_=ft[3][:], func=AF.Ln), R=[R_ft[3]], W=[R_ft[3]])
                                P.op("act", lambda e: e.activation(out=ft[3][:], in_=ft[3][:], func=AF.Exp, scale=-0.5), R=[R_ft[3]], W=[R_ft[3]])
                                P.op("dve", lambda e: e.scalar_tensor_tensor(out=mixT[:, h, J * 512:(J + 1) * 512], in0=ft[2][:], scalar=gscale, in1=ft[3][:], op0=ALU.mult, op1=ALU.mult),
                                     R=[R_ft[2], R_ft[3], R_sm], W=[R_mix[h][J]])
                            pend.append(fin1)
                            pend.append(fin2)
                    while pend:
                        pend.pop(0)()

                    for g in range(2):
                        bi = load_w(C_VB + g * 128, 128)
                        proj_tok(bi, 128, lambda st_, g=g: VV[:, st_, g * 128:(g + 1) * 128], R_VV)
                    for h in range(4):
                        z0 = 64 if h % 2 == 0 else 0
                        P.op("pool", lambda e: e.memset(QK[2 + h][z0:z0 + 64, :], 0.0), W=[R_QK[2 + h]])
                    for g in range(2):
                        bq = load_w(C_QB + g * 128, 128)
                        bk = load_w(C_KB + g * 128, 128)
                        proj_feat(bq, 0, 128, QK[g], R_QK[g], 0, 0.125)
                        proj_feat_split(bk, QK[2 + 2 * g], R_QK[2 + 2 * g], QK[3 + 2 * g], R_QK[3 + 2 * g], 1.0)
                    for h in range(4):
                        g = h // 2
                        r0 = (h % 2) * 64
                        for J in range(4):
                            i_list = list(range(max(0, 4 * J - 4), 4 * J + 4))

                            def colr(i, J=J):
                                a_lo = max(0, i - 4 * J); a_hi = min(3, i + 4 - 4 * J)
                                return (128 * a_lo, 128 * (a_hi + 1))

                            def smm(sci, i, m, c_lo, c_hi, J=J, h=h, g=g):
                                P.pe(lambda e: e.matmul(ps[sci][:, c_lo:c_hi], lhsT=QK[2 + h][:, i * 128:(i + 1) * 128], rhs=QK[g][:, J * 512 + c_lo:J * 512 + c_hi], start=True, stop=False),
                                     R=[R_QK[2 + h], R_QK[g]], W=[R_ps[sci]])
                                a_lo, a_hi = c_lo // 128, c_hi // 128 - 1
                                for a in range(a_lo, a_hi + 1):
                                    dl = 4 * J + a - i
                                    piece = {0: 0, 1: 1, 2: 2, 3: 2, 4: 3}[dl]
                                    P.pe(lambda e: e.matmul(ps[sci][:, a * 128:(a + 1) * 128], lhsT=ident_b[:], rhs=relb_b[:, h * 4 + piece, :], start=False, stop=(a == a_hi)),
                                         R=[R_const, R_relb], W=[R_ps[sci]])
                            attn_block(J, i_list, colr, smm, lambda i: 0.0, lambda i, g=g: VV[:, i, g * 128:(g + 1) * 128], R_VV, [(0, 1, 0)])
                            P.op("act", lambda e: e.activation(out=ft[0][r0:r0 + 64, :], in_=ps[0][r0:r0 + 64, :], func=AF.Copy), R=[R_ps[0]], W=[R_ft[0]])
                            P.op("dve", lambda e: e.tensor_copy(out=ft[1][r0:r0 + 64, :], in_=ps[1][r0:r0 + 64, :]), R=[R_ps[1]], W=[R_ft[1]])

                            def finB(r0=r0, cc=4 + g, J=J):
                                P.op("dve", lambda e: e.tensor_scalar(out=ft[1][r0:r0 + 64, :], in0=ft[1][r0:r0 + 64, :], scalar1=1e-30, scalar2=None, op0=ALU.max), R=[R_ft[1]], W=[R_ft[1]])
                                P.op("dve", lambda e: e.reciprocal(out=ft[1][r0:r0 + 64, :], in_=ft[1][r0:r0 + 64, :]), R=[R_ft[1]], W=[R_ft[1]])
                                P.op("dve", lambda e: e.tensor_tensor(out=mixT[r0:r0 + 64, cc, J * 512:(J + 1) * 512], in0=ft[0][r0:r0 + 64, :], in1=ft[1][r0:r0 + 64, :], op=ALU.mult),
                                     R=[R_ft[0], R_ft[1]], W=[R_mix[cc][J]])
                            pend.append(finB)

                    while pend:
                        pend.pop(0)()
                    for g in range(2):
                        bi = load_w(C_VC + g * 128, 128)
                        proj_tok(bi, 128, lambda st_, g=g: VV[:, st_, g * 128:(g + 1) * 128], R_VV)
                    for h in range(4):
                        P.dma("sp", lambda e: e.dma_start(out=QK[h][64:67, :], in_=c_kaug[4 + h]), W=[R_QK[h]])
                        P.dma("sp", lambda e: e.dma_start(out=QK[4 + h][64:67, :], in_=c_qaug), W=[R_QK[4 + h]])
                    for g in range(2):
                        bq = load_w(C_QC + g * 128, 128)
                        bk = load_w(C_KC + g * 128, 128)
                        proj_feat(bq, 0, 64, QK[4 + 2 * g], R_QK[4 + 2 * g], 0, 0.125)
                        proj_feat(bq, 64, 64, QK[5 + 2 * g], R_QK[5 + 2 * g], 0, 0.125)
                        proj_feat(bk, 0, 64, QK[2 * g], R_QK[2 * g], 0, 1.0)
                        proj_feat(bk, 64, 64, QK[2 * g + 1], R_QK[2 * g + 1], 0, 1.0)
                    for g, nh in ((0, 3), (1, 3), (2, 2)):
                        bi = load_w(C_QI + g * 96, 32 * nh)
                        proj_feat(bi, 0, 32 * nh, QK[8 + g], R_QK[8 + g], 0, 1.0)
                    bi = load_w(C_KI, 32, rep=3)
                    proj_feat(bi, 0, 96, QK[11], R_QK[11], 0, 1.0)
                    bi = load_w(C_WI, 8)
                    proj_tok(bi, 8, lambda st_: wtok[:, st_, :], R_wtok)

                    def indexer(j):
                        L = 128 * (j + 1)
                        sb_ = scoreb[j % 2]; Rsb = R_scoreb[j % 2]; dg = diag[j % 2]; Rdg = R_diag[j % 2]
                        for h8 in range(8):
                            P.op("pool", lambda e: e.tensor_scalar(out=dg[:, h8, :], in0=ident_f[:], scalar1=wtok[:, j, h8:h8 + 1], scalar2=W_SCALE, op0=ALU.mult, op1=ALU.mult),
                                 R=[R_const, R_wtok], W=[Rdg])
                        nsc = (L + 511) // 512
                        units = [(sc_i, h8) for sc_i in range(nsc) for h8 in range(8)]
                        gbank = [4 + nxt("gb", 2) for _ in range(nsc)]
                        stt = {}

                        def R_(u):
                            sc_i, h8 = units[u]
                            w_ = min(512, L - 512 * sc_i)
                            gq, rr = h8 // 3, h8 % 3
                            rb = nxt("rb", 4)
                            P.pe(lambda e: e.matmul(ps[rb][:, 0:w_], lhsT=QK[8 + gq][32 * rr:32 * rr + 32, j * 128:(j + 1) * 128], rhs=QK[11][32 * rr:32 * rr + 32, sc_i * 512:sc_i * 512 + w_], start=True, stop=True),
                                 R=[R_QK[8 + gq], R_QK[11]], W=[R_ps[rb]])
                            stt[u] = rb

                        def L_(u):
                            sc_i, h8 = units[u]
                            w_ = min(512, L - 512 * sc_i)
                            rb = stt[u]
                            ri = nxt("relu", 3)
                            P.op("act", lambda e: e.activation(out=relu_sb[ri][:, 0:w_], in_=ps[rb][:, 0:w_], func=AF.Relu), R=[R_ps[rb]], W=[R_relu[ri]])
                            stt[u] = ri

                        def G_(u):
                            sc_i, h8 = units[u]
                            w_ = min(512, L - 512 * sc_i)
                            ri = stt[u]
                            gb = gbank[sc_i]
                            P.pe(lambda e: e.matmul(ps[gb][:, 0:w_], lhsT=dg[:, h8, :], rhs=relu_sb[ri][:, 0:w_], start=(h8 == 0), stop=(h8 == 7)), R=[Rdg, R_relu[ri]], W=[R_ps[gb]])
                            if h8 == 7:
                                P.op("act", lambda e: e.activation(out=sb_[:, sc_i * 512:sc_i * 512 + w_], in_=ps[gb][:, 0:w_], func=AF.Copy), R=[R_ps[gb]], W=[Rsb])
                        n_u = len(units)
                        R_(0); R_(1)
                        for u in range(n_u):
                            L_(u)
                            if u + 2 < n_u:
                                R_(u + 2)
                            G_(u)

                    def select(j):
                        L = 128 * (j + 1)
                        a = j % 4
                        sb_ = scoreb[j % 2]; Rsb = R_scoreb[j % 2]
                        if j < 2:
                            P.op("pool", lambda e: e.memset(negmask[:, 0:L], 0.0), W=[R_negm])
                        else:
                            P.op("dve", lambda e: e.tensor_reduce(out=sm[:, 8:9], in_=sb_[:, 0:L], axis=AX.X, op=ALU.min), R=[Rsb], W=[R_sm])
                            P.op("pool", lambda e: e.memset(sb_[0:64, L - 64:L], -1e30), R=[R_sm], W=[Rsb])
                            P.op("dve", lambda e: e.reduce_max(out=sm[:, 9:10], in_=sb_[:, 0:L], axis=AX.X), R=[Rsb], W=[R_sm])
                            P.op("dve", lambda e: e.scalar_tensor_tensor(out=sm[:, 10:11], in0=sm[:, 9:10], scalar=1e-20, in1=sm[:, 8:9], op0=ALU.add, op1=ALU.subtract), R=[R_sm], W=[R_sm])
                            P.op("dve", lambda e: e.reciprocal(out=sm[:, 11:12], in_=sm[:, 10:11]), R=[R_sm], W=[R_sm])
                            P.op("dve", lambda e: e.tensor_scalar(out=sb_[:, 0:L], in0=sb_[:, 0:L], scalar1=sm[:, 8:9], scalar2=sm[:, 11:12], op0=ALU.subtract, op1=ALU.mult), R=[Rsb, R_sm], W=[Rsb])
                            P.op("dve", lambda e: e.memset(sm[:, 12:13], 0.5), W=[R_sm])
                            for n in range(NIT):
                                dlt = 2.0 ** -(n + 2)
                                P.op("dve", lambda e: e.tensor_scalar(out=negmask[:, 0:L], in0=sb_[:, 0:L], scalar1=sm[:, 12:13], scalar2=None, op0=ALU.is_gt, op1=ALU.add, accum_out=sm[:, 13:14]),
                                     R=[Rsb, R_sm], W=[R_negm, R_sm])
                                P.op("dve", lambda e: e.tensor_scalar(out=sm[:, 14:15], in0=sm[:, 13:14], scalar1=255.5, scalar2=2.0 * dlt, op0=ALU.is_gt, op1=ALU.mult), R=[R_sm], W=[R_sm])
                                P.op("dve", lambda e: e.scalar_tensor_tensor(out=sm[:, 12:13], in0=sm[:, 12:13], scalar=-dlt, in1=sm[:, 14:15], op0=ALU.add, op1=ALU.add), R=[R_sm], W=[R_sm])
                            P.op("dve", lambda e: e.tensor_scalar(out=negmask[:, 0:L], in0=sb_[:, 0:L], scalar1=sm[:, 12:13], scalar2=NEG, op0=ALU.is_le, op1=ALU.mult), R=[Rsb, R_sm], W=[R_negm])
                        for i0 in range(0, j + 1, 8):
                            n_ = min(8, j + 1 - i0)
                            for ii in range(n_):
                                P.pe(lambda e: e.transpose(out=psT[:, ii, :], in_=negmask[:, (i0 + ii) * 128:(i0 + ii + 1) * 128], identity=ident_b[:]), R=[R_negm, R_const], W=[R_psT])
                            P.op("act", lambda e: e.activation(out=nmT[:, i0:i0 + n_, a * 128:(a + 1) * 128], in_=psT[:, 0:n_, :], func=AF.Copy), R=[R_psT], W=[R_nmT])

                    for J in range(4):
                        for a in range(4):
                            j = 4 * J + a
                            if 2 <= j + 1 < 16:
                                indexer(j + 1)
                            select(j)
                        for h in range(4):
                            g = h // 2
                            r0 = (h % 2) * 64
                            sig = SIG_C[h]

                            def colr(i, J=J):
                                return (128 * max(0, i - 4 * J), 512)

                            def smm(sci, i, m, c_lo, c_hi, J=J, h=h):
                                P.pe(lambda e: e.matmul(ps[sci][:, c_lo:c_hi], lhsT=QK[h][0:67, i * 128:(i + 1) * 128], rhs=QK[4 + h][0:67, J * 512 + c_lo:J * 512 + c_hi], start=True, stop=False),
                                     R=[R_QK[h], R_QK[4 + h]], W=[R_ps[sci]])
                                if i >= 4 * J:
                                    a = i - 4 * J
                                    P.pe(lambda e: e.matmul(ps[sci][:, a * 128:(a + 1) * 128], lhsT=ident_b[:], rhs=dmask_b[:, 4 + h, :], start=False, stop=False), R=[R_const], W=[R_ps[sci]])
                                P.pe(lambda e: e.matmul(ps[sci][:, c_lo:c_hi], lhsT=ident_b[:], rhs=nmT[:, i, c_lo:c_hi], start=False, stop=True), R=[R_const, R_nmT], W=[R_ps[sci]])
                            attn_block(J, list(range(4 * J + 4)), colr, smm, lambda i, J=J, sig=sig: -sig * (512 * J - 128 * i),
                                       lambda i, g=g: VV[:, i, g * 128:(g + 1) * 128], R_VV, [(0, 1, 0)])
                            P.op("act", lambda e: e.activation(out=ft[0][r0:r0 + 64, :], in_=ps[0][r0:r0 + 64, :], func=AF.Copy), R=[R_ps[0]], W=[R_ft[0]])
                            P.op("dve", lambda e: e.tensor_copy(out=ft[1][r0:r0 + 64, :], in_=ps[1][r0:r0 + 64, :]), R=[R_ps[1]], W=[R_ft[1]])

                            def finB(r0=r0, cc=6 + g, J=J):
                                P.op("dve", lambda e: e.tensor_scalar(out=ft[1][r0:r0 + 64, :], in0=ft[1][r0:r0 + 64, :], scalar1=1e-30, scalar2=None, op0=ALU.max), R=[R_ft[1]], W=[R_ft[1]])
                                P.op("dve", lambda e: e.reciprocal(out=ft[1][r0:r0 + 64, :], in_=ft[1][r0:r0 + 64, :]), R=[R_ft[1]], W=[R_ft[1]])
                                P.op("dve", lambda e: e.tensor_tensor(out=mixT[r0:r0 + 64, cc, J * 512:(J + 1) * 512], in0=ft[0][r0:r0 + 64, :], in1=ft[1][r0:r0 + 64, :], op=ALU.mult),
                                     R=[R_ft[0], R_ft[1]], W=[R_mix[cc][J]])
                            pend.append(finB)
                    while pend:
                        pend.pop(0)()
                    P.barrier()

                if dbg == "mix":
                    allmix = [R_mix[c][j] for c in range(8) for j in range(4)]
                    P.dma("sp", lambda e: e.dma_start(out=dbg_out[b], in_=mixT[:]), R=allmix, W=[R_dbg])
                    P.barrier()
                    continue

                with ExitStack() as es:
                    WL = 3
                    wo_b = sbc(es, "wo_b", [128, 8, D], BF16); R_wo = Res("wo")
                    g1 = sbc(es, "g1", [128, D], F32); b1 = sbc(es, "b1", [128, D], F32); R_gb = Res("gb")
                    wr_f = sbc(es, "wr_f", [128, 8, NE], F32); br_t = sbc(es, "br_t", [128, NE], F32)
                    xt_ = [sbc(es, "xt%d" % i, [128, D], F32) for i in range(WL)]; R_xt = [Res("xt") for _ in range(WL)]
                    vt = [sbc(es, "vt%d" % i, [128, D], F32) for i in range(WL)]; R_vt = [Res("vt") for _ in range(WL)]
                    x1t = [sbc(es, "x1t%d" % i, [128, D], F32) for i in range(WL)]; R_x1t = [Res("x1t") for _ in range(WL)]
                    x1b = [sbc(es, "x1b%d" % i, [128, D], BF16) for i in range(WL)]; R_x1b = [Res("x1b") for _ in range(WL)]
                    x1T = [sbc(es, "x1T%d" % i, [128, 8, 128], F32) for i in range(WL)]; R_x1T = [Res("x1T") for _ in range(WL)]
                    junk = [sbc(es, "junk%d" % i, [128, D], BF16) for i in range(WL)]; R_junk = [Res("junk") for _ in range(WL)]
                    lst = [sbc(es, "lst%d" % i, [128, 12], F32) for i in range(WL)]; R_lst = [Res("lst") for _ in range(WL)]
                    lg = [sbc(es, "lg%d" % i, [128, NE], F32) for i in range(WL)]; R_lg = [Res("lg") for _ in range(WL)]
                    mx8 = [sbc(es, "mx8%d" % i, [128, 8], F32) for i in range(WL)]; R_mx = [Res("mx") for _ in range(WL)]
                    rs = [sbc(es, "rs%d" % i, [128, 16], F32) for i in range(WL)]; R_rs = [Res("rs") for _ in range(WL)]
                    maskb = [sbc(es, "maskb%d" % i, [128, NE], BF16) for i in range(WL)]; R_maskb = [Res("maskb") for _ in range(WL)]
                    slotm = [sbc(es, "slotm%d" % i, [128, NE], F32) for i in range(WL)]; R_slotm = [Res("slotm") for _ in range(WL)]
                    oh = [sbc(es, "oh%d" % i, [128, NE], F32) for i in range(WL)]; R_oh = [Res("oh") for _ in range(WL)]
                    R_p6 = [R_ps[6]] * WL
                    for c in range(8):
                        P.dma("pool", lambda e: e.dma_start(out=wo_b[:, c, :], in_=w_out[l, c * 128:(c + 1) * 128, :]), W=[R_wo])
                    P.dma("sp", lambda e: e.dma_start(out=g1[:], in_=ln1g[l:l + 1, :].to_broadcast([128, D])), W=[R_gb])
                    P.dma("sp", lambda e: e.dma_start(out=b1[:], in_=ln1b[l:l + 1, :].to_broadcast([128, D])), W=[R_gb])
                    P.dma("sp", lambda e: e.dma_start(out=wr_f[:], in_=w_rt[l].rearrange("(k p) c -> p k c", p=128)), W=[R_gb])
                    P.dma("sp", lambda e: e.dma_start(out=br_t[:], in_=b_rt[l:l + 1, :].to_broadcast([128, NE])), W=[R_gb])
                    P.op("dve", lambda e: e.memset(lst[0][:, 0:1], 0.0), R=[R_ps[6]], W=[R_lst[0]])

                    def ln1_tile(tt):
                        p = tt % WL
                        gt_ = b * 16 + tt
                        rows = slice(tok0 + tt * 128, tok0 + (tt + 1) * 128)
                        c6 = p * 128
                        P.dma("sp", lambda e: e.dma_start(out=xt_[p][:], in_=xsrc[rows, :]), R=[R_xsrc], W=[R_xt[p]])
                        yield
                        for half in range(2):
                            pi = p
                            for c in range(8):
                                P.pe(lambda e: e.matmul(ps[pi][:, :], lhsT=mixT[:, c, tt * 128:(tt + 1) * 128], rhs=wo_b[:, c, half * 512:(half + 1) * 512], start=(c == 0), stop=(c == 7)),
                                     R=[R_mix[c][tt // 4], R_wo], W=[R_ps[pi]])
                            P.op("dve", lambda e: e.scalar_tensor_tensor(out=vt[p][:, half * 512:(half + 1) * 512], in0=xt_[p][:, half * 512:(half + 1) * 512], scalar=ALPHA, in1=ps[pi][:, :], op0=ALU.mult, op1=ALU.add, accum_out=lst[p][:, 8 + half:9 + half]),
                                 R=[R_xt[p], R_ps[pi]], W=[R_vt[p], R_lst[p]])
                            yield
                        P.op("dve", lambda e: e.tensor_tensor(out=lst[p][:, 0:1], in0=lst[p][:, 8:9], in1=lst[p][:, 9:10], op=ALU.add), R=[R_lst[p]], W=[R_lst[p]])
                        yield
                        yield from ln_gen(vt[p], R_vt[p], g1, b1, R_gb, x1t[p], R_x1t[p], lst[p], R_lst[p], junk[p], R_junk[p])
                        if dbg == "x1":
                            P.dma("sp", lambda e: e.dma_start(out=dbg_out[rows, :], in_=x1t[p][:]), R=[R_x1t[p]], W=[R_dbg])
                        P.dma("sp", lambda e: e.dma_start(out=x1s[rows, :], in_=x1t[p][:]), R=[R_x1t[p]], W=[R_x1s])
                        P.op("act", lambda e: e.activation(out=x1b[p][:], in_=x1t[p][:], func=AF.Copy), R=[R_x1t[p]], W=[R_x1b[p]])
                        yield
                        for hf in range(2):
                            for k4 in range(4):
                                k = hf * 4 + k4
                                P.pe(lambda e: e.transpose(out=ps[3 + p][:, k4 * 128:(k4 + 1) * 128], in_=x1t[p][:, k * 128:(k + 1) * 128], identity=ident_f[:]), R=[R_x1t[p], R_const], W=[R_ps[3 + p]])
                            P.op("act", lambda e: e.activation(out=x1T[p][:, hf * 4:(hf + 1) * 4, :], in_=ps[3 + p][:, :].rearrange("p (a b) -> p a b", a=4), func=AF.Copy), R=[R_ps[3 + p]], W=[R_x1T[p]])
                            yield
                        for k in range(8):
                            P.pe(lambda e: e.matmul(ps[6][:, c6:c6 + NE], lhsT=x1T[p][:, k, :], rhs=wr_f[:, k, :], start=(k == 0), stop=(k == 7)), R=[R_x1T[p], R_gb], W=[R_p6[p]])
                        P.op("dve", lambda e: e.tensor_tensor(out=lg[p][:], in0=ps[6][:, c6:c6 + NE], in1=br_t[:], op=ALU.add), R=[R_p6[p], R_gb], W=[R_lg[p]])
                        yield
                        P.op("dve", lambda e: e.max(out=mx8[p][:], in_=lg[p][:]), R=[R_lg[p]], W=[R_mx[p]])
                        yield
                        P.op("dve", lambda e: e.tensor_scalar(out=rs[p][:, 0:1], in0=mx8[p][:, 0:1], scalar1=-1.0, scalar2=None, op0=ALU.mult), R=[R_mx[p]], W=[R_rs[p]])
                        P.op("dve", lambda e: e.tensor_scalar(out=maskb[p][:], in0=lg[p][:], scalar1=mx8[p][:, 3:4], scalar2=None, op0=ALU.is_ge), R=[R_lg[p], R_mx[p]], W=[R_maskb[p]])
                        yield
                        P.op("act", lambda e: e.activation(out=rs[p][:, 4:8], in_=mx8[p][:, 0:4], func=AF.Exp, bias=rs[p][:, 0:1], scale=1.0, accum_out=rs[p][:, 1:2]), R=[R_mx[p], R_rs[p]], W=[R_rs[p]])
                        P.pe(lambda e: e.matmul(ps[6][:, c6 + 32:c6 + 64], lhsT=ltri_b[:], rhs=maskb[p][:], start=True, stop=True), R=[R_maskb[p], R_const], W=[R_p6[p]])
                        P.pe(lambda e: e.matmul(ps[6][:, c6 + 64:c6 + 96], lhsT=ones_b[:], rhs=maskb[p][:], start=True, stop=True), R=[R_maskb[p], R_const], W=[R_p6[p]])
                        yield
                        P.op("dve", lambda e: e.tensor_tensor(out=slotm[p][:], in0=ps[6][:, c6 + 32:c6 + 64], in1=base_cnt[:], op=ALU.add), R=[R_p6[p], R_base], W=[R_slotm[p]])
                        P.op("dve", lambda e: e.tensor_tensor(out=base_cnt[:], in0=ps[6][:, c6 + 64:c6 + 96], in1=base_cnt[:], op=ALU.add), R=[R_p6[p], R_base], W=[R_base])
                        yield
                        P.op("dve", lambda e: e.tensor_tensor(out=slotm[p][:], in0=slotm[p][:], in1=eoff[:], op=ALU.add), R=[R_slotm[p], R_const], W=[R_slotm[p]])
                        P.op("dve", lambda e: e.reciprocal(out=rs[p][:, 2:3], in_=rs[p][:, 1:2]), R=[R_rs[p]], W=[R_rs[p]])
                        yield
                        P.op("dve", lambda e: e.tensor_scalar(out=gates[:, gt_ * 4:gt_ * 4 + 4], in0=rs[p][:, 4:8], scalar1=rs[p][:, 2:3], scalar2=None, op0=ALU.mult), R=[R_rs[p]], W=[R_gates])
                        yield
                        for k in range(4):
                            P.op("dve", lambda e: e.scalar_tensor_tensor(out=oh[p][:], in0=lg[p][:], scalar=mx8[p][:, k:k + 1], in1=slotm[p][:], op0=ALU.is_equal, op1=ALU.mult, accum_out=slotf[:, gt_ * 4 + k:gt_ * 4 + k + 1]),
                                 R=[R_lg[p], R_mx[p], R_slotm[p]], W=[R_oh[p], R_slot])
                            yield
                        P.op("dve", lambda e: e.tensor_copy(out=sloti[:, gt_ * 4:gt_ * 4 + 4], in_=slotf[:, gt_ * 4:gt_ * 4 + 4]), R=[R_slot], W=[R_slot])
                        yield
                        for k in range(4):
                            P.dma("pool", lambda e: e.indirect_dma_start(out=xg[:, :], out_offset=bass.IndirectOffsetOnAxis(ap=sloti[:, gt_ * 4 + k:gt_ * 4 + k + 1], axis=0), in_=x1b[p][:, :], in_offset=None),
                                  R=[R_x1b[p], R_slot], W=[R_xg])
                        yield

                    run_window([ln1_tile(tt) for tt in range(16)], WL, 11)
                    P.barrier()
        if dbg in ("mix", "x1"):
            break

        with ExitStack() as es:
            def sbm(name, shape, dt):
                return es.enter_context(nc.sbuf_tensor(name + "_m%d" % l, list(shape), dt))
            wgu_b = [sbm("wgu%d" % i, [128, 8, 2 * D], BF16) for i in range(2)]; R_wgu = [[Res("wgu") for _ in range(8)] for _ in range(2)]
            wdn_b = [sbm("wdn%d" % i, [128, 8, D], BF16) for i in range(2)]; R_wdn = [[Res("wdn") for _ in range(8)] for _ in range(2)]
            xr = [sbm("xr%d" % i, [128, D], BF16) for i in range(4)]; R_xr = [Res("xr") for _ in range(4)]
            xeT = [sbm("xeT%d" % i, [128, 8, CAP], BF16) for i in range(2)]; R_xeT = [Res("xeT") for _ in range(2)]
            GT = sbm("GT", [128, 8, CAP], BF16); R_GT = [Res("GT%d" % c) for c in range(8)]
            et = [sbm("et%d" % i, [128, 512], F32) for i in range(9)]; R_et = [Res("et") for _ in range(9)]
            yt = [sbm("yt%d" % i, [128, D], F32) for i in range(3)]; R_yt = [Res("yt") for _ in range(3)]
            bgT = sbm("bgT", [128, NE * 16], F32); R_bgT = Res("bgT")
            bgs = sbm("bgs", [128, 4, 128], F32); R_bgs = Res("bgs")
            bdb = [sbm("bdb%d" % i, [128, D], F32) for i in range(2)]; R_bdb = [Res("bdb") for _ in range(2)]
            P.dma("sp", lambda e: e.dma_start(out=bgs[:], in_=b_gu[l].rearrange("(a r) p -> r a p", r=128)), W=[R_bgs])
            for a in range(4):
                P.pe(lambda e: e.transpose(out=ps[6][:, a * 128:(a + 1) * 128], in_=bgs[:, a, :], identity=ident_f[:]), R=[R_bgs, R_const], W=[R_ps[6]])
            P.op("dve", lambda e: e.tensor_copy(out=bgT[:], in_=ps[6][:, :]), R=[R_ps[6]], W=[R_bgT])

            def load_expert(e_):
                s2 = e_ % 2
                for k in range(8):
                    P.dma("pool", lambda e: e.dma_start(out=wgu_b[s2][:, k, :], in_=w_gu[l, e_, k * 128:(k + 1) * 128, :]), W=[R_wgu[s2][k]])
                for k in range(8):
                    P.dma("pool", lambda e: e.dma_start(out=wdn_b[s2][:, k, :], in_=w_dn[l, e_, k * 128:(k + 1) * 128, :]), W=[R_wdn[s2][k]])
                P.dma("sp", lambda e: e.dma_start(out=bdb[s2][:], in_=b_dn[l, e_:e_ + 1, :].to_broadcast([128, D])), W=[R_bdb[s2]])

            def build_x(e_):
                s2 = e_ % 2
                for st_ in range(CAP // 128):
                    xi = nxt("xr", 4)
                    P.dma("sp", lambda e: e.dma_start(out=xr[xi][:], in_=xg[e_ * CAP + st_ * 128:e_ * CAP + (st_ + 1) * 128, :]), R=[R_xg], W=[R_xr[xi]])
                    tv, R_tv = (psT, R_psT) if st_ % 2 == 0 else (psT2, R_ps[6])
                    for k in range(8):
                        P.pe(lambda e: e.transpose(out=tv[:, k, :], in_=xr[xi][:, k * 128:(k + 1) * 128], identity=ident_b[:]), R=[R_xr[xi], R_const], W=[R_tv])
                    P.op("act", lambda e: e.activation(out=xeT[s2][:, :, st_ * 128:(st_ + 1) * 128], in_=tv[:, :, :], func=AF.Copy), R=[R_tv], W=[R_xeT[s2]])

            load_expert(0)
            build_x(0)
            for e_ in range(NE):
                s2 = e_ % 2
                if e_ + 1 < NE:
                    load_expert(e_ + 1)
                for c in range(8):
                    for (s0, sw) in ((0, 512), (512, CAP - 512)):
                        pg = nxt("pg", 2) * 2
                        for k in range(8):
                            P.pe(lambda e: e.matmul(ps[pg][:, 0:sw], lhsT=wgu_b[s2][:, k, c * 128:(c + 1) * 128], rhs=xeT[s2][:, k, s0:s0 + sw], start=(k == 0), stop=(k == 7)),
                                 R=[R_wgu[s2][k], R_xeT[s2]], W=[R_ps[pg]])
                        for k in range(8):
                            P.pe(lambda e: e.matmul(ps[pg + 1][:, 0:sw], lhsT=wgu_b[s2][:, k, D + c * 128:D + (c + 1) * 128], rhs=xeT[s2][:, k, s0:s0 + sw], start=(k == 0), stop=(k == 7)),
                                 R=[R_wgu[s2][k], R_xeT[s2]], W=[R_ps[pg + 1]])
                        ei = nxt("et", 3) * 3
                        bgc = bgT[:, e_ * 16 + c:e_ * 16 + c + 1]
                        buc = bgT[:, e_ * 16 + 8 + c:e_ * 16 + 8 + c + 1]
                        P.op("dve", lambda e: e.tensor_scalar(out=et[ei][:, 0:sw], in0=ps[pg][:, 0:sw], scalar1=bgc, scalar2=7.0, op0=ALU.add, op1=ALU.min), R=[R_ps[pg], R_bgT], W=[R_et[ei]])
                        P.op("act", lambda e: e.activation(out=et[ei + 1][:, 0:sw], in_=et[ei][:, 0:sw], func=AF.Sigmoid, scale=1.702), R=[R_et[ei]], W=[R_et[ei + 1]])
                        P.op("act", lambda e: e.activation(out=et[ei + 2][:, 0:sw], in_=ps[pg + 1][:, 0:sw], func=AF.Identity, bias=buc, scale=1.0), R=[R_ps[pg + 1], R_bgT], W=[R_et[ei + 2]])
                        P.op("dve", lambda e: e.tensor_scalar(out=et[ei + 2][:, 0:sw], in0=et[ei + 2][:, 0:sw], scalar1=7.0, scalar2=-7.0, op0=ALU.min, op1=ALU.max), R=[R_et[ei + 2]], W=[R_et[ei + 2]])
                        P.op("dve", lambda e: e.tensor_tensor(out=et[ei][:, 0:sw], in0=et[ei][:, 0:sw], in1=et[ei + 1][:, 0:sw], op=ALU.mult), R=[R_et[ei], R_et[ei + 1]], W=[R_et[ei]])
                        P.op("dve", lambda e: e.scalar_tensor_tensor(out=GT[:, c, s0:s0 + sw], in0=et[ei + 2][:, 0:sw], scalar=1.0, in1=et[ei][:, 0:sw], op0=ALU.add, op1=ALU.mult), R=[R_et[ei], R_et[ei + 2]], W=[R_GT[c]])
                if e_ + 1 < NE:
                    build_x(e_ + 1)
                for st_ in range(CAP // 128):
                    yi = nxt("yt", 3)
                    for half in range(2):
                        pi = 4 + nxt("pd", 2)
                        for c in range(8):
                            P.pe(lambda e: e.matmul(ps[pi][:, :], lhsT=GT[:, c, st_ * 128:(st_ + 1) * 128], rhs=wdn_b[s2][:, c, half * 512:(half + 1) * 512], start=(c == 0), stop=(c == 7)),
                                 R=[R_GT[c], R_wdn[s2][c]], W=[R_ps[pi]])
                        P.op("dve", lambda e: e.tensor_tensor(out=yt[yi][:, half * 512:(half + 1) * 512], in0=ps[pi][:, :], in1=bdb[s2][:, half * 512:(half + 1) * 512], op=ALU.add),
                             R=[R_ps[pi], R_bdb[s2]], W=[R_yt[yi]])
                    P.dma("sp", lambda e: e.dma_start(out=yg[e_ * CAP + st_ * 128:e_ * CAP + (st_ + 1) * 128, :], in_=yt[yi][:]), R=[R_yt[yi]], W=[R_yg])
            P.barrier()

        with ExitStack() as es:
            def sbm(name, shape, dt):
                return es.enter_context(nc.sbuf_tensor(name + "_c%d" % l, list(shape), dt))
            WC = 3
            yk = [sbm("yk%d" % i, [128, D], F32) for i in range(4 * WC)]; R_yk = [Res("yk") for _ in range(4 * WC)]
            xa = [sbm("xa%d" % i, [128, D], F32) for i in range(WC)]; R_xa = [Res("xa") for _ in range(WC)]
            xo = [sbm("xo%d" % i, [128, D], F32) for i in range(WC)]; R_xo = [Res("xo") for _ in range(WC)]
            g2 = sbm("g2", [128, D], F32); b2 = sbm("b2", [128, D], F32); R_gb2 = Res("gb2")
            junk2 = [sbm("junk2_%d" % i, [128, D], BF16) for i in range(WC)]; R_junk2 = [Res("junk2") for _ in range(WC)]
            lst2 = [sbm("lst2_%d" % i, [128, 12], F32) for i in range(WC)]; R_lst2 = [Res("lst2") for _ in range(WC)]
            P.dma("sp", lambda e: e.dma_start(out=g2[:], in_=ln2g[l:l + 1, :].to_broadcast([128, D])), W=[R_gb2])
            P.dma("sp", lambda e: e.dma_start(out=b2[:], in_=ln2b[l:l + 1, :].to_broadcast([128, D])), W=[R_gb2])

            def comb_tile(tt):
                p = tt % WC
                rows = slice(tt * 128, (tt + 1) * 128)
                P.dma("sp", lambda e: e.dma_start(out=xa[p][:], in_=x1s[rows, :]), R=[R_x1s], W=[R_xa[p]])
                for k in range(4):
                    yi = p * 4 + k
                    P.dma("pool", lambda e: e.indirect_dma_start(out=yk[yi][:, :], out_offset=None, in_=yg[:, :], in_offset=bass.IndirectOffsetOnAxis(ap=sloti[:, tt * 4 + k:tt * 4 + k + 1], axis=0)),
                          R=[R_yg, R_slot], W=[R_yk[yi]])
                yield
                P.op("act", lambda e: e.activation(out=xa[p][:], in_=xa[p][:], func=AF.Copy, scale=ALPHA), R=[R_xa[p]], W=[R_xa[p]])
                yield
                for k in range(4):
                    yi = p * 4 + k
                    if k < 3:
                        P.op("dve", lambda e: e.scalar_tensor_tensor(out=xa[p][:], in0=yk[yi][:], scalar=gates[:, tt * 4 + k:tt * 4 + k + 1], in1=xa[p][:], op0=ALU.mult, op1=ALU.add),
                             R=[R_yk[yi], R_gates, R_xa[p]], W=[R_xa[p]])
                    else:
                        P.op("dve", lambda e: e.scalar_tensor_tensor(out=xa[p][:], in0=yk[yi][:], scalar=gates[:, tt * 4 + k:tt * 4 + k + 1], in1=xa[p][:], op0=ALU.mult, op1=ALU.add, accum_out=lst2[p][:, 0:1]),
                             R=[R_yk[yi], R_gates, R_xa[p]], W=[R_xa[p], R_lst2[p]])
                    yield
                yield from ln_gen(xa[p], R_xa[p], g2, b2, R_gb2, xo[p], R_xo[p], lst2[p], R_lst2[p], junk2[p], R_junk2[p])
                P.dma("sp", lambda e: e.dma_start(out=xdst[rows, :], in_=xo[p][:]), R=[R_xo[p]], W=[R_xdst])
                yield

            run_window([comb_tile(tt) for tt in range(32)], WC, 5)
            P.barrier()

    P.finish()
    return nc, P


def _consts():
    bf = ml_dtypes.bfloat16
    ident = np.eye(128, dtype=np.float32)
    ltri = np.triu(np.ones((128, 128), np.float32), 1)
    si = np.arange(128)[:, None]; qi = np.arange(128)[None, :]
    dmask = np.zeros((128, 8, 128), np.float32)
    for h in range(8):
        sg = SIG8[h]
        d = np.where(si <= qi, 0.0, np.where((si // 64) == (qi // 64), -2.0 * sg * (si - qi), NEG))
        dmask[:, h, :] = d
    q = np.arange(S)
    qaug = np.stack([np.ones(S), -(q % 128).astype(np.float64), -(128.0 * ((q // 128) % 4))]).astype(np.float32)
    kaug = np.zeros((8, 3, S), np.float32)
    for h in range(8):
        kaug[h, 0] = SIG8[h] * (q % 128)
        kaug[h, 1] = SIG8[h]
        kaug[h, 2] = SIG8[h]
    eoff = np.tile((np.arange(NE, dtype=np.float32) * CAP)[None, :], (128, 1))
    return {"c_ident": ident.astype(bf), "c_identf": ident, "c_ltri": ltri.astype(bf), "c_dmask": dmask.astype(bf),
            "c_qaug": qaug.astype(bf), "c_kaug": kaug.astype(bf), "c_eoff": eoff}


def _relb_pieces(rel_bias):
    NLn = rel_bias.shape[0]
    si = np.arange(128)[:, None]; qi = np.arange(128)[None, :]
    out = np.empty((NLn, 128, 16, 128), np.float32)
    for h in range(4):
        rel0 = qi - si
        idx0 = np.clip(rel0, -128, 128) + 128
        m0 = ((si // 64) == 1) & ((qi // 64) == 0)
        idx1 = np.clip(128 + qi - si, -128, 128) + 128
        m4 = ((si // 64) == 0) & ((qi // 64) == 1)
        for ln in range(NLn):
            rb = rel_bias[ln, h]
            p0 = rb[idx0].copy(); p0[m0] = NEG
            p1 = rb[idx1]
            p2 = np.broadcast_to(rb[256], (128, 128))
            p4 = np.array(p2); p4[m4] = NEG
            out[ln, :, h * 4 + 0, :] = p0
            out[ln, :, h * 4 + 1, :] = p1
            out[ln, :, h * 4 + 2, :] = p2
            out[ln, :, h * 4 + 3, :] = p4
    return out


_CACHE = {}


def _get_prog(NL, lam_inits, dbg=None):
    key = (NL, tuple(lam_inits), dbg)
    if key not in _CACHE:
        _CACHE[key] = build(NL, lam_inits, dbg)[0]
    return _CACHE[key]


def _layer_inputs(inp, ls):
    f = lambda a: np.ascontiguousarray(a, dtype=np.float32)
    d = {
        "w_in": f(inp["w_in"][ls]),
        "lamv": f(np.stack([inp["lam_q1"][ls], inp["lam_k1"][ls], inp["lam_q2"][ls], inp["lam_k2"][ls]], axis=1)),
        "subln_g": f(inp["subln_g"][ls]),
        "relb": _relb_pieces(np.asarray(inp["rel_bias"][ls], np.float32)),
        "w_out": f(inp["w_out"][ls]),
        "ln1_g": f(inp["ln1_g"][ls]), "ln1_b": f(inp["ln1_b"][ls]),
        "w_router": f(inp["w_router"][ls]), "b_router": f(inp["b_router"][ls]),
        "w_gu": f(inp["w_gu"][ls]), "b_gu": f(inp["b_gu"][ls]).reshape(len(range(*ls.indices(DEPTH))), NE * 16, 128),
        "w_down": f(inp["w_down"][ls]), "b_down": f(inp["b_down"][ls]),
        "ln2_g": f(inp["ln2_g"][ls]), "ln2_b": f(inp["ln2_b"][ls]),
    }
    return d


FUSED = True


def kernel(**inp):
    x = np.ascontiguousarray(inp["x"], dtype=np.float32)
    consts = _consts()
    lam_inits_all = [0.8 - 0.6 * math.exp(-0.3 * l) for l in range(DEPTH)]
    xs = [x[c * NBL:(c + 1) * NBL].reshape(T, D) for c in range(NCORES)]
    if FUSED:
        nc = _get_prog(DEPTH, lam_inits_all)
        li = _layer_inputs(inp, slice(0, DEPTH))
        in_maps = [dict(li, x=xs[c], **consts) for c in range(NCORES)]
        res = run_bass_kernel_spmd(nc, in_maps, core_ids=list(range(NCORES)))
        xs = [res.results[c]["y"] for c in range(NCORES)]
    else:
        for l in range(DEPTH):
            nc = _get_prog(1, [lam_inits_all[l]])
            li = _layer_inputs(inp, slice(l, l + 1))
            in_maps = [dict(li, x=xs[c], **consts) for c in range(NCORES)]
            res = run_bass_kernel_spmd(nc, in_maps, core_ids=list(range(NCORES)))
            xs = [np.asarray(res.results[c]["y"]) for c in range(NCORES)]
    out = np.stack([xs[c].reshape(NBL, S, D) for c in range(NCORES)], axis=0).reshape(NCORES * NBL, S, D)
    return out.astype(np.float32)
```
